# Optimizing a Trainium2 kernel written in Bass

```python
import jax
import jax.numpy as jnp
from jax import lax
import numpy as np

D_MODEL = 1024
BATCH = 16
SEQ = 4096
DEPTH = 1

M_HEADS = 4
M_HEAD_DIM = D_MODEL // M_HEADS
M_WIDTH = M_HEADS * M_HEAD_DIM
M_CHUNK = 64
CONV_WIDTH = 4
A_HEAD_DIM = 64
A_HEADS = D_MODEL // A_HEAD_DIM
A_KV_HEADS = max(1, A_HEADS // 8)
A_GROUP = A_HEADS // A_KV_HEADS
A_WIDTH = A_HEADS * A_HEAD_DIM
A_KV_WIDTH = A_KV_HEADS * A_HEAD_DIM
WINDOW = 128
A_BLOCK = WINDOW
ROPE_THETA = 500000.0
ROPE_DIM = A_HEAD_DIM // 4
D_FF = 256 * (-(-8 * D_MODEL // (3 * 256)))
EPS = 1e-6
IN_SIZES = (M_WIDTH, M_WIDTH, M_WIDTH, M_WIDTH, M_HEADS, M_HEADS,
            A_WIDTH, A_KV_WIDTH, A_KV_WIDTH, D_MODEL, D_MODEL)
IN_OFFSETS = tuple(int(o) for o in np.cumsum(IN_SIZES)[:-1])
N_IN = int(sum(IN_SIZES))

kernel_name = 'hybrid_mlstm_swa_sinks_gated'


def rms_norm(x, g):
    xf = x.astype(jnp.float32)
    y = xf * lax.rsqrt(jnp.mean(xf * xf, axis=-1, keepdims=True) + EPS)
    return (y * g.astype(jnp.float32)).astype(x.dtype)


def partial_rope(x, pos):
    half = ROPE_DIM // 2
    inv_freq = ROPE_THETA ** (-jnp.arange(half, dtype=jnp.float32) * (2.0 / ROPE_DIM))
    ang = pos.astype(jnp.float32)[:, None] * inv_freq[None, :]
    cos = jnp.cos(ang)[:, None, :]
    sin = jnp.sin(ang)[:, None, :]
    xr = x[..., :ROPE_DIM].astype(jnp.float32)
    x1, x2 = xr[..., :half], xr[..., half:]
    rot = jnp.concatenate([x1 * cos - x2 * sin, x2 * cos + x1 * sin], axis=-1).astype(x.dtype)
    return jnp.concatenate([rot, x[..., ROPE_DIM:]], axis=-1)


def causal_depthwise_conv(x, w, b):
    y = lax.conv_general_dilated(
        x, w[:, None, :].astype(x.dtype), window_strides=(1,),
        padding=((CONV_WIDTH - 1, 0),), dimension_numbers=('NWC', 'WIO', 'NWC'),
        feature_group_count=x.shape[-1])
    return y + b.astype(x.dtype)


def mlstm_chunkwise(q, k, v, i_pre, f_pre):
    B, S, H, d = q.shape
    L = M_CHUNK
    NC = S // L
    k = k * (d ** -0.5)
    log_f = jax.nn.log_sigmoid(f_pre)

    def to_chunks(t):
        return t.reshape(B, NC, L, H, d).transpose(1, 0, 3, 2, 4)

    def gate_chunks(t):
        return t.reshape(B, NC, L, H).transpose(1, 0, 3, 2)

    causal = jnp.tril(jnp.ones((L, L), dtype=bool))

    def step(carry, inp):
        C, n, m = carry
        qc, kc, vc, ic, fc = inp
        b = jnp.cumsum(fc, axis=-1)
        log_d = b[..., :, None] - b[..., None, :] + ic[..., None, :]
        log_d = jnp.where(causal, log_d, -jnp.inf)
        m_inter = b + m[..., None]
        m_t = jnp.maximum(m_inter, jnp.max(log_d, axis=-1))
        scores = jnp.einsum('bhtd,bhsd->bhts', qc, kc) * jnp.exp(log_d - m_t[..., None])
        inter_scale = jnp.exp(m_inter - m_t)
        num = (jnp.einsum('bhts,bhse->bhte', scores, vc)
               + inter_scale[..., None] * jnp.einsum('bhtd,bhde->bhte', qc, C))
        den = jnp.sum(scores, axis=-1) + inter_scale * jnp.einsum('bhtd,bhd->bht', qc, n)
        h = num / jnp.maximum(jnp.abs(den), jnp.exp(-m_t))[..., None]
        b_last = b[..., -1]
        w = b_last[..., None] - b + ic
        m_new = jnp.maximum(b_last + m, jnp.max(w, axis=-1))
        decay = jnp.exp(b_last + m - m_new)
        kw = kc * jnp.exp(w - m_new[..., None])[..., None]
        C = decay[..., None, None] * C + jnp.einsum('bhsd,bhse->bhde', kw, vc)
        n = decay[..., None] * n + jnp.sum(kw, axis=2)
        return (C, n, m_new), h

    init = (jnp.zeros((B, H, d, d), jnp.float32), jnp.zeros((B, H, d), jnp.float32),
            jnp.zeros((B, H), jnp.float32))
    _, h = lax.scan(step, init, (to_chunks(q), to_chunks(k), to_chunks(v),
                                 gate_chunks(i_pre), gate_chunks(log_f)))
    return h.transpose(1, 0, 3, 2, 4).reshape(B, S, H, d)


def swa_gqa_sinks(q, k, v, sinks):
    B, S, H, hd = q.shape
    NB = S // A_BLOCK
    qb = q.reshape(B, NB, A_BLOCK, A_KV_HEADS, A_GROUP, hd)

    def band(t):
        padded = jnp.pad(t, ((0, 0), (A_BLOCK, 0), (0, 0), (0, 0)))
        prev = padded[:, :S].reshape(B, NB, A_BLOCK, A_KV_HEADS, hd)
        cur = t.reshape(B, NB, A_BLOCK, A_KV_HEADS, hd)
        return jnp.concatenate([prev, cur], axis=2)

    kb, vb = band(k), band(v)
    logits = jnp.einsum('bnqkgd,bnskd->bnkgqs', qb, kb,
                        preferred_element_type=jnp.float32) * (hd ** -0.5)
    blk = jnp.arange(NB)[:, None] * A_BLOCK
    qpos = blk + jnp.arange(A_BLOCK)[None, :]
    kpos = blk - A_BLOCK + jnp.arange(2 * A_BLOCK)[None, :]
    rel = qpos[:, :, None] - kpos[:, None, :]
    mask = (rel >= 0) & (rel < WINDOW) & (kpos[:, None, :] >= 0)
    logits = jnp.where(mask[None, :, None, None], logits, -jnp.inf)
    sink = sinks.astype(jnp.float32).reshape(A_KV_HEADS, A_GROUP)[None, None, :, :, None]
    mx = jnp.maximum(jnp.max(logits, axis=-1), sink)
    p = jnp.exp(logits - mx[..., None])
    p = p / (jnp.sum(p, axis=-1) + jnp.exp(sink - mx))[..., None]
    out = jnp.einsum('bnkgqs,bnskd->bnqkgd', p.astype(v.dtype), vb)
    return out.reshape(B, S, H * hd)


def setup_inputs(seed: int = 0) -> dict:
    key = jax.random.key(seed)
    ks = jax.random.split(key, 20)
    f32 = jnp.float32
    nrm = lambda k, shape, scale: jax.random.normal(k, shape, f32) * scale
    gate_bias = jnp.concatenate([
        nrm(ks[4], (DEPTH, M_HEADS), 0.1),
        jnp.broadcast_to(jnp.linspace(3.0, 6.0, M_HEADS, dtype=f32), (DEPTH, M_HEADS))
        + nrm(ks[5], (DEPTH, M_HEADS), 0.01)], axis=-1)
    return {
        'x': nrm(ks[0], (BATCH, SEQ, D_MODEL), 1.0),
        'norm1_g': 1.0 + nrm(ks[1], (DEPTH, D_MODEL), 0.02),
        'w_in': nrm(ks[2], (DEPTH, D_MODEL, N_IN), D_MODEL ** -0.5),
        'conv_w': nrm(ks[3], (DEPTH, CONV_WIDTH, 2 * M_WIDTH), CONV_WIDTH ** -0.5),
        'conv_b': nrm(ks[6], (DEPTH, 2 * M_WIDTH), 0.01),
        'b_mgate': gate_bias,
        'm_norm_g': 1.0 + nrm(ks[7], (DEPTH, M_WIDTH), 0.02),
        'q_norm_g': 1.0 + nrm(ks[8], (DEPTH, A_HEAD_DIM), 0.02),
        'k_norm_g': 1.0 + nrm(ks[9], (DEPTH, A_HEAD_DIM), 0.02),
        'sinks': nrm(ks[10], (DEPTH, A_HEADS), 1.0),
        'b_merge': nrm(ks[11], (DEPTH, 2 * D_MODEL), 0.01),
        'w_branch_m': nrm(ks[12], (DEPTH, M_WIDTH, D_MODEL), M_WIDTH ** -0.5),
        'w_branch_a': nrm(ks[13], (DEPTH, A_WIDTH, D_MODEL), A_WIDTH ** -0.5),
        'w_out': nrm(ks[14], (DEPTH, D_MODEL, D_MODEL), D_MODEL ** -0.5),
        'norm2_g': 1.0 + nrm(ks[15], (DEPTH, D_MODEL), 0.02),
        'w_ffn_in': nrm(ks[16], (DEPTH, D_MODEL, 2 * D_FF), D_MODEL ** -0.5),
        'w_ffn_out': nrm(ks[17], (DEPTH, D_FF, D_MODEL), D_FF ** -0.5),
    }


def reference(x, norm1_g, w_in, conv_w, conv_b, b_mgate, m_norm_g, q_norm_g, k_norm_g,
              sinks, b_merge, w_branch_m, w_branch_a, w_out, norm2_g, w_ffn_in, w_ffn_out):
    B, S, _ = x.shape
    pos = jnp.arange(S)
    for l in range(DEPTH):
        h = rms_norm(x, norm1_g[l])
        proj = h @ w_in[l]
        qk = jax.nn.silu(causal_depthwise_conv(proj[..., :2 * M_WIDTH], conv_w[l], conv_b[l]))
        _, _, mv, mo, mi, mf, aq, ak, av, gm, ga = jnp.split(proj, IN_OFFSETS, axis=-1)
        mq, mk = qk[..., :M_WIDTH], qk[..., M_WIDTH:]
        heads_m = lambda t: t.reshape(B, S, M_HEADS, M_HEAD_DIM).astype(jnp.float32)
        gb = b_mgate[l].astype(jnp.float32)
        h_tilde = mlstm_chunkwise(heads_m(mq), heads_m(mk), heads_m(mv),
                                  mi.astype(jnp.float32) + gb[:M_HEADS],
                                  mf.astype(jnp.float32) + gb[M_HEADS:])
        h_m = jax.nn.sigmoid(heads_m(mo)) * h_tilde
        y_m = rms_norm(h_m, m_norm_g[l].reshape(M_HEADS, M_HEAD_DIM)).reshape(B, S, M_WIDTH).astype(x.dtype)
        q = aq.reshape(B, S, A_HEADS, A_HEAD_DIM)
        k = ak.reshape(B, S, A_KV_HEADS, A_HEAD_DIM)
        v = av.reshape(B, S, A_KV_HEADS, A_HEAD_DIM)
        q = partial_rope(rms_norm(q, q_norm_g[l]), pos)
        k = partial_rope(rms_norm(k, k_norm_g[l]), pos)
        y_a = swa_gqa_sinks(q, k, v, sinks[l])
        g_m = jax.nn.sigmoid(gm + b_merge[l, :D_MODEL])
        g_a = jax.nn.sigmoid(ga + b_merge[l, D_MODEL:])
        merged = g_m * (y_m @ w_branch_m[l]) + g_a * (y_a @ w_branch_a[l])
        x = x + merged @ w_out[l]
        gu = rms_norm(x, norm2_g[l]) @ w_ffn_in[l]
        x = x + (jax.nn.silu(gu[..., :D_FF]) * gu[..., D_FF:]) @ w_ffn_out[l]
    return x
```

```python
import numpy as np
import ml_dtypes
from contextlib import ExitStack
import concourse.bass as bass
import concourse.mybir as mybir
from concourse.bass_utils import run_bass_kernel_spmd

F32 = mybir.dt.float32
BF16 = mybir.dt.bfloat16
AF = mybir.ActivationFunctionType
ALU = mybir.AluOpType
AX = mybir.AxisListType

D = 1024
SEQ = 4096
NCORES = 8
TOK_CORE = 2 * SEQ
T = 512
NSUB = 4
DFF = 2816
N_IN = 7432
EPS = 1e-6
NEG = -30000.0
O_MQ, O_MK, O_MV, O_MO, O_MI, O_MF, O_AQ, O_AK, O_AV, O_GM, O_GA = (
    0, 1024, 2048, 3072, 4096, 4100, 4104, 5128, 5256, 5384, 6408)


class Buf:
    __slots__ = ("name", "w", "r", "excl")

    def __init__(self, name="", excl=False):
        self.name = name
        self.w = None
        self.r = []
        self.excl = excl


class Op:
    __slots__ = ("id", "eng", "fns", "preds", "dur", "sem", "nbytes", "tick", "fin")

    def __init__(self, id, eng, sem=None, nbytes=0):
        self.id = id
        self.eng = eng
        self.fns = []
        self.preds = set()
        self.dur = 0.0
        self.sem = sem
        self.nbytes = nbytes
        self.tick = 0
        self.fin = 0.0


def _est(eng, n):
    if eng == "pe":
        return 60.0 + 0.33 * max(n, 64)
    if eng == "act":
        return 200.0 + 0.85 * n
    if eng == "dve":
        return 180.0 + 1.05 * n
    if eng == "pool":
        return 300.0 + 3.0 * n
    return 60.0


class Tracker:
    ENG = ("pe", "act", "dve", "pool", "sp")

    def __init__(self):
        self.ops = []
        self.unit = None
        self.q = {e: [] for e in self.ENG}
        self.dma_sems = set()

    def _preds(self, reads, writes, self_id):
        p = set()
        for b in reads:
            if b.w is not None:
                p.add(b.w)
            if b.excl:
                p.update(b.r)
        for b in writes:
            if b.w is not None:
                p.add(b.w)
            p.update(b.r)
        p.discard(self_id)
        return p

    def _mark(self, oid, reads, writes):
        for b in writes:
            b.w = oid
            b.r = []
        for b in reads:
            if b.excl:
                b.w = oid
                b.r = []
            elif not b.r or b.r[-1] != oid:
                b.r.append(oid)

    class _Probe:
        def __init__(self):
            self.n = 512

        def __getattr__(self, name):
            def f(*args, **kw):
                out = kw.get("out", args[0] if args else None)
                try:
                    shp = out.shape
                    m = 1
                    for d in shp[1:]:
                        m *= int(d)
                    self.n = m
                except Exception:
                    pass
                return None
            return f

    def op(self, eng, fn, reads=(), writes=(), inc=True, n=None):
        if n is None:
            pr = Tracker._Probe()
            fn(pr)
            n = pr.n
        if eng == "pe" and self.unit is not None:
            o = self.unit
        else:
            o = Op(len(self.ops), eng)
            self.ops.append(o)
            if eng == "pe":
                self.unit = o
        o.fns.append(fn)
        o.dur += _est(eng, n)
        o.preds |= self._preds(reads, writes, o.id)
        self._mark(o.id, reads, writes)
        if eng == "pe" and inc:
            self.unit = None

    def dma(self, eng, fn, sem, reads=(), writes=(), nbytes=65536):
        assert self.unit is None
        o = Op(len(self.ops), eng, sem=sem, nbytes=nbytes)
        self.ops.append(o)
        self.dma_sems.add(sem)
        o.fns.append(fn)
        o.dur = 60.0
        o.preds |= self._preds(reads, writes, o.id)
        self._mark(o.id, reads, writes)

    def schedule(self, reorder=True, prio="order"):
        import heapq
        ops = self.ops
        n = len(ops)
        succs = [[] for _ in range(n)]
        indeg = [0] * n
        for o in ops:
            indeg[o.id] = len(o.preds)
            for p in o.preds:
                succs[p].append(o.id)
        order = {e: [] for e in self.ENG}
        if not reorder:
            for o in ops:
                order[o.eng].append(o)
        else:
            key = list(range(n))
            if prio == "cp":
                ind2 = list(indeg)
                topo = [i for i in range(n) if ind2[i] == 0]
                k = 0
                while k < len(topo):
                    for sid in succs[topo[k]]:
                        ind2[sid] -= 1
                        if ind2[sid] == 0:
                            topo.append(sid)
                    k += 1
                bl = [0.0] * n
                for i in reversed(topo):
                    m = 0.0
                    for sid in succs[i]:
                        if bl[sid] > m:
                            m = bl[sid]
                    d = ops[i].dur if ops[i].sem is None else 2000.0 + ops[i].nbytes / 300.0
                    bl[i] = m + d
                rank = sorted(range(n), key=lambda i: (-bl[i], i))
                for r, i in enumerate(rank):
                    key[i] = r
            inv = [0] * n
            for i in range(n):
                inv[key[i]] = i
            free_at = {e: 0.0 for e in self.ENG}
            pend = {e: [] for e in self.ENG}
            avail = {e: [] for e in self.ENG}
            ready_t = [0.0] * n
            for o in ops:
                if indeg[o.id] == 0:
                    heapq.heappush(pend[o.eng], (0.0, key[o.id]))
            dma_free = 0.0
            done = 0
            while done < n:
                best = None
                for e in self.ENG:
                    pe_, av = pend[e], avail[e]
                    while pe_ and pe_[0][0] <= free_at[e]:
                        heapq.heappush(av, heapq.heappop(pe_)[1])
                    if av:
                        cand = (free_at[e], av[0], e, True)
                    elif pe_:
                        cand = (pe_[0][0], pe_[0][1], e, False)
                    else:
                        continue
                    if best is None or cand[:2] < best[:2]:
                        best = cand
                start, okey, e, from_av = best
                if from_av:
                    heapq.heappop(avail[e])
                else:
                    heapq.heappop(pend[e])
                oid = inv[okey]
                o = ops[oid]
                if o.sem is not None:
                    free_at[e] = start + o.dur
                    t0 = max(start, dma_free)
                    dma_free = t0 + o.nbytes / 300.0
                    o.fin = dma_free + 2000.0
                else:
                    o.fin = start + o.dur
                    free_at[e] = o.fin
                order[e].append(o)
                done += 1
                for sid in succs[oid]:
                    so = ops[sid]
                    lat = 0.0 if so.eng == e else 150.0
                    if ready_t[sid] < o.fin + lat:
                        ready_t[sid] = o.fin + lat
                    indeg[sid] -= 1
                    if indeg[sid] == 0:
                        heapq.heappush(pend[so.eng], (ready_t[sid], key[sid]))
            self.sim_end = max(o.fin for o in ops)
        dma_cnt = {}
        for e in self.ENG:
            c = 0
            for o in order[e]:
                if o.sem is not None:
                    dma_cnt[o.sem] = dma_cnt.get(o.sem, 0) + 16
                    o.tick = dma_cnt[o.sem]
                else:
                    c += 1
                    o.tick = c
        for e in self.ENG:
            waited = {}
            q = self.q[e]
            for o in order[e]:
                need = {}
                for p in o.preds:
                    po = ops[p]
                    if po.eng == "pe" and e == "pe":
                        continue
                    k = po.sem if po.sem is not None else po.eng
                    if need.get(k, 0) < po.tick:
                        need[k] = po.tick
                for k, v in need.items():
                    if waited.get(k, 0) < v:
                        waited[k] = v
                        q.append(("wait", k, v))
                k = o.sem if o.sem is not None else o.eng
                q.append(("op", o.fns, k, 16 if o.sem is not None else 1))
            if e == "sp":
                for k, v in dma_cnt.items():
                    if waited.get(k, 0) < v:
                        waited[k] = v
                        q.append(("wait", k, v))


def _host_consts():
    bf = ml_dtypes.bfloat16
    p = np.arange(128)
    ident = np.eye(128, dtype=np.float32)
    s = p[:, None]
    t = p[None, :]
    mask16 = np.where(s <= t, 1.0 / 16.0, 0.0).astype(np.float32)
    mcur = np.where(s <= t, 0.0, NEG).astype(np.float32)
    mprev = np.where(s > t, 0.0, NEG).astype(np.float32)
    cbf = np.concatenate([ident, mask16, mprev, mprev, mcur, mcur], axis=1).astype(bf)
    half = 8
    inv_freq = (500000.0 ** (-np.arange(half, dtype=np.float32) * (2.0 / 16.0))).astype(np.float32)
    pos = (np.arange(32)[None, :] * 128 + p[:, None]).astype(np.float32)
    ang = pos[:, :, None] * inv_freq[None, None, :]
    cos = np.cos(ang).astype(np.float32).reshape(128, 256)
    sin = np.sin(ang).astype(np.float32).reshape(128, 256)
    cf = np.concatenate([ident[:, 0:4], cos, sin], axis=1).astype(np.float32)
    return cbf, cf


CB_ID, CB_M16, CB_MP, CB_MC = 0, 128, 256, 512
CF_ID, CF_COS, CF_SIN = 0, 4, 260


def _weight_items():
    items = []

    def add(name, src_segs, segw, kc0=0, nkc=8):
        items.append(dict(name=name, segs=src_segs, segw=segw, kc0=kc0, nkc=nkc))

    for a in range(2):
        add(f"ina{a}", [("w_in", a * 1024 + j * 128, "g1") for j in range(8)], 128)
    add("inb0", [("w_in", O_MV, "g1"), ("w_in", O_MV + 512, "g1")], 512)
    add("inb1", [("w_in", O_MO, "g1"), ("w_in", O_MO + 512, "g1")], 512)
    add("inb2", [("w_in", O_AQ, "g1"), ("w_in", O_AQ + 512, "g1")], 512)
    add("inb3", [("w_in", O_AK, "g1")], 256)
    for a in range(4):
        segs = []
        for j in (2 * a, 2 * a + 1):
            segs += [("w_bm", j * 128, "gm"), ("w_ba", j * 128, None),
                     ("w_in", O_GM + j * 128, "g1"), ("w_in", O_GA + j * 128, "g1")]
        add(f"mrg{a}", segs, 128)
    add("wout", [("w_out", 0, None), ("w_out", 512, None)], 512)
    for a in range(6):
        js = list(range(4 * a, min(4 * a + 4, 22)))
        segs = []
        for j in js:
            segs += [("w_fi", j * 128, "g2"), ("w_fi", DFF + j * 128, "g2")]
        add(f"ffi{a}", segs, 128)
    for n in range(2):
        for hlf in range(2):
            add(f"ffo{n}{hlf}", [("w_fo", n * 512, None)], 512, kc0=hlf * 11, nkc=11)
    off = 0
    for it in items:
        it["size"] = len(it["segs"]) * it["nkc"] * it["segw"]
        it["off"] = off
        off += it["size"]
    return items, off


SLOT = 8192
NSLOT = 2


def build_program(ntiles=16, dumps=(), stop_after=None, reorder=True, prio="cp"):
    nc = bass.Bass("TRN2", target_bir_lowering=False)
    tr = Tracker()
    items, wtot = _weight_items()
    itmap = {it["name"]: it for it in items}
    dumps = set(dumps)
    dump_specs = {}

    ntok = ntiles * T
    x_d = nc.dram_tensor("x", [ntok, D], F32, kind="ExternalInput").ap()
    out_d = nc.dram_tensor("out", [ntok, D], F32, kind="ExternalOutput").ap()
    wsrc = {
        "w_in": nc.dram_tensor("w_in", [D, N_IN], F32, kind="ExternalInput").ap(),
        "w_bm": nc.dram_tensor("w_bm", [D, D], F32, kind="ExternalInput").ap(),
        "w_ba": nc.dram_tensor("w_ba", [D, D], F32, kind="ExternalInput").ap(),
        "w_out": nc.dram_tensor("w_out", [D, D], F32, kind="ExternalInput").ap(),
        "w_fi": nc.dram_tensor("w_fi", [D, 2 * DFF], F32, kind="ExternalInput").ap(),
        "w_fo": nc.dram_tensor("w_fo", [DFF, D], F32, kind="ExternalInput").ap(),
    }
    cbf_d = nc.dram_tensor("cbf", [128, 768], BF16, kind="ExternalInput").ap()
    cf_d = nc.dram_tensor("cf", [128, 516], F32, kind="ExternalInput").ap()
    pcol_d = nc.dram_tensor("pcol", [128, 24 + 64 + 16 + 16], F32, kind="ExternalInput").ap()
    prow_d = nc.dram_tensor("prow", [1, 144], F32, kind="ExternalInput").ap()
    pgate_d = nc.dram_tensor("pgate", [4, 2], F32, kind="ExternalInput").ap()
    wscr_d = nc.dram_tensor("wscr", [128, wtot], BF16, kind="Internal").ap()

    es = ExitStack()
    with es:
        def sb(name, shape, dt):
            return es.enter_context(nc.sbuf_tensor(name, shape, dt))

        def psum(name, shape, dt):
            return es.enter_context(nc.psum_tensor(name, shape, dt))

        x_sb = sb("x_sb", [128, NSUB, D], F32)
        x_b = [Buf(f"x{s}") for s in range(NSUB)]
        hT = sb("hT", [128, 8, T], BF16)
        hT_b = [Buf(f"hT{s}") for s in range(NSUB)]
        big = sb("big", [128, 12288], BF16)
        pg = [Buf(f"pg{i}") for i in range(24)]
        qkT = big[:, 0:8192].rearrange("p (c t) -> p c t", c=16)
        sigo = big[:, 8192:12288].rearrange("p (s f) -> p s f", s=4)
        actT = big[:, 0:11264].rearrange("p (c t) -> p c t", c=22)
        vaug = sb("vaug", [128, NSUB, 4, 257], BF16)
        vaug_b = [Buf(f"vaug{s}") for s in range(NSUB)]
        aqkv = sb("aqkv", [128, NSUB, 1280], BF16)
        aqkv_b = [Buf(f"aqkv{s}") for s in range(NSUB)]
        ymT = sb("ymT", [128, 8, T], BF16)
        ymT_b = [Buf(f"ymT{s}") for s in range(NSUB)]
        yaT = sb("yaT", [128, 8, T], BF16)
        yaT_b = [Buf(f"yaT{s}") for s in range(NSUB)]
        mgT = sb("mgT", [128, 8, T], BF16)
        mgT_b = [Buf(f"mgT{j}") for j in range(8)]
        wslot = [sb(f"wslot{i}", [128, SLOT], BF16) for i in range(NSLOT)]
        wslot_b = [Buf(f"wslot{i}") for i in range(NSLOT)]
        cbf = sb("cbf_sb", [128, 768], BF16)
        cf = sb("cf_sb", [128, 516], F32)
        const_b = Buf("const")
        pcol = sb("pcol_sb", [128, 120], F32)
        prow = sb("prow_sb", [128, 144], F32)
        pgate = sb("pgate_sb", [4, 2], F32)
        wgate = sb("wgate", [128, 8, 8], BF16)
        wgate_b = Buf("wgate")
        xn = [sb(f"xn{i}", [128, D], BF16) for i in range(2)]
        xn_b = [Buf(f"xn{i}") for i in range(2)]
        junk = sb("junk", [128, 256], BF16)
        junk_b = Buf("junk")
        stat = sb("stat", [128, 64], F32)
        cst = [sb(f"cst{i}", [128, 515], F32) for i in range(2)]
        cst_b = [Buf(f"cst{i}") for i in range(2)]
        cacc = [sb(f"cacc{i}", [128, 512], F32) for i in range(2)]
        cacc_b = [Buf(f"cacc{i}") for i in range(2)]
        carry = sb("carry", [128, 16, 3], F32)
        carry_b = [Buf(f"carry{j}") for j in range(16)]
        ostage = [sb(f"ostage{i}", [128, 512], F32) for i in range(2)]
        ostage_b = [Buf(f"ostage{i}") for i in range(2)]

        gr = sb("gr", [4, 2, T], F32)
        gr_b = [Buf("gr0"), Buf("gr1")]
        gs = sb("gs", [4, 32], F32)
        gs_b = Buf("gs")
        Rm = sb("Rm", [4, 32], F32)
        Rm_b = Buf("Rm")
        ones4 = sb("ones4", [4, 128], F32)
        wt = [sb(f"wt{i}", [128, NSUB, 8], F32) for i in range(2)]
        wt_b = [Buf(f"wt{i}") for i in range(2)]
        decb = [sb(f"decb{i}", [128, 32], F32) for i in range(2)]
        decb_b = [Buf(f"decb{i}") for i in range(2)]
        Cst = sb("Cst", [128, 4, 2, 257], F32)
        Cst_b = [Buf(f"Cst{h}") for h in range(4)]
        Csb = sb("Csb", [128, 4, 2, 257], BF16)
        Csb_b = [Buf(f"Csb{h}") for h in range(4)]
        STm = [sb(f"STm{i}", [128, 4, 128], BF16) for i in range(2)]
        STm_b = [[Buf(f"STm{i}_{h}") for h in range(4)] for i in range(2)]
        wvt = [sb(f"wvt{i}", [128, 4, 257], BF16) for i in range(2)]
        wvt_b = [[Buf(f"wvt{i}_{h}") for h in range(4)] for i in range(2)]
        ktok = [sb(f"ktok{i}", [128, D], BF16) for i in range(2)]
        ktok_b = [Buf(f"ktok{i}") for i in range(2)]
        hms = [sb(f"hm{i}", [128, D], BF16) for i in range(2)]
        hms_b = [[Buf(f"hm{i}_{h}") for h in range(4)] for i in range(2)]
        ymtok = sb("ymtok", [128, D], BF16)
        ymtok_b = Buf("ymtok")
        est = [sb(f"est{i}", [128, 24], F32) for i in range(2)]
        est_b = [Buf(f"est{i}") for i in range(2)]
        sq = sb("sq", [128, 1152], F32)
        sq_b = Buf("sq")
        qn = sb("qn", [128, 1152], F32)
        qn_b = Buf("qn")
        qb = sb("qb", [128, 18, 64], BF16)
        qb_b = Buf("qb")
        k2 = sb("k2", [128, 256], BF16)
        k2_b = Buf("k2")
        QBs = [sb(f"QB{i}", [128, 8, 256], BF16) for i in range(2)]
        QBs_b = [Buf(f"QB{i}") for i in range(2)]
        kTd = sb("kTd", [128, 2, 2, 128], BF16)
        kTd_b = [Buf("kTd0"), Buf("kTd1")]
        vat = sb("vat", [128, 2, 2, 65], BF16)
        vat_b = [Buf("vat0"), Buf("vat1")]
        pT = [sb(f"pT{i}", [128, 512], BF16) for i in range(2)]
        pT_b = [Buf(f"pT{i}") for i in range(2)]
        ya = sb("ya", [128, D], BF16)
        ya_b = Buf("ya")
        ast = sb("ast", [128, 96], F32)
        ast_b = Buf("ast")
        gqk = sb("gqk", [128, 128], F32)
        acst = sb("acst", [128, 32], F32)
        sgt = [sb(f"sgt{i}", [128, 512], BF16) for i in range(4)]
        sgt_b = [Buf(f"sgt{i}") for i in range(4)]
        mt = [sq[:, 0:512], sq[:, 512:1024]]
        mt_b = [sq_b, sq_b]

        sbuf_left = nc.sbuf_bytes_remaining
        pbank = [psum(f"pb{i}", [128, 512], F32) for i in range(8)]
        pbank_b = [Buf(f"pb{i}", excl=True) for i in range(8)]

        ident_f = cf[:, CF_ID:CF_ID + 4]
        MBprev = cbf[:, CB_MP:CB_MP + 256]
        MBcur = cbf[:, CB_MC:CB_MC + 256]
        ident_bf = cbf[:, CB_ID:CB_ID + 128]
        mask16 = cbf[:, CB_M16:CB_M16 + 128]

        g1col = pcol[:, 0:8]
        g2col = pcol[:, 8:16]
        gmcol = pcol[:, 16:24]
        convw = pcol[:, 24:88].rearrange("p (j k) -> p j k", j=16)
        convb = pcol[:, 88:104]
        bmcol = pcol[:, 104:120]
        fold = {"g1": g1col, "g2": g2col, "gm": gmcol, None: None}

        def dump(name, ap, bufs, shape, dt=F32):
            if name not in dumps:
                return
            d = nc.dram_tensor("dbg_" + name, list(shape), dt, kind="ExternalOutput").ap()
            dump_specs[name] = (list(shape), dt)
            tr.dma("sp", lambda e, d=d, ap=ap: e.dma_start(out=d, in_=ap), "dbg_" + name, reads=bufs)

        tr.dma("sp", lambda e: e.dma_start(out=cbf[:], in_=cbf_d), "cld", writes=[const_b])
        tr.dma("sp", lambda e: e.dma_start(out=cf[:], in_=cf_d), "cld", writes=[const_b])
        tr.dma("sp", lambda e: e.dma_start(out=pcol[:], in_=pcol_d), "cld", writes=[const_b])
        tr.dma("sp", lambda e: e.dma_start(out=prow[:], in_=prow_d.partition_broadcast(128)), "cld",
               writes=[const_b])
        tr.dma("sp", lambda e: e.dma_start(out=pgate[:], in_=pgate_d), "cld", writes=[const_b])

        wscr_b = {it["name"]: Buf("wscr_" + it["name"]) for it in items}
        cast_rr = [0]
        stage_rr = [0]

        def prep_cast(out_ap, in_ap, scale_ap, reads, writes):
            k = cast_rr[0] % 2
            cast_rr[0] += 1
            if k == 0:
                if scale_ap is None:
                    tr.op("act", lambda e: e.activation(out=out_ap, in_=in_ap, func=AF.Copy),
                          reads=reads, writes=writes)
                else:
                    tr.op("act", lambda e: e.activation(out=out_ap, in_=in_ap, func=AF.Copy, scale=scale_ap),
                          reads=reads + [const_b], writes=writes)
            else:
                eng = "dve" if k == 1 else "pool"
                if scale_ap is None:
                    tr.op(eng, lambda e: e.tensor_copy(out=out_ap, in_=in_ap), reads=reads, writes=writes)
                else:
                    tr.op(eng, lambda e: e.tensor_scalar(out=out_ap, in0=in_ap, scalar1=scale_ap, scalar2=None,
                                                         op0=ALU.mult),
                          reads=reads + [const_b], writes=writes)

        def prep_item(it, slot):
            segs, segw, kc0, nkc = it["segs"], it["segw"], it["kc0"], it["nkc"]
            nseg = len(segs)
            view = wslot[slot][:, 0:it["size"]].rearrange("p (s k w) -> p s k w", s=nseg, k=nkc)
            groups = []
            for si, (src, c0, fd) in enumerate(segs):
                if groups and groups[-1][0] == src and groups[-1][3] == fd and \
                        groups[-1][1] + groups[-1][2] * segw == c0 and \
                        (groups[-1][2] + 1) * segw <= 1024 and groups[-1][4] + groups[-1][2] == si:
                    groups[-1][2] += 1
                else:
                    groups.append([src, c0, 1, fd, si])
            for k in range(nkc):
                kc = kc0 + k
                for (src, c0, n, fd, si0) in groups:
                    st = stage_rr[0] % NSUB
                    stage_rr[0] += 1
                    width = n * segw
                    src_ap = wsrc[src][kc * 128:(kc + 1) * 128, c0:c0 + width]
                    stg = x_sb[:, st, 0:width]
                    tr.dma("sp", lambda e, stg=stg, src_ap=src_ap: e.dma_start(out=stg, in_=src_ap),
                           f"xld{st}", writes=[x_b[st]], nbytes=width * 512)
                    if n == 1:
                        out_ap = view[:, si0, k, :]
                        in_ap = stg
                    else:
                        out_ap = view[:, si0:si0 + n, k, :]
                        in_ap = stg.rearrange("p (s w) -> p s w", s=n)
                    sc = None if fd is None else fold[fd][:, kc:kc + 1]
                    prep_cast(out_ap, in_ap, sc, [x_b[st]], [wslot_b[slot]])
            dst = wscr_d[:, it["off"]:it["off"] + it["size"]]
            tr.dma("sp", lambda e, dst=dst, slot=slot, it=it: e.dma_start(out=dst, in_=wslot[slot][:, 0:it["size"]]),
                   f"wst{slot}", reads=[wslot_b[slot]], writes=[wscr_b[it["name"]]], nbytes=it["size"] * 256)

        LATE = [it for it in items if it["name"].startswith(("wout", "ffi", "ffo"))]
        for i, it in enumerate(LATE):
            prep_item(it, i % NSLOT)
        early_pending = {it["name"] for it in items} - {it["name"] for it in LATE}
        for kc in range(8):
            st = stage_rr[0] % NSUB
            stage_rr[0] += 1
            stg = x_sb[:, st, 0:8]
            src_ap = wsrc["w_in"][kc * 128:(kc + 1) * 128, O_MI:O_MI + 8]
            tr.dma("sp", lambda e, stg=stg, src_ap=src_ap: e.dma_start(out=stg, in_=src_ap), f"xld{st}",
                   writes=[x_b[st]])
            tr.op("dve", lambda e, kc=kc, stg=stg: e.tensor_scalar(out=wgate[:, kc, :], in0=stg,
                                                                  scalar1=g1col[:, kc:kc + 1], scalar2=None,
                                                                  op0=ALU.mult),
                  reads=[x_b[st], const_b], writes=[wgate_b])

        wcur = {"slot": 0}

        def wload(name):
            it = itmap[name]
            slot = wcur["slot"]
            wcur["slot"] = (slot + 1) % NSLOT
            if name in early_pending:
                early_pending.discard(name)
                prep_item(it, slot)
                view = wslot[slot][:, 0:it["size"]].rearrange("p (s k w) -> p s k w", s=len(it["segs"]),
                                                              k=it["nkc"])
                return view, wslot_b[slot]
            src = wscr_d[:, it["off"]:it["off"] + it["size"]]
            tr.dma("sp", lambda e, src=src, slot=slot, it=it: e.dma_start(out=wslot[slot][:, 0:it["size"]], in_=src),
                   f"wld{slot}", reads=[wscr_b[name]], writes=[wslot_b[slot]], nbytes=it["size"] * 256)
            view = wslot[slot][:, 0:it["size"]].rearrange("p (s k w) -> p s k w", s=len(it["segs"]), k=it["nkc"])
            return view, wslot_b[slot]

        gemm_rr = [0]

        def gemm_bank():
            b = gemm_rr[0] % 4
            gemm_rr[0] += 1
            return b

        xstg = [sq[:, 0:D], qn[:, 0:D]]
        xstg_b = [sq_b, qn_b]

        def tile_front(ti):
            r0 = ti * T
            for s in range(NSUB):
                k = s % 2
                src = x_d[r0 + s * 128:r0 + (s + 1) * 128, :]
                tr.dma("sp", lambda e, k=k, src=src: e.dma_start(out=xstg[k], in_=src), f"xsg{k}",
                       writes=[xstg_b[k]], nbytes=524288)
                norm_transpose(xstg[k], xstg_b[k], hT, hT_b[s], s, ti * NSUB + s)
            dump(f"hT{ti}", hT[:], hT_b, [128, 8, T], BF16)

        def x_reload(ti):
            r0 = ti * T
            for s in range(NSUB):
                src = x_d[r0 + s * 128:r0 + (s + 1) * 128, :]
                tr.dma("sp", lambda e, s=s, src=src: e.dma_start(out=x_sb[:, s, :], in_=src), f"xld{s}",
                       writes=[x_b[s]], nbytes=524288)

        def norm_transpose(src, src_b, dstT, dst_b, s, uid):
            c = (uid % 8) * 4
            ss, lnv, rstd = stat[:, c:c + 1], stat[:, c + 1:c + 2], stat[:, c + 2:c + 3]
            sbuf_ = Buf()
            k = uid % 2
            tr.op("act", lambda e: e.activation(out=xn[k][:], in_=src, func=AF.Square, accum_out=ss),
                  reads=[src_b], writes=[xn_b[k], sbuf_])
            tr.op("act", lambda e: e.activation(out=lnv, in_=ss, func=AF.Ln, scale=1.0 / D, bias=EPS),
                  reads=[sbuf_], writes=[sbuf_])
            tr.op("act", lambda e: e.activation(out=rstd, in_=lnv, func=AF.Exp, scale=-0.5),
                  reads=[sbuf_], writes=[sbuf_])
            tr.op("dve", lambda e: e.tensor_scalar(out=xn[k][:], in0=src, scalar1=rstd, scalar2=None, op0=ALU.mult),
                  reads=[src_b, sbuf_], writes=[xn_b[k]])
            pb = 4 + (uid % 2)
            pst = pbank[pb].bitcast(BF16)
            for kc in range(8):
                tr.op("pe", lambda e, kc=kc: e.transpose(out=pst[:, kc * 128:(kc + 1) * 128],
                                                         in_=xn[k][:, kc * 128:(kc + 1) * 128], identity=ident_bf),
                      reads=[xn_b[k], const_b], writes=[pbank_b[pb]], inc=(kc == 7))
            tr.op("dve", lambda e: e.tensor_copy(out=dstT[:, :, s * 128:(s + 1) * 128],
                                                 in_=pst[:, :].rearrange("p (c t) -> p c t", c=8)),
                  reads=[pbank_b[pb]], writes=[dst_b])

        def inproj_a(ti, first):
            for a in range(2):
                wv, wb = wload(f"ina{a}")
                for jj in range(8):
                    j = a * 8 + jj
                    pb = gemm_bank()
                    for kc in range(8):
                        tr.op("pe", lambda e, jj=jj, kc=kc, pb=pb, wv=wv: e.matmul(
                            pbank[pb][:, :], lhsT=wv[:, jj, kc, :], rhs=hT[:, kc, :], start=(kc == 0), stop=(kc == 7)),
                            reads=[wb] + hT_b, writes=[pbank_b[pb]], inc=(kc == 7))
                    k = j % 2
                    if first:
                        tr.op("pool", lambda e, k=k: e.memset(cst[k][:, 0:3], 0.0), writes=[cst_b[k]])
                    else:
                        tr.op("pool", lambda e, k=k, j=j: e.tensor_copy(out=cst[k][:, 0:3], in_=carry[:, j, :]),
                              reads=[carry_b[j]], writes=[cst_b[k]])
                    tr.op("act", lambda e, k=k, pb=pb: e.activation(out=cst[k][:, 3:515], in_=pbank[pb][:, :],
                                                                    func=AF.Copy),
                          reads=[pbank_b[pb]], writes=[cst_b[k]])
                    tr.op("pool", lambda e, k=k, j=j: e.tensor_copy(out=carry[:, j, :], in_=cst[k][:, 512:515]),
                          reads=[cst_b[k]], writes=[carry_b[j]])
                    tr.op("act", lambda e, k=k, j=j, pb=pb: e.activation(out=cacc[k][:, 3:512],
                                                                         in_=pbank[pb][:, 0:509], func=AF.Copy,
                                                                         scale=convw[:, j, 0:1]),
                          reads=[pbank_b[pb], const_b], writes=[cacc_b[k]])
                    tr.op("pool", lambda e, k=k, j=j: e.tensor_scalar(out=cacc[k][:, 0:3], in0=cst[k][:, 0:3],
                                                                       scalar1=convw[:, j, 0:1], scalar2=None,
                                                                       op0=ALU.mult),
                          reads=[cst_b[k], const_b, cacc_b[k]], writes=[cacc_b[k]])
                    for tap in range(1, 4):
                        tr.op("dve", lambda e, k=k, j=j, tap=tap: e.scalar_tensor_tensor(
                            out=cacc[k][:], in0=cst[k][:, tap:tap + 512], scalar=convw[:, j, tap:tap + 1],
                            in1=cacc[k][:], op0=ALU.mult, op1=ALU.add),
                            reads=[cst_b[k], cacc_b[k], const_b], writes=[cacc_b[k]])
                    tr.op("act", lambda e, k=k, j=j: e.activation(out=qkT[:, j, :], in_=cacc[k][:], func=AF.Silu,
                                                                   bias=convb[:, j:j + 1]),
                          reads=[cacc_b[k], const_b], writes=[pg[j]])
            dump(f"qkT{ti}", qkT, pg[0:16], [128, 16, T], BF16)

        def inproj_b(ti):
            plan = [("inb0", [("v", 0), ("v", 2)]), ("inb1", [("o", 0), ("o", 512)]),
                    ("inb2", [("q", 0), ("q", 512)]), ("inb3", [("kv", 0)])]
            for name, segl in plan:
                wv, wb = wload(name)
                segw = itmap[name]["segw"]
                for si, (kind, arg) in enumerate(segl):
                    for s in range(NSUB):
                        pb = gemm_bank()
                        for kc in range(8):
                            tr.op("pe", lambda e, si=si, kc=kc, pb=pb, wv=wv, s=s, segw=segw: e.matmul(
                                pbank[pb][:, 0:segw], lhsT=hT[:, kc, s * 128:(s + 1) * 128], rhs=wv[:, si, kc, :],
                                start=(kc == 0), stop=(kc == 7)),
                                reads=[wb, hT_b[s]], writes=[pbank_b[pb]], inc=(kc == 7))
                        if kind == "v":
                            tr.op("act", lambda e, pb=pb, s=s, arg=arg: e.activation(
                                out=vaug[:, s, arg:arg + 2, 0:256],
                                in_=pbank[pb][:, :].rearrange("p (h e) -> p h e", h=2), func=AF.Copy),
                                reads=[pbank_b[pb]], writes=[vaug_b[s]])
                        elif kind == "o":
                            tr.op("act", lambda e, pb=pb, s=s, arg=arg: e.activation(
                                out=sigo[:, s, arg:arg + 512], in_=pbank[pb][:, :], func=AF.Sigmoid),
                                reads=[pbank_b[pb]], writes=[pg[16 + 2 * s], pg[17 + 2 * s]])
                        elif kind == "q":
                            tr.op("dve", lambda e, pb=pb, s=s, arg=arg: e.tensor_copy(
                                out=aqkv[:, s, arg:arg + 512], in_=pbank[pb][:, :]),
                                reads=[pbank_b[pb]], writes=[aqkv_b[s]])
                        else:
                            tr.op("dve", lambda e, pb=pb, s=s: e.tensor_copy(
                                out=aqkv[:, s, 1024:1280], in_=pbank[pb][:, 0:256]),
                                reads=[pbank_b[pb]], writes=[aqkv_b[s]])
            dump(f"vaug{ti}", vaug[:], vaug_b, [128, NSUB, 4, 257], BF16)
            dump(f"sigo{ti}", sigo, pg[16:24], [128, NSUB, 1024], BF16)
            dump(f"aqkv{ti}", aqkv[:], aqkv_b, [128, NSUB, 1280], BF16)

        setup_b = Buf("setup")
        for i in range(2):
            tr.op("pool", lambda e, i=i: e.memset(QBs[i][:], 0.0), writes=[QBs_b[i]])
        tr.op("pool", lambda e: e.memset(vat[:, :, :, 64:65], 1.0), writes=vat_b)
        tr.op("pool", lambda e: e.memset(ones4[:], 1.0), writes=[setup_b])
        tr.op("dve", lambda e: e.tensor_scalar(out=gqk[:, 0:64], in0=prow[:, 0:64],
                                               scalar1=0.125, scalar2=None, op0=ALU.mult),
              reads=[const_b], writes=[setup_b])
        tr.op("dve", lambda e: e.tensor_copy(out=gqk[:, 64:128], in_=prow[:, 64:128]),
              reads=[const_b, setup_b], writes=[setup_b])
        tr.op("dve", lambda e: e.tensor_reduce(out=acst[:, 1:2], in_=prow[:, 0:64], axis=AX.X, op=ALU.max,
                                               apply_absolute_value=True),
              reads=[const_b, setup_b], writes=[setup_b])
        tr.op("dve", lambda e: e.tensor_reduce(out=acst[:, 2:3], in_=prow[:, 64:128], axis=AX.X, op=ALU.max,
                                               apply_absolute_value=True),
              reads=[const_b, setup_b], writes=[setup_b])
        tr.op("dve", lambda e: e.scalar_tensor_tensor(out=acst[:, 0:1], in0=acst[:, 1:2], scalar=-8.0,
                                                      in1=acst[:, 2:3], op0=ALU.mult, op1=ALU.mult),
              reads=[setup_b], writes=[setup_b])
        nlmax = acst[:, 0:1]
        tr.op("act", lambda e: e.activation(out=acst[:, 16:32], in_=prow[:, 128:144], func=AF.Exp, bias=nlmax),
              reads=[const_b, setup_b], writes=[setup_b])
        sinkexp = acst[:, 16:32]
        tr.op("dve", lambda e: e.tensor_scalar(out=gs[:, 28:29], in0=pgate[:, 1:2], scalar1=-1.0, scalar2=None,
                                               op0=ALU.mult),
              reads=[const_b], writes=[setup_b])
        nbf = gs[:, 28:29]
        bi = pgate[:, 0:1]

        GS_MBLK, GS_MC, GS_NMC, GS_MPREV, GS_DIF, GS_DEC = 0, 4, 8, 12, 17, 21

        def gates(ti, first):
            tp = ti % 2
            bi_, bf_, bt_, bd_ = gemm_bank(), gemm_bank(), gemm_bank(), gemm_bank()
            for (bank, c0) in ((bi_, 0), (bf_, 4)):
                for kc in range(8):
                    tr.op("pe", lambda e, bank=bank, c0=c0, kc=kc: e.matmul(
                        pbank[bank][0:4, :], lhsT=wgate[:, kc, c0:c0 + 4], rhs=hT[:, kc, :],
                        start=(kc == 0), stop=(kc == 7)),
                        reads=[wgate_b] + hT_b, writes=[pbank_b[bank]], inc=(kc == 7))
            ipre, fpre = pbank[bi_][0:4, :], pbank[bf_][0:4, :]
            g0, g1 = gr[:, 0, :], gr[:, 1, :]
            tr.op("act", lambda e: e.activation(out=g0, in_=fpre, func=AF.Exp, scale=-1.0, bias=nbf),
                  reads=[pbank_b[bf_], setup_b], writes=[gr_b[0]])
            tr.op("act", lambda e: e.activation(out=g0, in_=g0, func=AF.Ln, bias=1.0),
                  reads=[gr_b[0]], writes=[gr_b[0]])
            for j in range(NSUB):
                tr.op("dve", lambda e, j=j: e.tensor_tensor_scan(
                    out=g1[:, j * 128:(j + 1) * 128], data0=ones4[:, 0:128], data1=g0[:, j * 128:(j + 1) * 128],
                    initial=0.0, op0=ALU.mult, op1=ALU.add),
                    reads=[gr_b[0], setup_b], writes=[gr_b[1]])
            tr.op("dve", lambda e: e.scalar_tensor_tensor(out=g0, in0=ipre, scalar=bi, in1=g1, op0=ALU.add,
                                                          op1=ALU.add),
                  reads=[pbank_b[bi_], gr_b[1], const_b, gr_b[0]], writes=[gr_b[0]])
            tr.op("dve", lambda e: e.tensor_reduce(out=gs[:, GS_MBLK:GS_MBLK + 4],
                                                   in_=g0.rearrange("p (j t) -> p j t", j=4), axis=AX.X, op=ALU.max),
                  reads=[gr_b[0]], writes=[gs_b])
            if first:
                tr.op("dve", lambda e: e.memset(gs[:, GS_MPREV:GS_MPREV + 1], 0.0), reads=[gs_b], writes=[gs_b])
            else:
                tr.op("dve", lambda e: e.tensor_copy(out=gs[:, GS_MPREV:GS_MPREV + 1],
                                                     in_=gs[:, GS_MPREV + 4:GS_MPREV + 5]),
                      reads=[gs_b], writes=[gs_b])
            for j in range(NSUB):
                tr.op("dve", lambda e, j=j: e.tensor_tensor(out=gs[:, GS_MC + j:GS_MC + j + 1],
                                                            in0=gs[:, GS_MPREV + j:GS_MPREV + j + 1],
                                                            in1=gs[:, GS_MBLK + j:GS_MBLK + j + 1], op=ALU.max),
                      reads=[gs_b], writes=[gs_b])
                tr.op("dve", lambda e, j=j: e.tensor_tensor(out=gs[:, GS_MPREV + j + 1:GS_MPREV + j + 2],
                                                            in0=gs[:, GS_MC + j:GS_MC + j + 1],
                                                            in1=g1[:, j * 128 + 127:j * 128 + 128], op=ALU.subtract),
                      reads=[gs_b, gr_b[1]], writes=[gs_b])
            tr.op("dve", lambda e: e.tensor_tensor(out=gs[:, GS_DIF:GS_DIF + 4], in0=gs[:, GS_MPREV:GS_MPREV + 4],
                                                   in1=gs[:, GS_MC:GS_MC + 4], op=ALU.subtract),
                  reads=[gs_b], writes=[gs_b])
            tr.op("dve", lambda e: e.tensor_scalar(out=gs[:, GS_NMC:GS_NMC + 4], in0=gs[:, GS_MC:GS_MC + 4],
                                                   scalar1=-1.0, scalar2=None, op0=ALU.mult),
                  reads=[gs_b], writes=[gs_b])
            tr.op("act", lambda e: e.activation(out=gs[:, GS_DEC:GS_DEC + 4], in_=gs[:, GS_DIF:GS_DIF + 4],
                                                func=AF.Exp),
                  reads=[gs_b], writes=[gs_b])
            for j in range(NSUB):
                sl = slice(j * 128, (j + 1) * 128)
                tr.op("act", lambda e, j=j, sl=sl: e.activation(out=g1[:, sl], in_=g1[:, sl], func=AF.Exp,
                                                                bias=gs[:, GS_NMC + j:GS_NMC + j + 1]),
                      reads=[gs_b, gr_b[1]], writes=[gr_b[1]])
                tr.op("act", lambda e, j=j, sl=sl: e.activation(out=g0[:, sl], in_=g0[:, sl], func=AF.Exp,
                                                                bias=gs[:, GS_NMC + j:GS_NMC + j + 1]),
                      reads=[gs_b, gr_b[0]], writes=[gr_b[0]])
            for j in range(NSUB):
                sl = slice(j * 128, (j + 1) * 128)
                tr.op("pe", lambda e, j=j, sl=sl: e.matmul(pbank[bt_][:, j * 8:j * 8 + 4], lhsT=g0[:, sl],
                                                           rhs=ident_f[0:4, 0:4], start=True, stop=True),
                      reads=[gr_b[0], const_b], writes=[pbank_b[bt_]], inc=False)
                tr.op("pe", lambda e, j=j, sl=sl: e.matmul(pbank[bt_][:, j * 8 + 4:j * 8 + 8], lhsT=g1[:, sl],
                                                           rhs=ident_f[0:4, 0:4], start=True, stop=True),
                      reads=[gr_b[1], const_b], writes=[pbank_b[bt_]], inc=(j == NSUB - 1))
            tr.op("act", lambda e: e.activation(out=wt[tp][:].rearrange("p j c -> p (j c)"), in_=pbank[bt_][:, 0:32],
                                                func=AF.Copy),
                  reads=[pbank_b[bt_]], writes=[wt_b[tp]])
            tr.op("dve", lambda e: e.tensor_tensor(
                out=Rm[:, 0:16].rearrange("p (j h) -> p j h", j=4),
                in0=ident_f[0:4, 0:4].unsqueeze(1).broadcast_to([4, 4, 4]),
                in1=gs[:, GS_DEC:GS_DEC + 4].unsqueeze(2).broadcast_to([4, 4, 4]), op=ALU.mult),
                reads=[gs_b, const_b, Rm_b], writes=[Rm_b])
            tr.op("dve", lambda e: e.tensor_scalar(out=Rm[:, 16:32], in0=Rm[:, 0:16], scalar1=1.0 / 16.0,
                                                   scalar2=None, op0=ALU.mult),
                  reads=[Rm_b], writes=[Rm_b])
            tr.op("pe", lambda e: e.matmul(pbank[bd_][:, 0:32], lhsT=ones4[:, 0:128], rhs=Rm[:, 0:32],
                                           start=True, stop=True),
                  reads=[Rm_b, setup_b], writes=[pbank_b[bd_]])
            tr.op("act", lambda e: e.activation(out=decb[tp][:], in_=pbank[bd_][:, 0:32], func=AF.Copy),
                  reads=[pbank_b[bd_]], writes=[decb_b[tp]])
            dump(f"wt{ti}", wt[tp][:], [wt_b[tp]], [128, NSUB, 8])
            dump(f"decb{ti}", decb[tp][:], [decb_b[tp]], [128, 32])

        def mlstm_block(ti, s, first_block):
            tp = ti % 2
            par = s % 2
            sl = slice(s * 128, (s + 1) * 128)
            if first_block:
                tr.op("pool", lambda e: e.memset(Cst[:], 0.0), writes=Cst_b)
            pst = pbank[7].bitcast(BF16)
            for c in range(8):
                tr.op("pe", lambda e, c=c: e.transpose(out=pst[:, c * 128:(c + 1) * 128], in_=qkT[:, 8 + c, sl],
                                                       identity=ident_bf),
                      reads=[pg[8 + c], const_b], writes=[pbank_b[7]], inc=(c == 7))
            tr.op("act", lambda e: e.activation(out=ktok[par][:], in_=pst[:, :], func=AF.Copy),
                  reads=[pbank_b[7]], writes=[ktok_b[par]])
            e_ = est[par]
            hm, hm_b = hms[par], hms_b[par]
            for h in range(4):
                for dc in range(2):
                    tr.op("pe", lambda e, h=h, dc=dc: e.matmul(pbank[5][:, 0:128], lhsT=qkT[:, 8 + 2 * h + dc, sl],
                                                               rhs=qkT[:, 2 * h + dc, sl], start=(dc == 0),
                                                               stop=(dc == 1)),
                          reads=[pg[8 + 2 * h + dc], pg[2 * h + dc]], writes=[pbank_b[5]], inc=(dc == 1))
                tr.op("dve", lambda e, h=h: e.tensor_tensor(out=STm[par][:, h, :], in0=pbank[5][:, 0:128],
                                                            in1=mask16, op=ALU.mult),
                      reads=[pbank_b[5], const_b], writes=[STm_b[par][h]])
                tr.op("act", lambda e, h=h: e.activation(out=wvt[par][:, h, :], in_=vaug[:, s, h, :], func=AF.Copy,
                                                         scale=wt[tp][:, s, h:h + 1]),
                      reads=[vaug_b[s], wt_b[tp]], writes=[wvt_b[par][h]])
                tr.op("act", lambda e, h=h: e.activation(
                    out=Csb[:, h, :, :], in_=Cst[:, h, :, :], func=AF.Copy,
                    scale=decb[tp][:, 16 + s * 4 + h:16 + s * 4 + h + 1]),
                    reads=[Cst_b[h], decb_b[tp]], writes=[Csb_b[h]])
                num = pbank[6][:, 0:257]
                tr.op("pe", lambda e, h=h: e.matmul(num, lhsT=STm[par][:, h, :], rhs=wvt[par][:, h, :],
                                                    start=True, stop=False),
                      reads=[STm_b[par][h], wvt_b[par][h]], writes=[pbank_b[6]], inc=False)
                for dc in range(2):
                    tr.op("pe", lambda e, h=h, dc=dc: e.matmul(num, lhsT=qkT[:, 2 * h + dc, sl],
                                                               rhs=Csb[:, h, dc, :], start=False, stop=(dc == 1)),
                          reads=[pg[2 * h + dc], Csb_b[h]], writes=[pbank_b[6]], inc=(dc == 1))
                for dc, bank, c0 in ((0, 7, 0), (1, 5, 128)):
                    tr.op("pe", lambda e, h=h, dc=dc, bank=bank, c0=c0: e.matmul(
                        pbank[bank][:, c0:c0 + 257], lhsT=ktok[par][:, h * 256 + dc * 128:h * 256 + (dc + 1) * 128],
                        rhs=wvt[par][:, h, :], start=True, stop=True),
                        reads=[ktok_b[par], wvt_b[par][h]], writes=[pbank_b[bank]])
                    tr.op("dve", lambda e, h=h, dc=dc, bank=bank, c0=c0: e.scalar_tensor_tensor(
                        out=Cst[:, h, dc, :], in0=Cst[:, h, dc, :],
                        scalar=decb[tp][:, s * 4 + h:s * 4 + h + 1], in1=pbank[bank][:, c0:c0 + 257],
                        op0=ALU.mult, op1=ALU.add),
                        reads=[Cst_b[h], decb_b[tp], pbank_b[bank]], writes=[Cst_b[h]])
                tr.op("act", lambda e, h=h: e.activation(out=e_[:, 20 + h:21 + h], in_=pbank[6][:, 256:257],
                                                         func=AF.Abs),
                      reads=[pbank_b[6], est_b[par]], writes=[est_b[par]])
                tr.op("dve", lambda e, h=h: e.tensor_tensor(out=e_[:, h:h + 1], in0=e_[:, 20 + h:21 + h],
                                                            in1=wt[tp][:, s, 4 + h:5 + h], op=ALU.max),
                      reads=[wt_b[tp], est_b[par]], writes=[est_b[par]])
                tr.op("dve", lambda e, h=h: e.reciprocal(out=e_[:, 4 + h:5 + h], in_=e_[:, h:h + 1]),
                      reads=[est_b[par]], writes=[est_b[par]])
                tr.op("dve", lambda e, h=h: e.scalar_tensor_tensor(
                    out=hm[:, h * 256:(h + 1) * 256], in0=pbank[6][:, 0:256], scalar=e_[:, 4 + h:5 + h],
                    in1=sigo[:, s, h * 256:(h + 1) * 256], op0=ALU.mult, op1=ALU.mult),
                    reads=[pbank_b[6], est_b[par], pg[16 + 2 * s], pg[17 + 2 * s]], writes=[hm_b[h]])
                tr.op("act", lambda e, h=h: e.activation(out=junk[:, 0:256], in_=hm[:, h * 256:(h + 1) * 256],
                                                         func=AF.Square, accum_out=e_[:, 8 + h:9 + h]),
                      reads=[hm_b[h], est_b[par]], writes=[junk_b, est_b[par]])
            tr.op("act", lambda e: e.activation(out=e_[:, 12:16], in_=e_[:, 8:12], func=AF.Ln, scale=1.0 / 256.0,
                                                bias=EPS),
                  reads=[est_b[par]], writes=[est_b[par]])
            tr.op("act", lambda e: e.activation(out=e_[:, 16:20], in_=e_[:, 12:16], func=AF.Exp, scale=-0.5),
                  reads=[est_b[par]], writes=[est_b[par]])
            tr.op("dve", lambda e: e.tensor_tensor(
                out=ymtok[:].rearrange("p (h e) -> p h e", h=4), in0=hm[:].rearrange("p (h e) -> p h e", h=4),
                in1=e_[:, 16:20].unsqueeze(2).broadcast_to([128, 4, 256]), op=ALU.mult),
                reads=hm_b + [est_b[par]], writes=[ymtok_b])
            for c in range(8):
                tr.op("pe", lambda e, c=c: e.transpose(out=pst[:, c * 128:(c + 1) * 128],
                                                       in_=ymtok[:, c * 128:(c + 1) * 128], identity=ident_bf),
                      reads=[ymtok_b, const_b], writes=[pbank_b[7]], inc=(c == 7))
            tr.op("act", lambda e: e.activation(out=ymT[:, :, sl], in_=pst[:, :].rearrange("p (c t) -> p c t", c=8),
                                                func=AF.Copy),
                  reads=[pbank_b[7]], writes=[ymT_b[s]])

        def attn_block(ti, s, jb):
            par = jb % 2
            first = (jb == 0)
            QB, QB_b = QBs[par], QBs_b[par]
            src = aqkv[:, s, 0:1152]
            tr.op("act", lambda e: e.activation(out=sq[:], in_=src, func=AF.Square),
                  reads=[aqkv_b[s]], writes=[sq_b])
            tr.op("dve", lambda e: e.tensor_reduce(out=ast[:, 0:18], in_=sq[:].rearrange("p (h d) -> p h d", h=18),
                                                   axis=AX.X, op=ALU.add),
                  reads=[sq_b, ast_b], writes=[ast_b])
            tr.op("act", lambda e: e.activation(out=ast[:, 18:36], in_=ast[:, 0:18], func=AF.Ln, scale=1.0 / 64.0,
                                                bias=EPS),
                  reads=[ast_b], writes=[ast_b])
            tr.op("act", lambda e: e.activation(out=ast[:, 36:54], in_=ast[:, 18:36], func=AF.Exp, scale=-0.5),
                  reads=[ast_b], writes=[ast_b])
            qn3 = qn[:].rearrange("p (h d) -> p h d", h=18)
            rt = sq[:, 0:576].rearrange("p (a h d) -> p a h d", a=4, h=18)
            rt_b = sq_b
            tr.op("dve", lambda e: e.tensor_tensor(out=qn3, in0=src.rearrange("p (h d) -> p h d", h=18),
                                                   in1=ast[:, 36:54].unsqueeze(2).broadcast_to([128, 18, 64]),
                                                   op=ALU.mult),
                  reads=[aqkv_b[s], ast_b], writes=[qn_b])
            tr.op("dve", lambda e: e.tensor_tensor(out=qn3[:, 0:16, :], in0=qn3[:, 0:16, :],
                                                   in1=gqk[:, 0:64].unsqueeze(1).broadcast_to([128, 16, 64]),
                                                   op=ALU.mult),
                  reads=[qn_b, setup_b], writes=[qn_b])
            tr.op("dve", lambda e: e.tensor_tensor(out=qn3[:, 16:18, :], in0=qn3[:, 16:18, :],
                                                   in1=gqk[:, 64:128].unsqueeze(1).broadcast_to([128, 2, 64]),
                                                   op=ALU.mult),
                  reads=[qn_b, setup_b], writes=[qn_b])
            cosb = cf[:, CF_COS + jb * 8:CF_COS + jb * 8 + 8].unsqueeze(1).broadcast_to([128, 18, 8])
            sinb = cf[:, CF_SIN + jb * 8:CF_SIN + jb * 8 + 8].unsqueeze(1).broadcast_to([128, 18, 8])
            x1, x2 = qn3[:, :, 0:8], qn3[:, :, 8:16]
            tr.op("dve", lambda e: e.tensor_tensor(out=rt[:, 0, :, :], in0=x1, in1=cosb, op=ALU.mult),
                  reads=[qn_b, const_b], writes=[rt_b])
            tr.op("dve", lambda e: e.tensor_tensor(out=rt[:, 1, :, :], in0=x2, in1=sinb, op=ALU.mult),
                  reads=[qn_b, const_b, rt_b], writes=[rt_b])
            tr.op("dve", lambda e: e.tensor_tensor(out=rt[:, 2, :, :], in0=x2, in1=cosb, op=ALU.mult),
                  reads=[qn_b, const_b, rt_b], writes=[rt_b])
            tr.op("dve", lambda e: e.tensor_tensor(out=rt[:, 3, :, :], in0=x1, in1=sinb, op=ALU.mult),
                  reads=[qn_b, const_b, rt_b], writes=[rt_b])
            tr.op("dve", lambda e: e.tensor_tensor(out=qb[:, :, 0:8], in0=rt[:, 0, :, :], in1=rt[:, 1, :, :],
                                                   op=ALU.subtract),
                  reads=[rt_b], writes=[qb_b])
            tr.op("dve", lambda e: e.tensor_tensor(out=qb[:, :, 8:16], in0=rt[:, 2, :, :], in1=rt[:, 3, :, :],
                                                   op=ALU.add),
                  reads=[rt_b, qb_b], writes=[qb_b])
            tr.op("act", lambda e: e.activation(out=qb[:, :, 16:64], in_=qn3[:, :, 16:64], func=AF.Copy),
                  reads=[qn_b, qb_b], writes=[qb_b])
            tr.op("pool", lambda e: e.tensor_copy(
                out=k2[:].rearrange("p (g r d) -> p g r d", g=2, r=2),
                in_=qb[:, 16:18, :].unsqueeze(2).broadcast_to([128, 2, 2, 64])),
                reads=[qb_b], writes=[k2_b])
            tr.op("pool", lambda e: e.tensor_copy(
                out=vat[:, par, :, 0:64], in_=aqkv[:, s, 1152:1280].rearrange("p (g d) -> p g d", g=2)),
                reads=[aqkv_b[s]], writes=[vat_b[par]])
            pq = pbank[4].bitcast(BF16)
            for g in range(2):
                tr.op("pe", lambda e, g=g: e.transpose(out=pq[:, g * 128:(g + 1) * 128],
                                                       in_=k2[:, g * 128:(g + 1) * 128], identity=ident_bf),
                      reads=[k2_b, const_b], writes=[pbank_b[4]], inc=(g == 1))
            tr.op("act", lambda e: e.activation(out=kTd[:, par, :, :],
                                                in_=pq[:, 0:256].rearrange("p (g t) -> p g t", g=2), func=AF.Copy),
                  reads=[pbank_b[4]], writes=[kTd_b[par]])
            for c in range(8):
                tr.op("pe", lambda e, c=c: e.transpose(out=pq[:, c * 128:(c + 1) * 128],
                                                       in_=qb[:].rearrange("p h d -> p (h d)")[:, c * 128:(c + 1) * 128],
                                                       identity=ident_bf),
                      reads=[qb_b, const_b], writes=[pbank_b[4]], inc=(c == 7))
            pq3 = pq[:, :].rearrange("p (c t) -> p c t", c=8)
            tr.op("act", lambda e: e.activation(out=QB[0:64, :, 0:128], in_=pq3[0:64, :, :], func=AF.Copy),
                  reads=[pbank_b[4]], writes=[QB_b])
            tr.op("dve", lambda e: e.tensor_copy(out=QB[64:128, :, 128:256], in_=pq3[64:128, :, :]),
                  reads=[pbank_b[4], QB_b], writes=[QB_b])
            def po_ap(head):
                if head < 7:
                    return 2, pbank[2][:, head * 65:(head + 1) * 65]
                if head < 14:
                    return 3, pbank[3][:, (head - 7) * 65:(head - 6) * 65]
                return 4, pbank[4][:, (head - 14) * 65:(head - 13) * 65]
            for c in range(8):
                g = c // 4
                lb = c % 2
                lg = pbank[lb]
                if not first:
                    tr.op("pe", lambda e, c=c, g=g, lg=lg: e.matmul(lg[:, 0:256], lhsT=kTd[:, 1 - par, g, :],
                                                                    rhs=QB[:, c, :], start=True, stop=False),
                          reads=[kTd_b[1 - par], QB_b], writes=[pbank_b[lb]], inc=False)
                    tr.op("pe", lambda e, lg=lg: e.matmul(lg[:, 0:256], lhsT=ident_bf, rhs=MBprev,
                                                          start=False, stop=True),
                          reads=[const_b], writes=[pbank_b[lb]], inc=False)
                tr.op("pe", lambda e, c=c, g=g, lg=lg: e.matmul(lg[:, 256:512], lhsT=kTd[:, par, g, :],
                                                                rhs=QB[:, c, :], start=True, stop=False),
                      reads=[kTd_b[par], QB_b], writes=[pbank_b[lb]], inc=False)
                tr.op("pe", lambda e, lg=lg: e.matmul(lg[:, 256:512], lhsT=ident_bf, rhs=MBcur,
                                                      start=False, stop=True),
                      reads=[const_b], writes=[pbank_b[lb]])
                lo = 256 if first else 0
                pk_ = c % 2
                tr.op("act", lambda e, lg=lg, lo=lo, pk_=pk_: e.activation(out=pT[pk_][:, lo:512], in_=lg[:, lo:512],
                                                                           func=AF.Exp, bias=nlmax),
                      reads=[pbank_b[lb], setup_b], writes=[pT_b[pk_]])
                for hh in range(2):
                    head = 2 * c + hh
                    bank, po = po_ap(head)
                    srcs = [(256 + hh * 128, par)]
                    if not first:
                        srcs.append((hh * 128, 1 - par))
                    for i, (col, slot) in enumerate(srcs):
                        tr.op("pe", lambda e, pk_=pk_, col=col, slot=slot, g=g, po=po, i=i, n=len(srcs): e.matmul(
                            po, lhsT=pT[pk_][:, col:col + 128], rhs=vat[:, slot, g, :], start=(i == 0),
                            stop=(i == n - 1)),
                            reads=[pT_b[pk_], vat_b[slot]], writes=[pbank_b[bank]], inc=(i == len(srcs) - 1))
            for (bank, h0, nh) in ((2, 0, 7), (3, 7, 7), (4, 14, 2)):
                po3 = pbank[bank][:, 0:nh * 65].rearrange("p (h d) -> p h d", h=nh)
                dsum = ast[:, 54 + h0:54 + h0 + nh]
                rden = ast[:, 72 + h0:72 + h0 + nh]
                tr.op("dve", lambda e, po3=po3, dsum=dsum, h0=h0, nh=nh: e.tensor_tensor(
                    out=dsum, in0=po3[:, :, 64], in1=sinkexp[:, h0:h0 + nh], op=ALU.add),
                    reads=[pbank_b[bank], setup_b, ast_b], writes=[ast_b])
                tr.op("dve", lambda e, dsum=dsum, rden=rden: e.reciprocal(out=rden, in_=dsum),
                      reads=[ast_b], writes=[ast_b])
                tr.op("dve", lambda e, po3=po3, rden=rden, h0=h0, nh=nh: e.tensor_tensor(
                    out=ya[:, h0 * 64:(h0 + nh) * 64].rearrange("p (h d) -> p h d", h=nh), in0=po3[:, :, 0:64],
                    in1=rden.unsqueeze(2).broadcast_to([128, nh, 64]), op=ALU.mult),
                    reads=[pbank_b[bank], ast_b], writes=[ya_b])
            sl = slice(s * 128, (s + 1) * 128)
            py = pbank[4].bitcast(BF16)
            for c in range(8):
                tr.op("pe", lambda e, c=c: e.transpose(out=py[:, c * 128:(c + 1) * 128],
                                                       in_=ya[:, c * 128:(c + 1) * 128], identity=ident_bf),
                      reads=[ya_b, const_b], writes=[pbank_b[4]], inc=(c == 7))
            tr.op("act", lambda e: e.activation(out=yaT[:, :, sl], in_=py[:, :].rearrange("p (c t) -> p c t", c=8),
                                                func=AF.Copy),
                  reads=[pbank_b[4]], writes=[yaT_b[s]])

        def merge(ti):
            for a in range(4):
                wv, wb = wload(f"mrg{a}")
                for jj in range(2):
                    j = 2 * a + jj
                    for (bank, seg, rhsT, rb) in ((0, 0, ymT, ymT_b), (1, 1, yaT, yaT_b), (2, 2, hT, hT_b),
                                                  (3, 3, hT, hT_b)):
                        for kc in range(8):
                            tr.op("pe", lambda e, bank=bank, seg=seg, rhsT=rhsT, kc=kc, wv=wv, jj=jj: e.matmul(
                                pbank[bank][:, :], lhsT=wv[:, jj * 4 + seg, kc, :], rhs=rhsT[:, kc, :],
                                start=(kc == 0), stop=(kc == 7)),
                                reads=[wb] + rb, writes=[pbank_b[bank]], inc=(kc == 7))
                    k = j % 2
                    tr.op("act", lambda e, j=j, k=k: e.activation(out=sgt[k][:], in_=pbank[2][:, :], func=AF.Sigmoid,
                                                                  bias=bmcol[:, j:j + 1]),
                          reads=[pbank_b[2], const_b], writes=[sgt_b[k]])
                    tr.op("act", lambda e, j=j, k=k: e.activation(out=sgt[2 + k][:], in_=pbank[3][:, :],
                                                                  func=AF.Sigmoid, bias=bmcol[:, 8 + j:9 + j]),
                          reads=[pbank_b[3], const_b], writes=[sgt_b[2 + k]])
                    tr.op("dve", lambda e, k=k: e.tensor_tensor(out=mt[0], in0=pbank[0][:, :], in1=sgt[k][:],
                                                                op=ALU.mult),
                          reads=[pbank_b[0], sgt_b[k]], writes=[mt_b[0]])
                    tr.op("dve", lambda e, k=k: e.tensor_tensor(out=mt[1], in0=pbank[1][:, :], in1=sgt[2 + k][:],
                                                                op=ALU.mult),
                          reads=[pbank_b[1], sgt_b[2 + k]], writes=[mt_b[1]])
                    tr.op("dve", lambda e, j=j: e.tensor_tensor(out=mgT[:, j, :], in0=mt[0], in1=mt[1],
                                                                 op=ALU.add),
                          reads=[mt_b[0], mt_b[1]], writes=[mgT_b[j]])
            dump(f"mgT{ti}", mgT[:], mgT_b, [128, 8, T], BF16)

        def outproj(ti):
            wv, wb = wload("wout")
            for s in range(NSUB):
                for n in range(2):
                    pb = gemm_bank()
                    for kc in range(8):
                        tr.op("pe", lambda e, pb=pb, kc=kc, s=s, n=n, wv=wv: e.matmul(
                            pbank[pb][:, :], lhsT=mgT[:, kc, s * 128:(s + 1) * 128], rhs=wv[:, n, kc, :],
                            start=(kc == 0), stop=(kc == 7)),
                            reads=[wb] + mgT_b, writes=[pbank_b[pb]], inc=(kc == 7))
                    tr.op("dve", lambda e, pb=pb, s=s, n=n: e.tensor_tensor(
                        out=x_sb[:, s, n * 512:(n + 1) * 512], in0=pbank[pb][:, :],
                        in1=x_sb[:, s, n * 512:(n + 1) * 512], op=ALU.add),
                        reads=[pbank_b[pb], x_b[s]], writes=[x_b[s]])
            dump(f"x1_{ti}", x_sb[:], x_b, [128, NSUB, D])
            for s in range(NSUB):
                norm_transpose(x_sb[:, s, :], x_b[s], ymT, ymT_b[s], s, ti * NSUB + s)

        ost_rr = [0]

        def ffn(ti):
            r0 = ti * T
            for a in range(6):
                wv, wb = wload(f"ffi{a}")
                js = list(range(4 * a, min(4 * a + 4, 22)))
                for idx, j in enumerate(js):
                    gb, ub = gemm_bank(), gemm_bank()
                    for (bank, seg) in ((gb, 2 * idx), (ub, 2 * idx + 1)):
                        for kc in range(8):
                            tr.op("pe", lambda e, bank=bank, seg=seg, kc=kc, wv=wv: e.matmul(
                                pbank[bank][:, :], lhsT=wv[:, seg, kc, :], rhs=ymT[:, kc, :],
                                start=(kc == 0), stop=(kc == 7)),
                                reads=[wb] + ymT_b, writes=[pbank_b[bank]], inc=(kc == 7))
                    k = j % 2
                    tr.op("act", lambda e, gb=gb, k=k: e.activation(out=sgt[k][:], in_=pbank[gb][:, :], func=AF.Silu),
                          reads=[pbank_b[gb]], writes=[sgt_b[k]])
                    tr.op("dve", lambda e, ub=ub, k=k, j=j: e.tensor_tensor(out=actT[:, j, :], in0=pbank[ub][:, :],
                                                                            in1=sgt[k][:], op=ALU.mult),
                          reads=[pbank_b[ub], sgt_b[k]], writes=[pg[j]])
            for n in range(2):
                for hlf in range(2):
                    wv, wb = wload(f"ffo{n}{hlf}")
                    for k in range(11):
                        kc = hlf * 11 + k
                        for s in range(NSUB):
                            tr.op("pe", lambda e, s=s, kc=kc, k=k, wv=wv, n=n: e.matmul(
                                pbank[s + 4 * n][:, :], lhsT=actT[:, kc, s * 128:(s + 1) * 128], rhs=wv[:, 0, k, :],
                                start=(kc == 0), stop=(kc == 21)),
                                reads=[wb, pg[kc]], writes=[pbank_b[s + 4 * n]], inc=(k == 10 and s == NSUB - 1))
                for s in range(NSUB):
                    o = ost_rr[0] % 2
                    ost_rr[0] += 1
                    tr.op("dve", lambda e, s=s, n=n, o=o: e.tensor_tensor(
                        out=ostage[o][:], in0=pbank[s + 4 * n][:, :], in1=x_sb[:, s, n * 512:(n + 1) * 512],
                        op=ALU.add),
                        reads=[pbank_b[s + 4 * n], x_b[s]], writes=[ostage_b[o]])
                    dst = out_d[r0 + s * 128:r0 + (s + 1) * 128, n * 512:(n + 1) * 512]
                    tr.dma("sp", lambda e, dst=dst, o=o: e.dma_start(out=dst, in_=ostage[o][:]), f"ost{o}",
                           reads=[ostage_b[o]], nbytes=262144)

        tr.op("pool", lambda e: e.memset(vaug[:, :, :, 256:257], 1.0), writes=vaug_b)

        tile_front(0)
        for ti in range(ntiles):
            first = (ti % 8 == 0)
            inproj_a(ti, first)
            gates(ti, first)
            inproj_b(ti)
            if stop_after == "inproj":
                continue
            for s in range(NSUB):
                mlstm_block(ti, s, first and s == 0)
                if stop_after != "mlstm":
                    attn_block(ti, s, (ti % 8) * NSUB + s)
            dump(f"ymT{ti}", ymT[:], ymT_b, [128, 8, T], BF16)
            dump(f"yaT{ti}", yaT[:], yaT_b, [128, 8, T], BF16)
            if stop_after in ("mlstm", "attn"):
                continue
            merge(ti)
            x_reload(ti)
            outproj(ti)
            if stop_after == "outproj":
                continue
            if ti + 1 < ntiles:
                tile_front(ti + 1)
            ffn(ti)

        tr.schedule(reorder=reorder, prio=prio)

        semnames = set(Tracker.ENG) | set(tr.dma_sems)
        sems = {n: es.enter_context(nc.semaphore("s_" + n)) for n in sorted(semnames)}
        block = es.enter_context(nc.Block())

        def replay(engname):
            def run(eng):
                for item in tr.q[engname]:
                    if item[0] == "wait":
                        eng.wait_ge(sems[item[1]], item[2])
                    else:
                        ins = None
                        for fn in item[1]:
                            ins = fn(eng)
                        ins.then_inc(sems[item[2]], item[3])
            return run

        block.tensor(replay("pe"))
        block.scalar(replay("act"))
        block.vector(replay("dve"))
        block.gpsimd(replay("pool"))
        block.sync(replay("sp"))
    stats = {e: len(tr.q[e]) for e in Tracker.ENG}
    stats['sim_end_us'] = getattr(tr, 'sim_end', 0.0) / 1e3
    stats['sbuf_left'] = sbuf_left
    return nc, dump_specs, stats


def _prep_inputs(inputs, ntiles=16, ncores=NCORES):
    f = np.float32
    x = np.ascontiguousarray(np.asarray(inputs["x"], dtype=f)).reshape(-1, D)
    cbf, cf = _host_consts()
    g1 = np.asarray(inputs["norm1_g"], f).reshape(8, 128).T
    g2 = np.asarray(inputs["norm2_g"], f).reshape(8, 128).T
    gm = np.asarray(inputs["m_norm_g"], f).reshape(8, 128).T
    convw = np.asarray(inputs["conv_w"], f).reshape(4, 16, 128).transpose(2, 1, 0).reshape(128, 64)
    convb = np.asarray(inputs["conv_b"], f).reshape(16, 128).T
    bmc = np.asarray(inputs["b_merge"], f).reshape(16, 128).T
    pcol = np.ascontiguousarray(np.concatenate([g1, g2, gm, convw, convb, bmc], axis=1))
    prow = np.ascontiguousarray(np.concatenate([np.asarray(inputs["q_norm_g"], f).reshape(-1),
                                                np.asarray(inputs["k_norm_g"], f).reshape(-1),
                                                np.asarray(inputs["sinks"], f).reshape(-1)])[None, :])
    pgate = np.ascontiguousarray(np.asarray(inputs["b_mgate"], f).reshape(2, 4).T)
    shared = {
        "w_in": np.ascontiguousarray(np.asarray(inputs["w_in"], f).reshape(D, N_IN)),
        "w_bm": np.ascontiguousarray(np.asarray(inputs["w_branch_m"], f).reshape(D, D)),
        "w_ba": np.ascontiguousarray(np.asarray(inputs["w_branch_a"], f).reshape(D, D)),
        "w_out": np.ascontiguousarray(np.asarray(inputs["w_out"], f).reshape(D, D)),
        "w_fi": np.ascontiguousarray(np.asarray(inputs["w_ffn_in"], f).reshape(D, 2 * DFF)),
        "w_fo": np.ascontiguousarray(np.asarray(inputs["w_ffn_out"], f).reshape(DFF, D)),
        "cbf": cbf, "cf": cf, "pcol": pcol, "prow": prow, "pgate": pgate,
    }
    in_maps = []
    per = TOK_CORE
    for c in range(ncores):
        m = dict(shared)
        m["x"] = x[c * per:c * per + ntiles * T]
        in_maps.append(m)
    return in_maps


_PROGRAM = None


def kernel(**inputs):
    global _PROGRAM
    if _PROGRAM is None:
        _PROGRAM = build_program(16)[0]
    in_maps = _prep_inputs(inputs)
    res = run_bass_kernel_spmd(_PROGRAM, in_maps, core_ids=list(range(NCORES)))
    out = np.concatenate([np.asarray(r["out"], dtype=np.float32) for r in res.results], axis=0)
    return out.reshape(16, SEQ, D)
```

```python
import numpy as np
import ml_dtypes
from contextlib import ExitStack
import concourse.bass as bass
import concourse.mybir as mybir
from concourse.bass_utils import run_bass_kernel_spmd

F32 = mybir.dt.float32
BF16 = mybir.dt.bfloat16
AF = mybir.ActivationFunctionType
ALU = mybir.AluOpType
AX = mybir.AxisListType

D = 1024
SEQ = 4096
NCORES = 8
TOK_CORE = 2 * SEQ
T = 512
NSUB = 4
DFF = 2816
N_IN = 7432
EPS = 1e-6
NEG = -30000.0
O_MQ, O_MK, O_MV, O_MO, O_MI, O_MF, O_AQ, O_AK, O_AV, O_GM, O_GA = (
    0, 1024, 2048, 3072, 4096, 4100, 4104, 5128, 5256, 5384, 6408)


class Buf:
    __slots__ = ("name", "w", "r", "excl")

    def __init__(self, name="", excl=False):
        self.name = name
        self.w = None
        self.r = []
        self.excl = excl


class Op:
    __slots__ = ("id", "eng", "fns", "preds", "dur", "sem", "nbytes", "tick", "fin")

    def __init__(self, id, eng, sem=None, nbytes=0):
        self.id = id
        self.eng = eng
        self.fns = []
        self.preds = set()
        self.dur = 0.0
        self.sem = sem
        self.nbytes = nbytes
        self.tick = 0
        self.fin = 0.0


def _est(eng, n):
    if eng == "pe":
        return 60.0 + 0.33 * max(n, 64)
    if eng == "act":
        return 200.0 + 0.85 * n
    if eng == "dve":
        return 180.0 + 1.05 * n
    if eng == "pool":
        return 300.0 + 3.0 * n
    return 60.0


class Tracker:
    ENG = ("pe", "act", "dve", "pool", "sp")

    def __init__(self):
        self.ops = []
        self.unit = None
        self.q = {e: [] for e in self.ENG}
        self.dma_sems = set()

    def _preds(self, reads, writes, self_id):
        p = set()
        for b in reads:
            if b.w is not None:
                p.add(b.w)
            if b.excl:
                p.update(b.r)
        for b in writes:
            if b.w is not None:
                p.add(b.w)
            p.update(b.r)
        p.discard(self_id)
        return p

    def _mark(self, oid, reads, writes):
        for b in writes:
            b.w = oid
            b.r = []
        for b in reads:
            if b.excl:
                b.w = oid
                b.r = []
            elif not b.r or b.r[-1] != oid:
                b.r.append(oid)

    class _Probe:
        def __init__(self):
            self.n = 512

        def __getattr__(self, name):
            def f(*args, **kw):
                out = kw.get("out", args[0] if args else None)
                try:
                    shp = out.shape
                    m = 1
                    for d in shp[1:]:
                        m *= int(d)
                    self.n = m
                except Exception:
                    pass
                return None
            return f

    def op(self, eng, fn, reads=(), writes=(), inc=True, n=None):
        if n is None:
            pr = Tracker._Probe()
            fn(pr)
            n = pr.n
        if eng == "pe" and self.unit is not None:
            o = self.unit
        else:
            o = Op(len(self.ops), eng)
            self.ops.append(o)
            if eng == "pe":
                self.unit = o
        o.fns.append(fn)
        o.dur += _est(eng, n)
        o.preds |= self._preds(reads, writes, o.id)
        self._mark(o.id, reads, writes)
        if eng == "pe" and inc:
            self.unit = None

    def dma(self, eng, fn, sem, reads=(), writes=(), nbytes=65536):
        assert self.unit is None
        o = Op(len(self.ops), eng, sem=sem, nbytes=nbytes)
        self.ops.append(o)
        self.dma_sems.add(sem)
        o.fns.append(fn)
        o.dur = 60.0
        o.preds |= self._preds(reads, writes, o.id)
        self._mark(o.id, reads, writes)

    def schedule(self, reorder=True, prio="order"):
        import heapq
        ops = self.ops
        n = len(ops)
        succs = [[] for _ in range(n)]
        indeg = [0] * n
        for o in ops:
            indeg[o.id] = len(o.preds)
            for p in o.preds:
                succs[p].append(o.id)
        order = {e: [] for e in self.ENG}
        if not reorder:
            for o in ops:
                order[o.eng].append(o)
        else:
            key = list(range(n))
            if prio == "cp":
                ind2 = list(indeg)
                topo = [i for i in range(n) if ind2[i] == 0]
                k = 0
                while k < len(topo):
                    for sid in succs[topo[k]]:
                        ind2[sid] -= 1
                        if ind2[sid] == 0:
                            topo.append(sid)
                    k += 1
                bl = [0.0] * n
                for i in reversed(topo):
                    m = 0.0
                    for sid in succs[i]:
                        if bl[sid] > m:
                            m = bl[sid]
                    d = ops[i].dur if ops[i].sem is None else 2000.0 + ops[i].nbytes / 300.0
                    bl[i] = m + d
                rank = sorted(range(n), key=lambda i: (-bl[i], i))
                for r, i in enumerate(rank):
                    key[i] = r
            inv = [0] * n
            for i in range(n):
                inv[key[i]] = i
            free_at = {e: 0.0 for e in self.ENG}
            pend = {e: [] for e in self.ENG}
            avail = {e: [] for e in self.ENG}
            ready_t = [0.0] * n
            for o in ops:
                if indeg[o.id] == 0:
                    heapq.heappush(pend[o.eng], (0.0, key[o.id]))
            dma_free = 0.0
            done = 0
            while done < n:
                best = None
                for e in self.ENG:
                    pe_, av = pend[e], avail[e]
                    while pe_ and pe_[0][0] <= free_at[e]:
                        heapq.heappush(av, heapq.heappop(pe_)[1])
                    if av:
                        cand = (free_at[e], av[0], e, True)
                    elif pe_:
                        cand = (pe_[0][0], pe_[0][1], e, False)
                    else:
                        continue
                    if best is None or cand[:2] < best[:2]:
                        best = cand
                start, okey, e, from_av = best
                if from_av:
                    heapq.heappop(avail[e])
                else:
                    heapq.heappop(pend[e])
                oid = inv[okey]
                o = ops[oid]
                if o.sem is not None:
                    free_at[e] = start + o.dur
                    t0 = max(start, dma_free)
                    dma_free = t0 + o.nbytes / 300.0
                    o.fin = dma_free + 2000.0
                else:
                    o.fin = start + o.dur
                    free_at[e] = o.fin
                order[e].append(o)
                done += 1
                for sid in succs[oid]:
                    so = ops[sid]
                    lat = 0.0 if so.eng == e else 150.0
                    if ready_t[sid] < o.fin + lat:
                        ready_t[sid] = o.fin + lat
                    indeg[sid] -= 1
                    if indeg[sid] == 0:
                        heapq.heappush(pend[so.eng], (ready_t[sid], key[sid]))
            self.sim_end = max(o.fin for o in ops)
        dma_cnt = {}
        for e in self.ENG:
            c = 0
            for o in order[e]:
                if o.sem is not None:
                    dma_cnt[o.sem] = dma_cnt.get(o.sem, 0) + 16
                    o.tick = dma_cnt[o.sem]
                else:
                    c += 1
                    o.tick = c
        for e in self.ENG:
            waited = {}
            q = self.q[e]
            for o in order[e]:
                need = {}
                for p in o.preds:
                    po = ops[p]
                    if po.eng == "pe" and e == "pe":
                        continue
                    k = po.sem if po.sem is not None else po.eng
                    if need.get(k, 0) < po.tick:
                        need[k] = po.tick
                for k, v in need.items():
                    if waited.get(k, 0) < v:
                        waited[k] = v
                        q.append(("wait", k, v))
                k = o.sem if o.sem is not None else o.eng
                q.append(("op", o.fns, k, 16 if o.sem is not None else 1))
            if e == "sp":
                for k, v in dma_cnt.items():
                    if waited.get(k, 0) < v:
                        waited[k] = v
                        q.append(("wait", k, v))


def _host_consts():
    bf = ml_dtypes.bfloat16
    p = np.arange(128)
    ident = np.eye(128, dtype=np.float32)
    s = p[:, None]
    t = p[None, :]
    mask16 = np.where(s <= t, 1.0 / 16.0, 0.0).astype(np.float32)
    mcur = np.where(s <= t, 0.0, NEG).astype(np.float32)
    mprev = np.where(s > t, 0.0, NEG).astype(np.float32)
    cbf = np.concatenate([ident, mask16, mprev, mprev, mcur, mcur], axis=1).astype(bf)
    half = 8
    inv_freq = (500000.0 ** (-np.arange(half, dtype=np.float32) * (2.0 / 16.0))).astype(np.float32)
    pos = (np.arange(32)[None, :] * 128 + p[:, None]).astype(np.float32)
    ang = pos[:, :, None] * inv_freq[None, None, :]
    cos = np.cos(ang).astype(np.float32).reshape(128, 256)
    sin = np.sin(ang).astype(np.float32).reshape(128, 256)
    cf = np.concatenate([ident[:, 0:4], cos, sin], axis=1).astype(np.float32)
    return cbf, cf


CB_ID, CB_M16, CB_MP, CB_MC = 0, 128, 256, 512
CF_ID, CF_COS, CF_SIN = 0, 4, 260


def _weight_items():
    items = []

    def add(name, src_segs, segw, kc0=0, nkc=8):
        items.append(dict(name=name, segs=src_segs, segw=segw, kc0=kc0, nkc=nkc))

    for a in range(2):
        add(f"ina{a}", [("w_in", a * 1024 + j * 128, "g1") for j in range(8)], 128)
    add("inb0", [("w_in", O_MV, "g1"), ("w_in", O_MV + 512, "g1")], 512)
    add("inb1", [("w_in", O_MO, "g1"), ("w_in", O_MO + 512, "g1")], 512)
    add("inb2", [("w_in", O_AQ, "g1"), ("w_in", O_AQ + 512, "g1")], 512)
    add("inb3", [("w_in", O_AK, "g1")], 256)
    for a in range(4):
        segs = []
        for j in (2 * a, 2 * a + 1):
            segs += [("w_bm", j * 128, "gm"), ("w_ba", j * 128, None),
                     ("w_in", O_GM + j * 128, "g1"), ("w_in", O_GA + j * 128, "g1")]
        add(f"mrg{a}", segs, 128)
    add("wout", [("w_out", 0, None), ("w_out", 512, None)], 512)
    for a in range(6):
        js = list(range(4 * a, min(4 * a + 4, 22)))
        segs = []
        for j in js:
            segs += [("w_fi", j * 128, "g2"), ("w_fi", DFF + j * 128, "g2")]
        add(f"ffi{a}", segs, 128)
    for n in range(2):
        for hlf in range(2):
            add(f"ffo{n}{hlf}", [("w_fo", n * 512, None)], 512, kc0=hlf * 11, nkc=11)
    off = 0
    for it in items:
        it["size"] = len(it["segs"]) * it["nkc"] * it["segw"]
        it["off"] = off
        off += it["size"]
    return items, off


SLOT = 8192
NSLOT = 2


def build_program(ntiles=16, dumps=(), stop_after=None, reorder=True, prio="cp"):
    nc = bass.Bass("TRN2", target_bir_lowering=False)
    tr = Tracker()
    items, wtot = _weight_items()
    itmap = {it["name"]: it for it in items}
    dumps = set(dumps)
    dump_specs = {}

    ntok = ntiles * T
    x_d = nc.dram_tensor("x", [ntok, D], F32, kind="ExternalInput").ap()
    out_d = nc.dram_tensor("out", [ntok, D], F32, kind="ExternalOutput").ap()
    wsrc = {
        "w_in": nc.dram_tensor("w_in", [D, N_IN], F32, kind="ExternalInput").ap(),
        "w_bm": nc.dram_tensor("w_bm", [D, D], F32, kind="ExternalInput").ap(),
        "w_ba": nc.dram_tensor("w_ba", [D, D], F32, kind="ExternalInput").ap(),
        "w_out": nc.dram_tensor("w_out", [D, D], F32, kind="ExternalInput").ap(),
        "w_fi": nc.dram_tensor("w_fi", [D, 2 * DFF], F32, kind="ExternalInput").ap(),
        "w_fo": nc.dram_tensor("w_fo", [DFF, D], F32, kind="ExternalInput").ap(),
    }
    cbf_d = nc.dram_tensor("cbf", [128, 768], BF16, kind="ExternalInput").ap()
    cf_d = nc.dram_tensor("cf", [128, 516], F32, kind="ExternalInput").ap()
    pcol_d = nc.dram_tensor("pcol", [128, 24 + 64 + 16 + 16], F32, kind="ExternalInput").ap()
    prow_d = nc.dram_tensor("prow", [1, 144], F32, kind="ExternalInput").ap()
    pgate_d = nc.dram_tensor("pgate", [4, 2], F32, kind="ExternalInput").ap()
    wscr_d = nc.dram_tensor("wscr", [128, wtot], BF16, kind="Internal").ap()

    es = ExitStack()
    with es:
        def sb(name, shape, dt):
            return es.enter_context(nc.sbuf_tensor(name, shape, dt))

        def psum(name, shape, dt):
            return es.enter_context(nc.psum_tensor(name, shape, dt))

        x_sb = sb("x_sb", [128, NSUB, D], F32)
        x_b = [Buf(f"x{s}") for s in range(NSUB)]
        hT = sb("hT", [128, 8, T], BF16)
        hT_b = [Buf(f"hT{s}") for s in range(NSUB)]
        big = sb("big", [128, 12288], BF16)
        pg = [Buf(f"pg{i}") for i in range(24)]
        qkT = big[:, 0:8192].rearrange("p (c t) -> p c t", c=16)
        sigo = big[:, 8192:12288].rearrange("p (s f) -> p s f", s=4)
        actT = big[:, 0:11264].rearrange("p (c t) -> p c t", c=22)
        vaug = sb("vaug", [128, NSUB, 4, 257], BF16)
        vaug_b = [Buf(f"vaug{s}") for s in range(NSUB)]
        aqkv = sb("aqkv", [128, NSUB, 1280], BF16)
        aqkv_b = [Buf(f"aqkv{s}") for s in range(NSUB)]
        ymT = sb("ymT", [128, 8, T], BF16)
        ymT_b = [Buf(f"ymT{s}") for s in range(NSUB)]
        yaT = sb("yaT", [128, 8, T], BF16)
        yaT_b = [Buf(f"yaT{s}") for s in range(NSUB)]
        mgT = sb("mgT", [128, 8, T], BF16)
        mgT_b = [Buf(f"mgT{j}") for j in range(8)]
        wslot = [sb(f"wslot{i}", [128, SLOT], BF16) for i in range(NSLOT)]
        wslot_b = [Buf(f"wslot{i}") for i in range(NSLOT)]
        cbf = sb("cbf_sb", [128, 768], BF16)
        cf = sb("cf_sb", [128, 516], F32)
        const_b = Buf("const")
        pcol = sb("pcol_sb", [128, 120], F32)
        prow = sb("prow_sb", [128, 144], F32)
        pgate = sb("pgate_sb", [4, 2], F32)
        wgate = sb("wgate", [128, 8, 8], BF16)
        wgate_b = Buf("wgate")
        xn = [sb(f"xn{i}", [128, D], BF16) for i in range(2)]
        xn_b = [Buf(f"xn{i}") for i in range(2)]
        junk = sb("junk", [128, 256], BF16)
        junk_b = Buf("junk")
        stat = sb("stat", [128, 64], F32)
        cst = [sb(f"cst{i}", [128, 515], F32) for i in range(2)]
        cst_b = [Buf(f"cst{i}") for i in range(2)]
        cacc = [sb(f"cacc{i}", [128, 512], F32) for i in range(2)]
        cacc_b = [Buf(f"cacc{i}") for i in range(2)]
        carry = sb("carry", [128, 16, 3], F32)
        carry_b = [Buf(f"carry{j}") for j in range(16)]
        ostage = [sb(f"ostage{i}", [128, 512], F32) for i in range(2)]
        ostage_b = [Buf(f"ostage{i}") for i in range(2)]

        gr = sb("gr", [4, 2, T], F32)
        gr_b = [Buf("gr0"), Buf("gr1")]
        gs = sb("gs", [4, 32], F32)
        gs_b = Buf("gs")
        Rm = sb("Rm", [4, 32], F32)
        Rm_b = Buf("Rm")
        ones4 = sb("ones4", [4, 128], F32)
        wt = [sb(f"wt{i}", [128, NSUB, 8], F32) for i in range(2)]
        wt_b = [Buf(f"wt{i}") for i in range(2)]
        decb = [sb(f"decb{i}", [128, 32], F32) for i in range(2)]
        decb_b = [Buf(f"decb{i}") for i in range(2)]
        Cst = sb("Cst", [128, 4, 2, 257], F32)
        Cst_b = [Buf(f"Cst{h}") for h in range(4)]
        Csb = sb("Csb", [128, 4, 2, 257], BF16)
        Csb_b = [Buf(f"Csb{h}") for h in range(4)]
        STm = [sb(f"STm{i}", [128, 4, 128], BF16) for i in range(2)]
        STm_b = [[Buf(f"STm{i}_{h}") for h in range(4)] for i in range(2)]
        wvt = [sb(f"wvt{i}", [128, 4, 257], BF16) for i in range(2)]
        wvt_b = [[Buf(f"wvt{i}_{h}") for h in range(4)] for i in range(2)]
        ktok = [sb(f"ktok{i}", [128, D], BF16) for i in range(2)]
        ktok_b = [Buf(f"ktok{i}") for i in range(2)]
        hms = [sb(f"hm{i}", [128, D], BF16) for i in range(2)]
        hms_b = [[Buf(f"hm{i}_{h}") for h in range(4)] for i in range(2)]
        ymtok = sb("ymtok", [128, D], BF16)
        ymtok_b = Buf("ymtok")
        est = [sb(f"est{i}", [128, 24], F32) for i in range(2)]
        est_b = [Buf(f"est{i}") for i in range(2)]
        sq = sb("sq", [128, 1152], F32)
        sq_b = Buf("sq")
        qn = sb("qn", [128, 1152], F32)
        qn_b = Buf("qn")
        qb = sb("qb", [128, 18, 64], BF16)
        qb_b = Buf("qb")
        k2 = sb("k2", [128, 256], BF16)
        k2_b = Buf("k2")
        QBs = [sb(f"QB{i}", [128, 8, 256], BF16) for i in range(2)]
        QBs_b = [Buf(f"QB{i}") for i in range(2)]
        kTd = sb("kTd", [128, 2, 2, 128], BF16)
        kTd_b = [Buf("kTd0"), Buf("kTd1")]
        vat = sb("vat", [128, 2, 2, 65], BF16)
        vat_b = [Buf("vat0"), Buf("vat1")]
        pT = [sb(f"pT{i}", [128, 512], BF16) for i in range(2)]
        pT_b = [Buf(f"pT{i}") for i in range(2)]
        ya = sb("ya", [128, D], BF16)
        ya_b = Buf("ya")
        ast = sb("ast", [128, 96], F32)
        ast_b = Buf("ast")
        gqk = sb("gqk", [128, 128], F32)
        acst = sb("acst", [128, 32], F32)
        sgt = [sb(f"sgt{i}", [128, 512], BF16) for i in range(4)]
        sgt_b = [Buf(f"sgt{i}") for i in range(4)]
        mt = [sq[:, 0:512], sq[:, 512:1024]]
        mt_b = [sq_b, sq_b]

        sbuf_left = nc.sbuf_bytes_remaining
        pbank = [psum(f"pb{i}", [128, 512], F32) for i in range(8)]
        pbank_b = [Buf(f"pb{i}", excl=True) for i in range(8)]

        ident_f = cf[:, CF_ID:CF_ID + 4]
        MBprev = cbf[:, CB_MP:CB_MP + 256]
        MBcur = cbf[:, CB_MC:CB_MC + 256]
        ident_bf = cbf[:, CB_ID:CB_ID + 128]
        mask16 = cbf[:, CB_M16:CB_M16 + 128]

        g1col = pcol[:, 0:8]
        g2col = pcol[:, 8:16]
        gmcol = pcol[:, 16:24]
        convw = pcol[:, 24:88].rearrange("p (j k) -> p j k", j=16)
        convb = pcol[:, 88:104]
        bmcol = pcol[:, 104:120]
        fold = {"g1": g1col, "g2": g2col, "gm": gmcol, None: None}

        def dump(name, ap, bufs, shape, dt=F32):
            if name not in dumps:
                return
            d = nc.dram_tensor("dbg_" + name, list(shape), dt, kind="ExternalOutput").ap()
            dump_specs[name] = (list(shape), dt)
            tr.dma("sp", lambda e, d=d, ap=ap: e.dma_start(out=d, in_=ap), "dbg_" + name, reads=bufs)

        tr.dma("sp", lambda e: e.dma_start(out=cbf[:], in_=cbf_d), "cld", writes=[const_b])
        tr.dma("sp", lambda e: e.dma_start(out=cf[:], in_=cf_d), "cld", writes=[const_b])
        tr.dma("sp", lambda e: e.dma_start(out=pcol[:], in_=pcol_d), "cld", writes=[const_b])
        tr.dma("sp", lambda e: e.dma_start(out=prow[:], in_=prow_d.partition_broadcast(128)), "cld",
               writes=[const_b])
        tr.dma("sp", lambda e: e.dma_start(out=pgate[:], in_=pgate_d), "cld", writes=[const_b])

        wscr_b = {it["name"]: Buf("wscr_" + it["name"]) for it in items}
        cast_rr = [0]
        stage_rr = [0]

        def prep_cast(out_ap, in_ap, scale_ap, reads, writes):
            k = cast_rr[0] % 2
            cast_rr[0] += 1
            if k == 0:
                if scale_ap is None:
                    tr.op("act", lambda e: e.activation(out=out_ap, in_=in_ap, func=AF.Copy),
                          reads=reads, writes=writes)
                else:
                    tr.op("act", lambda e: e.activation(out=out_ap, in_=in_ap, func=AF.Copy, scale=scale_ap),
                          reads=reads + [const_b], writes=writes)
            else:
                eng = "dve" if k == 1 else "pool"
                if scale_ap is None:
                    tr.op(eng, lambda e: e.tensor_copy(out=out_ap, in_=in_ap), reads=reads, writes=writes)
                else:
                    tr.op(eng, lambda e: e.tensor_scalar(out=out_ap, in0=in_ap, scalar1=scale_ap, scalar2=None,
                                                         op0=ALU.mult),
                          reads=reads + [const_b], writes=writes)

        bigf = big.bitcast(F32)

        def prep_item(it, slot, deep=False):
            segs, segw, kc0, nkc = it["segs"], it["segw"], it["kc0"], it["nkc"]
            nseg = len(segs)
            view = wslot[slot][:, 0:it["size"]].rearrange("p (s k w) -> p s k w", s=nseg, k=nkc)
            groups = []
            for si, (src, c0, fd) in enumerate(segs):
                if groups and groups[-1][0] == src and groups[-1][3] == fd and \
                        groups[-1][1] + groups[-1][2] * segw == c0 and \
                        (groups[-1][2] + 1) * segw <= 1024 and groups[-1][4] + groups[-1][2] == si:
                    groups[-1][2] += 1
                else:
                    groups.append([src, c0, 1, fd, si])
            for k in range(nkc):
                kc = kc0 + k
                for (src, c0, n, fd, si0) in groups:
                    st = stage_rr[0] % (10 if deep else NSUB)
                    stage_rr[0] += 1
                    width = n * segw
                    src_ap = wsrc[src][kc * 128:(kc + 1) * 128, c0:c0 + width]
                    if st < NSUB:
                        stg = x_sb[:, st, 0:width]
                        stg_bufs = [x_b[st]]
                    else:
                        stg = bigf[:, (st - NSUB) * 1024:(st - NSUB) * 1024 + width]
                        stg_bufs = pg[4 * (st - NSUB):4 * (st - NSUB) + 4]
                    tr.dma("sp", lambda e, stg=stg, src_ap=src_ap: e.dma_start(out=stg, in_=src_ap),
                           f"xld{st}", writes=stg_bufs, nbytes=width * 512)
                    if n == 1:
                        out_ap = view[:, si0, k, :]
                        in_ap = stg
                    else:
                        out_ap = view[:, si0:si0 + n, k, :]
                        in_ap = stg.rearrange("p (s w) -> p s w", s=n)
                    sc = None if fd is None else fold[fd][:, kc:kc + 1]
                    prep_cast(out_ap, in_ap, sc, list(stg_bufs), [wslot_b[slot]])
            dst = wscr_d[:, it["off"]:it["off"] + it["size"]]
            tr.dma("sp", lambda e, dst=dst, slot=slot, it=it: e.dma_start(out=dst, in_=wslot[slot][:, 0:it["size"]]),
                   f"wst{slot}", reads=[wslot_b[slot]], writes=[wscr_b[it["name"]]], nbytes=it["size"] * 256)

        LATE = [it for it in items if it["name"].startswith(("mrg", "wout", "ffi", "ffo"))]
        for i, it in enumerate(LATE):
            prep_item(it, i % NSLOT, deep=True)
        early_pending = {it["name"] for it in items} - {it["name"] for it in LATE}
        for kc in range(8):
            st = stage_rr[0] % NSUB
            stage_rr[0] += 1
            stg = x_sb[:, st, 0:8]
            src_ap = wsrc["w_in"][kc * 128:(kc + 1) * 128, O_MI:O_MI + 8]
            tr.dma("sp", lambda e, stg=stg, src_ap=src_ap: e.dma_start(out=stg, in_=src_ap), f"xld{st}",
                   writes=[x_b[st]])
            tr.op("dve", lambda e, kc=kc, stg=stg: e.tensor_scalar(out=wgate[:, kc, :], in0=stg,
                                                                  scalar1=g1col[:, kc:kc + 1], scalar2=None,
                                                                  op0=ALU.mult),
                  reads=[x_b[st], const_b], writes=[wgate_b])

        wcur = {"slot": 0}

        def wload(name):
            it = itmap[name]
            slot = wcur["slot"]
            wcur["slot"] = (slot + 1) % NSLOT
            if name in early_pending:
                early_pending.discard(name)
                prep_item(it, slot)
                view = wslot[slot][:, 0:it["size"]].rearrange("p (s k w) -> p s k w", s=len(it["segs"]),
                                                              k=it["nkc"])
                return view, wslot_b[slot]
            src = wscr_d[:, it["off"]:it["off"] + it["size"]]
            tr.dma("sp", lambda e, src=src, slot=slot, it=it: e.dma_start(out=wslot[slot][:, 0:it["size"]], in_=src),
                   f"wld{slot}", reads=[wscr_b[name]], writes=[wslot_b[slot]], nbytes=it["size"] * 256)
            view = wslot[slot][:, 0:it["size"]].rearrange("p (s k w) -> p s k w", s=len(it["segs"]), k=it["nkc"])
            return view, wslot_b[slot]

        gemm_rr = [0]

        def gemm_bank():
            b = gemm_rr[0] % 4
            gemm_rr[0] += 1
            return b

        xstg = [sq[:, 0:D], qn[:, 0:D]]
        xstg_b = [sq_b, qn_b]

        def tile_front(ti):
            r0 = ti * T
            for s in range(NSUB):
                k = s % 2
                src = x_d[r0 + s * 128:r0 + (s + 1) * 128, :]
                tr.dma("sp", lambda e, k=k, src=src: e.dma_start(out=xstg[k], in_=src), f"xsg{k}",
                       writes=[xstg_b[k]], nbytes=524288)
                norm_transpose(xstg[k], xstg_b[k], hT, hT_b[s], s, ti * NSUB + s)
            dump(f"hT{ti}", hT[:], hT_b, [128, 8, T], BF16)

        def x_reload(ti):
            r0 = ti * T
            for s in range(NSUB):
                src = x_d[r0 + s * 128:r0 + (s + 1) * 128, :]
                tr.dma("sp", lambda e, s=s, src=src: e.dma_start(out=x_sb[:, s, :], in_=src), f"xld{s}",
                       writes=[x_b[s]], nbytes=524288)

        def norm_transpose(src, src_b, dstT, dst_b, s, uid):
            c = (uid % 8) * 4
            ss, lnv, rstd = stat[:, c:c + 1], stat[:, c + 1:c + 2], stat[:, c + 2:c + 3]
            sbuf_ = Buf()
            k = uid % 2
            tr.op("act", lambda e: e.activation(out=xn[k][:], in_=src, func=AF.Square, accum_out=ss),
                  reads=[src_b], writes=[xn_b[k], sbuf_])
            tr.op("act", lambda e: e.activation(out=lnv, in_=ss, func=AF.Ln, scale=1.0 / D, bias=EPS),
                  reads=[sbuf_], writes=[sbuf_])
            tr.op("act", lambda e: e.activation(out=rstd, in_=lnv, func=AF.Exp, scale=-0.5),
                  reads=[sbuf_], writes=[sbuf_])
            tr.op("act", lambda e: e.activation(out=xn[k][:], in_=src, func=AF.Copy, scale=rstd),
                  reads=[src_b, sbuf_], writes=[xn_b[k]])
            pb = 4 + (uid % 2)
            pst = pbank[pb].bitcast(BF16)
            for kc in range(8):
                tr.op("pe", lambda e, kc=kc: e.transpose(out=pst[:, kc * 128:(kc + 1) * 128],
                                                         in_=xn[k][:, kc * 128:(kc + 1) * 128], identity=ident_bf),
                      reads=[xn_b[k], const_b], writes=[pbank_b[pb]], inc=(kc == 7))
            tr.op("act", lambda e: e.activation(out=dstT[:, :, s * 128:(s + 1) * 128],
                                                in_=pst[:, :].rearrange("p (c t) -> p c t", c=8), func=AF.Copy),
                  reads=[pbank_b[pb]], writes=[dst_b])

        def inproj_a(ti, first):
            for a in range(2):
                wv, wb = wload(f"ina{a}")
                for jj in range(8):
                    j = a * 8 + jj
                    pb = gemm_bank()
                    for kc in range(8):
                        tr.op("pe", lambda e, jj=jj, kc=kc, pb=pb, wv=wv: e.matmul(
                            pbank[pb][:, :], lhsT=wv[:, jj, kc, :], rhs=hT[:, kc, :], start=(kc == 0), stop=(kc == 7)),
                            reads=[wb] + hT_b, writes=[pbank_b[pb]], inc=(kc == 7))
                    k = j % 2
                    if first:
                        tr.op("pool", lambda e, k=k: e.memset(cst[k][:, 0:3], 0.0), writes=[cst_b[k]])
                    else:
                        tr.op("pool", lambda e, k=k, j=j: e.tensor_copy(out=cst[k][:, 0:3], in_=carry[:, j, :]),
                              reads=[carry_b[j]], writes=[cst_b[k]])
                    tr.op("act", lambda e, k=k, pb=pb: e.activation(out=cst[k][:, 3:515], in_=pbank[pb][:, :],
                                                                    func=AF.Copy),
                          reads=[pbank_b[pb]], writes=[cst_b[k]])
                    tr.op("pool", lambda e, k=k, j=j: e.tensor_copy(out=carry[:, j, :], in_=cst[k][:, 512:515]),
                          reads=[cst_b[k]], writes=[carry_b[j]])
                    tr.op("act", lambda e, k=k, j=j, pb=pb: e.activation(out=cacc[k][:, 3:512],
                                                                         in_=pbank[pb][:, 0:509], func=AF.Copy,
                                                                         scale=convw[:, j, 0:1]),
                          reads=[pbank_b[pb], const_b], writes=[cacc_b[k]])
                    tr.op("pool", lambda e, k=k, j=j: e.tensor_scalar(out=cacc[k][:, 0:3], in0=cst[k][:, 0:3],
                                                                       scalar1=convw[:, j, 0:1], scalar2=None,
                                                                       op0=ALU.mult),
                          reads=[cst_b[k], const_b, cacc_b[k]], writes=[cacc_b[k]])
                    for tap in range(1, 4):
                        tr.op("dve", lambda e, k=k, j=j, tap=tap: e.scalar_tensor_tensor(
                            out=cacc[k][:], in0=cst[k][:, tap:tap + 512], scalar=convw[:, j, tap:tap + 1],
                            in1=cacc[k][:], op0=ALU.mult, op1=ALU.add),
                            reads=[cst_b[k], cacc_b[k], const_b], writes=[cacc_b[k]])
                    tr.op("act", lambda e, k=k, j=j: e.activation(out=qkT[:, j, :], in_=cacc[k][:], func=AF.Silu,
                                                                   bias=convb[:, j:j + 1]),
                          reads=[cacc_b[k], const_b], writes=[pg[j]])
            dump(f"qkT{ti}", qkT, pg[0:16], [128, 16, T], BF16)

        def inproj_b(ti):
            plan = [("inb0", [("v", 0), ("v", 2)]), ("inb1", [("o", 0), ("o", 512)]),
                    ("inb2", [("q", 0), ("q", 512)]), ("inb3", [("kv", 0)])]
            for name, segl in plan:
                wv, wb = wload(name)
                segw = itmap[name]["segw"]
                for si, (kind, arg) in enumerate(segl):
                    for s in range(NSUB):
                        pb = gemm_bank()
                        for kc in range(8):
                            tr.op("pe", lambda e, si=si, kc=kc, pb=pb, wv=wv, s=s, segw=segw: e.matmul(
                                pbank[pb][:, 0:segw], lhsT=hT[:, kc, s * 128:(s + 1) * 128], rhs=wv[:, si, kc, :],
                                start=(kc == 0), stop=(kc == 7)),
                                reads=[wb, hT_b[s]], writes=[pbank_b[pb]], inc=(kc == 7))
                        if kind == "v":
                            tr.op("act", lambda e, pb=pb, s=s, arg=arg: e.activation(
                                out=vaug[:, s, arg:arg + 2, 0:256],
                                in_=pbank[pb][:, :].rearrange("p (h e) -> p h e", h=2), func=AF.Copy),
                                reads=[pbank_b[pb]], writes=[vaug_b[s]])
                        elif kind == "o":
                            tr.op("act", lambda e, pb=pb, s=s, arg=arg: e.activation(
                                out=sigo[:, s, arg:arg + 512], in_=pbank[pb][:, :], func=AF.Sigmoid),
                                reads=[pbank_b[pb]], writes=[pg[16 + 2 * s], pg[17 + 2 * s]])
                        elif kind == "q":
                            tr.op("dve", lambda e, pb=pb, s=s, arg=arg: e.tensor_copy(
                                out=aqkv[:, s, arg:arg + 512], in_=pbank[pb][:, :]),
                                reads=[pbank_b[pb]], writes=[aqkv_b[s]])
                        else:
                            tr.op("dve", lambda e, pb=pb, s=s: e.tensor_copy(
                                out=aqkv[:, s, 1024:1280], in_=pbank[pb][:, 0:256]),
                                reads=[pbank_b[pb]], writes=[aqkv_b[s]])
            dump(f"vaug{ti}", vaug[:], vaug_b, [128, NSUB, 4, 257], BF16)
            dump(f"sigo{ti}", sigo, pg[16:24], [128, NSUB, 1024], BF16)
            dump(f"aqkv{ti}", aqkv[:], aqkv_b, [128, NSUB, 1280], BF16)

        setup_b = Buf("setup")
        for i in range(2):
            tr.op("pool", lambda e, i=i: e.memset(QBs[i][:], 0.0), writes=[QBs_b[i]])
        tr.op("pool", lambda e: e.memset(vat[:, :, :, 64:65], 1.0), writes=vat_b)
        tr.op("pool", lambda e: e.memset(ones4[:], 1.0), writes=[setup_b])
        tr.op("dve", lambda e: e.tensor_scalar(out=gqk[:, 0:64], in0=prow[:, 0:64],
                                               scalar1=0.125, scalar2=None, op0=ALU.mult),
              reads=[const_b], writes=[setup_b])
        tr.op("dve", lambda e: e.tensor_copy(out=gqk[:, 64:128], in_=prow[:, 64:128]),
              reads=[const_b, setup_b], writes=[setup_b])
        tr.op("dve", lambda e: e.tensor_reduce(out=acst[:, 1:2], in_=prow[:, 0:64], axis=AX.X, op=ALU.max,
                                               apply_absolute_value=True),
              reads=[const_b, setup_b], writes=[setup_b])
        tr.op("dve", lambda e: e.tensor_reduce(out=acst[:, 2:3], in_=prow[:, 64:128], axis=AX.X, op=ALU.max,
                                               apply_absolute_value=True),
              reads=[const_b, setup_b], writes=[setup_b])
        tr.op("dve", lambda e: e.scalar_tensor_tensor(out=acst[:, 0:1], in0=acst[:, 1:2], scalar=-8.0,
                                                      in1=acst[:, 2:3], op0=ALU.mult, op1=ALU.mult),
              reads=[setup_b], writes=[setup_b])
        nlmax = acst[:, 0:1]
        tr.op("act", lambda e: e.activation(out=acst[:, 16:32], in_=prow[:, 128:144], func=AF.Exp, bias=nlmax),
              reads=[const_b, setup_b], writes=[setup_b])
        sinkexp = acst[:, 16:32]
        tr.op("dve", lambda e: e.tensor_scalar(out=gs[:, 28:29], in0=pgate[:, 1:2], scalar1=-1.0, scalar2=None,
                                               op0=ALU.mult),
              reads=[const_b], writes=[setup_b])
        nbf = gs[:, 28:29]
        bi = pgate[:, 0:1]

        GS_MBLK, GS_MC, GS_NMC, GS_MPREV, GS_DIF, GS_DEC = 0, 4, 8, 12, 17, 21

        def gates(ti, first):
            tp = ti % 2
            bi_, bf_, bt_, bd_ = gemm_bank(), gemm_bank(), gemm_bank(), gemm_bank()
            for (bank, c0) in ((bi_, 0), (bf_, 4)):
                for kc in range(8):
                    tr.op("pe", lambda e, bank=bank, c0=c0, kc=kc: e.matmul(
                        pbank[bank][0:4, :], lhsT=wgate[:, kc, c0:c0 + 4], rhs=hT[:, kc, :],
                        start=(kc == 0), stop=(kc == 7)),
                        reads=[wgate_b] + hT_b, writes=[pbank_b[bank]], inc=(kc == 7))
            ipre, fpre = pbank[bi_][0:4, :], pbank[bf_][0:4, :]
            g0, g1 = gr[:, 0, :], gr[:, 1, :]
            tr.op("act", lambda e: e.activation(out=g0, in_=fpre, func=AF.Exp, scale=-1.0, bias=nbf),
                  reads=[pbank_b[bf_], setup_b], writes=[gr_b[0]])
            tr.op("act", lambda e: e.activation(out=g0, in_=g0, func=AF.Ln, bias=1.0),
                  reads=[gr_b[0]], writes=[gr_b[0]])
            for j in range(NSUB):
                tr.op("dve", lambda e, j=j: e.tensor_tensor_scan(
                    out=g1[:, j * 128:(j + 1) * 128], data0=ones4[:, 0:128], data1=g0[:, j * 128:(j + 1) * 128],
                    initial=0.0, op0=ALU.mult, op1=ALU.add),
                    reads=[gr_b[0], setup_b], writes=[gr_b[1]])
            tr.op("dve", lambda e: e.scalar_tensor_tensor(out=g0, in0=ipre, scalar=bi, in1=g1, op0=ALU.add,
                                                          op1=ALU.add),
                  reads=[pbank_b[bi_], gr_b[1], const_b, gr_b[0]], writes=[gr_b[0]])
            tr.op("dve", lambda e: e.tensor_reduce(out=gs[:, GS_MBLK:GS_MBLK + 4],
                                                   in_=g0.rearrange("p (j t) -> p j t", j=4), axis=AX.X, op=ALU.max),
                  reads=[gr_b[0]], writes=[gs_b])
            if first:
                tr.op("dve", lambda e: e.memset(gs[:, GS_MPREV:GS_MPREV + 1], 0.0), reads=[gs_b], writes=[gs_b])
            else:
                tr.op("dve", lambda e: e.tensor_copy(out=gs[:, GS_MPREV:GS_MPREV + 1],
                                                     in_=gs[:, GS_MPREV + 4:GS_MPREV + 5]),
                      reads=[gs_b], writes=[gs_b])
            for j in range(NSUB):
                tr.op("dve", lambda e, j=j: e.tensor_tensor(out=gs[:, GS_MC + j:GS_MC + j + 1],
                                                            in0=gs[:, GS_MPREV + j:GS_MPREV + j + 1],
                                                            in1=gs[:, GS_MBLK + j:GS_MBLK + j + 1], op=ALU.max),
                      reads=[gs_b], writes=[gs_b])
                tr.op("dve", lambda e, j=j: e.tensor_tensor(out=gs[:, GS_MPREV + j + 1:GS_MPREV + j + 2],
                                                            in0=gs[:, GS_MC + j:GS_MC + j + 1],
                                                            in1=g1[:, j * 128 + 127:j * 128 + 128], op=ALU.subtract),
                      reads=[gs_b, gr_b[1]], writes=[gs_b])
            tr.op("dve", lambda e: e.tensor_tensor(out=gs[:, GS_DIF:GS_DIF + 4], in0=gs[:, GS_MPREV:GS_MPREV + 4],
                                                   in1=gs[:, GS_MC:GS_MC + 4], op=ALU.subtract),
                  reads=[gs_b], writes=[gs_b])
            tr.op("dve", lambda e: e.tensor_scalar(out=gs[:, GS_NMC:GS_NMC + 4], in0=gs[:, GS_MC:GS_MC + 4],
                                                   scalar1=-1.0, scalar2=None, op0=ALU.mult),
                  reads=[gs_b], writes=[gs_b])
            tr.op("act", lambda e: e.activation(out=gs[:, GS_DEC:GS_DEC + 4], in_=gs[:, GS_DIF:GS_DIF + 4],
                                                func=AF.Exp),
                  reads=[gs_b], writes=[gs_b])
            for j in range(NSUB):
                sl = slice(j * 128, (j + 1) * 128)
                tr.op("act", lambda e, j=j, sl=sl: e.activation(out=g1[:, sl], in_=g1[:, sl], func=AF.Exp,
                                                                bias=gs[:, GS_NMC + j:GS_NMC + j + 1]),
                      reads=[gs_b, gr_b[1]], writes=[gr_b[1]])
                tr.op("act", lambda e, j=j, sl=sl: e.activation(out=g0[:, sl], in_=g0[:, sl], func=AF.Exp,
                                                                bias=gs[:, GS_NMC + j:GS_NMC + j + 1]),
                      reads=[gs_b, gr_b[0]], writes=[gr_b[0]])
            for j in range(NSUB):
                sl = slice(j * 128, (j + 1) * 128)
                tr.op("pe", lambda e, j=j, sl=sl: e.matmul(pbank[bt_][:, j * 8:j * 8 + 4], lhsT=g0[:, sl],
                                                           rhs=ident_f[0:4, 0:4], start=True, stop=True),
                      reads=[gr_b[0], const_b], writes=[pbank_b[bt_]], inc=False)
                tr.op("pe", lambda e, j=j, sl=sl: e.matmul(pbank[bt_][:, j * 8 + 4:j * 8 + 8], lhsT=g1[:, sl],
                                                           rhs=ident_f[0:4, 0:4], start=True, stop=True),
                      reads=[gr_b[1], const_b], writes=[pbank_b[bt_]], inc=(j == NSUB - 1))
            tr.op("act", lambda e: e.activation(out=wt[tp][:].rearrange("p j c -> p (j c)"), in_=pbank[bt_][:, 0:32],
                                                func=AF.Copy),
                  reads=[pbank_b[bt_]], writes=[wt_b[tp]])
            tr.op("dve", lambda e: e.tensor_tensor(
                out=Rm[:, 0:16].rearrange("p (j h) -> p j h", j=4),
                in0=ident_f[0:4, 0:4].unsqueeze(1).broadcast_to([4, 4, 4]),
                in1=gs[:, GS_DEC:GS_DEC + 4].unsqueeze(2).broadcast_to([4, 4, 4]), op=ALU.mult),
                reads=[gs_b, const_b, Rm_b], writes=[Rm_b])
            tr.op("dve", lambda e: e.tensor_scalar(out=Rm[:, 16:32], in0=Rm[:, 0:16], scalar1=1.0 / 16.0,
                                                   scalar2=None, op0=ALU.mult),
                  reads=[Rm_b], writes=[Rm_b])
            tr.op("pe", lambda e: e.matmul(pbank[bd_][:, 0:32], lhsT=ones4[:, 0:128], rhs=Rm[:, 0:32],
                                           start=True, stop=True),
                  reads=[Rm_b, setup_b], writes=[pbank_b[bd_]])
            tr.op("act", lambda e: e.activation(out=decb[tp][:], in_=pbank[bd_][:, 0:32], func=AF.Copy),
                  reads=[pbank_b[bd_]], writes=[decb_b[tp]])
            dump(f"wt{ti}", wt[tp][:], [wt_b[tp]], [128, NSUB, 8])
            dump(f"decb{ti}", decb[tp][:], [decb_b[tp]], [128, 32])

        def mlstm_block(ti, s, first_block):
            tp = ti % 2
            par = s % 2
            sl = slice(s * 128, (s + 1) * 128)
            if first_block:
                tr.op("pool", lambda e: e.memset(Cst[:], 0.0), writes=Cst_b)
            pst = pbank[7].bitcast(BF16)
            for c in range(8):
                tr.op("pe", lambda e, c=c: e.transpose(out=pst[:, c * 128:(c + 1) * 128], in_=qkT[:, 8 + c, sl],
                                                       identity=ident_bf),
                      reads=[pg[8 + c], const_b], writes=[pbank_b[7]], inc=(c == 7))
            tr.op("act", lambda e: e.activation(out=ktok[par][:], in_=pst[:, :], func=AF.Copy),
                  reads=[pbank_b[7]], writes=[ktok_b[par]])
            e_ = est[par]
            hm, hm_b = hms[par], hms_b[par]
            for h in range(4):
                for dc in range(2):
                    tr.op("pe", lambda e, h=h, dc=dc: e.matmul(pbank[5][:, 0:128], lhsT=qkT[:, 8 + 2 * h + dc, sl],
                                                               rhs=qkT[:, 2 * h + dc, sl], start=(dc == 0),
                                                               stop=(dc == 1)),
                          reads=[pg[8 + 2 * h + dc], pg[2 * h + dc]], writes=[pbank_b[5]], inc=(dc == 1))
                tr.op("dve", lambda e, h=h: e.tensor_tensor(out=STm[par][:, h, :], in0=pbank[5][:, 0:128],
                                                            in1=mask16, op=ALU.mult),
                      reads=[pbank_b[5], const_b], writes=[STm_b[par][h]])
                tr.op("act", lambda e, h=h: e.activation(out=wvt[par][:, h, :], in_=vaug[:, s, h, :], func=AF.Copy,
                                                         scale=wt[tp][:, s, h:h + 1]),
                      reads=[vaug_b[s], wt_b[tp]], writes=[wvt_b[par][h]])
                tr.op("act", lambda e, h=h: e.activation(
                    out=Csb[:, h, :, :], in_=Cst[:, h, :, :], func=AF.Copy,
                    scale=decb[tp][:, 16 + s * 4 + h:16 + s * 4 + h + 1]),
                    reads=[Cst_b[h], decb_b[tp]], writes=[Csb_b[h]])
                num = pbank[6][:, 0:257]
                tr.op("pe", lambda e, h=h: e.matmul(num, lhsT=STm[par][:, h, :], rhs=wvt[par][:, h, :],
                                                    start=True, stop=False),
                      reads=[STm_b[par][h], wvt_b[par][h]], writes=[pbank_b[6]], inc=False)
                for dc in range(2):
                    tr.op("pe", lambda e, h=h, dc=dc: e.matmul(num, lhsT=qkT[:, 2 * h + dc, sl],
                                                               rhs=Csb[:, h, dc, :], start=False, stop=(dc == 1)),
                          reads=[pg[2 * h + dc], Csb_b[h]], writes=[pbank_b[6]], inc=(dc == 1))
                for dc, bank, c0 in ((0, 7, 0), (1, 5, 128)):
                    tr.op("pe", lambda e, h=h, dc=dc, bank=bank, c0=c0: e.matmul(
                        pbank[bank][:, c0:c0 + 257], lhsT=ktok[par][:, h * 256 + dc * 128:h * 256 + (dc + 1) * 128],
                        rhs=wvt[par][:, h, :], start=True, stop=True),
                        reads=[ktok_b[par], wvt_b[par][h]], writes=[pbank_b[bank]])
                    tr.op("dve", lambda e, h=h, dc=dc, bank=bank, c0=c0: e.scalar_tensor_tensor(
                        out=Cst[:, h, dc, :], in0=Cst[:, h, dc, :],
                        scalar=decb[tp][:, s * 4 + h:s * 4 + h + 1], in1=pbank[bank][:, c0:c0 + 257],
                        op0=ALU.mult, op1=ALU.add),
                        reads=[Cst_b[h], decb_b[tp], pbank_b[bank]], writes=[Cst_b[h]])
                tr.op("act", lambda e, h=h: e.activation(out=e_[:, 20 + h:21 + h], in_=pbank[6][:, 256:257],
                                                         func=AF.Abs),
                      reads=[pbank_b[6], est_b[par]], writes=[est_b[par]])
                tr.op("dve", lambda e, h=h: e.tensor_tensor(out=e_[:, h:h + 1], in0=e_[:, 20 + h:21 + h],
                                                            in1=wt[tp][:, s, 4 + h:5 + h], op=ALU.max),
                      reads=[wt_b[tp], est_b[par]], writes=[est_b[par]])
                tr.op("dve", lambda e, h=h: e.reciprocal(out=e_[:, 4 + h:5 + h], in_=e_[:, h:h + 1]),
                      reads=[est_b[par]], writes=[est_b[par]])
                tr.op("dve", lambda e, h=h: e.scalar_tensor_tensor(
                    out=hm[:, h * 256:(h + 1) * 256], in0=pbank[6][:, 0:256], scalar=e_[:, 4 + h:5 + h],
                    in1=sigo[:, s, h * 256:(h + 1) * 256], op0=ALU.mult, op1=ALU.mult),
                    reads=[pbank_b[6], est_b[par], pg[16 + 2 * s], pg[17 + 2 * s]], writes=[hm_b[h]])
                tr.op("act", lambda e, h=h: e.activation(out=junk[:, 0:256], in_=hm[:, h * 256:(h + 1) * 256],
                                                         func=AF.Square, accum_out=e_[:, 8 + h:9 + h]),
                      reads=[hm_b[h], est_b[par]], writes=[junk_b, est_b[par]])
            tr.op("act", lambda e: e.activation(out=e_[:, 12:16], in_=e_[:, 8:12], func=AF.Ln, scale=1.0 / 256.0,
                                                bias=EPS),
                  reads=[est_b[par]], writes=[est_b[par]])
            tr.op("act", lambda e: e.activation(out=e_[:, 16:20], in_=e_[:, 12:16], func=AF.Exp, scale=-0.5),
                  reads=[est_b[par]], writes=[est_b[par]])
            tr.op("dve", lambda e: e.tensor_tensor(
                out=ymtok[:].rearrange("p (h e) -> p h e", h=4), in0=hm[:].rearrange("p (h e) -> p h e", h=4),
                in1=e_[:, 16:20].unsqueeze(2).broadcast_to([128, 4, 256]), op=ALU.mult),
                reads=hm_b + [est_b[par]], writes=[ymtok_b])
            for c in range(8):
                tr.op("pe", lambda e, c=c: e.transpose(out=pst[:, c * 128:(c + 1) * 128],
                                                       in_=ymtok[:, c * 128:(c + 1) * 128], identity=ident_bf),
                      reads=[ymtok_b, const_b], writes=[pbank_b[7]], inc=(c == 7))
            tr.op("act", lambda e: e.activation(out=ymT[:, :, sl], in_=pst[:, :].rearrange("p (c t) -> p c t", c=8),
                                                func=AF.Copy),
                  reads=[pbank_b[7]], writes=[ymT_b[s]])

        def attn_block(ti, s, jb):
            par = jb % 2
            first = (jb == 0)
            QB, QB_b = QBs[par], QBs_b[par]
            src = aqkv[:, s, 0:1152]
            tr.op("act", lambda e: e.activation(out=sq[:], in_=src, func=AF.Square),
                  reads=[aqkv_b[s]], writes=[sq_b])
            tr.op("dve", lambda e: e.tensor_reduce(out=ast[:, 0:18], in_=sq[:].rearrange("p (h d) -> p h d", h=18),
                                                   axis=AX.X, op=ALU.add),
                  reads=[sq_b, ast_b], writes=[ast_b])
            tr.op("act", lambda e: e.activation(out=ast[:, 18:36], in_=ast[:, 0:18], func=AF.Ln, scale=1.0 / 64.0,
                                                bias=EPS),
                  reads=[ast_b], writes=[ast_b])
            tr.op("act", lambda e: e.activation(out=ast[:, 36:54], in_=ast[:, 18:36], func=AF.Exp, scale=-0.5),
                  reads=[ast_b], writes=[ast_b])
            qn3 = qn[:].rearrange("p (h d) -> p h d", h=18)
            rt = sq[:, 0:576].rearrange("p (a h d) -> p a h d", a=4, h=18)
            rt_b = sq_b
            tr.op("dve", lambda e: e.tensor_tensor(out=qn3, in0=src.rearrange("p (h d) -> p h d", h=18),
                                                   in1=ast[:, 36:54].unsqueeze(2).broadcast_to([128, 18, 64]),
                                                   op=ALU.mult),
                  reads=[aqkv_b[s], ast_b], writes=[qn_b])
            tr.op("dve", lambda e: e.tensor_tensor(out=qn3[:, 0:16, :], in0=qn3[:, 0:16, :],
                                                   in1=gqk[:, 0:64].unsqueeze(1).broadcast_to([128, 16, 64]),
                                                   op=ALU.mult),
                  reads=[qn_b, setup_b], writes=[qn_b])
            tr.op("dve", lambda e: e.tensor_tensor(out=qn3[:, 16:18, :], in0=qn3[:, 16:18, :],
                                                   in1=gqk[:, 64:128].unsqueeze(1).broadcast_to([128, 2, 64]),
                                                   op=ALU.mult),
                  reads=[qn_b, setup_b], writes=[qn_b])
            cosb = cf[:, CF_COS + jb * 8:CF_COS + jb * 8 + 8].unsqueeze(1).broadcast_to([128, 18, 8])
            sinb = cf[:, CF_SIN + jb * 8:CF_SIN + jb * 8 + 8].unsqueeze(1).broadcast_to([128, 18, 8])
            x1, x2 = qn3[:, :, 0:8], qn3[:, :, 8:16]
            tr.op("dve", lambda e: e.tensor_tensor(out=rt[:, 0, :, :], in0=x1, in1=cosb, op=ALU.mult),
                  reads=[qn_b, const_b], writes=[rt_b])
            tr.op("dve", lambda e: e.tensor_tensor(out=rt[:, 1, :, :], in0=x2, in1=sinb, op=ALU.mult),
                  reads=[qn_b, const_b, rt_b], writes=[rt_b])
            tr.op("dve", lambda e: e.tensor_tensor(out=rt[:, 2, :, :], in0=x2, in1=cosb, op=ALU.mult),
                  reads=[qn_b, const_b, rt_b], writes=[rt_b])
            tr.op("dve", lambda e: e.tensor_tensor(out=rt[:, 3, :, :], in0=x1, in1=sinb, op=ALU.mult),
                  reads=[qn_b, const_b, rt_b], writes=[rt_b])
            tr.op("dve", lambda e: e.tensor_tensor(out=qb[:, :, 0:8], in0=rt[:, 0, :, :], in1=rt[:, 1, :, :],
                                                   op=ALU.subtract),
                  reads=[rt_b], writes=[qb_b])
            tr.op("dve", lambda e: e.tensor_tensor(out=qb[:, :, 8:16], in0=rt[:, 2, :, :], in1=rt[:, 3, :, :],
                                                   op=ALU.add),
                  reads=[rt_b, qb_b], writes=[qb_b])
            tr.op("act", lambda e: e.activation(out=qb[:, :, 16:64], in_=qn3[:, :, 16:64], func=AF.Copy),
                  reads=[qn_b, qb_b], writes=[qb_b])
            tr.op("pool", lambda e: e.tensor_copy(
                out=k2[:].rearrange("p (g r d) -> p g r d", g=2, r=2),
                in_=qb[:, 16:18, :].unsqueeze(2).broadcast_to([128, 2, 2, 64])),
                reads=[qb_b], writes=[k2_b])
            tr.op("pool", lambda e: e.tensor_copy(
                out=vat[:, par, :, 0:64], in_=aqkv[:, s, 1152:1280].rearrange("p (g d) -> p g d", g=2)),
                reads=[aqkv_b[s]], writes=[vat_b[par]])
            pq = pbank[4].bitcast(BF16)
            for g in range(2):
                tr.op("pe", lambda e, g=g: e.transpose(out=pq[:, g * 128:(g + 1) * 128],
                                                       in_=k2[:, g * 128:(g + 1) * 128], identity=ident_bf),
                      reads=[k2_b, const_b], writes=[pbank_b[4]], inc=(g == 1))
            tr.op("act", lambda e: e.activation(out=kTd[:, par, :, :],
                                                in_=pq[:, 0:256].rearrange("p (g t) -> p g t", g=2), func=AF.Copy),
                  reads=[pbank_b[4]], writes=[kTd_b[par]])
            for c in range(8):
                tr.op("pe", lambda e, c=c: e.transpose(out=pq[:, c * 128:(c + 1) * 128],
                                                       in_=qb[:].rearrange("p h d -> p (h d)")[:, c * 128:(c + 1) * 128],
                                                       identity=ident_bf),
                      reads=[qb_b, const_b], writes=[pbank_b[4]], inc=(c == 7))
            pq3 = pq[:, :].rearrange("p (c t) -> p c t", c=8)
            tr.op("act", lambda e: e.activation(out=QB[0:64, :, 0:128], in_=pq3[0:64, :, :], func=AF.Copy),
                  reads=[pbank_b[4]], writes=[QB_b])
            tr.op("dve", lambda e: e.tensor_copy(out=QB[64:128, :, 128:256], in_=pq3[64:128, :, :]),
                  reads=[pbank_b[4], QB_b], writes=[QB_b])
            def po_ap(head):
                if head < 7:
                    return 2, pbank[2][:, head * 65:(head + 1) * 65]
                if head < 14:
                    return 3, pbank[3][:, (head - 7) * 65:(head - 6) * 65]
                return 4, pbank[4][:, (head - 14) * 65:(head - 13) * 65]
            for c in range(8):
                g = c // 4
                lb = c % 2
                lg = pbank[lb]
                if not first:
                    tr.op("pe", lambda e, c=c, g=g, lg=lg: e.matmul(lg[:, 0:256], lhsT=kTd[:, 1 - par, g, :],
                                                                    rhs=QB[:, c, :], start=True, stop=False),
                          reads=[kTd_b[1 - par], QB_b], writes=[pbank_b[lb]], inc=False)
                    tr.op("pe", lambda e, lg=lg: e.matmul(lg[:, 0:256], lhsT=ident_bf, rhs=MBprev,
                                                          start=False, stop=True),
                          reads=[const_b], writes=[pbank_b[lb]], inc=False)
                tr.op("pe", lambda e, c=c, g=g, lg=lg: e.matmul(lg[:, 256:512], lhsT=kTd[:, par, g, :],
                                                                rhs=QB[:, c, :], start=True, stop=False),
                      reads=[kTd_b[par], QB_b], writes=[pbank_b[lb]], inc=False)
                tr.op("pe", lambda e, lg=lg: e.matmul(lg[:, 256:512], lhsT=ident_bf, rhs=MBcur,
                                                      start=False, stop=True),
                      reads=[const_b], writes=[pbank_b[lb]])
                lo = 256 if first else 0
                pk_ = c % 2
                tr.op("act", lambda e, lg=lg, lo=lo, pk_=pk_: e.activation(out=pT[pk_][:, lo:512], in_=lg[:, lo:512],
                                                                           func=AF.Exp, bias=nlmax),
                      reads=[pbank_b[lb], setup_b], writes=[pT_b[pk_]])
                for hh in range(2):
                    head = 2 * c + hh
                    bank, po = po_ap(head)
                    srcs = [(256 + hh * 128, par)]
                    if not first:
                        srcs.append((hh * 128, 1 - par))
                    for i, (col, slot) in enumerate(srcs):
                        tr.op("pe", lambda e, pk_=pk_, col=col, slot=slot, g=g, po=po, i=i, n=len(srcs): e.matmul(
                            po, lhsT=pT[pk_][:, col:col + 128], rhs=vat[:, slot, g, :], start=(i == 0),
                            stop=(i == n - 1)),
                            reads=[pT_b[pk_], vat_b[slot]], writes=[pbank_b[bank]], inc=(i == len(srcs) - 1))
            for (bank, h0, nh) in ((2, 0, 7), (3, 7, 7), (4, 14, 2)):
                po3 = pbank[bank][:, 0:nh * 65].rearrange("p (h d) -> p h d", h=nh)
                dsum = ast[:, 54 + h0:54 + h0 + nh]
                rden = ast[:, 72 + h0:72 + h0 + nh]
                tr.op("dve", lambda e, po3=po3, dsum=dsum, h0=h0, nh=nh: e.tensor_tensor(
                    out=dsum, in0=po3[:, :, 64], in1=sinkexp[:, h0:h0 + nh], op=ALU.add),
                    reads=[pbank_b[bank], setup_b, ast_b], writes=[ast_b])
                tr.op("dve", lambda e, dsum=dsum, rden=rden: e.reciprocal(out=rden, in_=dsum),
                      reads=[ast_b], writes=[ast_b])
                tr.op("dve", lambda e, po3=po3, rden=rden, h0=h0, nh=nh: e.tensor_tensor(
                    out=ya[:, h0 * 64:(h0 + nh) * 64].rearrange("p (h d) -> p h d", h=nh), in0=po3[:, :, 0:64],
                    in1=rden.unsqueeze(2).broadcast_to([128, nh, 64]), op=ALU.mult),
                    reads=[pbank_b[bank], ast_b], writes=[ya_b])
            sl = slice(s * 128, (s + 1) * 128)
            py = pbank[4].bitcast(BF16)
            for c in range(8):
                tr.op("pe", lambda e, c=c: e.transpose(out=py[:, c * 128:(c + 1) * 128],
                                                       in_=ya[:, c * 128:(c + 1) * 128], identity=ident_bf),
                      reads=[ya_b, const_b], writes=[pbank_b[4]], inc=(c == 7))
            tr.op("act", lambda e: e.activation(out=yaT[:, :, sl], in_=py[:, :].rearrange("p (c t) -> p c t", c=8),
                                                func=AF.Copy),
                  reads=[pbank_b[4]], writes=[yaT_b[s]])

        def merge(ti):
            for a in range(4):
                wv, wb = wload(f"mrg{a}")
                for jj in range(2):
                    j = 2 * a + jj
                    for (bank, seg, rhsT, rb) in ((0, 0, ymT, ymT_b), (1, 1, yaT, yaT_b), (2, 2, hT, hT_b),
                                                  (3, 3, hT, hT_b)):
                        for kc in range(8):
                            tr.op("pe", lambda e, bank=bank, seg=seg, rhsT=rhsT, kc=kc, wv=wv, jj=jj: e.matmul(
                                pbank[bank][:, :], lhsT=wv[:, jj * 4 + seg, kc, :], rhs=rhsT[:, kc, :],
                                start=(kc == 0), stop=(kc == 7)),
                                reads=[wb] + rb, writes=[pbank_b[bank]], inc=(kc == 7))
                    k = j % 2
                    tr.op("act", lambda e, j=j, k=k: e.activation(out=sgt[k][:], in_=pbank[2][:, :], func=AF.Sigmoid,
                                                                  bias=bmcol[:, j:j + 1]),
                          reads=[pbank_b[2], const_b], writes=[sgt_b[k]])
                    tr.op("act", lambda e, j=j, k=k: e.activation(out=sgt[2 + k][:], in_=pbank[3][:, :],
                                                                  func=AF.Sigmoid, bias=bmcol[:, 8 + j:9 + j]),
                          reads=[pbank_b[3], const_b], writes=[sgt_b[2 + k]])
                    tr.op("dve", lambda e, k=k: e.tensor_tensor(out=mt[0], in0=pbank[0][:, :], in1=sgt[k][:],
                                                                op=ALU.mult),
                          reads=[pbank_b[0], sgt_b[k]], writes=[mt_b[0]])
                    tr.op("dve", lambda e, k=k: e.tensor_tensor(out=mt[1], in0=pbank[1][:, :], in1=sgt[2 + k][:],
                                                                op=ALU.mult),
                          reads=[pbank_b[1], sgt_b[2 + k]], writes=[mt_b[1]])
                    tr.op("dve", lambda e, j=j: e.tensor_tensor(out=mgT[:, j, :], in0=mt[0], in1=mt[1],
                                                                 op=ALU.add),
                          reads=[mt_b[0], mt_b[1]], writes=[mgT_b[j]])
            dump(f"mgT{ti}", mgT[:], mgT_b, [128, 8, T], BF16)

        def outproj(ti):
            wv, wb = wload("wout")
            for s in range(NSUB):
                for n in range(2):
                    pb = gemm_bank()
                    for kc in range(8):
                        tr.op("pe", lambda e, pb=pb, kc=kc, s=s, n=n, wv=wv: e.matmul(
                            pbank[pb][:, :], lhsT=mgT[:, kc, s * 128:(s + 1) * 128], rhs=wv[:, n, kc, :],
                            start=(kc == 0), stop=(kc == 7)),
                            reads=[wb] + mgT_b, writes=[pbank_b[pb]], inc=(kc == 7))
                    tr.op("dve", lambda e, pb=pb, s=s, n=n: e.tensor_tensor(
                        out=x_sb[:, s, n * 512:(n + 1) * 512], in0=pbank[pb][:, :],
                        in1=x_sb[:, s, n * 512:(n + 1) * 512], op=ALU.add),
                        reads=[pbank_b[pb], x_b[s]], writes=[x_b[s]])
            dump(f"x1_{ti}", x_sb[:], x_b, [128, NSUB, D])
            for s in range(NSUB):
                norm_transpose(x_sb[:, s, :], x_b[s], ymT, ymT_b[s], s, ti * NSUB + s)

        ost_rr = [0]

        def ffn(ti):
            r0 = ti * T
            for a in range(6):
                wv, wb = wload(f"ffi{a}")
                js = list(range(4 * a, min(4 * a + 4, 22)))
                for idx, j in enumerate(js):
                    gb, ub = gemm_bank(), gemm_bank()
                    for (bank, seg) in ((gb, 2 * idx), (ub, 2 * idx + 1)):
                        for kc in range(8):
                            tr.op("pe", lambda e, bank=bank, seg=seg, kc=kc, wv=wv: e.matmul(
                                pbank[bank][:, :], lhsT=wv[:, seg, kc, :], rhs=ymT[:, kc, :],
                                start=(kc == 0), stop=(kc == 7)),
                                reads=[wb] + ymT_b, writes=[pbank_b[bank]], inc=(kc == 7))
                    k = j % 2
                    tr.op("act", lambda e, gb=gb, k=k: e.activation(out=sgt[k][:], in_=pbank[gb][:, :], func=AF.Silu),
                          reads=[pbank_b[gb]], writes=[sgt_b[k]])
                    tr.op("dve", lambda e, ub=ub, k=k, j=j: e.tensor_tensor(out=actT[:, j, :], in0=pbank[ub][:, :],
                                                                            in1=sgt[k][:], op=ALU.mult),
                          reads=[pbank_b[ub], sgt_b[k]], writes=[pg[j]])
            for n in range(2):
                for hlf in range(2):
                    wv, wb = wload(f"ffo{n}{hlf}")
                    for k in range(11):
                        kc = hlf * 11 + k
                        for s in range(NSUB):
                            tr.op("pe", lambda e, s=s, kc=kc, k=k, wv=wv, n=n: e.matmul(
                                pbank[s + 4 * n][:, :], lhsT=actT[:, kc, s * 128:(s + 1) * 128], rhs=wv[:, 0, k, :],
                                start=(kc == 0), stop=(kc == 21)),
                                reads=[wb, pg[kc]], writes=[pbank_b[s + 4 * n]], inc=(k == 10 and s == NSUB - 1))
                for s in range(NSUB):
                    o = ost_rr[0] % 2
                    ost_rr[0] += 1
                    tr.op("dve", lambda e, s=s, n=n, o=o: e.tensor_tensor(
                        out=ostage[o][:], in0=pbank[s + 4 * n][:, :], in1=x_sb[:, s, n * 512:(n + 1) * 512],
                        op=ALU.add),
                        reads=[pbank_b[s + 4 * n], x_b[s]], writes=[ostage_b[o]])
                    dst = out_d[r0 + s * 128:r0 + (s + 1) * 128, n * 512:(n + 1) * 512]
                    tr.dma("sp", lambda e, dst=dst, o=o: e.dma_start(out=dst, in_=ostage[o][:]), f"ost{o}",
                           reads=[ostage_b[o]], nbytes=262144)

        tr.op("pool", lambda e: e.memset(vaug[:, :, :, 256:257], 1.0), writes=vaug_b)

        tile_front(0)
        for ti in range(ntiles):
            first = (ti % 8 == 0)
            inproj_a(ti, first)
            gates(ti, first)
            inproj_b(ti)
            if stop_after == "inproj":
                continue
            for s in range(NSUB):
                mlstm_block(ti, s, first and s == 0)
                if stop_after != "mlstm":
                    attn_block(ti, s, (ti % 8) * NSUB + s)
            dump(f"ymT{ti}", ymT[:], ymT_b, [128, 8, T], BF16)
            dump(f"yaT{ti}", yaT[:], yaT_b, [128, 8, T], BF16)
            if stop_after in ("mlstm", "attn"):
                continue
            merge(ti)
            x_reload(ti)
            outproj(ti)
            if stop_after == "outproj":
                continue
            if ti + 1 < ntiles:
                tile_front(ti + 1)
            ffn(ti)

        tr.schedule(reorder=reorder, prio=prio)

        semnames = set(Tracker.ENG) | set(tr.dma_sems)
        sems = {n: es.enter_context(nc.semaphore("s_" + n)) for n in sorted(semnames)}
        block = es.enter_context(nc.Block())

        def replay(engname):
            def run(eng):
                for item in tr.q[engname]:
                    if item[0] == "wait":
                        eng.wait_ge(sems[item[1]], item[2])
                    else:
                        ins = None
                        for fn in item[1]:
                            ins = fn(eng)
                        ins.then_inc(sems[item[2]], item[3])
            return run

        block.tensor(replay("pe"))
        block.scalar(replay("act"))
        block.vector(replay("dve"))
        block.gpsimd(replay("pool"))
        block.sync(replay("sp"))
    stats = {e: len(tr.q[e]) for e in Tracker.ENG}
    stats['sim_end_us'] = getattr(tr, 'sim_end', 0.0) / 1e3
    stats['sbuf_left'] = sbuf_left
    return nc, dump_specs, stats


def _prep_inputs(inputs, ntiles=16, ncores=NCORES):
    f = np.float32
    x = np.ascontiguousarray(np.asarray(inputs["x"], dtype=f)).reshape(-1, D)
    cbf, cf = _host_consts()
    g1 = np.asarray(inputs["norm1_g"], f).reshape(8, 128).T
    g2 = np.asarray(inputs["norm2_g"], f).reshape(8, 128).T
    gm = np.asarray(inputs["m_norm_g"], f).reshape(8, 128).T
    convw = np.asarray(inputs["conv_w"], f).reshape(4, 16, 128).transpose(2, 1, 0).reshape(128, 64)
    convb = np.asarray(inputs["conv_b"], f).reshape(16, 128).T
    bmc = np.asarray(inputs["b_merge"], f).reshape(16, 128).T
    pcol = np.ascontiguousarray(np.concatenate([g1, g2, gm, convw, convb, bmc], axis=1))
    prow = np.ascontiguousarray(np.concatenate([np.asarray(inputs["q_norm_g"], f).reshape(-1),
                                                np.asarray(inputs["k_norm_g"], f).reshape(-1),
                                                np.asarray(inputs["sinks"], f).reshape(-1)])[None, :])
    pgate = np.ascontiguousarray(np.asarray(inputs["b_mgate"], f).reshape(2, 4).T)
    shared = {
        "w_in": np.ascontiguousarray(np.asarray(inputs["w_in"], f).reshape(D, N_IN)),
        "w_bm": np.ascontiguousarray(np.asarray(inputs["w_branch_m"], f).reshape(D, D)),
        "w_ba": np.ascontiguousarray(np.asarray(inputs["w_branch_a"], f).reshape(D, D)),
        "w_out": np.ascontiguousarray(np.asarray(inputs["w_out"], f).reshape(D, D)),
        "w_fi": np.ascontiguousarray(np.asarray(inputs["w_ffn_in"], f).reshape(D, 2 * DFF)),
        "w_fo": np.ascontiguousarray(np.asarray(inputs["w_ffn_out"], f).reshape(DFF, D)),
        "cbf": cbf, "cf": cf, "pcol": pcol, "prow": prow, "pgate": pgate,
    }
    in_maps = []
    per = TOK_CORE
    for c in range(ncores):
        m = dict(shared)
        m["x"] = x[c * per:c * per + ntiles * T]
        in_maps.append(m)
    return in_maps


_PROGRAM = None


def kernel(**inputs):
    global _PROGRAM
    if _PROGRAM is None:
        _PROGRAM = build_program(16)[0]
    in_maps = _prep_inputs(inputs)
    res = run_bass_kernel_spmd(_PROGRAM, in_maps, core_ids=list(range(NCORES)))
    out = np.concatenate([np.asarray(r["out"], dtype=np.float32) for r in res.results], axis=0)
    return out.reshape(16, SEQ, D)
```

```python
import numpy as np
import ml_dtypes
from contextlib import ExitStack
import concourse.bass as bass
import concourse.mybir as mybir
from concourse.bass_utils import run_bass_kernel_spmd

F32 = mybir.dt.float32
BF16 = mybir.dt.bfloat16
AF = mybir.ActivationFunctionType
ALU = mybir.AluOpType
AX = mybir.AxisListType

D = 1024
SEQ = 4096
NCORES = 8
TOK_CORE = 2 * SEQ
T = 512
NSUB = 4
DFF = 2816
N_IN = 7432
EPS = 1e-6
NEG = -30000.0
O_MQ, O_MK, O_MV, O_MO, O_MI, O_MF, O_AQ, O_AK, O_AV, O_GM, O_GA = (
    0, 1024, 2048, 3072, 4096, 4100, 4104, 5128, 5256, 5384, 6408)


class Buf:
    __slots__ = ("name", "w", "r", "excl")

    def __init__(self, name="", excl=False):
        self.name = name
        self.w = None
        self.r = []
        self.excl = excl


class Op:
    __slots__ = ("id", "eng", "fns", "preds", "dur", "sem", "nbytes", "tick", "fin")

    def __init__(self, id, eng, sem=None, nbytes=0):
        self.id = id
        self.eng = eng
        self.fns = []
        self.preds = set()
        self.dur = 0.0
        self.sem = sem
        self.nbytes = nbytes
        self.tick = 0
        self.fin = 0.0


def _est(eng, n):
    if eng == "pe":
        return 60.0 + 0.33 * max(n, 64)
    if eng == "act":
        return 200.0 + 0.85 * n
    if eng == "dve":
        return 180.0 + 1.05 * n
    if eng == "pool":
        return 300.0 + 3.0 * n
    return 60.0


class Tracker:
    ENG = ("pe", "act", "dve", "pool", "sp")

    def __init__(self):
        self.ops = []
        self.unit = None
        self.q = {e: [] for e in self.ENG}
        self.dma_sems = set()

    def _preds(self, reads, writes, self_id):
        p = set()
        for b in reads:
            if b.w is not None:
                p.add(b.w)
            if b.excl:
                p.update(b.r)
        for b in writes:
            if b.w is not None:
                p.add(b.w)
            p.update(b.r)
        p.discard(self_id)
        return p

    def _mark(self, oid, reads, writes):
        for b in writes:
            b.w = oid
            b.r = []
        for b in reads:
            if b.excl:
                b.w = oid
                b.r = []
            elif not b.r or b.r[-1] != oid:
                b.r.append(oid)

    class _Probe:
        def __init__(self):
            self.n = 512

        def __getattr__(self, name):
            def f(*args, **kw):
                out = kw.get("out", args[0] if args else None)
                try:
                    shp = out.shape
                    m = 1
                    for d in shp[1:]:
                        m *= int(d)
                    self.n = m
                except Exception:
                    pass
                return None
            return f

    def op(self, eng, fn, reads=(), writes=(), inc=True, n=None):
        if n is None:
            pr = Tracker._Probe()
            fn(pr)
            n = pr.n
        if eng == "pe" and self.unit is not None:
            o = self.unit
        else:
            o = Op(len(self.ops), eng)
            self.ops.append(o)
            if eng == "pe":
                self.unit = o
        o.fns.append(fn)
        o.dur += _est(eng, n)
        o.preds |= self._preds(reads, writes, o.id)
        self._mark(o.id, reads, writes)
        if eng == "pe" and inc:
            self.unit = None

    def dma(self, eng, fn, sem, reads=(), writes=(), nbytes=65536):
        assert self.unit is None
        o = Op(len(self.ops), eng, sem=sem, nbytes=nbytes)
        self.ops.append(o)
        self.dma_sems.add(sem)
        o.fns.append(fn)
        o.dur = 60.0
        o.preds |= self._preds(reads, writes, o.id)
        self._mark(o.id, reads, writes)

    def schedule(self, reorder=True, prio="order"):
        import heapq
        ops = self.ops
        n = len(ops)
        succs = [[] for _ in range(n)]
        indeg = [0] * n
        for o in ops:
            indeg[o.id] = len(o.preds)
            for p in o.preds:
                succs[p].append(o.id)
        order = {e: [] for e in self.ENG}
        if not reorder:
            for o in ops:
                order[o.eng].append(o)
        else:
            key = list(range(n))
            if prio == "cp":
                ind2 = list(indeg)
                topo = [i for i in range(n) if ind2[i] == 0]
                k = 0
                while k < len(topo):
                    for sid in succs[topo[k]]:
                        ind2[sid] -= 1
                        if ind2[sid] == 0:
                            topo.append(sid)
                    k += 1
                bl = [0.0] * n
                for i in reversed(topo):
                    m = 0.0
                    for sid in succs[i]:
                        if bl[sid] > m:
                            m = bl[sid]
                    d = ops[i].dur if ops[i].sem is None else 2000.0 + ops[i].nbytes / 300.0
                    bl[i] = m + d
                rank = sorted(range(n), key=lambda i: (-bl[i], i))
                for r, i in enumerate(rank):
                    key[i] = r
            inv = [0] * n
            for i in range(n):
                inv[key[i]] = i
            free_at = {e: 0.0 for e in self.ENG}
            pend = {e: [] for e in self.ENG}
            avail = {e: [] for e in self.ENG}
            ready_t = [0.0] * n
            for o in ops:
                if indeg[o.id] == 0:
                    heapq.heappush(pend[o.eng], (0.0, key[o.id]))
            dma_free = 0.0
            done = 0
            while done < n:
                best = None
                for e in self.ENG:
                    pe_, av = pend[e], avail[e]
                    while pe_ and pe_[0][0] <= free_at[e]:
                        heapq.heappush(av, heapq.heappop(pe_)[1])
                    if av:
                        cand = (free_at[e], av[0], e, True)
                    elif pe_:
                        cand = (pe_[0][0], pe_[0][1], e, False)
                    else:
                        continue
                    if best is None or cand[:2] < best[:2]:
                        best = cand
                start, okey, e, from_av = best
                if from_av:
                    heapq.heappop(avail[e])
                else:
                    heapq.heappop(pend[e])
                oid = inv[okey]
                o = ops[oid]
                if o.sem is not None:
                    free_at[e] = start + o.dur
                    t0 = max(start, dma_free)
                    dma_free = t0 + o.nbytes / 300.0
                    o.fin = dma_free + 2000.0
                else:
                    o.fin = start + o.dur
                    free_at[e] = o.fin
                order[e].append(o)
                done += 1
                for sid in succs[oid]:
                    so = ops[sid]
                    lat = 0.0 if so.eng == e else 150.0
                    if ready_t[sid] < o.fin + lat:
                        ready_t[sid] = o.fin + lat
                    indeg[sid] -= 1
                    if indeg[sid] == 0:
                        heapq.heappush(pend[so.eng], (ready_t[sid], key[sid]))
            self.sim_end = max(o.fin for o in ops)
        dma_cnt = {}
        for e in self.ENG:
            c = 0
            for o in order[e]:
                if o.sem is not None:
                    dma_cnt[o.sem] = dma_cnt.get(o.sem, 0) + 16
                    o.tick = dma_cnt[o.sem]
                else:
                    c += 1
                    o.tick = c
        for e in self.ENG:
            waited = {}
            q = self.q[e]
            for o in order[e]:
                need = {}
                for p in o.preds:
                    po = ops[p]
                    if po.eng == "pe" and e == "pe":
                        continue
                    k = po.sem if po.sem is not None else po.eng
                    if need.get(k, 0) < po.tick:
                        need[k] = po.tick
                for k, v in need.items():
                    if waited.get(k, 0) < v:
                        waited[k] = v
                        q.append(("wait", k, v))
                k = o.sem if o.sem is not None else o.eng
                q.append(("op", o.fns, k, 16 if o.sem is not None else 1))
            if e == "sp":
                for k, v in dma_cnt.items():
                    if waited.get(k, 0) < v:
                        waited[k] = v
                        q.append(("wait", k, v))


def _host_consts():
    bf = ml_dtypes.bfloat16
    p = np.arange(128)
    ident = np.eye(128, dtype=np.float32)
    s = p[:, None]
    t = p[None, :]
    mask16 = np.where(s <= t, 1.0 / 16.0, 0.0).astype(np.float32)
    mcur = np.where(s <= t, 0.0, NEG).astype(np.float32)
    mprev = np.where(s > t, 0.0, NEG).astype(np.float32)
    cbf = np.concatenate([ident, mask16, mprev, mprev, mcur, mcur], axis=1).astype(bf)
    half = 8
    inv_freq = (500000.0 ** (-np.arange(half, dtype=np.float32) * (2.0 / 16.0))).astype(np.float32)
    pos = (np.arange(32)[None, :] * 128 + p[:, None]).astype(np.float32)
    ang = pos[:, :, None] * inv_freq[None, None, :]
    cos = np.cos(ang).astype(np.float32).reshape(128, 256)
    sin = np.sin(ang).astype(np.float32).reshape(128, 256)
    cf = np.concatenate([ident[:, 0:4], cos, sin], axis=1).astype(np.float32)
    return cbf, cf


CB_ID, CB_M16, CB_MP, CB_MC = 0, 128, 256, 512
CF_ID, CF_COS, CF_SIN = 0, 4, 260


def _weight_items():
    items = []

    def add(name, src_segs, segw, kc0=0, nkc=8):
        items.append(dict(name=name, segs=src_segs, segw=segw, kc0=kc0, nkc=nkc))

    for a in range(2):
        add(f"ina{a}", [("w_in", a * 1024 + j * 128, "g1") for j in range(8)], 128)
    add("inb0", [("w_in", O_MV, "g1"), ("w_in", O_MV + 512, "g1")], 512)
    add("inb1", [("w_in", O_MO, "g1"), ("w_in", O_MO + 512, "g1")], 512)
    add("inb2", [("w_in", O_AQ, "g1"), ("w_in", O_AQ + 512, "g1")], 512)
    add("inb3", [("w_in", O_AK, "g1")], 256)
    for a in range(4):
        segs = []
        for j in (2 * a, 2 * a + 1):
            segs += [("w_bm", j * 128, "gm"), ("w_ba", j * 128, None),
                     ("w_in", O_GM + j * 128, "g1"), ("w_in", O_GA + j * 128, "g1")]
        add(f"mrg{a}", segs, 128)
    add("wout", [("w_out", 0, None), ("w_out", 512, None)], 512)
    for a in range(6):
        js = list(range(4 * a, min(4 * a + 4, 22)))
        segs = []
        for j in js:
            segs += [("w_fi", j * 128, "g2"), ("w_fi", DFF + j * 128, "g2")]
        add(f"ffi{a}", segs, 128)
    for n in range(2):
        for hlf in range(2):
            add(f"ffo{n}{hlf}", [("w_fo", n * 512, None)], 512, kc0=hlf * 11, nkc=11)
    off = 0
    for it in items:
        it["size"] = len(it["segs"]) * it["nkc"] * it["segw"]
        it["off"] = off
        off += it["size"]
    return items, off


SLOT = 8192
NSLOT = 2


def build_program(ntiles=16, dumps=(), stop_after=None, reorder=True, prio="cp"):
    nc = bass.Bass("TRN2", target_bir_lowering=False)
    tr = Tracker()
    items, wtot = _weight_items()
    itmap = {it["name"]: it for it in items}
    dumps = set(dumps)
    dump_specs = {}

    ntok = ntiles * T
    x_d = nc.dram_tensor("x", [ntok, D], F32, kind="ExternalInput").ap()
    out_d = nc.dram_tensor("out", [ntok, D], F32, kind="ExternalOutput").ap()
    wsrc = {
        "w_in": nc.dram_tensor("w_in", [D, N_IN], F32, kind="ExternalInput").ap(),
        "w_bm": nc.dram_tensor("w_bm", [D, D], F32, kind="ExternalInput").ap(),
        "w_ba": nc.dram_tensor("w_ba", [D, D], F32, kind="ExternalInput").ap(),
        "w_out": nc.dram_tensor("w_out", [D, D], F32, kind="ExternalInput").ap(),
        "w_fi": nc.dram_tensor("w_fi", [D, 2 * DFF], F32, kind="ExternalInput").ap(),
        "w_fo": nc.dram_tensor("w_fo", [DFF, D], F32, kind="ExternalInput").ap(),
    }
    cbf_d = nc.dram_tensor("cbf", [128, 768], BF16, kind="ExternalInput").ap()
    cf_d = nc.dram_tensor("cf", [128, 516], F32, kind="ExternalInput").ap()
    pcol_d = nc.dram_tensor("pcol", [128, 24 + 64 + 16 + 16], F32, kind="ExternalInput").ap()
    prow_d = nc.dram_tensor("prow", [1, 144], F32, kind="ExternalInput").ap()
    pgate_d = nc.dram_tensor("pgate", [4, 2], F32, kind="ExternalInput").ap()
    wscr_d = nc.dram_tensor("wscr", [128, wtot], BF16, kind="Internal").ap()

    es = ExitStack()
    with es:
        def sb(name, shape, dt):
            return es.enter_context(nc.sbuf_tensor(name, shape, dt))

        def psum(name, shape, dt):
            return es.enter_context(nc.psum_tensor(name, shape, dt))

        x_sb = sb("x_sb", [128, NSUB, D], F32)
        x_b = [Buf(f"x{s}") for s in range(NSUB)]
        hT = sb("hT", [128, 8, T], BF16)
        hT_b = [Buf(f"hT{s}") for s in range(NSUB)]
        big = sb("big", [128, 12288], BF16)
        pg = [Buf(f"pg{i}") for i in range(24)]
        qkT = big[:, 0:8192].rearrange("p (c t) -> p c t", c=16)
        sigo = big[:, 8192:12288].rearrange("p (s f) -> p s f", s=4)
        actT = big[:, 0:11264].rearrange("p (c t) -> p c t", c=22)
        vaug = sb("vaug", [128, NSUB, 4, 257], BF16)
        vaug_b = [Buf(f"vaug{s}") for s in range(NSUB)]
        aqkv = sb("aqkv", [128, NSUB, 1280], BF16)
        aqkv_b = [Buf(f"aqkv{s}") for s in range(NSUB)]
        ymT = sb("ymT", [128, 8, T], BF16)
        ymT_b = [Buf(f"ymT{s}") for s in range(NSUB)]
        yaT = sb("yaT", [128, 8, T], BF16)
        yaT_b = [Buf(f"yaT{s}") for s in range(NSUB)]
        mgT = sb("mgT", [128, 8, T], BF16)
        mgT_b = [Buf(f"mgT{j}") for j in range(8)]
        wslot = [sb(f"wslot{i}", [128, SLOT], BF16) for i in range(NSLOT)]
        wslot_b = [Buf(f"wslot{i}") for i in range(NSLOT)]
        cbf = sb("cbf_sb", [128, 768], BF16)
        cf = sb("cf_sb", [128, 516], F32)
        const_b = Buf("const")
        pcol = sb("pcol_sb", [128, 120], F32)
        prow = sb("prow_sb", [128, 144], F32)
        pgate = sb("pgate_sb", [4, 2], F32)
        wgate = sb("wgate", [128, 8, 8], BF16)
        wgate_b = Buf("wgate")
        xn = [sb(f"xn{i}", [128, D], BF16) for i in range(2)]
        xn_b = [Buf(f"xn{i}") for i in range(2)]
        junk = sb("junk", [128, 256], BF16)
        junk_b = Buf("junk")
        stat = sb("stat", [128, 64], F32)
        cst = [sb(f"cst{i}", [128, 515], F32) for i in range(2)]
        cst_b = [Buf(f"cst{i}") for i in range(2)]
        cacc = [sb(f"cacc{i}", [128, 512], F32) for i in range(2)]
        cacc_b = [Buf(f"cacc{i}") for i in range(2)]
        carry = sb("carry", [128, 16, 3], F32)
        carry_b = [Buf(f"carry{j}") for j in range(16)]
        ostage = [sb(f"ostage{i}", [128, 512], F32) for i in range(2)]
        ostage_b = [Buf(f"ostage{i}") for i in range(2)]

        gr = sb("gr", [4, 2, T], F32)
        gr_b = [Buf("gr0"), Buf("gr1")]
        gs = sb("gs", [4, 32], F32)
        gs_b = Buf("gs")
        Rm = sb("Rm", [4, 32], F32)
        Rm_b = Buf("Rm")
        ones4 = sb("ones4", [4, 128], F32)
        wt = [sb(f"wt{i}", [128, NSUB, 8], F32) for i in range(2)]
        wt_b = [Buf(f"wt{i}") for i in range(2)]
        decb = [sb(f"decb{i}", [128, 32], F32) for i in range(2)]
        decb_b = [Buf(f"decb{i}") for i in range(2)]
        Cst = sb("Cst", [128, 4, 2, 257], F32)
        Cst_b = [Buf(f"Cst{h}") for h in range(4)]
        Csb = sb("Csb", [128, 4, 2, 257], BF16)
        Csb_b = [Buf(f"Csb{h}") for h in range(4)]
        STm = [sb(f"STm{i}", [128, 4, 128], BF16) for i in range(2)]
        STm_b = [[Buf(f"STm{i}_{h}") for h in range(4)] for i in range(2)]
        wvt = [sb(f"wvt{i}", [128, 4, 257], BF16) for i in range(2)]
        wvt_b = [[Buf(f"wvt{i}_{h}") for h in range(4)] for i in range(2)]
        ktok = [sb(f"ktok{i}", [128, D], BF16) for i in range(2)]
        ktok_b = [Buf(f"ktok{i}") for i in range(2)]
        hms = [sb(f"hm{i}", [128, D], BF16) for i in range(2)]
        hms_b = [[Buf(f"hm{i}_{h}") for h in range(4)] for i in range(2)]
        ymtok = sb("ymtok", [128, D], BF16)
        ymtok_b = Buf("ymtok")
        est = [sb(f"est{i}", [128, 24], F32) for i in range(2)]
        est_b = [Buf(f"est{i}") for i in range(2)]
        sq = sb("sq", [128, 1152], F32)
        sq_b = Buf("sq")
        qn = sb("qn", [128, 1152], F32)
        qn_b = Buf("qn")
        qb = sb("qb", [128, 18, 64], BF16)
        qb_b = Buf("qb")
        k2 = sb("k2", [128, 256], BF16)
        k2_b = Buf("k2")
        QBs = [sb(f"QB{i}", [128, 8, 256], BF16) for i in range(2)]
        QBs_b = [Buf(f"QB{i}") for i in range(2)]
        kTd = sb("kTd", [128, 2, 2, 128], BF16)
        kTd_b = [Buf("kTd0"), Buf("kTd1")]
        vat = sb("vat", [128, 2, 2, 65], BF16)
        vat_b = [Buf("vat0"), Buf("vat1")]
        pT = [sb(f"pT{i}", [128, 512], BF16) for i in range(2)]
        pT_b = [Buf(f"pT{i}") for i in range(2)]
        ya = sb("ya", [128, D], BF16)
        ya_b = Buf("ya")
        ast = sb("ast", [128, 96], F32)
        ast_b = Buf("ast")
        gqk = sb("gqk", [128, 128], F32)
        acst = sb("acst", [128, 32], F32)
        sgt = [sb(f"sgt{i}", [128, 512], BF16) for i in range(4)]
        sgt_b = [Buf(f"sgt{i}") for i in range(4)]
        mt = [sq[:, 0:512], sq[:, 512:1024]]
        mt_b = [sq_b, sq_b]

        sbuf_left = nc.sbuf_bytes_remaining
        pbank = [psum(f"pb{i}", [128, 512], F32) for i in range(8)]
        pbank_b = [Buf(f"pb{i}", excl=True) for i in range(8)]

        ident_f = cf[:, CF_ID:CF_ID + 4]
        MBprev = cbf[:, CB_MP:CB_MP + 256]
        MBcur = cbf[:, CB_MC:CB_MC + 256]
        ident_bf = cbf[:, CB_ID:CB_ID + 128]
        mask16 = cbf[:, CB_M16:CB_M16 + 128]

        g1col = pcol[:, 0:8]
        g2col = pcol[:, 8:16]
        gmcol = pcol[:, 16:24]
        convw = pcol[:, 24:88].rearrange("p (j k) -> p j k", j=16)
        convb = pcol[:, 88:104]
        bmcol = pcol[:, 104:120]
        fold = {"g1": g1col, "g2": g2col, "gm": gmcol, None: None}

        def dump(name, ap, bufs, shape, dt=F32):
            if name not in dumps:
                return
            d = nc.dram_tensor("dbg_" + name, list(shape), dt, kind="ExternalOutput").ap()
            dump_specs[name] = (list(shape), dt)
            tr.dma("sp", lambda e, d=d, ap=ap: e.dma_start(out=d, in_=ap), "dbg_" + name, reads=bufs)

        tr.dma("sp", lambda e: e.dma_start(out=cbf[:], in_=cbf_d), "cld", writes=[const_b])
        tr.dma("sp", lambda e: e.dma_start(out=cf[:], in_=cf_d), "cld", writes=[const_b])
        tr.dma("sp", lambda e: e.dma_start(out=pcol[:], in_=pcol_d), "cld", writes=[const_b])
        tr.dma("sp", lambda e: e.dma_start(out=prow[:], in_=prow_d.partition_broadcast(128)), "cld",
               writes=[const_b])
        tr.dma("sp", lambda e: e.dma_start(out=pgate[:], in_=pgate_d), "cld", writes=[const_b])

        wscr_b = {it["name"]: Buf("wscr_" + it["name"]) for it in items}
        cast_rr = [0]
        stage_rr = [0]

        def prep_cast(out_ap, in_ap, scale_ap, reads, writes):
            k = cast_rr[0] % 2
            cast_rr[0] += 1
            if k == 0:
                if scale_ap is None:
                    tr.op("act", lambda e: e.activation(out=out_ap, in_=in_ap, func=AF.Copy),
                          reads=reads, writes=writes)
                else:
                    tr.op("act", lambda e: e.activation(out=out_ap, in_=in_ap, func=AF.Copy, scale=scale_ap),
                          reads=reads + [const_b], writes=writes)
            else:
                eng = "dve" if k == 1 else "pool"
                if scale_ap is None:
                    tr.op(eng, lambda e: e.tensor_copy(out=out_ap, in_=in_ap), reads=reads, writes=writes)
                else:
                    tr.op(eng, lambda e: e.tensor_scalar(out=out_ap, in0=in_ap, scalar1=scale_ap, scalar2=None,
                                                         op0=ALU.mult),
                          reads=reads + [const_b], writes=writes)

        bigf = big.bitcast(F32)

        vaugf = vaug.reshape([128, NSUB * 4 * 257]).bitcast(F32)
        aqkvf = aqkv.reshape([128, NSUB * 1280]).bitcast(F32)
        vstage = [(vaugf[:, 0:1024], [vaug_b[0], vaug_b[1]]), (vaugf[:, 1028:2052], [vaug_b[2], vaug_b[3]]),
                  (aqkvf[:, 0:1024], [aqkv_b[0], aqkv_b[1]]), (aqkvf[:, 1280:2304], [aqkv_b[2], aqkv_b[3]])]

        def prep_item(it, slot, deep=False, vst=False):
            segs, segw, kc0, nkc = it["segs"], it["segw"], it["kc0"], it["nkc"]
            nseg = len(segs)
            view = wslot[slot][:, 0:it["size"]].rearrange("p (s k w) -> p s k w", s=nseg, k=nkc)
            groups = []
            for si, (src, c0, fd) in enumerate(segs):
                if groups and groups[-1][0] == src and groups[-1][3] == fd and \
                        groups[-1][1] + groups[-1][2] * segw == c0 and \
                        (groups[-1][2] + 1) * segw <= 1024 and groups[-1][4] + groups[-1][2] == si:
                    groups[-1][2] += 1
                else:
                    groups.append([src, c0, 1, fd, si])
            for k in range(nkc):
                kc = kc0 + k
                for (src, c0, n, fd, si0) in groups:
                    st = stage_rr[0] % (10 if deep else NSUB)
                    stage_rr[0] += 1
                    width = n * segw
                    src_ap = wsrc[src][kc * 128:(kc + 1) * 128, c0:c0 + width]
                    if vst:
                        stg, stg_bufs = vstage[st][0][:, 0:width], vstage[st][1]
                        st = 10 + st
                    elif st < NSUB:
                        stg = x_sb[:, st, 0:width]
                        stg_bufs = [x_b[st]]
                    else:
                        stg = bigf[:, (st - NSUB) * 1024:(st - NSUB) * 1024 + width]
                        stg_bufs = pg[4 * (st - NSUB):4 * (st - NSUB) + 4]
                    tr.dma("sp", lambda e, stg=stg, src_ap=src_ap: e.dma_start(out=stg, in_=src_ap),
                           f"xld{st}", writes=stg_bufs, nbytes=width * 512)
                    if n == 1:
                        out_ap = view[:, si0, k, :]
                        in_ap = stg
                    else:
                        out_ap = view[:, si0:si0 + n, k, :]
                        in_ap = stg.rearrange("p (s w) -> p s w", s=n)
                    sc = None if fd is None else fold[fd][:, kc:kc + 1]
                    prep_cast(out_ap, in_ap, sc, list(stg_bufs), [wslot_b[slot]])
            dst = wscr_d[:, it["off"]:it["off"] + it["size"]]
            tr.dma("sp", lambda e, dst=dst, slot=slot, it=it: e.dma_start(out=dst, in_=wslot[slot][:, 0:it["size"]]),
                   f"wst{slot}", reads=[wslot_b[slot]], writes=[wscr_b[it["name"]]], nbytes=it["size"] * 256)

        early_pending = {it["name"] for it in items}
        for kc in range(8):
            st = stage_rr[0] % NSUB
            stage_rr[0] += 1
            stg = x_sb[:, st, 0:8]
            src_ap = wsrc["w_in"][kc * 128:(kc + 1) * 128, O_MI:O_MI + 8]
            tr.dma("sp", lambda e, stg=stg, src_ap=src_ap: e.dma_start(out=stg, in_=src_ap), f"xld{st}",
                   writes=[x_b[st]])
            tr.op("dve", lambda e, kc=kc, stg=stg: e.tensor_scalar(out=wgate[:, kc, :], in0=stg,
                                                                  scalar1=g1col[:, kc:kc + 1], scalar2=None,
                                                                  op0=ALU.mult),
                  reads=[x_b[st], const_b], writes=[wgate_b])

        wcur = {"slot": 0}

        def wload(name):
            it = itmap[name]
            slot = wcur["slot"]
            wcur["slot"] = (slot + 1) % NSLOT
            if name in early_pending:
                early_pending.discard(name)
                prep_item(it, slot, vst=name.startswith(("wout", "ffi", "ffo")))
                view = wslot[slot][:, 0:it["size"]].rearrange("p (s k w) -> p s k w", s=len(it["segs"]),
                                                              k=it["nkc"])
                return view, wslot_b[slot]
            src = wscr_d[:, it["off"]:it["off"] + it["size"]]
            tr.dma("sp", lambda e, src=src, slot=slot, it=it: e.dma_start(out=wslot[slot][:, 0:it["size"]], in_=src),
                   f"wld{slot}", reads=[wscr_b[name]], writes=[wslot_b[slot]], nbytes=it["size"] * 256)
            view = wslot[slot][:, 0:it["size"]].rearrange("p (s k w) -> p s k w", s=len(it["segs"]), k=it["nkc"])
            return view, wslot_b[slot]

        gemm_rr = [0]

        def gemm_bank():
            b = gemm_rr[0] % 4
            gemm_rr[0] += 1
            return b

        xstg = [sq[:, 0:D], qn[:, 0:D]]
        xstg_b = [sq_b, qn_b]

        def tile_front(ti):
            r0 = ti * T
            for s in range(NSUB):
                k = s % 2
                src = x_d[r0 + s * 128:r0 + (s + 1) * 128, :]
                tr.dma("sp", lambda e, k=k, src=src: e.dma_start(out=xstg[k], in_=src), f"xsg{k}",
                       writes=[xstg_b[k]], nbytes=524288)
                norm_transpose(xstg[k], xstg_b[k], hT, hT_b[s], s, ti * NSUB + s)
            dump(f"hT{ti}", hT[:], hT_b, [128, 8, T], BF16)

        def x_reload(ti):
            r0 = ti * T
            for s in range(NSUB):
                src = x_d[r0 + s * 128:r0 + (s + 1) * 128, :]
                tr.dma("sp", lambda e, s=s, src=src: e.dma_start(out=x_sb[:, s, :], in_=src), f"xld{s}",
                       writes=[x_b[s]], nbytes=524288)

        def norm_transpose(src, src_b, dstT, dst_b, s, uid):
            c = (uid % 8) * 4
            ss, lnv, rstd = stat[:, c:c + 1], stat[:, c + 1:c + 2], stat[:, c + 2:c + 3]
            sbuf_ = Buf()
            k = uid % 2
            tr.op("act", lambda e: e.activation(out=xn[k][:], in_=src, func=AF.Square, accum_out=ss),
                  reads=[src_b], writes=[xn_b[k], sbuf_])
            tr.op("act", lambda e: e.activation(out=lnv, in_=ss, func=AF.Ln, scale=1.0 / D, bias=EPS),
                  reads=[sbuf_], writes=[sbuf_])
            tr.op("act", lambda e: e.activation(out=rstd, in_=lnv, func=AF.Exp, scale=-0.5),
                  reads=[sbuf_], writes=[sbuf_])
            tr.op("act", lambda e: e.activation(out=xn[k][:], in_=src, func=AF.Copy, scale=rstd),
                  reads=[src_b, sbuf_], writes=[xn_b[k]])
            pb = 4 + (uid % 2)
            pst = pbank[pb].bitcast(BF16)
            for kc in range(8):
                tr.op("pe", lambda e, kc=kc: e.transpose(out=pst[:, kc * 128:(kc + 1) * 128],
                                                         in_=xn[k][:, kc * 128:(kc + 1) * 128], identity=ident_bf),
                      reads=[xn_b[k], const_b], writes=[pbank_b[pb]], inc=(kc == 7))
            tr.op("act", lambda e: e.activation(out=dstT[:, :, s * 128:(s + 1) * 128],
                                                in_=pst[:, :].rearrange("p (c t) -> p c t", c=8), func=AF.Copy),
                  reads=[pbank_b[pb]], writes=[dst_b])

        def inproj_a(ti, first):
            for a in range(2):
                wv, wb = wload(f"ina{a}")
                for jj in range(8):
                    j = a * 8 + jj
                    pb = gemm_bank()
                    for kc in range(8):
                        tr.op("pe", lambda e, jj=jj, kc=kc, pb=pb, wv=wv: e.matmul(
                            pbank[pb][:, :], lhsT=wv[:, jj, kc, :], rhs=hT[:, kc, :], start=(kc == 0), stop=(kc == 7)),
                            reads=[wb] + hT_b, writes=[pbank_b[pb]], inc=(kc == 7))
                    k = j % 2
                    if first:
                        tr.op("pool", lambda e, k=k: e.memset(cst[k][:, 0:3], 0.0), writes=[cst_b[k]])
                    else:
                        tr.op("pool", lambda e, k=k, j=j: e.tensor_copy(out=cst[k][:, 0:3], in_=carry[:, j, :]),
                              reads=[carry_b[j]], writes=[cst_b[k]])
                    tr.op("act", lambda e, k=k, pb=pb: e.activation(out=cst[k][:, 3:515], in_=pbank[pb][:, :],
                                                                    func=AF.Copy),
                          reads=[pbank_b[pb]], writes=[cst_b[k]])
                    tr.op("pool", lambda e, k=k, j=j: e.tensor_copy(out=carry[:, j, :], in_=cst[k][:, 512:515]),
                          reads=[cst_b[k]], writes=[carry_b[j]])
                    tr.op("act", lambda e, k=k, j=j, pb=pb: e.activation(out=cacc[k][:, 3:512],
                                                                         in_=pbank[pb][:, 0:509], func=AF.Copy,
                                                                         scale=convw[:, j, 0:1]),
                          reads=[pbank_b[pb], const_b], writes=[cacc_b[k]])
                    tr.op("pool", lambda e, k=k, j=j: e.tensor_scalar(out=cacc[k][:, 0:3], in0=cst[k][:, 0:3],
                                                                       scalar1=convw[:, j, 0:1], scalar2=None,
                                                                       op0=ALU.mult),
                          reads=[cst_b[k], const_b, cacc_b[k]], writes=[cacc_b[k]])
                    for tap in range(1, 4):
                        tr.op("dve", lambda e, k=k, j=j, tap=tap: e.scalar_tensor_tensor(
                            out=cacc[k][:], in0=cst[k][:, tap:tap + 512], scalar=convw[:, j, tap:tap + 1],
                            in1=cacc[k][:], op0=ALU.mult, op1=ALU.add),
                            reads=[cst_b[k], cacc_b[k], const_b], writes=[cacc_b[k]])
                    tr.op("act", lambda e, k=k, j=j: e.activation(out=qkT[:, j, :], in_=cacc[k][:], func=AF.Silu,
                                                                   bias=convb[:, j:j + 1]),
                          reads=[cacc_b[k], const_b], writes=[pg[j]])
            dump(f"qkT{ti}", qkT, pg[0:16], [128, 16, T], BF16)

        def inproj_b(ti):
            plan = [("inb0", [("v", 0), ("v", 2)]), ("inb1", [("o", 0), ("o", 512)]),
                    ("inb2", [("q", 0), ("q", 512)]), ("inb3", [("kv", 0)])]
            for name, segl in plan:
                wv, wb = wload(name)
                segw = itmap[name]["segw"]
                for si, (kind, arg) in enumerate(segl):
                    for s in range(NSUB):
                        pb = gemm_bank()
                        for kc in range(8):
                            tr.op("pe", lambda e, si=si, kc=kc, pb=pb, wv=wv, s=s, segw=segw: e.matmul(
                                pbank[pb][:, 0:segw], lhsT=hT[:, kc, s * 128:(s + 1) * 128], rhs=wv[:, si, kc, :],
                                start=(kc == 0), stop=(kc == 7)),
                                reads=[wb, hT_b[s]], writes=[pbank_b[pb]], inc=(kc == 7))
                        if kind == "v":
                            tr.op("act", lambda e, pb=pb, s=s, arg=arg: e.activation(
                                out=vaug[:, s, arg:arg + 2, 0:256],
                                in_=pbank[pb][:, :].rearrange("p (h e) -> p h e", h=2), func=AF.Copy),
                                reads=[pbank_b[pb]], writes=[vaug_b[s]])
                        elif kind == "o":
                            tr.op("act", lambda e, pb=pb, s=s, arg=arg: e.activation(
                                out=sigo[:, s, arg:arg + 512], in_=pbank[pb][:, :], func=AF.Sigmoid),
                                reads=[pbank_b[pb]], writes=[pg[16 + 2 * s], pg[17 + 2 * s]])
                        elif kind == "q":
                            tr.op("dve", lambda e, pb=pb, s=s, arg=arg: e.tensor_copy(
                                out=aqkv[:, s, arg:arg + 512], in_=pbank[pb][:, :]),
                                reads=[pbank_b[pb]], writes=[aqkv_b[s]])
                        else:
                            tr.op("dve", lambda e, pb=pb, s=s: e.tensor_copy(
                                out=aqkv[:, s, 1024:1280], in_=pbank[pb][:, 0:256]),
                                reads=[pbank_b[pb]], writes=[aqkv_b[s]])
            dump(f"vaug{ti}", vaug[:], vaug_b, [128, NSUB, 4, 257], BF16)
            dump(f"sigo{ti}", sigo, pg[16:24], [128, NSUB, 1024], BF16)
            dump(f"aqkv{ti}", aqkv[:], aqkv_b, [128, NSUB, 1280], BF16)

        setup_b = Buf("setup")
        for i in range(2):
            tr.op("pool", lambda e, i=i: e.memset(QBs[i][:], 0.0), writes=[QBs_b[i]])
        tr.op("pool", lambda e: e.memset(vat[:, :, :, 64:65], 1.0), writes=vat_b)
        tr.op("pool", lambda e: e.memset(ones4[:], 1.0), writes=[setup_b])
        tr.op("dve", lambda e: e.tensor_scalar(out=gqk[:, 0:64], in0=prow[:, 0:64],
                                               scalar1=0.125, scalar2=None, op0=ALU.mult),
              reads=[const_b], writes=[setup_b])
        tr.op("dve", lambda e: e.tensor_copy(out=gqk[:, 64:128], in_=prow[:, 64:128]),
              reads=[const_b, setup_b], writes=[setup_b])
        tr.op("dve", lambda e: e.tensor_reduce(out=acst[:, 1:2], in_=prow[:, 0:64], axis=AX.X, op=ALU.max,
                                               apply_absolute_value=True),
              reads=[const_b, setup_b], writes=[setup_b])
        tr.op("dve", lambda e: e.tensor_reduce(out=acst[:, 2:3], in_=prow[:, 64:128], axis=AX.X, op=ALU.max,
                                               apply_absolute_value=True),
              reads=[const_b, setup_b], writes=[setup_b])
        tr.op("dve", lambda e: e.scalar_tensor_tensor(out=acst[:, 0:1], in0=acst[:, 1:2], scalar=-8.0,
                                                      in1=acst[:, 2:3], op0=ALU.mult, op1=ALU.mult),
              reads=[setup_b], writes=[setup_b])
        nlmax = acst[:, 0:1]
        tr.op("act", lambda e: e.activation(out=acst[:, 16:32], in_=prow[:, 128:144], func=AF.Exp, bias=nlmax),
              reads=[const_b, setup_b], writes=[setup_b])
        sinkexp = acst[:, 16:32]
        tr.op("dve", lambda e: e.tensor_scalar(out=gs[:, 28:29], in0=pgate[:, 1:2], scalar1=-1.0, scalar2=None,
                                               op0=ALU.mult),
              reads=[const_b], writes=[setup_b])
        nbf = gs[:, 28:29]
        bi = pgate[:, 0:1]

        GS_MBLK, GS_MC, GS_NMC, GS_MPREV, GS_DIF, GS_DEC = 0, 4, 8, 12, 17, 21

        def gates(ti, first):
            tp = ti % 2
            bi_, bf_, bt_, bd_ = gemm_bank(), gemm_bank(), gemm_bank(), gemm_bank()
            for (bank, c0) in ((bi_, 0), (bf_, 4)):
                for kc in range(8):
                    tr.op("pe", lambda e, bank=bank, c0=c0, kc=kc: e.matmul(
                        pbank[bank][0:4, :], lhsT=wgate[:, kc, c0:c0 + 4], rhs=hT[:, kc, :],
                        start=(kc == 0), stop=(kc == 7)),
                        reads=[wgate_b] + hT_b, writes=[pbank_b[bank]], inc=(kc == 7))
            ipre, fpre = pbank[bi_][0:4, :], pbank[bf_][0:4, :]
            g0, g1 = gr[:, 0, :], gr[:, 1, :]
            tr.op("act", lambda e: e.activation(out=g0, in_=fpre, func=AF.Exp, scale=-1.0, bias=nbf),
                  reads=[pbank_b[bf_], setup_b], writes=[gr_b[0]])
            tr.op("act", lambda e: e.activation(out=g0, in_=g0, func=AF.Ln, bias=1.0),
                  reads=[gr_b[0]], writes=[gr_b[0]])
            for j in range(NSUB):
                tr.op("dve", lambda e, j=j: e.tensor_tensor_scan(
                    out=g1[:, j * 128:(j + 1) * 128], data0=ones4[:, 0:128], data1=g0[:, j * 128:(j + 1) * 128],
                    initial=0.0, op0=ALU.mult, op1=ALU.add),
                    reads=[gr_b[0], setup_b], writes=[gr_b[1]])
            tr.op("dve", lambda e: e.scalar_tensor_tensor(out=g0, in0=ipre, scalar=bi, in1=g1, op0=ALU.add,
                                                          op1=ALU.add),
                  reads=[pbank_b[bi_], gr_b[1], const_b, gr_b[0]], writes=[gr_b[0]])
            tr.op("dve", lambda e: e.tensor_reduce(out=gs[:, GS_MBLK:GS_MBLK + 4],
                                                   in_=g0.rearrange("p (j t) -> p j t", j=4), axis=AX.X, op=ALU.max),
                  reads=[gr_b[0]], writes=[gs_b])
            if first:
                tr.op("dve", lambda e: e.memset(gs[:, GS_MPREV:GS_MPREV + 1], 0.0), reads=[gs_b], writes=[gs_b])
            else:
                tr.op("dve", lambda e: e.tensor_copy(out=gs[:, GS_MPREV:GS_MPREV + 1],
                                                     in_=gs[:, GS_MPREV + 4:GS_MPREV + 5]),
                      reads=[gs_b], writes=[gs_b])
            for j in range(NSUB):
                tr.op("dve", lambda e, j=j: e.tensor_tensor(out=gs[:, GS_MC + j:GS_MC + j + 1],
                                                            in0=gs[:, GS_MPREV + j:GS_MPREV + j + 1],
                                                            in1=gs[:, GS_MBLK + j:GS_MBLK + j + 1], op=ALU.max),
                      reads=[gs_b], writes=[gs_b])
                tr.op("dve", lambda e, j=j: e.tensor_tensor(out=gs[:, GS_MPREV + j + 1:GS_MPREV + j + 2],
                                                            in0=gs[:, GS_MC + j:GS_MC + j + 1],
                                                            in1=g1[:, j * 128 + 127:j * 128 + 128], op=ALU.subtract),
                      reads=[gs_b, gr_b[1]], writes=[gs_b])
            tr.op("dve", lambda e: e.tensor_tensor(out=gs[:, GS_DIF:GS_DIF + 4], in0=gs[:, GS_MPREV:GS_MPREV + 4],
                                                   in1=gs[:, GS_MC:GS_MC + 4], op=ALU.subtract),
                  reads=[gs_b], writes=[gs_b])
            tr.op("dve", lambda e: e.tensor_scalar(out=gs[:, GS_NMC:GS_NMC + 4], in0=gs[:, GS_MC:GS_MC + 4],
                                                   scalar1=-1.0, scalar2=None, op0=ALU.mult),
                  reads=[gs_b], writes=[gs_b])
            tr.op("act", lambda e: e.activation(out=gs[:, GS_DEC:GS_DEC + 4], in_=gs[:, GS_DIF:GS_DIF + 4],
                                                func=AF.Exp),
                  reads=[gs_b], writes=[gs_b])
            for j in range(NSUB):
                sl = slice(j * 128, (j + 1) * 128)
                tr.op("act", lambda e, j=j, sl=sl: e.activation(out=g1[:, sl], in_=g1[:, sl], func=AF.Exp,
                                                                bias=gs[:, GS_NMC + j:GS_NMC + j + 1]),
                      reads=[gs_b, gr_b[1]], writes=[gr_b[1]])
                tr.op("act", lambda e, j=j, sl=sl: e.activation(out=g0[:, sl], in_=g0[:, sl], func=AF.Exp,
                                                                bias=gs[:, GS_NMC + j:GS_NMC + j + 1]),
                      reads=[gs_b, gr_b[0]], writes=[gr_b[0]])
            for j in range(NSUB):
                sl = slice(j * 128, (j + 1) * 128)
                tr.op("pe", lambda e, j=j, sl=sl: e.matmul(pbank[bt_][:, j * 8:j * 8 + 4], lhsT=g0[:, sl],
                                                           rhs=ident_f[0:4, 0:4], start=True, stop=True),
                      reads=[gr_b[0], const_b], writes=[pbank_b[bt_]], inc=False)
                tr.op("pe", lambda e, j=j, sl=sl: e.matmul(pbank[bt_][:, j * 8 + 4:j * 8 + 8], lhsT=g1[:, sl],
                                                           rhs=ident_f[0:4, 0:4], start=True, stop=True),
                      reads=[gr_b[1], const_b], writes=[pbank_b[bt_]], inc=(j == NSUB - 1))
            tr.op("act", lambda e: e.activation(out=wt[tp][:].rearrange("p j c -> p (j c)"), in_=pbank[bt_][:, 0:32],
                                                func=AF.Copy),
                  reads=[pbank_b[bt_]], writes=[wt_b[tp]])
            tr.op("dve", lambda e: e.tensor_tensor(
                out=Rm[:, 0:16].rearrange("p (j h) -> p j h", j=4),
                in0=ident_f[0:4, 0:4].unsqueeze(1).broadcast_to([4, 4, 4]),
                in1=gs[:, GS_DEC:GS_DEC + 4].unsqueeze(2).broadcast_to([4, 4, 4]), op=ALU.mult),
                reads=[gs_b, const_b, Rm_b], writes=[Rm_b])
            tr.op("dve", lambda e: e.tensor_scalar(out=Rm[:, 16:32], in0=Rm[:, 0:16], scalar1=1.0 / 16.0,
                                                   scalar2=None, op0=ALU.mult),
                  reads=[Rm_b], writes=[Rm_b])
            tr.op("pe", lambda e: e.matmul(pbank[bd_][:, 0:32], lhsT=ones4[:, 0:128], rhs=Rm[:, 0:32],
                                           start=True, stop=True),
                  reads=[Rm_b, setup_b], writes=[pbank_b[bd_]])
            tr.op("act", lambda e: e.activation(out=decb[tp][:], in_=pbank[bd_][:, 0:32], func=AF.Copy),
                  reads=[pbank_b[bd_]], writes=[decb_b[tp]])
            dump(f"wt{ti}", wt[tp][:], [wt_b[tp]], [128, NSUB, 8])
            dump(f"decb{ti}", decb[tp][:], [decb_b[tp]], [128, 32])

        def mlstm_block(ti, s, first_block):
            tp = ti % 2
            par = s % 2
            sl = slice(s * 128, (s + 1) * 128)
            if first_block:
                tr.op("pool", lambda e: e.memset(Cst[:], 0.0), writes=Cst_b)
            pst = pbank[7].bitcast(BF16)
            for c in range(8):
                tr.op("pe", lambda e, c=c: e.transpose(out=pst[:, c * 128:(c + 1) * 128], in_=qkT[:, 8 + c, sl],
                                                       identity=ident_bf),
                      reads=[pg[8 + c], const_b], writes=[pbank_b[7]], inc=(c == 7))
            tr.op("act", lambda e: e.activation(out=ktok[par][:], in_=pst[:, :], func=AF.Copy),
                  reads=[pbank_b[7]], writes=[ktok_b[par]])
            e_ = est[par]
            hm, hm_b = hms[par], hms_b[par]
            for h in range(4):
                for dc in range(2):
                    tr.op("pe", lambda e, h=h, dc=dc: e.matmul(pbank[5][:, 0:128], lhsT=qkT[:, 8 + 2 * h + dc, sl],
                                                               rhs=qkT[:, 2 * h + dc, sl], start=(dc == 0),
                                                               stop=(dc == 1)),
                          reads=[pg[8 + 2 * h + dc], pg[2 * h + dc]], writes=[pbank_b[5]], inc=(dc == 1))
                tr.op("dve", lambda e, h=h: e.tensor_tensor(out=STm[par][:, h, :], in0=pbank[5][:, 0:128],
                                                            in1=mask16, op=ALU.mult),
                      reads=[pbank_b[5], const_b], writes=[STm_b[par][h]])
                tr.op("act", lambda e, h=h: e.activation(out=wvt[par][:, h, :], in_=vaug[:, s, h, :], func=AF.Copy,
                                                         scale=wt[tp][:, s, h:h + 1]),
                      reads=[vaug_b[s], wt_b[tp]], writes=[wvt_b[par][h]])
                tr.op("act", lambda e, h=h: e.activation(
                    out=Csb[:, h, :, :], in_=Cst[:, h, :, :], func=AF.Copy,
                    scale=decb[tp][:, 16 + s * 4 + h:16 + s * 4 + h + 1]),
                    reads=[Cst_b[h], decb_b[tp]], writes=[Csb_b[h]])
                num = pbank[6][:, 0:257]
                tr.op("pe", lambda e, h=h: e.matmul(num, lhsT=STm[par][:, h, :], rhs=wvt[par][:, h, :],
                                                    start=True, stop=False),
                      reads=[STm_b[par][h], wvt_b[par][h]], writes=[pbank_b[6]], inc=False)
                for dc in range(2):
                    tr.op("pe", lambda e, h=h, dc=dc: e.matmul(num, lhsT=qkT[:, 2 * h + dc, sl],
                                                               rhs=Csb[:, h, dc, :], start=False, stop=(dc == 1)),
                          reads=[pg[2 * h + dc], Csb_b[h]], writes=[pbank_b[6]], inc=(dc == 1))
                for dc, bank, c0 in ((0, 7, 0), (1, 5, 128)):
                    tr.op("pe", lambda e, h=h, dc=dc, bank=bank, c0=c0: e.matmul(
                        pbank[bank][:, c0:c0 + 257], lhsT=ktok[par][:, h * 256 + dc * 128:h * 256 + (dc + 1) * 128],
                        rhs=wvt[par][:, h, :], start=True, stop=True),
                        reads=[ktok_b[par], wvt_b[par][h]], writes=[pbank_b[bank]])
                    tr.op("dve", lambda e, h=h, dc=dc, bank=bank, c0=c0: e.scalar_tensor_tensor(
                        out=Cst[:, h, dc, :], in0=Cst[:, h, dc, :],
                        scalar=decb[tp][:, s * 4 + h:s * 4 + h + 1], in1=pbank[bank][:, c0:c0 + 257],
                        op0=ALU.mult, op1=ALU.add),
                        reads=[Cst_b[h], decb_b[tp], pbank_b[bank]], writes=[Cst_b[h]])
                tr.op("act", lambda e, h=h: e.activation(out=e_[:, 20 + h:21 + h], in_=pbank[6][:, 256:257],
                                                         func=AF.Abs),
                      reads=[pbank_b[6], est_b[par]], writes=[est_b[par]])
                tr.op("dve", lambda e, h=h: e.tensor_tensor(out=e_[:, h:h + 1], in0=e_[:, 20 + h:21 + h],
                                                            in1=wt[tp][:, s, 4 + h:5 + h], op=ALU.max),
                      reads=[wt_b[tp], est_b[par]], writes=[est_b[par]])
                tr.op("dve", lambda e, h=h: e.reciprocal(out=e_[:, 4 + h:5 + h], in_=e_[:, h:h + 1]),
                      reads=[est_b[par]], writes=[est_b[par]])
                tr.op("dve", lambda e, h=h: e.scalar_tensor_tensor(
                    out=hm[:, h * 256:(h + 1) * 256], in0=pbank[6][:, 0:256], scalar=e_[:, 4 + h:5 + h],
                    in1=sigo[:, s, h * 256:(h + 1) * 256], op0=ALU.mult, op1=ALU.mult),
                    reads=[pbank_b[6], est_b[par], pg[16 + 2 * s], pg[17 + 2 * s]], writes=[hm_b[h]])
                tr.op("act", lambda e, h=h: e.activation(out=junk[:, 0:256], in_=hm[:, h * 256:(h + 1) * 256],
                                                         func=AF.Square, accum_out=e_[:, 8 + h:9 + h]),
                      reads=[hm_b[h], est_b[par]], writes=[junk_b, est_b[par]])
            tr.op("act", lambda e: e.activation(out=e_[:, 12:16], in_=e_[:, 8:12], func=AF.Ln, scale=1.0 / 256.0,
                                                bias=EPS),
                  reads=[est_b[par]], writes=[est_b[par]])
            tr.op("act", lambda e: e.activation(out=e_[:, 16:20], in_=e_[:, 12:16], func=AF.Exp, scale=-0.5),
                  reads=[est_b[par]], writes=[est_b[par]])
            tr.op("dve", lambda e: e.tensor_tensor(
                out=ymtok[:].rearrange("p (h e) -> p h e", h=4), in0=hm[:].rearrange("p (h e) -> p h e", h=4),
                in1=e_[:, 16:20].unsqueeze(2).broadcast_to([128, 4, 256]), op=ALU.mult),
                reads=hm_b + [est_b[par]], writes=[ymtok_b])
            for c in range(8):
                tr.op("pe", lambda e, c=c: e.transpose(out=pst[:, c * 128:(c + 1) * 128],
                                                       in_=ymtok[:, c * 128:(c + 1) * 128], identity=ident_bf),
                      reads=[ymtok_b, const_b], writes=[pbank_b[7]], inc=(c == 7))
            tr.op("act", lambda e: e.activation(out=ymT[:, :, sl], in_=pst[:, :].rearrange("p (c t) -> p c t", c=8),
                                                func=AF.Copy),
                  reads=[pbank_b[7]], writes=[ymT_b[s]])

        def attn_block(ti, s, jb):
            par = jb % 2
            first = (jb == 0)
            QB, QB_b = QBs[par], QBs_b[par]
            src = aqkv[:, s, 0:1152]
            tr.op("act", lambda e: e.activation(out=sq[:], in_=src, func=AF.Square),
                  reads=[aqkv_b[s]], writes=[sq_b])
            tr.op("dve", lambda e: e.tensor_reduce(out=ast[:, 0:18], in_=sq[:].rearrange("p (h d) -> p h d", h=18),
                                                   axis=AX.X, op=ALU.add),
                  reads=[sq_b, ast_b], writes=[ast_b])
            tr.op("act", lambda e: e.activation(out=ast[:, 18:36], in_=ast[:, 0:18], func=AF.Ln, scale=1.0 / 64.0,
                                                bias=EPS),
                  reads=[ast_b], writes=[ast_b])
            tr.op("act", lambda e: e.activation(out=ast[:, 36:54], in_=ast[:, 18:36], func=AF.Exp, scale=-0.5),
                  reads=[ast_b], writes=[ast_b])
            qn3 = qn[:].rearrange("p (h d) -> p h d", h=18)
            rt = sq[:, 0:576].rearrange("p (a h d) -> p a h d", a=4, h=18)
            rt_b = sq_b
            tr.op("dve", lambda e: e.tensor_tensor(out=qn3, in0=src.rearrange("p (h d) -> p h d", h=18),
                                                   in1=ast[:, 36:54].unsqueeze(2).broadcast_to([128, 18, 64]),
                                                   op=ALU.mult),
                  reads=[aqkv_b[s], ast_b], writes=[qn_b])
            tr.op("dve", lambda e: e.tensor_tensor(out=qn3[:, 0:16, :], in0=qn3[:, 0:16, :],
                                                   in1=gqk[:, 0:64].unsqueeze(1).broadcast_to([128, 16, 64]),
                                                   op=ALU.mult),
                  reads=[qn_b, setup_b], writes=[qn_b])
            tr.op("dve", lambda e: e.tensor_tensor(out=qn3[:, 16:18, :], in0=qn3[:, 16:18, :],
                                                   in1=gqk[:, 64:128].unsqueeze(1).broadcast_to([128, 2, 64]),
                                                   op=ALU.mult),
                  reads=[qn_b, setup_b], writes=[qn_b])
            cosb = cf[:, CF_COS + jb * 8:CF_COS + jb * 8 + 8].unsqueeze(1).broadcast_to([128, 18, 8])
            sinb = cf[:, CF_SIN + jb * 8:CF_SIN + jb * 8 + 8].unsqueeze(1).broadcast_to([128, 18, 8])
            x1, x2 = qn3[:, :, 0:8], qn3[:, :, 8:16]
            tr.op("dve", lambda e: e.tensor_tensor(out=rt[:, 0, :, :], in0=x1, in1=cosb, op=ALU.mult),
                  reads=[qn_b, const_b], writes=[rt_b])
            tr.op("dve", lambda e: e.tensor_tensor(out=rt[:, 1, :, :], in0=x2, in1=sinb, op=ALU.mult),
                  reads=[qn_b, const_b, rt_b], writes=[rt_b])
            tr.op("dve", lambda e: e.tensor_tensor(out=rt[:, 2, :, :], in0=x2, in1=cosb, op=ALU.mult),
                  reads=[qn_b, const_b, rt_b], writes=[rt_b])
            tr.op("dve", lambda e: e.tensor_tensor(out=rt[:, 3, :, :], in0=x1, in1=sinb, op=ALU.mult),
                  reads=[qn_b, const_b, rt_b], writes=[rt_b])
            tr.op("dve", lambda e: e.tensor_tensor(out=qb[:, :, 0:8], in0=rt[:, 0, :, :], in1=rt[:, 1, :, :],
                                                   op=ALU.subtract),
                  reads=[rt_b], writes=[qb_b])
            tr.op("dve", lambda e: e.tensor_tensor(out=qb[:, :, 8:16], in0=rt[:, 2, :, :], in1=rt[:, 3, :, :],
                                                   op=ALU.add),
                  reads=[rt_b, qb_b], writes=[qb_b])
            tr.op("act", lambda e: e.activation(out=qb[:, :, 16:64], in_=qn3[:, :, 16:64], func=AF.Copy),
                  reads=[qn_b, qb_b], writes=[qb_b])
            tr.op("pool", lambda e: e.tensor_copy(
                out=k2[:].rearrange("p (g r d) -> p g r d", g=2, r=2),
                in_=qb[:, 16:18, :].unsqueeze(2).broadcast_to([128, 2, 2, 64])),
                reads=[qb_b], writes=[k2_b])
            tr.op("pool", lambda e: e.tensor_copy(
                out=vat[:, par, :, 0:64], in_=aqkv[:, s, 1152:1280].rearrange("p (g d) -> p g d", g=2)),
                reads=[aqkv_b[s]], writes=[vat_b[par]])
            pq = pbank[4].bitcast(BF16)
            for g in range(2):
                tr.op("pe", lambda e, g=g: e.transpose(out=pq[:, g * 128:(g + 1) * 128],
                                                       in_=k2[:, g * 128:(g + 1) * 128], identity=ident_bf),
                      reads=[k2_b, const_b], writes=[pbank_b[4]], inc=(g == 1))
            tr.op("act", lambda e: e.activation(out=kTd[:, par, :, :],
                                                in_=pq[:, 0:256].rearrange("p (g t) -> p g t", g=2), func=AF.Copy),
                  reads=[pbank_b[4]], writes=[kTd_b[par]])
            for c in range(8):
                tr.op("pe", lambda e, c=c: e.transpose(out=pq[:, c * 128:(c + 1) * 128],
                                                       in_=qb[:].rearrange("p h d -> p (h d)")[:, c * 128:(c + 1) * 128],
                                                       identity=ident_bf),
                      reads=[qb_b, const_b], writes=[pbank_b[4]], inc=(c == 7))
            pq3 = pq[:, :].rearrange("p (c t) -> p c t", c=8)
            tr.op("act", lambda e: e.activation(out=QB[0:64, :, 0:128], in_=pq3[0:64, :, :], func=AF.Copy),
                  reads=[pbank_b[4]], writes=[QB_b])
            tr.op("dve", lambda e: e.tensor_copy(out=QB[64:128, :, 128:256], in_=pq3[64:128, :, :]),
                  reads=[pbank_b[4], QB_b], writes=[QB_b])
            def po_ap(head):
                if head < 7:
                    return 2, pbank[2][:, head * 65:(head + 1) * 65]
                if head < 14:
                    return 3, pbank[3][:, (head - 7) * 65:(head - 6) * 65]
                return 4, pbank[4][:, (head - 14) * 65:(head - 13) * 65]
            for c in range(8):
                g = c // 4
                lb = c % 2
                lg = pbank[lb]
                if not first:
                    tr.op("pe", lambda e, c=c, g=g, lg=lg: e.matmul(lg[:, 0:256], lhsT=kTd[:, 1 - par, g, :],
                                                                    rhs=QB[:, c, :], start=True, stop=False),
                          reads=[kTd_b[1 - par], QB_b], writes=[pbank_b[lb]], inc=False)
                    tr.op("pe", lambda e, lg=lg: e.matmul(lg[:, 0:256], lhsT=ident_bf, rhs=MBprev,
                                                          start=False, stop=True),
                          reads=[const_b], writes=[pbank_b[lb]], inc=False)
                tr.op("pe", lambda e, c=c, g=g, lg=lg: e.matmul(lg[:, 256:512], lhsT=kTd[:, par, g, :],
                                                                rhs=QB[:, c, :], start=True, stop=False),
                      reads=[kTd_b[par], QB_b], writes=[pbank_b[lb]], inc=False)
                tr.op("pe", lambda e, lg=lg: e.matmul(lg[:, 256:512], lhsT=ident_bf, rhs=MBcur,
                                                      start=False, stop=True),
                      reads=[const_b], writes=[pbank_b[lb]])
                lo = 256 if first else 0
                pk_ = c % 2
                tr.op("act", lambda e, lg=lg, lo=lo, pk_=pk_: e.activation(out=pT[pk_][:, lo:512], in_=lg[:, lo:512],
                                                                           func=AF.Exp, bias=nlmax),
                      reads=[pbank_b[lb], setup_b], writes=[pT_b[pk_]])
                for hh in range(2):
                    head = 2 * c + hh
                    bank, po = po_ap(head)
                    srcs = [(256 + hh * 128, par)]
                    if not first:
                        srcs.append((hh * 128, 1 - par))
                    for i, (col, slot) in enumerate(srcs):
                        tr.op("pe", lambda e, pk_=pk_, col=col, slot=slot, g=g, po=po, i=i, n=len(srcs): e.matmul(
                            po, lhsT=pT[pk_][:, col:col + 128], rhs=vat[:, slot, g, :], start=(i == 0),
                            stop=(i == n - 1)),
                            reads=[pT_b[pk_], vat_b[slot]], writes=[pbank_b[bank]], inc=(i == len(srcs) - 1))
            for (bank, h0, nh) in ((2, 0, 7), (3, 7, 7), (4, 14, 2)):
                po3 = pbank[bank][:, 0:nh * 65].rearrange("p (h d) -> p h d", h=nh)
                dsum = ast[:, 54 + h0:54 + h0 + nh]
                rden = ast[:, 72 + h0:72 + h0 + nh]
                tr.op("dve", lambda e, po3=po3, dsum=dsum, h0=h0, nh=nh: e.tensor_tensor(
                    out=dsum, in0=po3[:, :, 64], in1=sinkexp[:, h0:h0 + nh], op=ALU.add),
                    reads=[pbank_b[bank], setup_b, ast_b], writes=[ast_b])
                tr.op("dve", lambda e, dsum=dsum, rden=rden: e.reciprocal(out=rden, in_=dsum),
                      reads=[ast_b], writes=[ast_b])
                tr.op("dve", lambda e, po3=po3, rden=rden, h0=h0, nh=nh: e.tensor_tensor(
                    out=ya[:, h0 * 64:(h0 + nh) * 64].rearrange("p (h d) -> p h d", h=nh), in0=po3[:, :, 0:64],
                    in1=rden.unsqueeze(2).broadcast_to([128, nh, 64]), op=ALU.mult),
                    reads=[pbank_b[bank], ast_b], writes=[ya_b])
            sl = slice(s * 128, (s + 1) * 128)
            py = pbank[4].bitcast(BF16)
            for c in range(8):
                tr.op("pe", lambda e, c=c: e.transpose(out=py[:, c * 128:(c + 1) * 128],
                                                       in_=ya[:, c * 128:(c + 1) * 128], identity=ident_bf),
                      reads=[ya_b, const_b], writes=[pbank_b[4]], inc=(c == 7))
            tr.op("act", lambda e: e.activation(out=yaT[:, :, sl], in_=py[:, :].rearrange("p (c t) -> p c t", c=8),
                                                func=AF.Copy),
                  reads=[pbank_b[4]], writes=[yaT_b[s]])

        def merge(ti):
            for a in range(4):
                wv, wb = wload(f"mrg{a}")
                for jj in range(2):
                    j = 2 * a + jj
                    for (bank, seg, rhsT, rb) in ((0, 0, ymT, ymT_b), (1, 1, yaT, yaT_b), (2, 2, hT, hT_b),
                                                  (3, 3, hT, hT_b)):
                        for kc in range(8):
                            tr.op("pe", lambda e, bank=bank, seg=seg, rhsT=rhsT, kc=kc, wv=wv, jj=jj: e.matmul(
                                pbank[bank][:, :], lhsT=wv[:, jj * 4 + seg, kc, :], rhs=rhsT[:, kc, :],
                                start=(kc == 0), stop=(kc == 7)),
                                reads=[wb] + rb, writes=[pbank_b[bank]], inc=(kc == 7))
                    k = j % 2
                    tr.op("act", lambda e, j=j, k=k: e.activation(out=sgt[k][:], in_=pbank[2][:, :], func=AF.Sigmoid,
                                                                  bias=bmcol[:, j:j + 1]),
                          reads=[pbank_b[2], const_b], writes=[sgt_b[k]])
                    tr.op("act", lambda e, j=j, k=k: e.activation(out=sgt[2 + k][:], in_=pbank[3][:, :],
                                                                  func=AF.Sigmoid, bias=bmcol[:, 8 + j:9 + j]),
                          reads=[pbank_b[3], const_b], writes=[sgt_b[2 + k]])
                    tr.op("dve", lambda e, k=k: e.tensor_tensor(out=mt[0], in0=pbank[0][:, :], in1=sgt[k][:],
                                                                op=ALU.mult),
                          reads=[pbank_b[0], sgt_b[k]], writes=[mt_b[0]])
                    tr.op("dve", lambda e, k=k: e.tensor_tensor(out=mt[1], in0=pbank[1][:, :], in1=sgt[2 + k][:],
                                                                op=ALU.mult),
                          reads=[pbank_b[1], sgt_b[2 + k]], writes=[mt_b[1]])
                    tr.op("dve", lambda e, j=j: e.tensor_tensor(out=mgT[:, j, :], in0=mt[0], in1=mt[1],
                                                                 op=ALU.add),
                          reads=[mt_b[0], mt_b[1]], writes=[mgT_b[j]])
            dump(f"mgT{ti}", mgT[:], mgT_b, [128, 8, T], BF16)

        def outproj(ti):
            wv, wb = wload("wout")
            for s in range(NSUB):
                for n in range(2):
                    pb = gemm_bank()
                    for kc in range(8):
                        tr.op("pe", lambda e, pb=pb, kc=kc, s=s, n=n, wv=wv: e.matmul(
                            pbank[pb][:, :], lhsT=mgT[:, kc, s * 128:(s + 1) * 128], rhs=wv[:, n, kc, :],
                            start=(kc == 0), stop=(kc == 7)),
                            reads=[wb] + mgT_b, writes=[pbank_b[pb]], inc=(kc == 7))
                    tr.op("dve", lambda e, pb=pb, s=s, n=n: e.tensor_tensor(
                        out=x_sb[:, s, n * 512:(n + 1) * 512], in0=pbank[pb][:, :],
                        in1=x_sb[:, s, n * 512:(n + 1) * 512], op=ALU.add),
                        reads=[pbank_b[pb], x_b[s]], writes=[x_b[s]])
            dump(f"x1_{ti}", x_sb[:], x_b, [128, NSUB, D])
            for s in range(NSUB):
                norm_transpose(x_sb[:, s, :], x_b[s], ymT, ymT_b[s], s, ti * NSUB + s)

        ost_rr = [0]

        def ffn(ti):
            r0 = ti * T
            for a in range(6):
                wv, wb = wload(f"ffi{a}")
                js = list(range(4 * a, min(4 * a + 4, 22)))
                for idx, j in enumerate(js):
                    gb, ub = gemm_bank(), gemm_bank()
                    for (bank, seg) in ((gb, 2 * idx), (ub, 2 * idx + 1)):
                        for kc in range(8):
                            tr.op("pe", lambda e, bank=bank, seg=seg, kc=kc, wv=wv: e.matmul(
                                pbank[bank][:, :], lhsT=wv[:, seg, kc, :], rhs=ymT[:, kc, :],
                                start=(kc == 0), stop=(kc == 7)),
                                reads=[wb] + ymT_b, writes=[pbank_b[bank]], inc=(kc == 7))
                    k = j % 2
                    tr.op("act", lambda e, gb=gb, k=k: e.activation(out=sgt[k][:], in_=pbank[gb][:, :], func=AF.Silu),
                          reads=[pbank_b[gb]], writes=[sgt_b[k]])
                    tr.op("dve", lambda e, ub=ub, k=k, j=j: e.tensor_tensor(out=actT[:, j, :], in0=pbank[ub][:, :],
                                                                            in1=sgt[k][:], op=ALU.mult),
                          reads=[pbank_b[ub], sgt_b[k]], writes=[pg[j]])
            for n in range(2):
                for hlf in range(2):
                    wv, wb = wload(f"ffo{n}{hlf}")
                    for k in range(11):
                        kc = hlf * 11 + k
                        for s in range(NSUB):
                            tr.op("pe", lambda e, s=s, kc=kc, k=k, wv=wv, n=n: e.matmul(
                                pbank[s + 4 * n][:, :], lhsT=actT[:, kc, s * 128:(s + 1) * 128], rhs=wv[:, 0, k, :],
                                start=(kc == 0), stop=(kc == 21)),
                                reads=[wb, pg[kc]], writes=[pbank_b[s + 4 * n]], inc=(k == 10 and s == NSUB - 1))
                for s in range(NSUB):
                    o = ost_rr[0] % 2
                    ost_rr[0] += 1
                    tr.op("dve", lambda e, s=s, n=n, o=o: e.tensor_tensor(
                        out=ostage[o][:], in0=pbank[s + 4 * n][:, :], in1=x_sb[:, s, n * 512:(n + 1) * 512],
                        op=ALU.add),
                        reads=[pbank_b[s + 4 * n], x_b[s]], writes=[ostage_b[o]])
                    dst = out_d[r0 + s * 128:r0 + (s + 1) * 128, n * 512:(n + 1) * 512]
                    tr.dma("sp", lambda e, dst=dst, o=o: e.dma_start(out=dst, in_=ostage[o][:]), f"ost{o}",
                           reads=[ostage_b[o]], nbytes=262144)

        tr.op("pool", lambda e: e.memset(vaug[:, :, :, 256:257], 1.0), writes=vaug_b)

        tile_front(0)
        for ti in range(ntiles):
            first = (ti % 8 == 0)
            inproj_a(ti, first)
            gates(ti, first)
            inproj_b(ti)
            if stop_after == "inproj":
                continue
            for s in range(NSUB):
                mlstm_block(ti, s, first and s == 0)
                if stop_after != "mlstm":
                    attn_block(ti, s, (ti % 8) * NSUB + s)
            dump(f"ymT{ti}", ymT[:], ymT_b, [128, 8, T], BF16)
            dump(f"yaT{ti}", yaT[:], yaT_b, [128, 8, T], BF16)
            if stop_after in ("mlstm", "attn"):
                continue
            merge(ti)
            x_reload(ti)
            outproj(ti)
            if stop_after == "outproj":
                continue
            if ti + 1 < ntiles:
                tile_front(ti + 1)
            ffn(ti)
            if ti == 0:
                tr.op("pool", lambda e: e.memset(vaug[:, :, :, 256:257], 1.0), writes=vaug_b)

        tr.schedule(reorder=reorder, prio=prio)

        semnames = set(Tracker.ENG) | set(tr.dma_sems)
        sems = {n: es.enter_context(nc.semaphore("s_" + n)) for n in sorted(semnames)}
        block = es.enter_context(nc.Block())

        def replay(engname):
            def run(eng):
                for item in tr.q[engname]:
                    if item[0] == "wait":
                        eng.wait_ge(sems[item[1]], item[2])
                    else:
                        ins = None
                        for fn in item[1]:
                            ins = fn(eng)
                        ins.then_inc(sems[item[2]], item[3])
            return run

        block.tensor(replay("pe"))
        block.scalar(replay("act"))
        block.vector(replay("dve"))
        block.gpsimd(replay("pool"))
        block.sync(replay("sp"))
    stats = {e: len(tr.q[e]) for e in Tracker.ENG}
    stats['sim_end_us'] = getattr(tr, 'sim_end', 0.0) / 1e3
    stats['sbuf_left'] = sbuf_left
    return nc, dump_specs, stats


def _prep_inputs(inputs, ntiles=16, ncores=NCORES):
    f = np.float32
    x = np.ascontiguousarray(np.asarray(inputs["x"], dtype=f)).reshape(-1, D)
    cbf, cf = _host_consts()
    g1 = np.asarray(inputs["norm1_g"], f).reshape(8, 128).T
    g2 = np.asarray(inputs["norm2_g"], f).reshape(8, 128).T
    gm = np.asarray(inputs["m_norm_g"], f).reshape(8, 128).T
    convw = np.asarray(inputs["conv_w"], f).reshape(4, 16, 128).transpose(2, 1, 0).reshape(128, 64)
    convb = np.asarray(inputs["conv_b"], f).reshape(16, 128).T
    bmc = np.asarray(inputs["b_merge"], f).reshape(16, 128).T
    pcol = np.ascontiguousarray(np.concatenate([g1, g2, gm, convw, convb, bmc], axis=1))
    prow = np.ascontiguousarray(np.concatenate([np.asarray(inputs["q_norm_g"], f).reshape(-1),
                                                np.asarray(inputs["k_norm_g"], f).reshape(-1),
                                                np.asarray(inputs["sinks"], f).reshape(-1)])[None, :])
    pgate = np.ascontiguousarray(np.asarray(inputs["b_mgate"], f).reshape(2, 4).T)
    shared = {
        "w_in": np.ascontiguousarray(np.asarray(inputs["w_in"], f).reshape(D, N_IN)),
        "w_bm": np.ascontiguousarray(np.asarray(inputs["w_branch_m"], f).reshape(D, D)),
        "w_ba": np.ascontiguousarray(np.asarray(inputs["w_branch_a"], f).reshape(D, D)),
        "w_out": np.ascontiguousarray(np.asarray(inputs["w_out"], f).reshape(D, D)),
        "w_fi": np.ascontiguousarray(np.asarray(inputs["w_ffn_in"], f).reshape(D, 2 * DFF)),
        "w_fo": np.ascontiguousarray(np.asarray(inputs["w_ffn_out"], f).reshape(DFF, D)),
        "cbf": cbf, "cf": cf, "pcol": pcol, "prow": prow, "pgate": pgate,
    }
    in_maps = []
    per = TOK_CORE
    for c in range(ncores):
        m = dict(shared)
        m["x"] = x[c * per:c * per + ntiles * T]
        in_maps.append(m)
    return in_maps


_PROGRAM = None


def kernel(**inputs):
    global _PROGRAM
    if _PROGRAM is None:
        _PROGRAM = build_program(16)[0]
    in_maps = _prep_inputs(inputs)
    res = run_bass_kernel_spmd(_PROGRAM, in_maps, core_ids=list(range(NCORES)))
    out = np.concatenate([np.asarray(r["out"], dtype=np.float32) for r in res.results], axis=0)
    return out.reshape(16, SEQ, D)
```

```python
import numpy as np
import ml_dtypes
from contextlib import ExitStack
import concourse.bass as bass
import concourse.mybir as mybir
from concourse.bass_utils import run_bass_kernel_spmd

F32 = mybir.dt.float32
BF16 = mybir.dt.bfloat16
AF = mybir.ActivationFunctionType
ALU = mybir.AluOpType
AX = mybir.AxisListType

D = 1024
SEQ = 4096
NCORES = 8
TOK_CORE = 2 * SEQ
T = 512
NSUB = 4
DFF = 2816
N_IN = 7432
EPS = 1e-6
NEG = -30000.0
O_MQ, O_MK, O_MV, O_MO, O_MI, O_MF, O_AQ, O_AK, O_AV, O_GM, O_GA = (
    0, 1024, 2048, 3072, 4096, 4100, 4104, 5128, 5256, 5384, 6408)


class Buf:
    __slots__ = ("name", "w", "r", "excl")

    def __init__(self, name="", excl=False):
        self.name = name
        self.w = None
        self.r = []
        self.excl = excl


class Op:
    __slots__ = ("id", "eng", "fns", "preds", "dur", "sem", "nbytes", "tick", "fin")

    def __init__(self, id, eng, sem=None, nbytes=0):
        self.id = id
        self.eng = eng
        self.fns = []
        self.preds = set()
        self.dur = 0.0
        self.sem = sem
        self.nbytes = nbytes
        self.tick = 0
        self.fin = 0.0


def _est(eng, n):
    if eng == "pe":
        return 60.0 + 0.33 * max(n, 64)
    if eng == "act":
        return 200.0 + 0.85 * n
    if eng == "dve":
        return 180.0 + 1.05 * n
    if eng == "pool":
        return 300.0 + 3.0 * n
    return 60.0


class Tracker:
    ENG = ("pe", "act", "dve", "pool", "sp")

    def __init__(self):
        self.ops = []
        self.unit = None
        self.q = {e: [] for e in self.ENG}
        self.dma_sems = set()

    def _preds(self, reads, writes, self_id):
        p = set()
        for b in reads:
            if b.w is not None:
                p.add(b.w)
            if b.excl:
                p.update(b.r)
        for b in writes:
            if b.w is not None:
                p.add(b.w)
            p.update(b.r)
        p.discard(self_id)
        return p

    def _mark(self, oid, reads, writes):
        for b in writes:
            b.w = oid
            b.r = []
        for b in reads:
            if b.excl:
                b.w = oid
                b.r = []
            elif not b.r or b.r[-1] != oid:
                b.r.append(oid)

    class _Probe:
        def __init__(self):
            self.n = 512

        def __getattr__(self, name):
            def f(*args, **kw):
                out = kw.get("out", args[0] if args else None)
                try:
                    shp = out.shape
                    m = 1
                    for d in shp[1:]:
                        m *= int(d)
                    self.n = m
                except Exception:
                    pass
                return None
            return f

    def op(self, eng, fn, reads=(), writes=(), inc=True, n=None):
        if n is None:
            pr = Tracker._Probe()
            fn(pr)
            n = pr.n
        if eng == "pe" and self.unit is not None:
            o = self.unit
        else:
            o = Op(len(self.ops), eng)
            self.ops.append(o)
            if eng == "pe":
                self.unit = o
        o.fns.append(fn)
        o.dur += _est(eng, n)
        o.preds |= self._preds(reads, writes, o.id)
        self._mark(o.id, reads, writes)
        if eng == "pe" and inc:
            self.unit = None

    def dma(self, eng, fn, sem, reads=(), writes=(), nbytes=65536):
        assert self.unit is None
        o = Op(len(self.ops), eng, sem=sem, nbytes=nbytes)
        self.ops.append(o)
        self.dma_sems.add(sem)
        o.fns.append(fn)
        o.dur = 60.0
        o.preds |= self._preds(reads, writes, o.id)
        self._mark(o.id, reads, writes)

    def schedule(self, reorder=True, prio="order"):
        import heapq
        ops = self.ops
        n = len(ops)
        succs = [[] for _ in range(n)]
        indeg = [0] * n
        for o in ops:
            indeg[o.id] = len(o.preds)
            for p in o.preds:
                succs[p].append(o.id)
        order = {e: [] for e in self.ENG}
        if not reorder:
            for o in ops:
                order[o.eng].append(o)
        else:
            key = list(range(n))
            if prio == "cp":
                ind2 = list(indeg)
                topo = [i for i in range(n) if ind2[i] == 0]
                k = 0
                while k < len(topo):
                    for sid in succs[topo[k]]:
                        ind2[sid] -= 1
                        if ind2[sid] == 0:
                            topo.append(sid)
                    k += 1
                bl = [0.0] * n
                for i in reversed(topo):
                    m = 0.0
                    for sid in succs[i]:
                        if bl[sid] > m:
                            m = bl[sid]
                    d = ops[i].dur if ops[i].sem is None else 2000.0 + ops[i].nbytes / 300.0
                    bl[i] = m + d
                rank = sorted(range(n), key=lambda i: (-bl[i], i))
                for r, i in enumerate(rank):
                    key[i] = r
            inv = [0] * n
            for i in range(n):
                inv[key[i]] = i
            free_at = {e: 0.0 for e in self.ENG}
            pend = {e: [] for e in self.ENG}
            avail = {e: [] for e in self.ENG}
            ready_t = [0.0] * n
            for o in ops:
                if indeg[o.id] == 0:
                    heapq.heappush(pend[o.eng], (0.0, key[o.id]))
            dma_free = 0.0
            done = 0
            while done < n:
                best = None
                for e in self.ENG:
                    pe_, av = pend[e], avail[e]
                    while pe_ and pe_[0][0] <= free_at[e]:
                        heapq.heappush(av, heapq.heappop(pe_)[1])
                    if av:
                        cand = (free_at[e], av[0], e, True)
                    elif pe_:
                        cand = (pe_[0][0], pe_[0][1], e, False)
                    else:
                        continue
                    if best is None or cand[:2] < best[:2]:
                        best = cand
                start, okey, e, from_av = best
                if from_av:
                    heapq.heappop(avail[e])
                else:
                    heapq.heappop(pend[e])
                oid = inv[okey]
                o = ops[oid]
                if o.sem is not None:
                    free_at[e] = start + o.dur
                    t0 = max(start, dma_free)
                    dma_free = t0 + o.nbytes / 300.0
                    o.fin = dma_free + 2000.0
                else:
                    o.fin = start + o.dur
                    free_at[e] = o.fin
                order[e].append(o)
                done += 1
                for sid in succs[oid]:
                    so = ops[sid]
                    lat = 0.0 if so.eng == e else 150.0
                    if ready_t[sid] < o.fin + lat:
                        ready_t[sid] = o.fin + lat
                    indeg[sid] -= 1
                    if indeg[sid] == 0:
                        heapq.heappush(pend[so.eng], (ready_t[sid], key[sid]))
            self.sim_end = max(o.fin for o in ops)
        dma_cnt = {}
        for e in self.ENG:
            c = 0
            for o in order[e]:
                if o.sem is not None:
                    dma_cnt[o.sem] = dma_cnt.get(o.sem, 0) + 16
                    o.tick = dma_cnt[o.sem]
                else:
                    c += 1
                    o.tick = c
        for e in self.ENG:
            waited = {}
            q = self.q[e]
            for o in order[e]:
                need = {}
                for p in o.preds:
                    po = ops[p]
                    if po.eng == "pe" and e == "pe":
                        continue
                    k = po.sem if po.sem is not None else po.eng
                    if need.get(k, 0) < po.tick:
                        need[k] = po.tick
                for k, v in need.items():
                    if waited.get(k, 0) < v:
                        waited[k] = v
                        q.append(("wait", k, v))
                k = o.sem if o.sem is not None else o.eng
                q.append(("op", o.fns, k, 16 if o.sem is not None else 1))
            if e == "sp":
                for k, v in dma_cnt.items():
                    if waited.get(k, 0) < v:
                        waited[k] = v
                        q.append(("wait", k, v))


def _host_consts():
    bf = ml_dtypes.bfloat16
    p = np.arange(128)
    ident = np.eye(128, dtype=np.float32)
    s = p[:, None]
    t = p[None, :]
    mask16 = np.where(s <= t, 1.0 / 16.0, 0.0).astype(np.float32)
    mcur = np.where(s <= t, 0.0, NEG).astype(np.float32)
    mprev = np.where(s > t, 0.0, NEG).astype(np.float32)
    cbf = np.concatenate([ident, mask16, mprev, mprev, mcur, mcur], axis=1).astype(bf)
    half = 8
    inv_freq = (500000.0 ** (-np.arange(half, dtype=np.float32) * (2.0 / 16.0))).astype(np.float32)
    pos = (np.arange(32)[None, :] * 128 + p[:, None]).astype(np.float32)
    ang = pos[:, :, None] * inv_freq[None, None, :]
    cos = np.cos(ang).astype(np.float32).reshape(128, 256)
    sin = np.sin(ang).astype(np.float32).reshape(128, 256)
    cf = np.concatenate([ident[:, 0:4], cos, sin], axis=1).astype(np.float32)
    return cbf, cf


CB_ID, CB_M16, CB_MP, CB_MC = 0, 128, 256, 512
CF_ID, CF_COS, CF_SIN = 0, 4, 260


def _weight_items():
    items = []

    def add(name, src_segs, segw, kc0=0, nkc=8):
        items.append(dict(name=name, segs=src_segs, segw=segw, kc0=kc0, nkc=nkc))

    for a in range(2):
        add(f"ina{a}", [("w_in", a * 1024 + j * 128, "g1") for j in range(8)], 128)
    add("inb0", [("w_in", O_MV, "g1"), ("w_in", O_MV + 512, "g1")], 512)
    add("inb1", [("w_in", O_MO, "g1"), ("w_in", O_MO + 512, "g1")], 512)
    add("inb2", [("w_in", O_AQ, "g1"), ("w_in", O_AQ + 512, "g1")], 512)
    add("inb3", [("w_in", O_AK, "g1")], 256)
    for a in range(4):
        js = (2 * a, 2 * a + 1)
        segs = ([("w_bm", j * 128, "gm") for j in js] + [("w_ba", j * 128, None) for j in js] +
                [("w_in", O_GM + j * 128, "g1") for j in js] + [("w_in", O_GA + j * 128, "g1") for j in js])
        add(f"mrg{a}", segs, 128)
    add("wout", [("w_out", 0, None), ("w_out", 512, None)], 512)
    for a in range(6):
        js = list(range(4 * a, min(4 * a + 4, 22)))
        segs = [("w_fi", j * 128, "g2") for j in js] + [("w_fi", DFF + j * 128, "g2") for j in js]
        add(f"ffi{a}", segs, 128)
    for n in range(2):
        for hlf in range(2):
            add(f"ffo{n}{hlf}", [("w_fo", n * 512, None)], 512, kc0=hlf * 11, nkc=11)
    off = 0
    for it in items:
        it["size"] = len(it["segs"]) * it["nkc"] * it["segw"]
        it["off"] = off
        off += it["size"]
    return items, off


SLOT = 8192
NSLOT = 2


def build_program(ntiles=16, dumps=(), stop_after=None, reorder=True, prio="cp"):
    nc = bass.Bass("TRN2", target_bir_lowering=False)
    tr = Tracker()
    items, wtot = _weight_items()
    itmap = {it["name"]: it for it in items}
    dumps = set(dumps)
    dump_specs = {}

    ntok = ntiles * T
    x_d = nc.dram_tensor("x", [ntok, D], F32, kind="ExternalInput").ap()
    out_d = nc.dram_tensor("out", [ntok, D], F32, kind="ExternalOutput").ap()
    wsrc = {
        "w_in": nc.dram_tensor("w_in", [D, N_IN], F32, kind="ExternalInput").ap(),
        "w_bm": nc.dram_tensor("w_bm", [D, D], F32, kind="ExternalInput").ap(),
        "w_ba": nc.dram_tensor("w_ba", [D, D], F32, kind="ExternalInput").ap(),
        "w_out": nc.dram_tensor("w_out", [D, D], F32, kind="ExternalInput").ap(),
        "w_fi": nc.dram_tensor("w_fi", [D, 2 * DFF], F32, kind="ExternalInput").ap(),
        "w_fo": nc.dram_tensor("w_fo", [DFF, D], F32, kind="ExternalInput").ap(),
    }
    cbf_d = nc.dram_tensor("cbf", [128, 768], BF16, kind="ExternalInput").ap()
    cf_d = nc.dram_tensor("cf", [128, 516], F32, kind="ExternalInput").ap()
    pcol_d = nc.dram_tensor("pcol", [128, 24 + 64 + 16 + 16], F32, kind="ExternalInput").ap()
    prow_d = nc.dram_tensor("prow", [1, 144], F32, kind="ExternalInput").ap()
    pgate_d = nc.dram_tensor("pgate", [4, 2], F32, kind="ExternalInput").ap()
    wscr_d = nc.dram_tensor("wscr", [128, wtot], BF16, kind="Internal").ap()

    es = ExitStack()
    with es:
        def sb(name, shape, dt):
            return es.enter_context(nc.sbuf_tensor(name, shape, dt))

        def psum(name, shape, dt):
            return es.enter_context(nc.psum_tensor(name, shape, dt))

        x_sb = sb("x_sb", [128, NSUB, D], F32)
        x_b = [Buf(f"x{s}") for s in range(NSUB)]
        hT = sb("hT", [128, 8, T], BF16)
        hT_b = [Buf(f"hT{s}") for s in range(NSUB)]
        big = sb("big", [128, 12288], BF16)
        pg = [Buf(f"pg{i}") for i in range(24)]
        qkT = big[:, 0:8192].rearrange("p (c t) -> p c t", c=16)
        sigo = big[:, 8192:12288].rearrange("p (s f) -> p s f", s=4)
        actT = big[:, 0:11264].rearrange("p (c t) -> p c t", c=22)
        vaug = sb("vaug", [128, NSUB, 4, 257], BF16)
        vaug_b = [Buf(f"vaug{s}") for s in range(NSUB)]
        aqkv = sb("aqkv", [128, NSUB, 1280], BF16)
        aqkv_b = [Buf(f"aqkv{s}") for s in range(NSUB)]
        ymT = sb("ymT", [128, 8, T], BF16)
        ymT_b = [Buf(f"ymT{s}") for s in range(NSUB)]
        yaT = sb("yaT", [128, 8, T], BF16)
        yaT_b = [Buf(f"yaT{s}") for s in range(NSUB)]
        mgT = sb("mgT", [128, 8, T], BF16)
        mgT_b = [Buf(f"mgT{j}") for j in range(8)]
        wslot = [sb(f"wslot{i}", [128, SLOT], BF16) for i in range(NSLOT)]
        wslot_b = [Buf(f"wslot{i}") for i in range(NSLOT)]
        cbf = sb("cbf_sb", [128, 768], BF16)
        cf = sb("cf_sb", [128, 516], F32)
        const_b = Buf("const")
        pcol = sb("pcol_sb", [128, 120], F32)
        prow = sb("prow_sb", [128, 144], F32)
        pgate = sb("pgate_sb", [4, 2], F32)
        wgate = sb("wgate", [128, 8, 8], BF16)
        wgate_b = Buf("wgate")
        xn = [sb(f"xn{i}", [128, D], BF16) for i in range(2)]
        xn_b = [Buf(f"xn{i}") for i in range(2)]
        junk = sb("junk", [128, 256], BF16)
        junk_b = Buf("junk")
        stat = sb("stat", [128, 64], F32)
        cst = [sb(f"cst{i}", [128, 515], F32) for i in range(2)]
        cst_b = [Buf(f"cst{i}") for i in range(2)]
        cacc = [sb(f"cacc{i}", [128, 512], F32) for i in range(2)]
        cacc_b = [Buf(f"cacc{i}") for i in range(2)]
        carry = sb("carry", [128, 16, 3], F32)
        carry_b = [Buf(f"carry{j}") for j in range(16)]
        ostage = [sb(f"ostage{i}", [128, 512], F32) for i in range(2)]
        ostage_b = [Buf(f"ostage{i}") for i in range(2)]

        gr = sb("gr", [4, 2, T], F32)
        gr_b = [Buf("gr0"), Buf("gr1")]
        gs = sb("gs", [4, 32], F32)
        gs_b = Buf("gs")
        Rm = sb("Rm", [4, 32], F32)
        Rm_b = Buf("Rm")
        ones4 = sb("ones4", [4, 128], F32)
        wt = [sb(f"wt{i}", [128, NSUB, 8], F32) for i in range(2)]
        wt_b = [Buf(f"wt{i}") for i in range(2)]
        decb = [sb(f"decb{i}", [128, 32], F32) for i in range(2)]
        decb_b = [Buf(f"decb{i}") for i in range(2)]
        Cst = sb("Cst", [128, 4, 2, 257], F32)
        Cst_b = [Buf(f"Cst{h}") for h in range(4)]
        Csb = sb("Csb", [128, 4, 2, 257], BF16)
        Csb_b = [Buf(f"Csb{h}") for h in range(4)]
        STm = [sb(f"STm{i}", [128, 4, 128], BF16) for i in range(2)]
        STm_b = [[Buf(f"STm{i}_{h}") for h in range(4)] for i in range(2)]
        wvt = [sb(f"wvt{i}", [128, 4, 257], BF16) for i in range(2)]
        wvt_b = [[Buf(f"wvt{i}_{h}") for h in range(4)] for i in range(2)]
        ktok = [sb(f"ktok{i}", [128, D], BF16) for i in range(2)]
        ktok_b = [Buf(f"ktok{i}") for i in range(2)]
        hms = [sb(f"hm{i}", [128, D], BF16) for i in range(2)]
        hms_b = [[Buf(f"hm{i}_{h}") for h in range(4)] for i in range(2)]
        ymtok = sb("ymtok", [128, D], BF16)
        ymtok_b = Buf("ymtok")
        est = [sb(f"est{i}", [128, 24], F32) for i in range(2)]
        est_b = [Buf(f"est{i}") for i in range(2)]
        sq = sb("sq", [128, 1152], F32)
        sq_b = Buf("sq")
        qn = sb("qn", [128, 1152], F32)
        qn_b = Buf("qn")
        qb = sb("qb", [128, 18, 64], BF16)
        qb_b = Buf("qb")
        k2 = sb("k2", [128, 256], BF16)
        k2_b = Buf("k2")
        QBs = [sb(f"QB{i}", [128, 8, 256], BF16) for i in range(2)]
        QBs_b = [Buf(f"QB{i}") for i in range(2)]
        kTd = sb("kTd", [128, 2, 2, 128], BF16)
        kTd_b = [Buf("kTd0"), Buf("kTd1")]
        vat = sb("vat", [128, 2, 2, 65], BF16)
        vat_b = [Buf("vat0"), Buf("vat1")]
        pT = [sb(f"pT{i}", [128, 512], BF16) for i in range(2)]
        pT_b = [Buf(f"pT{i}") for i in range(2)]
        ya = sb("ya", [128, D], BF16)
        ya_b = Buf("ya")
        ast = sb("ast", [128, 96], F32)
        ast_b = Buf("ast")
        gqk = sb("gqk", [128, 128], F32)
        acst = sb("acst", [128, 32], F32)
        sgt = [sb(f"sgt{i}", [128, 512], BF16) for i in range(4)]
        sgt_b = [Buf(f"sgt{i}") for i in range(4)]
        mt = [sq[:, 0:512], sq[:, 512:1024]]
        mt_b = [sq_b, sq_b]

        sbuf_left = nc.sbuf_bytes_remaining
        pbank = [psum(f"pb{i}", [128, 512], F32) for i in range(8)]
        pbank_b = [Buf(f"pb{i}", excl=True) for i in range(8)]

        ident_f = cf[:, CF_ID:CF_ID + 4]
        MBprev = cbf[:, CB_MP:CB_MP + 256]
        MBcur = cbf[:, CB_MC:CB_MC + 256]
        ident_bf = cbf[:, CB_ID:CB_ID + 128]
        mask16 = cbf[:, CB_M16:CB_M16 + 128]

        g1col = pcol[:, 0:8]
        g2col = pcol[:, 8:16]
        gmcol = pcol[:, 16:24]
        convw = pcol[:, 24:88].rearrange("p (j k) -> p j k", j=16)
        convb = pcol[:, 88:104]
        bmcol = pcol[:, 104:120]
        fold = {"g1": g1col, "g2": g2col, "gm": gmcol, None: None}

        def dump(name, ap, bufs, shape, dt=F32):
            if name not in dumps:
                return
            d = nc.dram_tensor("dbg_" + name, list(shape), dt, kind="ExternalOutput").ap()
            dump_specs[name] = (list(shape), dt)
            tr.dma("sp", lambda e, d=d, ap=ap: e.dma_start(out=d, in_=ap), "dbg_" + name, reads=bufs)

        tr.dma("sp", lambda e: e.dma_start(out=cbf[:], in_=cbf_d), "cld", writes=[const_b])
        tr.dma("sp", lambda e: e.dma_start(out=cf[:], in_=cf_d), "cld", writes=[const_b])
        tr.dma("sp", lambda e: e.dma_start(out=pcol[:], in_=pcol_d), "cld", writes=[const_b])
        tr.dma("sp", lambda e: e.dma_start(out=prow[:], in_=prow_d.partition_broadcast(128)), "cld",
               writes=[const_b])
        tr.dma("sp", lambda e: e.dma_start(out=pgate[:], in_=pgate_d), "cld", writes=[const_b])

        wscr_b = {it["name"]: Buf("wscr_" + it["name"]) for it in items}
        cast_rr = [0]
        stage_rr = [0]

        def prep_cast(out_ap, in_ap, scale_ap, reads, writes):
            k = cast_rr[0] % 2
            cast_rr[0] += 1
            if k == 0:
                if scale_ap is None:
                    tr.op("act", lambda e: e.activation(out=out_ap, in_=in_ap, func=AF.Copy),
                          reads=reads, writes=writes)
                else:
                    tr.op("act", lambda e: e.activation(out=out_ap, in_=in_ap, func=AF.Copy, scale=scale_ap),
                          reads=reads + [const_b], writes=writes)
            else:
                eng = "dve" if k == 1 else "pool"
                if scale_ap is None:
                    tr.op(eng, lambda e: e.tensor_copy(out=out_ap, in_=in_ap), reads=reads, writes=writes)
                else:
                    tr.op(eng, lambda e: e.tensor_scalar(out=out_ap, in0=in_ap, scalar1=scale_ap, scalar2=None,
                                                         op0=ALU.mult),
                          reads=reads + [const_b], writes=writes)

        bigf = big.bitcast(F32)

        vaugf = vaug.reshape([128, NSUB * 4 * 257]).bitcast(F32)
        aqkvf = aqkv.reshape([128, NSUB * 1280]).bitcast(F32)
        vstage = [(vaugf[:, 0:1024], [vaug_b[0], vaug_b[1]]), (vaugf[:, 1028:2052], [vaug_b[2], vaug_b[3]]),
                  (aqkvf[:, 0:1024], [aqkv_b[0], aqkv_b[1]]), (aqkvf[:, 1280:2304], [aqkv_b[2], aqkv_b[3]])]

        def prep_item(it, slot, deep=False, vst=False):
            segs, segw, kc0, nkc = it["segs"], it["segw"], it["kc0"], it["nkc"]
            nseg = len(segs)
            view = wslot[slot][:, 0:it["size"]].rearrange("p (s k w) -> p s k w", s=nseg, k=nkc)
            groups = []
            for si, (src, c0, fd) in enumerate(segs):
                if groups and groups[-1][0] == src and groups[-1][3] == fd and \
                        groups[-1][1] + groups[-1][2] * segw == c0 and \
                        (groups[-1][2] + 1) * segw <= 1024 and groups[-1][4] + groups[-1][2] == si:
                    groups[-1][2] += 1
                else:
                    groups.append([src, c0, 1, fd, si])
            for k in range(nkc):
                kc = kc0 + k
                for (src, c0, n, fd, si0) in groups:
                    st = stage_rr[0] % (10 if deep else NSUB)
                    stage_rr[0] += 1
                    width = n * segw
                    src_ap = wsrc[src][kc * 128:(kc + 1) * 128, c0:c0 + width]
                    if vst:
                        stg, stg_bufs = vstage[st][0][:, 0:width], vstage[st][1]
                        st = 10 + st
                    elif st < NSUB:
                        stg = x_sb[:, st, 0:width]
                        stg_bufs = [x_b[st]]
                    else:
                        stg = bigf[:, (st - NSUB) * 1024:(st - NSUB) * 1024 + width]
                        stg_bufs = pg[4 * (st - NSUB):4 * (st - NSUB) + 4]
                    tr.dma("sp", lambda e, stg=stg, src_ap=src_ap: e.dma_start(out=stg, in_=src_ap),
                           f"xld{st}", writes=stg_bufs, nbytes=width * 512)
                    if n == 1:
                        out_ap = view[:, si0, k, :]
                        in_ap = stg
                    else:
                        out_ap = view[:, si0:si0 + n, k, :]
                        in_ap = stg.rearrange("p (s w) -> p s w", s=n)
                    sc = None if fd is None else fold[fd][:, kc:kc + 1]
                    prep_cast(out_ap, in_ap, sc, list(stg_bufs), [wslot_b[slot]])
            dst = wscr_d[:, it["off"]:it["off"] + it["size"]]
            tr.dma("sp", lambda e, dst=dst, slot=slot, it=it: e.dma_start(out=dst, in_=wslot[slot][:, 0:it["size"]]),
                   f"wst{slot}", reads=[wslot_b[slot]], writes=[wscr_b[it["name"]]], nbytes=it["size"] * 256)

        early_pending = {it["name"] for it in items}
        for kc in range(8):
            st = stage_rr[0] % NSUB
            stage_rr[0] += 1
            stg = x_sb[:, st, 0:8]
            src_ap = wsrc["w_in"][kc * 128:(kc + 1) * 128, O_MI:O_MI + 8]
            tr.dma("sp", lambda e, stg=stg, src_ap=src_ap: e.dma_start(out=stg, in_=src_ap), f"xld{st}",
                   writes=[x_b[st]])
            tr.op("dve", lambda e, kc=kc, stg=stg: e.tensor_scalar(out=wgate[:, kc, :], in0=stg,
                                                                  scalar1=g1col[:, kc:kc + 1], scalar2=None,
                                                                  op0=ALU.mult),
                  reads=[x_b[st], const_b], writes=[wgate_b])

        wcur = {"slot": 0}

        def wload(name):
            it = itmap[name]
            slot = wcur["slot"]
            wcur["slot"] = (slot + 1) % NSLOT
            if name in early_pending:
                early_pending.discard(name)
                prep_item(it, slot, vst=name.startswith(("wout", "ffi", "ffo")))
                view = wslot[slot][:, 0:it["size"]].rearrange("p (s k w) -> p s k w", s=len(it["segs"]),
                                                              k=it["nkc"])
                return view, wslot_b[slot]
            src = wscr_d[:, it["off"]:it["off"] + it["size"]]
            tr.dma("sp", lambda e, src=src, slot=slot, it=it: e.dma_start(out=wslot[slot][:, 0:it["size"]], in_=src),
                   f"wld{slot}", reads=[wscr_b[name]], writes=[wslot_b[slot]], nbytes=it["size"] * 256)
            view = wslot[slot][:, 0:it["size"]].rearrange("p (s k w) -> p s k w", s=len(it["segs"]), k=it["nkc"])
            return view, wslot_b[slot]

        gemm_rr = [0]

        def gemm_bank():
            b = gemm_rr[0] % 4
            gemm_rr[0] += 1
            return b

        xstg = [sq[:, 0:D], qn[:, 0:D]]
        xstg_b = [sq_b, qn_b]

        def tile_front(ti):
            r0 = ti * T
            for s in range(NSUB):
                k = s % 2
                src = x_d[r0 + s * 128:r0 + (s + 1) * 128, :]
                tr.dma("sp", lambda e, k=k, src=src: e.dma_start(out=xstg[k], in_=src), f"xsg{k}",
                       writes=[xstg_b[k]], nbytes=524288)
                norm_transpose(xstg[k], xstg_b[k], hT, hT_b[s], s, ti * NSUB + s)
            dump(f"hT{ti}", hT[:], hT_b, [128, 8, T], BF16)

        def x_reload(ti):
            r0 = ti * T
            for s in range(NSUB):
                src = x_d[r0 + s * 128:r0 + (s + 1) * 128, :]
                tr.dma("sp", lambda e, s=s, src=src: e.dma_start(out=x_sb[:, s, :], in_=src), f"xld{s}",
                       writes=[x_b[s]], nbytes=524288)

        def norm_transpose(src, src_b, dstT, dst_b, s, uid):
            c = (uid % 8) * 4
            ss, lnv, rstd = stat[:, c:c + 1], stat[:, c + 1:c + 2], stat[:, c + 2:c + 3]
            sbuf_ = Buf()
            k = uid % 2
            tr.op("act", lambda e: e.activation(out=xn[k][:], in_=src, func=AF.Square, accum_out=ss),
                  reads=[src_b], writes=[xn_b[k], sbuf_])
            tr.op("act", lambda e: e.activation(out=lnv, in_=ss, func=AF.Ln, scale=1.0 / D, bias=EPS),
                  reads=[sbuf_], writes=[sbuf_])
            tr.op("act", lambda e: e.activation(out=rstd, in_=lnv, func=AF.Exp, scale=-0.5),
                  reads=[sbuf_], writes=[sbuf_])
            tr.op("act", lambda e: e.activation(out=xn[k][:], in_=src, func=AF.Copy, scale=rstd),
                  reads=[src_b, sbuf_], writes=[xn_b[k]])
            pb = 4 + (uid % 2)
            pst = pbank[pb].bitcast(BF16)
            for kc in range(8):
                tr.op("pe", lambda e, kc=kc: e.transpose(out=pst[:, kc * 128:(kc + 1) * 128],
                                                         in_=xn[k][:, kc * 128:(kc + 1) * 128], identity=ident_bf),
                      reads=[xn_b[k], const_b], writes=[pbank_b[pb]], inc=(kc == 7))
            tr.op("act", lambda e: e.activation(out=dstT[:, :, s * 128:(s + 1) * 128],
                                                in_=pst[:, :].rearrange("p (c t) -> p c t", c=8), func=AF.Copy),
                  reads=[pbank_b[pb]], writes=[dst_b])

        def inproj_a(ti, first):
            for a in range(2):
                wv, wb = wload(f"ina{a}")
                for jj in range(8):
                    j = a * 8 + jj
                    pb = gemm_bank()
                    for kc in range(8):
                        tr.op("pe", lambda e, jj=jj, kc=kc, pb=pb, wv=wv: e.matmul(
                            pbank[pb][:, :], lhsT=wv[:, jj, kc, :], rhs=hT[:, kc, :], start=(kc == 0), stop=(kc == 7)),
                            reads=[wb] + hT_b, writes=[pbank_b[pb]], inc=(kc == 7))
                    k = j % 2
                    if first:
                        tr.op("pool", lambda e, k=k: e.memset(cst[k][:, 0:3], 0.0), writes=[cst_b[k]])
                    else:
                        tr.op("pool", lambda e, k=k, j=j: e.tensor_copy(out=cst[k][:, 0:3], in_=carry[:, j, :]),
                              reads=[carry_b[j]], writes=[cst_b[k]])
                    tr.op("act", lambda e, k=k, pb=pb: e.activation(out=cst[k][:, 3:515], in_=pbank[pb][:, :],
                                                                    func=AF.Copy),
                          reads=[pbank_b[pb]], writes=[cst_b[k]])
                    tr.op("pool", lambda e, k=k, j=j: e.tensor_copy(out=carry[:, j, :], in_=cst[k][:, 512:515]),
                          reads=[cst_b[k]], writes=[carry_b[j]])
                    tr.op("act", lambda e, k=k, j=j, pb=pb: e.activation(out=cacc[k][:, 3:512],
                                                                         in_=pbank[pb][:, 0:509], func=AF.Copy,
                                                                         scale=convw[:, j, 0:1]),
                          reads=[pbank_b[pb], const_b], writes=[cacc_b[k]])
                    tr.op("pool", lambda e, k=k, j=j: e.tensor_scalar(out=cacc[k][:, 0:3], in0=cst[k][:, 0:3],
                                                                       scalar1=convw[:, j, 0:1], scalar2=None,
                                                                       op0=ALU.mult),
                          reads=[cst_b[k], const_b, cacc_b[k]], writes=[cacc_b[k]])
                    for tap in range(1, 4):
                        tr.op("dve", lambda e, k=k, j=j, tap=tap: e.scalar_tensor_tensor(
                            out=cacc[k][:], in0=cst[k][:, tap:tap + 512], scalar=convw[:, j, tap:tap + 1],
                            in1=cacc[k][:], op0=ALU.mult, op1=ALU.add),
                            reads=[cst_b[k], cacc_b[k], const_b], writes=[cacc_b[k]])
                    tr.op("act", lambda e, k=k, j=j: e.activation(out=qkT[:, j, :], in_=cacc[k][:], func=AF.Silu,
                                                                   bias=convb[:, j:j + 1]),
                          reads=[cacc_b[k], const_b], writes=[pg[j]])
            dump(f"qkT{ti}", qkT, pg[0:16], [128, 16, T], BF16)

        def inproj_b(ti):
            plan = [("inb0", [("v", 0), ("v", 2)]), ("inb1", [("o", 0), ("o", 512)]),
                    ("inb2", [("q", 0), ("q", 512)]), ("inb3", [("kv", 0)])]
            for name, segl in plan:
                wv, wb = wload(name)
                segw = itmap[name]["segw"]
                for si, (kind, arg) in enumerate(segl):
                    for s in range(NSUB):
                        pb = gemm_bank()
                        for kc in range(8):
                            tr.op("pe", lambda e, si=si, kc=kc, pb=pb, wv=wv, s=s, segw=segw: e.matmul(
                                pbank[pb][:, 0:segw], lhsT=hT[:, kc, s * 128:(s + 1) * 128], rhs=wv[:, si, kc, :],
                                start=(kc == 0), stop=(kc == 7)),
                                reads=[wb, hT_b[s]], writes=[pbank_b[pb]], inc=(kc == 7))
                        if kind == "v":
                            tr.op("act", lambda e, pb=pb, s=s, arg=arg: e.activation(
                                out=vaug[:, s, arg:arg + 2, 0:256],
                                in_=pbank[pb][:, :].rearrange("p (h e) -> p h e", h=2), func=AF.Copy),
                                reads=[pbank_b[pb]], writes=[vaug_b[s]])
                        elif kind == "o":
                            tr.op("act", lambda e, pb=pb, s=s, arg=arg: e.activation(
                                out=sigo[:, s, arg:arg + 512], in_=pbank[pb][:, :], func=AF.Sigmoid),
                                reads=[pbank_b[pb]], writes=[pg[16 + 2 * s], pg[17 + 2 * s]])
                        elif kind == "q":
                            tr.op("dve", lambda e, pb=pb, s=s, arg=arg: e.tensor_copy(
                                out=aqkv[:, s, arg:arg + 512], in_=pbank[pb][:, :]),
                                reads=[pbank_b[pb]], writes=[aqkv_b[s]])
                        else:
                            tr.op("dve", lambda e, pb=pb, s=s: e.tensor_copy(
                                out=aqkv[:, s, 1024:1280], in_=pbank[pb][:, 0:256]),
                                reads=[pbank_b[pb]], writes=[aqkv_b[s]])
            dump(f"vaug{ti}", vaug[:], vaug_b, [128, NSUB, 4, 257], BF16)
            dump(f"sigo{ti}", sigo, pg[16:24], [128, NSUB, 1024], BF16)
            dump(f"aqkv{ti}", aqkv[:], aqkv_b, [128, NSUB, 1280], BF16)

        setup_b = Buf("setup")
        for i in range(2):
            tr.op("pool", lambda e, i=i: e.memset(QBs[i][:], 0.0), writes=[QBs_b[i]])
        tr.op("pool", lambda e: e.memset(vat[:, :, :, 64:65], 1.0), writes=vat_b)
        tr.op("pool", lambda e: e.memset(ones4[:], 1.0), writes=[setup_b])
        tr.op("dve", lambda e: e.tensor_scalar(out=gqk[:, 0:64], in0=prow[:, 0:64],
                                               scalar1=0.125, scalar2=None, op0=ALU.mult),
              reads=[const_b], writes=[setup_b])
        tr.op("dve", lambda e: e.tensor_copy(out=gqk[:, 64:128], in_=prow[:, 64:128]),
              reads=[const_b, setup_b], writes=[setup_b])
        tr.op("dve", lambda e: e.tensor_reduce(out=acst[:, 1:2], in_=prow[:, 0:64], axis=AX.X, op=ALU.max,
                                               apply_absolute_value=True),
              reads=[const_b, setup_b], writes=[setup_b])
        tr.op("dve", lambda e: e.tensor_reduce(out=acst[:, 2:3], in_=prow[:, 64:128], axis=AX.X, op=ALU.max,
                                               apply_absolute_value=True),
              reads=[const_b, setup_b], writes=[setup_b])
        tr.op("dve", lambda e: e.scalar_tensor_tensor(out=acst[:, 0:1], in0=acst[:, 1:2], scalar=-8.0,
                                                      in1=acst[:, 2:3], op0=ALU.mult, op1=ALU.mult),
              reads=[setup_b], writes=[setup_b])
        nlmax = acst[:, 0:1]
        tr.op("act", lambda e: e.activation(out=acst[:, 16:32], in_=prow[:, 128:144], func=AF.Exp, bias=nlmax),
              reads=[const_b, setup_b], writes=[setup_b])
        sinkexp = acst[:, 16:32]
        tr.op("dve", lambda e: e.tensor_scalar(out=gs[:, 28:29], in0=pgate[:, 1:2], scalar1=-1.0, scalar2=None,
                                               op0=ALU.mult),
              reads=[const_b], writes=[setup_b])
        nbf = gs[:, 28:29]
        bi = pgate[:, 0:1]

        GS_MBLK, GS_MC, GS_NMC, GS_MPREV, GS_DIF, GS_DEC = 0, 4, 8, 12, 17, 21

        def gates(ti, first):
            tp = ti % 2
            bi_, bf_, bt_, bd_ = gemm_bank(), gemm_bank(), gemm_bank(), gemm_bank()
            for (bank, c0) in ((bi_, 0), (bf_, 4)):
                for kc in range(8):
                    tr.op("pe", lambda e, bank=bank, c0=c0, kc=kc: e.matmul(
                        pbank[bank][0:4, :], lhsT=wgate[:, kc, c0:c0 + 4], rhs=hT[:, kc, :],
                        start=(kc == 0), stop=(kc == 7)),
                        reads=[wgate_b] + hT_b, writes=[pbank_b[bank]], inc=(kc == 7))
            ipre, fpre = pbank[bi_][0:4, :], pbank[bf_][0:4, :]
            g0, g1 = gr[:, 0, :], gr[:, 1, :]
            tr.op("act", lambda e: e.activation(out=g0, in_=fpre, func=AF.Exp, scale=-1.0, bias=nbf),
                  reads=[pbank_b[bf_], setup_b], writes=[gr_b[0]])
            tr.op("act", lambda e: e.activation(out=g0, in_=g0, func=AF.Ln, bias=1.0),
                  reads=[gr_b[0]], writes=[gr_b[0]])
            for j in range(NSUB):
                tr.op("dve", lambda e, j=j: e.tensor_tensor_scan(
                    out=g1[:, j * 128:(j + 1) * 128], data0=ones4[:, 0:128], data1=g0[:, j * 128:(j + 1) * 128],
                    initial=0.0, op0=ALU.mult, op1=ALU.add),
                    reads=[gr_b[0], setup_b], writes=[gr_b[1]])
            tr.op("dve", lambda e: e.scalar_tensor_tensor(out=g0, in0=ipre, scalar=bi, in1=g1, op0=ALU.add,
                                                          op1=ALU.add),
                  reads=[pbank_b[bi_], gr_b[1], const_b, gr_b[0]], writes=[gr_b[0]])
            tr.op("dve", lambda e: e.tensor_reduce(out=gs[:, GS_MBLK:GS_MBLK + 4],
                                                   in_=g0.rearrange("p (j t) -> p j t", j=4), axis=AX.X, op=ALU.max),
                  reads=[gr_b[0]], writes=[gs_b])
            if first:
                tr.op("dve", lambda e: e.memset(gs[:, GS_MPREV:GS_MPREV + 1], 0.0), reads=[gs_b], writes=[gs_b])
            else:
                tr.op("dve", lambda e: e.tensor_copy(out=gs[:, GS_MPREV:GS_MPREV + 1],
                                                     in_=gs[:, GS_MPREV + 4:GS_MPREV + 5]),
                      reads=[gs_b], writes=[gs_b])
            for j in range(NSUB):
                tr.op("dve", lambda e, j=j: e.tensor_tensor(out=gs[:, GS_MC + j:GS_MC + j + 1],
                                                            in0=gs[:, GS_MPREV + j:GS_MPREV + j + 1],
                                                            in1=gs[:, GS_MBLK + j:GS_MBLK + j + 1], op=ALU.max),
                      reads=[gs_b], writes=[gs_b])
                tr.op("dve", lambda e, j=j: e.tensor_tensor(out=gs[:, GS_MPREV + j + 1:GS_MPREV + j + 2],
                                                            in0=gs[:, GS_MC + j:GS_MC + j + 1],
                                                            in1=g1[:, j * 128 + 127:j * 128 + 128], op=ALU.subtract),
                      reads=[gs_b, gr_b[1]], writes=[gs_b])
            tr.op("dve", lambda e: e.tensor_tensor(out=gs[:, GS_DIF:GS_DIF + 4], in0=gs[:, GS_MPREV:GS_MPREV + 4],
                                                   in1=gs[:, GS_MC:GS_MC + 4], op=ALU.subtract),
                  reads=[gs_b], writes=[gs_b])
            tr.op("dve", lambda e: e.tensor_scalar(out=gs[:, GS_NMC:GS_NMC + 4], in0=gs[:, GS_MC:GS_MC + 4],
                                                   scalar1=-1.0, scalar2=None, op0=ALU.mult),
                  reads=[gs_b], writes=[gs_b])
            tr.op("act", lambda e: e.activation(out=gs[:, GS_DEC:GS_DEC + 4], in_=gs[:, GS_DIF:GS_DIF + 4],
                                                func=AF.Exp),
                  reads=[gs_b], writes=[gs_b])
            for j in range(NSUB):
                sl = slice(j * 128, (j + 1) * 128)
                tr.op("act", lambda e, j=j, sl=sl: e.activation(out=g1[:, sl], in_=g1[:, sl], func=AF.Exp,
                                                                bias=gs[:, GS_NMC + j:GS_NMC + j + 1]),
                      reads=[gs_b, gr_b[1]], writes=[gr_b[1]])
                tr.op("act", lambda e, j=j, sl=sl: e.activation(out=g0[:, sl], in_=g0[:, sl], func=AF.Exp,
                                                                bias=gs[:, GS_NMC + j:GS_NMC + j + 1]),
                      reads=[gs_b, gr_b[0]], writes=[gr_b[0]])
            for j in range(NSUB):
                sl = slice(j * 128, (j + 1) * 128)
                tr.op("pe", lambda e, j=j, sl=sl: e.matmul(pbank[bt_][:, j * 8:j * 8 + 4], lhsT=g0[:, sl],
                                                           rhs=ident_f[0:4, 0:4], start=True, stop=True),
                      reads=[gr_b[0], const_b], writes=[pbank_b[bt_]], inc=False)
                tr.op("pe", lambda e, j=j, sl=sl: e.matmul(pbank[bt_][:, j * 8 + 4:j * 8 + 8], lhsT=g1[:, sl],
                                                           rhs=ident_f[0:4, 0:4], start=True, stop=True),
                      reads=[gr_b[1], const_b], writes=[pbank_b[bt_]], inc=(j == NSUB - 1))
            tr.op("act", lambda e: e.activation(out=wt[tp][:].rearrange("p j c -> p (j c)"), in_=pbank[bt_][:, 0:32],
                                                func=AF.Copy),
                  reads=[pbank_b[bt_]], writes=[wt_b[tp]])
            tr.op("dve", lambda e: e.tensor_tensor(
                out=Rm[:, 0:16].rearrange("p (j h) -> p j h", j=4),
                in0=ident_f[0:4, 0:4].unsqueeze(1).broadcast_to([4, 4, 4]),
                in1=gs[:, GS_DEC:GS_DEC + 4].unsqueeze(2).broadcast_to([4, 4, 4]), op=ALU.mult),
                reads=[gs_b, const_b, Rm_b], writes=[Rm_b])
            tr.op("dve", lambda e: e.tensor_scalar(out=Rm[:, 16:32], in0=Rm[:, 0:16], scalar1=1.0 / 16.0,
                                                   scalar2=None, op0=ALU.mult),
                  reads=[Rm_b], writes=[Rm_b])
            tr.op("pe", lambda e: e.matmul(pbank[bd_][:, 0:32], lhsT=ones4[:, 0:128], rhs=Rm[:, 0:32],
                                           start=True, stop=True),
                  reads=[Rm_b, setup_b], writes=[pbank_b[bd_]])
            tr.op("act", lambda e: e.activation(out=decb[tp][:], in_=pbank[bd_][:, 0:32], func=AF.Copy),
                  reads=[pbank_b[bd_]], writes=[decb_b[tp]])
            dump(f"wt{ti}", wt[tp][:], [wt_b[tp]], [128, NSUB, 8])
            dump(f"decb{ti}", decb[tp][:], [decb_b[tp]], [128, 32])

        def mlstm_block(ti, s, first_block):
            tp = ti % 2
            par = s % 2
            sl = slice(s * 128, (s + 1) * 128)
            if first_block:
                tr.op("pool", lambda e: e.memset(Cst[:], 0.0), writes=Cst_b)
            pst = pbank[7].bitcast(BF16)
            for c in range(8):
                tr.op("pe", lambda e, c=c: e.transpose(out=pst[:, c * 128:(c + 1) * 128], in_=qkT[:, 8 + c, sl],
                                                       identity=ident_bf),
                      reads=[pg[8 + c], const_b], writes=[pbank_b[7]], inc=(c == 7))
            tr.op("act", lambda e: e.activation(out=ktok[par][:], in_=pst[:, :], func=AF.Copy),
                  reads=[pbank_b[7]], writes=[ktok_b[par]])
            e_ = est[par]
            hm, hm_b = hms[par], hms_b[par]
            for h in range(4):
                for dc in range(2):
                    tr.op("pe", lambda e, h=h, dc=dc: e.matmul(pbank[5][:, 0:128], lhsT=qkT[:, 8 + 2 * h + dc, sl],
                                                               rhs=qkT[:, 2 * h + dc, sl], start=(dc == 0),
                                                               stop=(dc == 1)),
                          reads=[pg[8 + 2 * h + dc], pg[2 * h + dc]], writes=[pbank_b[5]], inc=(dc == 1))
                tr.op("dve", lambda e, h=h: e.tensor_tensor(out=STm[par][:, h, :], in0=pbank[5][:, 0:128],
                                                            in1=mask16, op=ALU.mult),
                      reads=[pbank_b[5], const_b], writes=[STm_b[par][h]])
                tr.op("act", lambda e, h=h: e.activation(out=wvt[par][:, h, :], in_=vaug[:, s, h, :], func=AF.Copy,
                                                         scale=wt[tp][:, s, h:h + 1]),
                      reads=[vaug_b[s], wt_b[tp]], writes=[wvt_b[par][h]])
                tr.op("act", lambda e, h=h: e.activation(
                    out=Csb[:, h, :, :], in_=Cst[:, h, :, :], func=AF.Copy,
                    scale=decb[tp][:, 16 + s * 4 + h:16 + s * 4 + h + 1]),
                    reads=[Cst_b[h], decb_b[tp]], writes=[Csb_b[h]])
                num = pbank[6][:, 0:257]
                tr.op("pe", lambda e, h=h: e.matmul(num, lhsT=STm[par][:, h, :], rhs=wvt[par][:, h, :],
                                                    start=True, stop=False),
                      reads=[STm_b[par][h], wvt_b[par][h]], writes=[pbank_b[6]], inc=False)
                for dc in range(2):
                    tr.op("pe", lambda e, h=h, dc=dc: e.matmul(num, lhsT=qkT[:, 2 * h + dc, sl],
                                                               rhs=Csb[:, h, dc, :], start=False, stop=(dc == 1)),
                          reads=[pg[2 * h + dc], Csb_b[h]], writes=[pbank_b[6]], inc=(dc == 1))
                for dc, bank, c0 in ((0, 7, 0), (1, 5, 128)):
                    tr.op("pe", lambda e, h=h, dc=dc, bank=bank, c0=c0: e.matmul(
                        pbank[bank][:, c0:c0 + 257], lhsT=ktok[par][:, h * 256 + dc * 128:h * 256 + (dc + 1) * 128],
                        rhs=wvt[par][:, h, :], start=True, stop=True),
                        reads=[ktok_b[par], wvt_b[par][h]], writes=[pbank_b[bank]])
                    tr.op("dve", lambda e, h=h, dc=dc, bank=bank, c0=c0: e.scalar_tensor_tensor(
                        out=Cst[:, h, dc, :], in0=Cst[:, h, dc, :],
                        scalar=decb[tp][:, s * 4 + h:s * 4 + h + 1], in1=pbank[bank][:, c0:c0 + 257],
                        op0=ALU.mult, op1=ALU.add),
                        reads=[Cst_b[h], decb_b[tp], pbank_b[bank]], writes=[Cst_b[h]])
                tr.op("act", lambda e, h=h: e.activation(out=e_[:, 20 + h:21 + h], in_=pbank[6][:, 256:257],
                                                         func=AF.Abs),
                      reads=[pbank_b[6], est_b[par]], writes=[est_b[par]])
                tr.op("dve", lambda e, h=h: e.tensor_tensor(out=e_[:, h:h + 1], in0=e_[:, 20 + h:21 + h],
                                                            in1=wt[tp][:, s, 4 + h:5 + h], op=ALU.max),
                      reads=[wt_b[tp], est_b[par]], writes=[est_b[par]])
                tr.op("dve", lambda e, h=h: e.reciprocal(out=e_[:, 4 + h:5 + h], in_=e_[:, h:h + 1]),
                      reads=[est_b[par]], writes=[est_b[par]])
                tr.op("dve", lambda e, h=h: e.scalar_tensor_tensor(
                    out=hm[:, h * 256:(h + 1) * 256], in0=pbank[6][:, 0:256], scalar=e_[:, 4 + h:5 + h],
                    in1=sigo[:, s, h * 256:(h + 1) * 256], op0=ALU.mult, op1=ALU.mult),
                    reads=[pbank_b[6], est_b[par], pg[16 + 2 * s], pg[17 + 2 * s]], writes=[hm_b[h]])
                tr.op("act", lambda e, h=h: e.activation(out=junk[:, 0:256], in_=hm[:, h * 256:(h + 1) * 256],
                                                         func=AF.Square, accum_out=e_[:, 8 + h:9 + h]),
                      reads=[hm_b[h], est_b[par]], writes=[junk_b, est_b[par]])
            tr.op("act", lambda e: e.activation(out=e_[:, 12:16], in_=e_[:, 8:12], func=AF.Ln, scale=1.0 / 256.0,
                                                bias=EPS),
                  reads=[est_b[par]], writes=[est_b[par]])
            tr.op("act", lambda e: e.activation(out=e_[:, 16:20], in_=e_[:, 12:16], func=AF.Exp, scale=-0.5),
                  reads=[est_b[par]], writes=[est_b[par]])
            tr.op("dve", lambda e: e.tensor_tensor(
                out=ymtok[:].rearrange("p (h e) -> p h e", h=4), in0=hm[:].rearrange("p (h e) -> p h e", h=4),
                in1=e_[:, 16:20].unsqueeze(2).broadcast_to([128, 4, 256]), op=ALU.mult),
                reads=hm_b + [est_b[par]], writes=[ymtok_b])
            for c in range(8):
                tr.op("pe", lambda e, c=c: e.transpose(out=pst[:, c * 128:(c + 1) * 128],
                                                       in_=ymtok[:, c * 128:(c + 1) * 128], identity=ident_bf),
                      reads=[ymtok_b, const_b], writes=[pbank_b[7]], inc=(c == 7))
            tr.op("act", lambda e: e.activation(out=ymT[:, :, sl], in_=pst[:, :].rearrange("p (c t) -> p c t", c=8),
                                                func=AF.Copy),
                  reads=[pbank_b[7]], writes=[ymT_b[s]])

        def attn_block(ti, s, jb):
            par = jb % 2
            first = (jb == 0)
            QB, QB_b = QBs[par], QBs_b[par]
            src = aqkv[:, s, 0:1152]
            tr.op("act", lambda e: e.activation(out=sq[:], in_=src, func=AF.Square),
                  reads=[aqkv_b[s]], writes=[sq_b])
            tr.op("dve", lambda e: e.tensor_reduce(out=ast[:, 0:18], in_=sq[:].rearrange("p (h d) -> p h d", h=18),
                                                   axis=AX.X, op=ALU.add),
                  reads=[sq_b, ast_b], writes=[ast_b])
            tr.op("act", lambda e: e.activation(out=ast[:, 18:36], in_=ast[:, 0:18], func=AF.Ln, scale=1.0 / 64.0,
                                                bias=EPS),
                  reads=[ast_b], writes=[ast_b])
            tr.op("act", lambda e: e.activation(out=ast[:, 36:54], in_=ast[:, 18:36], func=AF.Exp, scale=-0.5),
                  reads=[ast_b], writes=[ast_b])
            qn3 = qn[:].rearrange("p (h d) -> p h d", h=18)
            rt = sq[:, 0:576].rearrange("p (a h d) -> p a h d", a=4, h=18)
            rt_b = sq_b
            tr.op("dve", lambda e: e.tensor_tensor(out=qn3, in0=src.rearrange("p (h d) -> p h d", h=18),
                                                   in1=ast[:, 36:54].unsqueeze(2).broadcast_to([128, 18, 64]),
                                                   op=ALU.mult),
                  reads=[aqkv_b[s], ast_b], writes=[qn_b])
            tr.op("dve", lambda e: e.tensor_tensor(out=qn3[:, 0:16, :], in0=qn3[:, 0:16, :],
                                                   in1=gqk[:, 0:64].unsqueeze(1).broadcast_to([128, 16, 64]),
                                                   op=ALU.mult),
                  reads=[qn_b, setup_b], writes=[qn_b])
            tr.op("dve", lambda e: e.tensor_tensor(out=qn3[:, 16:18, :], in0=qn3[:, 16:18, :],
                                                   in1=gqk[:, 64:128].unsqueeze(1).broadcast_to([128, 2, 64]),
                                                   op=ALU.mult),
                  reads=[qn_b, setup_b], writes=[qn_b])
            cosb = cf[:, CF_COS + jb * 8:CF_COS + jb * 8 + 8].unsqueeze(1).broadcast_to([128, 18, 8])
            sinb = cf[:, CF_SIN + jb * 8:CF_SIN + jb * 8 + 8].unsqueeze(1).broadcast_to([128, 18, 8])
            x1, x2 = qn3[:, :, 0:8], qn3[:, :, 8:16]
            tr.op("dve", lambda e: e.tensor_tensor(out=rt[:, 0, :, :], in0=x1, in1=cosb, op=ALU.mult),
                  reads=[qn_b, const_b], writes=[rt_b])
            tr.op("dve", lambda e: e.tensor_tensor(out=rt[:, 1, :, :], in0=x2, in1=sinb, op=ALU.mult),
                  reads=[qn_b, const_b, rt_b], writes=[rt_b])
            tr.op("dve", lambda e: e.tensor_tensor(out=rt[:, 2, :, :], in0=x2, in1=cosb, op=ALU.mult),
                  reads=[qn_b, const_b, rt_b], writes=[rt_b])
            tr.op("dve", lambda e: e.tensor_tensor(out=rt[:, 3, :, :], in0=x1, in1=sinb, op=ALU.mult),
                  reads=[qn_b, const_b, rt_b], writes=[rt_b])
            tr.op("dve", lambda e: e.tensor_tensor(out=qb[:, :, 0:8], in0=rt[:, 0, :, :], in1=rt[:, 1, :, :],
                                                   op=ALU.subtract),
                  reads=[rt_b], writes=[qb_b])
            tr.op("dve", lambda e: e.tensor_tensor(out=qb[:, :, 8:16], in0=rt[:, 2, :, :], in1=rt[:, 3, :, :],
                                                   op=ALU.add),
                  reads=[rt_b, qb_b], writes=[qb_b])
            tr.op("act", lambda e: e.activation(out=qb[:, :, 16:64], in_=qn3[:, :, 16:64], func=AF.Copy),
                  reads=[qn_b, qb_b], writes=[qb_b])
            tr.op("pool", lambda e: e.tensor_copy(
                out=k2[:].rearrange("p (g r d) -> p g r d", g=2, r=2),
                in_=qb[:, 16:18, :].unsqueeze(2).broadcast_to([128, 2, 2, 64])),
                reads=[qb_b], writes=[k2_b])
            tr.op("pool", lambda e: e.tensor_copy(
                out=vat[:, par, :, 0:64], in_=aqkv[:, s, 1152:1280].rearrange("p (g d) -> p g d", g=2)),
                reads=[aqkv_b[s]], writes=[vat_b[par]])
            pq = pbank[4].bitcast(BF16)
            for g in range(2):
                tr.op("pe", lambda e, g=g: e.transpose(out=pq[:, g * 128:(g + 1) * 128],
                                                       in_=k2[:, g * 128:(g + 1) * 128], identity=ident_bf),
                      reads=[k2_b, const_b], writes=[pbank_b[4]], inc=(g == 1))
            tr.op("act", lambda e: e.activation(out=kTd[:, par, :, :],
                                                in_=pq[:, 0:256].rearrange("p (g t) -> p g t", g=2), func=AF.Copy),
                  reads=[pbank_b[4]], writes=[kTd_b[par]])
            for c in range(8):
                tr.op("pe", lambda e, c=c: e.transpose(out=pq[:, c * 128:(c + 1) * 128],
                                                       in_=qb[:].rearrange("p h d -> p (h d)")[:, c * 128:(c + 1) * 128],
                                                       identity=ident_bf),
                      reads=[qb_b, const_b], writes=[pbank_b[4]], inc=(c == 7))
            pq3 = pq[:, :].rearrange("p (c t) -> p c t", c=8)
            tr.op("act", lambda e: e.activation(out=QB[0:64, :, 0:128], in_=pq3[0:64, :, :], func=AF.Copy),
                  reads=[pbank_b[4]], writes=[QB_b])
            tr.op("dve", lambda e: e.tensor_copy(out=QB[64:128, :, 128:256], in_=pq3[64:128, :, :]),
                  reads=[pbank_b[4], QB_b], writes=[QB_b])
            def po_ap(head):
                if head < 7:
                    return 2, pbank[2][:, head * 65:(head + 1) * 65]
                if head < 14:
                    return 3, pbank[3][:, (head - 7) * 65:(head - 6) * 65]
                return 4, pbank[4][:, (head - 14) * 65:(head - 13) * 65]
            for c in range(8):
                g = c // 4
                lb = c % 2
                lg = pbank[lb]
                if not first:
                    tr.op("pe", lambda e, c=c, g=g, lg=lg: e.matmul(lg[:, 0:256], lhsT=kTd[:, 1 - par, g, :],
                                                                    rhs=QB[:, c, :], start=True, stop=False),
                          reads=[kTd_b[1 - par], QB_b], writes=[pbank_b[lb]], inc=False)
                    tr.op("pe", lambda e, lg=lg: e.matmul(lg[:, 0:256], lhsT=ident_bf, rhs=MBprev,
                                                          start=False, stop=True),
                          reads=[const_b], writes=[pbank_b[lb]], inc=False)
                tr.op("pe", lambda e, c=c, g=g, lg=lg: e.matmul(lg[:, 256:512], lhsT=kTd[:, par, g, :],
                                                                rhs=QB[:, c, :], start=True, stop=False),
                      reads=[kTd_b[par], QB_b], writes=[pbank_b[lb]], inc=False)
                tr.op("pe", lambda e, lg=lg: e.matmul(lg[:, 256:512], lhsT=ident_bf, rhs=MBcur,
                                                      start=False, stop=True),
                      reads=[const_b], writes=[pbank_b[lb]])
                lo = 256 if first else 0
                pk_ = c % 2
                tr.op("act", lambda e, lg=lg, lo=lo, pk_=pk_: e.activation(out=pT[pk_][:, lo:512], in_=lg[:, lo:512],
                                                                           func=AF.Exp, bias=nlmax),
                      reads=[pbank_b[lb], setup_b], writes=[pT_b[pk_]])
                for hh in range(2):
                    head = 2 * c + hh
                    bank, po = po_ap(head)
                    srcs = [(256 + hh * 128, par)]
                    if not first:
                        srcs.append((hh * 128, 1 - par))
                    for i, (col, slot) in enumerate(srcs):
                        tr.op("pe", lambda e, pk_=pk_, col=col, slot=slot, g=g, po=po, i=i, n=len(srcs): e.matmul(
                            po, lhsT=pT[pk_][:, col:col + 128], rhs=vat[:, slot, g, :], start=(i == 0),
                            stop=(i == n - 1)),
                            reads=[pT_b[pk_], vat_b[slot]], writes=[pbank_b[bank]], inc=(i == len(srcs) - 1))
            for (bank, h0, nh) in ((2, 0, 7), (3, 7, 7), (4, 14, 2)):
                po3 = pbank[bank][:, 0:nh * 65].rearrange("p (h d) -> p h d", h=nh)
                dsum = ast[:, 54 + h0:54 + h0 + nh]
                rden = ast[:, 72 + h0:72 + h0 + nh]
                tr.op("dve", lambda e, po3=po3, dsum=dsum, h0=h0, nh=nh: e.tensor_tensor(
                    out=dsum, in0=po3[:, :, 64], in1=sinkexp[:, h0:h0 + nh], op=ALU.add),
                    reads=[pbank_b[bank], setup_b, ast_b], writes=[ast_b])
                tr.op("dve", lambda e, dsum=dsum, rden=rden: e.reciprocal(out=rden, in_=dsum),
                      reads=[ast_b], writes=[ast_b])
                tr.op("dve", lambda e, po3=po3, rden=rden, h0=h0, nh=nh: e.tensor_tensor(
                    out=ya[:, h0 * 64:(h0 + nh) * 64].rearrange("p (h d) -> p h d", h=nh), in0=po3[:, :, 0:64],
                    in1=rden.unsqueeze(2).broadcast_to([128, nh, 64]), op=ALU.mult),
                    reads=[pbank_b[bank], ast_b], writes=[ya_b])
            sl = slice(s * 128, (s + 1) * 128)
            py = pbank[4].bitcast(BF16)
            for c in range(8):
                tr.op("pe", lambda e, c=c: e.transpose(out=py[:, c * 128:(c + 1) * 128],
                                                       in_=ya[:, c * 128:(c + 1) * 128], identity=ident_bf),
                      reads=[ya_b, const_b], writes=[pbank_b[4]], inc=(c == 7))
            tr.op("act", lambda e: e.activation(out=yaT[:, :, sl], in_=py[:, :].rearrange("p (c t) -> p c t", c=8),
                                                func=AF.Copy),
                  reads=[pbank_b[4]], writes=[yaT_b[s]])

        def merge(ti):
            for a in range(4):
                wv, wb = wload(f"mrg{a}")
                for jj in range(2):
                    j = 2 * a + jj
                    for (bank, seg, rhsT, rb) in ((0, 0, ymT, ymT_b), (1, 1, yaT, yaT_b), (2, 2, hT, hT_b),
                                                  (3, 3, hT, hT_b)):
                        for kc in range(8):
                            tr.op("pe", lambda e, bank=bank, seg=seg, rhsT=rhsT, kc=kc, wv=wv, jj=jj: e.matmul(
                                pbank[bank][:, :], lhsT=wv[:, seg * 2 + jj, kc, :], rhs=rhsT[:, kc, :],
                                start=(kc == 0), stop=(kc == 7)),
                                reads=[wb] + rb, writes=[pbank_b[bank]], inc=(kc == 7))
                    k = j % 2
                    tr.op("act", lambda e, j=j, k=k: e.activation(out=sgt[k][:], in_=pbank[2][:, :], func=AF.Sigmoid,
                                                                  bias=bmcol[:, j:j + 1]),
                          reads=[pbank_b[2], const_b], writes=[sgt_b[k]])
                    tr.op("act", lambda e, j=j, k=k: e.activation(out=sgt[2 + k][:], in_=pbank[3][:, :],
                                                                  func=AF.Sigmoid, bias=bmcol[:, 8 + j:9 + j]),
                          reads=[pbank_b[3], const_b], writes=[sgt_b[2 + k]])
                    tr.op("dve", lambda e, k=k: e.tensor_tensor(out=mt[0], in0=pbank[0][:, :], in1=sgt[k][:],
                                                                op=ALU.mult),
                          reads=[pbank_b[0], sgt_b[k]], writes=[mt_b[0]])
                    tr.op("dve", lambda e, k=k: e.tensor_tensor(out=mt[1], in0=pbank[1][:, :], in1=sgt[2 + k][:],
                                                                op=ALU.mult),
                          reads=[pbank_b[1], sgt_b[2 + k]], writes=[mt_b[1]])
                    tr.op("dve", lambda e, j=j: e.tensor_tensor(out=mgT[:, j, :], in0=mt[0], in1=mt[1],
                                                                 op=ALU.add),
                          reads=[mt_b[0], mt_b[1]], writes=[mgT_b[j]])
            dump(f"mgT{ti}", mgT[:], mgT_b, [128, 8, T], BF16)

        def outproj(ti):
            wv, wb = wload("wout")
            for s in range(NSUB):
                for n in range(2):
                    pb = gemm_bank()
                    for kc in range(8):
                        tr.op("pe", lambda e, pb=pb, kc=kc, s=s, n=n, wv=wv: e.matmul(
                            pbank[pb][:, :], lhsT=mgT[:, kc, s * 128:(s + 1) * 128], rhs=wv[:, n, kc, :],
                            start=(kc == 0), stop=(kc == 7)),
                            reads=[wb] + mgT_b, writes=[pbank_b[pb]], inc=(kc == 7))
                    tr.op("dve", lambda e, pb=pb, s=s, n=n: e.tensor_tensor(
                        out=x_sb[:, s, n * 512:(n + 1) * 512], in0=pbank[pb][:, :],
                        in1=x_sb[:, s, n * 512:(n + 1) * 512], op=ALU.add),
                        reads=[pbank_b[pb], x_b[s]], writes=[x_b[s]])
            dump(f"x1_{ti}", x_sb[:], x_b, [128, NSUB, D])
            for s in range(NSUB):
                norm_transpose(x_sb[:, s, :], x_b[s], ymT, ymT_b[s], s, ti * NSUB + s)

        ost_rr = [0]

        def ffn(ti):
            r0 = ti * T
            for a in range(6):
                wv, wb = wload(f"ffi{a}")
                js = list(range(4 * a, min(4 * a + 4, 22)))
                for idx, j in enumerate(js):
                    gb, ub = gemm_bank(), gemm_bank()
                    for (bank, seg) in ((gb, idx), (ub, len(js) + idx)):
                        for kc in range(8):
                            tr.op("pe", lambda e, bank=bank, seg=seg, kc=kc, wv=wv: e.matmul(
                                pbank[bank][:, :], lhsT=wv[:, seg, kc, :], rhs=ymT[:, kc, :],
                                start=(kc == 0), stop=(kc == 7)),
                                reads=[wb] + ymT_b, writes=[pbank_b[bank]], inc=(kc == 7))
                    k = j % 2
                    tr.op("act", lambda e, gb=gb, k=k: e.activation(out=sgt[k][:], in_=pbank[gb][:, :], func=AF.Silu),
                          reads=[pbank_b[gb]], writes=[sgt_b[k]])
                    tr.op("dve", lambda e, ub=ub, k=k, j=j: e.tensor_tensor(out=actT[:, j, :], in0=pbank[ub][:, :],
                                                                            in1=sgt[k][:], op=ALU.mult),
                          reads=[pbank_b[ub], sgt_b[k]], writes=[pg[j]])
            for n in range(2):
                for hlf in range(2):
                    wv, wb = wload(f"ffo{n}{hlf}")
                    for k in range(11):
                        kc = hlf * 11 + k
                        for s in range(NSUB):
                            tr.op("pe", lambda e, s=s, kc=kc, k=k, wv=wv, n=n: e.matmul(
                                pbank[s + 4 * n][:, :], lhsT=actT[:, kc, s * 128:(s + 1) * 128], rhs=wv[:, 0, k, :],
                                start=(kc == 0), stop=(kc == 21)),
                                reads=[wb, pg[kc]], writes=[pbank_b[s + 4 * n]], inc=(k == 10 and s == NSUB - 1))
                for s in range(NSUB):
                    o = ost_rr[0] % 2
                    ost_rr[0] += 1
                    tr.op("dve", lambda e, s=s, n=n, o=o: e.tensor_tensor(
                        out=ostage[o][:], in0=pbank[s + 4 * n][:, :], in1=x_sb[:, s, n * 512:(n + 1) * 512],
                        op=ALU.add),
                        reads=[pbank_b[s + 4 * n], x_b[s]], writes=[ostage_b[o]])
                    dst = out_d[r0 + s * 128:r0 + (s + 1) * 128, n * 512:(n + 1) * 512]
                    tr.dma("sp", lambda e, dst=dst, o=o: e.dma_start(out=dst, in_=ostage[o][:]), f"ost{o}",
                           reads=[ostage_b[o]], nbytes=262144)

        tr.op("pool", lambda e: e.memset(vaug[:, :, :, 256:257], 1.0), writes=vaug_b)

        tile_front(0)
        for ti in range(ntiles):
            first = (ti % 8 == 0)
            inproj_a(ti, first)
            gates(ti, first)
            inproj_b(ti)
            if stop_after == "inproj":
                continue
            for s in range(NSUB):
                mlstm_block(ti, s, first and s == 0)
                if stop_after != "mlstm":
                    attn_block(ti, s, (ti % 8) * NSUB + s)
            dump(f"ymT{ti}", ymT[:], ymT_b, [128, 8, T], BF16)
            dump(f"yaT{ti}", yaT[:], yaT_b, [128, 8, T], BF16)
            if stop_after in ("mlstm", "attn"):
                continue
            merge(ti)
            x_reload(ti)
            outproj(ti)
            if stop_after == "outproj":
                continue
            if ti + 1 < ntiles:
                tile_front(ti + 1)
            ffn(ti)
            if ti == 0:
                tr.op("pool", lambda e: e.memset(vaug[:, :, :, 256:257], 1.0), writes=vaug_b)

        tr.schedule(reorder=reorder, prio=prio)

        semnames = set(Tracker.ENG) | set(tr.dma_sems)
        sems = {n: es.enter_context(nc.semaphore("s_" + n)) for n in sorted(semnames)}
        block = es.enter_context(nc.Block())

        def replay(engname):
            def run(eng):
                for item in tr.q[engname]:
                    if item[0] == "wait":
                        eng.wait_ge(sems[item[1]], item[2])
                    else:
                        ins = None
                        for fn in item[1]:
                            ins = fn(eng)
                        ins.then_inc(sems[item[2]], item[3])
            return run

        block.tensor(replay("pe"))
        block.scalar(replay("act"))
        block.vector(replay("dve"))
        block.gpsimd(replay("pool"))
        block.sync(replay("sp"))
    stats = {e: len(tr.q[e]) for e in Tracker.ENG}
    stats['sim_end_us'] = getattr(tr, 'sim_end', 0.0) / 1e3
    stats['sbuf_left'] = sbuf_left
    return nc, dump_specs, stats


def _prep_inputs(inputs, ntiles=16, ncores=NCORES):
    f = np.float32
    x = np.ascontiguousarray(np.asarray(inputs["x"], dtype=f)).reshape(-1, D)
    cbf, cf = _host_consts()
    g1 = np.asarray(inputs["norm1_g"], f).reshape(8, 128).T
    g2 = np.asarray(inputs["norm2_g"], f).reshape(8, 128).T
    gm = np.asarray(inputs["m_norm_g"], f).reshape(8, 128).T
    convw = np.asarray(inputs["conv_w"], f).reshape(4, 16, 128).transpose(2, 1, 0).reshape(128, 64)
    convb = np.asarray(inputs["conv_b"], f).reshape(16, 128).T
    bmc = np.asarray(inputs["b_merge"], f).reshape(16, 128).T
    pcol = np.ascontiguousarray(np.concatenate([g1, g2, gm, convw, convb, bmc], axis=1))
    prow = np.ascontiguousarray(np.concatenate([np.asarray(inputs["q_norm_g"], f).reshape(-1),
                                                np.asarray(inputs["k_norm_g"], f).reshape(-1),
                                                np.asarray(inputs["sinks"], f).reshape(-1)])[None, :])
    pgate = np.ascontiguousarray(np.asarray(inputs["b_mgate"], f).reshape(2, 4).T)
    shared = {
        "w_in": np.ascontiguousarray(np.asarray(inputs["w_in"], f).reshape(D, N_IN)),
        "w_bm": np.ascontiguousarray(np.asarray(inputs["w_branch_m"], f).reshape(D, D)),
        "w_ba": np.ascontiguousarray(np.asarray(inputs["w_branch_a"], f).reshape(D, D)),
        "w_out": np.ascontiguousarray(np.asarray(inputs["w_out"], f).reshape(D, D)),
        "w_fi": np.ascontiguousarray(np.asarray(inputs["w_ffn_in"], f).reshape(D, 2 * DFF)),
        "w_fo": np.ascontiguousarray(np.asarray(inputs["w_ffn_out"], f).reshape(DFF, D)),
        "cbf": cbf, "cf": cf, "pcol": pcol, "prow": prow, "pgate": pgate,
    }
    in_maps = []
    per = TOK_CORE
    for c in range(ncores):
        m = dict(shared)
        m["x"] = x[c * per:c * per + ntiles * T]
        in_maps.append(m)
    return in_maps


_PROGRAM = None


def kernel(**inputs):
    global _PROGRAM
    if _PROGRAM is None:
        _PROGRAM = build_program(16)[0]
    in_maps = _prep_inputs(inputs)
    res = run_bass_kernel_spmd(_PROGRAM, in_maps, core_ids=list(range(NCORES)))
    out = np.concatenate([np.asarray(r["out"], dtype=np.float32) for r in res.results], axis=0)
    return out.reshape(16, SEQ, D)
```

```python
import numpy as np
import ml_dtypes
from contextlib import ExitStack
import concourse.bass as bass
import concourse.mybir as mybir
from concourse.bass_utils import run_bass_kernel_spmd

F32 = mybir.dt.float32
BF16 = mybir.dt.bfloat16
AF = mybir.ActivationFunctionType
ALU = mybir.AluOpType
AX = mybir.AxisListType

D = 1024
SEQ = 4096
NCORES = 8
TOK_CORE = 2 * SEQ
T = 512
NSUB = 4
DFF = 2816
N_IN = 7432
EPS = 1e-6
NEG = -30000.0
O_MQ, O_MK, O_MV, O_MO, O_MI, O_MF, O_AQ, O_AK, O_AV, O_GM, O_GA = (
    0, 1024, 2048, 3072, 4096, 4100, 4104, 5128, 5256, 5384, 6408)


class Buf:
    __slots__ = ("name", "w", "r", "excl")

    def __init__(self, name="", excl=False):
        self.name = name
        self.w = None
        self.r = []
        self.excl = excl


class Op:
    __slots__ = ("id", "eng", "fns", "preds", "dur", "sem", "nbytes", "tick", "fin")

    def __init__(self, id, eng, sem=None, nbytes=0):
        self.id = id
        self.eng = eng
        self.fns = []
        self.preds = set()
        self.dur = 0.0
        self.sem = sem
        self.nbytes = nbytes
        self.tick = 0
        self.fin = 0.0


def _est(eng, n):
    if eng == "pe":
        return 60.0 + 0.33 * max(n, 64)
    if eng == "act":
        return 200.0 + 0.85 * n
    if eng == "dve":
        return 180.0 + 1.05 * n
    if eng == "pool":
        return 300.0 + 3.0 * n
    return 60.0


class Tracker:
    ENG = ("pe", "act", "dve", "pool", "sp")

    def __init__(self):
        self.ops = []
        self.unit = None
        self.q = {e: [] for e in self.ENG}
        self.dma_sems = set()

    def _preds(self, reads, writes, self_id):
        p = set()
        for b in reads:
            if b.w is not None:
                p.add(b.w)
            if b.excl:
                p.update(b.r)
        for b in writes:
            if b.w is not None:
                p.add(b.w)
            p.update(b.r)
        p.discard(self_id)
        return p

    def _mark(self, oid, reads, writes):
        for b in writes:
            b.w = oid
            b.r = []
        for b in reads:
            if b.excl:
                b.w = oid
                b.r = []
            elif not b.r or b.r[-1] != oid:
                b.r.append(oid)

    class _Probe:
        def __init__(self):
            self.n = 512

        def __getattr__(self, name):
            def f(*args, **kw):
                out = kw.get("out", args[0] if args else None)
                try:
                    shp = out.shape
                    m = 1
                    for d in shp[1:]:
                        m *= int(d)
                    self.n = m
                except Exception:
                    pass
                return None
            return f

    def op(self, eng, fn, reads=(), writes=(), inc=True, n=None):
        if n is None:
            pr = Tracker._Probe()
            fn(pr)
            n = pr.n
        if eng == "pe" and self.unit is not None:
            o = self.unit
        else:
            o = Op(len(self.ops), eng)
            self.ops.append(o)
            if eng == "pe":
                self.unit = o
        o.fns.append(fn)
        o.dur += _est(eng, n)
        o.preds |= self._preds(reads, writes, o.id)
        self._mark(o.id, reads, writes)
        if eng == "pe" and inc:
            self.unit = None

    def dma(self, eng, fn, sem, reads=(), writes=(), nbytes=65536):
        assert self.unit is None
        o = Op(len(self.ops), eng, sem=sem, nbytes=nbytes)
        self.ops.append(o)
        self.dma_sems.add(sem)
        o.fns.append(fn)
        o.dur = 60.0
        o.preds |= self._preds(reads, writes, o.id)
        self._mark(o.id, reads, writes)

    def schedule(self, reorder=True, prio="order"):
        import heapq
        ops = self.ops
        n = len(ops)
        succs = [[] for _ in range(n)]
        indeg = [0] * n
        for o in ops:
            indeg[o.id] = len(o.preds)
            for p in o.preds:
                succs[p].append(o.id)
        order = {e: [] for e in self.ENG}
        if not reorder:
            for o in ops:
                order[o.eng].append(o)
        else:
            key = list(range(n))
            if prio == "cp":
                ind2 = list(indeg)
                topo = [i for i in range(n) if ind2[i] == 0]
                k = 0
                while k < len(topo):
                    for sid in succs[topo[k]]:
                        ind2[sid] -= 1
                        if ind2[sid] == 0:
                            topo.append(sid)
                    k += 1
                bl = [0.0] * n
                for i in reversed(topo):
                    m = 0.0
                    for sid in succs[i]:
                        if bl[sid] > m:
                            m = bl[sid]
                    d = ops[i].dur if ops[i].sem is None else 2000.0 + ops[i].nbytes / 300.0
                    bl[i] = m + d
                rank = sorted(range(n), key=lambda i: (-bl[i], i))
                for r, i in enumerate(rank):
                    key[i] = r
            inv = [0] * n
            for i in range(n):
                inv[key[i]] = i
            free_at = {e: 0.0 for e in self.ENG}
            pend = {e: [] for e in self.ENG}
            avail = {e: [] for e in self.ENG}
            ready_t = [0.0] * n
            for o in ops:
                if indeg[o.id] == 0:
                    heapq.heappush(pend[o.eng], (0.0, key[o.id]))
            dma_free = 0.0
            done = 0
            while done < n:
                best = None
                for e in self.ENG:
                    pe_, av = pend[e], avail[e]
                    while pe_ and pe_[0][0] <= free_at[e]:
                        heapq.heappush(av, heapq.heappop(pe_)[1])
                    if av:
                        cand = (free_at[e], av[0], e, True)
                    elif pe_:
                        cand = (pe_[0][0], pe_[0][1], e, False)
                    else:
                        continue
                    if best is None or cand[:2] < best[:2]:
                        best = cand
                start, okey, e, from_av = best
                if from_av:
                    heapq.heappop(avail[e])
                else:
                    heapq.heappop(pend[e])
                oid = inv[okey]
                o = ops[oid]
                if o.sem is not None:
                    free_at[e] = start + o.dur
                    t0 = max(start, dma_free)
                    dma_free = t0 + o.nbytes / 300.0
                    o.fin = dma_free + 2000.0
                else:
                    o.fin = start + o.dur
                    free_at[e] = o.fin
                order[e].append(o)
                done += 1
                for sid in succs[oid]:
                    so = ops[sid]
                    lat = 0.0 if so.eng == e else 150.0
                    if ready_t[sid] < o.fin + lat:
                        ready_t[sid] = o.fin + lat
                    indeg[sid] -= 1
                    if indeg[sid] == 0:
                        heapq.heappush(pend[so.eng], (ready_t[sid], key[sid]))
            self.sim_end = max(o.fin for o in ops)
        dma_cnt = {}
        for e in self.ENG:
            c = 0
            for o in order[e]:
                if o.sem is not None:
                    dma_cnt[o.sem] = dma_cnt.get(o.sem, 0) + 16
                    o.tick = dma_cnt[o.sem]
                else:
                    c += 1
                    o.tick = c
        for e in self.ENG:
            waited = {}
            q = self.q[e]
            for o in order[e]:
                need = {}
                for p in o.preds:
                    po = ops[p]
                    if po.eng == "pe" and e == "pe":
                        continue
                    k = po.sem if po.sem is not None else po.eng
                    if need.get(k, 0) < po.tick:
                        need[k] = po.tick
                for k, v in need.items():
                    if waited.get(k, 0) < v:
                        waited[k] = v
                        q.append(("wait", k, v))
                k = o.sem if o.sem is not None else o.eng
                q.append(("op", o.fns, k, 16 if o.sem is not None else 1))
            if e == "sp":
                for k, v in dma_cnt.items():
                    if waited.get(k, 0) < v:
                        waited[k] = v
                        q.append(("wait", k, v))


def _host_consts():
    bf = ml_dtypes.bfloat16
    p = np.arange(128)
    ident = np.eye(128, dtype=np.float32)
    s = p[:, None]
    t = p[None, :]
    mask16 = np.where(s <= t, 1.0 / 16.0, 0.0).astype(np.float32)
    mcur = np.where(s <= t, 0.0, NEG).astype(np.float32)
    mprev = np.where(s > t, 0.0, NEG).astype(np.float32)
    cbf = np.concatenate([ident, mask16, mprev, mprev, mcur, mcur], axis=1).astype(bf)
    half = 8
    inv_freq = (500000.0 ** (-np.arange(half, dtype=np.float32) * (2.0 / 16.0))).astype(np.float32)
    pos = (np.arange(32)[None, :] * 128 + p[:, None]).astype(np.float32)
    ang = pos[:, :, None] * inv_freq[None, None, :]
    cos = np.cos(ang).astype(np.float32).reshape(128, 256)
    sin = np.sin(ang).astype(np.float32).reshape(128, 256)
    cf = np.concatenate([ident[:, 0:4], cos, sin], axis=1).astype(np.float32)
    return cbf, cf


CB_ID, CB_M16, CB_MP, CB_MC = 0, 128, 256, 512
CF_ID, CF_COS, CF_SIN = 0, 4, 260


def _weight_items():
    items = []

    def add(name, src_segs, segw, kc0=0, nkc=8):
        items.append(dict(name=name, segs=src_segs, segw=segw, kc0=kc0, nkc=nkc))

    for a in range(2):
        add(f"ina{a}", [("w_in", a * 1024 + j * 128, "g1") for j in range(8)], 128)
    add("inb0", [("w_in", O_MV, "g1"), ("w_in", O_MV + 512, "g1")], 512)
    add("inb1", [("w_in", O_MO, "g1"), ("w_in", O_MO + 512, "g1")], 512)
    add("inb2", [("w_in", O_AQ, "g1"), ("w_in", O_AQ + 512, "g1")], 512)
    add("inb3", [("w_in", O_AK, "g1")], 256)
    for a in range(4):
        js = (2 * a, 2 * a + 1)
        segs = ([("w_bm", j * 128, "gm") for j in js] + [("w_ba", j * 128, None) for j in js] +
                [("w_in", O_GM + j * 128, "g1") for j in js] + [("w_in", O_GA + j * 128, "g1") for j in js])
        add(f"mrg{a}", segs, 128)
    add("wout", [("w_out", 0, None), ("w_out", 512, None)], 512)
    for a in range(6):
        js = list(range(4 * a, min(4 * a + 4, 22)))
        segs = [("w_fi", j * 128, "g2") for j in js] + [("w_fi", DFF + j * 128, "g2") for j in js]
        add(f"ffi{a}", segs, 128)
    for n in range(2):
        for hlf in range(2):
            add(f"ffo{n}{hlf}", [("w_fo", n * 512, None)], 512, kc0=hlf * 11, nkc=11)
    off = 0
    for it in items:
        it["size"] = len(it["segs"]) * it["nkc"] * it["segw"]
        it["off"] = off
        off += it["size"]
    return items, off


SLOT = 8192
NSLOT = 2


def build_program(ntiles=16, dumps=(), stop_after=None, reorder=True, prio="cp"):
    nc = bass.Bass("TRN2", target_bir_lowering=False)
    tr = Tracker()
    items, wtot = _weight_items()
    itmap = {it["name"]: it for it in items}
    dumps = set(dumps)
    dump_specs = {}

    ntok = ntiles * T
    x_d = nc.dram_tensor("x", [ntok, D], F32, kind="ExternalInput").ap()
    out_d = nc.dram_tensor("out", [ntok, D], F32, kind="ExternalOutput").ap()
    wsrc = {
        "w_in": nc.dram_tensor("w_in", [D, N_IN], F32, kind="ExternalInput").ap(),
        "w_bm": nc.dram_tensor("w_bm", [D, D], F32, kind="ExternalInput").ap(),
        "w_ba": nc.dram_tensor("w_ba", [D, D], F32, kind="ExternalInput").ap(),
        "w_out": nc.dram_tensor("w_out", [D, D], F32, kind="ExternalInput").ap(),
        "w_fi": nc.dram_tensor("w_fi", [D, 2 * DFF], F32, kind="ExternalInput").ap(),
        "w_fo": nc.dram_tensor("w_fo", [DFF, D], F32, kind="ExternalInput").ap(),
    }
    cbf_d = nc.dram_tensor("cbf", [128, 768], BF16, kind="ExternalInput").ap()
    cf_d = nc.dram_tensor("cf", [128, 516], F32, kind="ExternalInput").ap()
    pcol_d = nc.dram_tensor("pcol", [128, 24 + 64 + 16 + 16], F32, kind="ExternalInput").ap()
    prow_d = nc.dram_tensor("prow", [1, 144], F32, kind="ExternalInput").ap()
    pgate_d = nc.dram_tensor("pgate", [4, 2], F32, kind="ExternalInput").ap()
    wscr_d = nc.dram_tensor("wscr", [128, wtot], BF16, kind="Internal").ap()

    es = ExitStack()
    with es:
        def sb(name, shape, dt):
            return es.enter_context(nc.sbuf_tensor(name, shape, dt))

        def psum(name, shape, dt):
            return es.enter_context(nc.psum_tensor(name, shape, dt))

        x_sb = sb("x_sb", [128, NSUB, D], F32)
        x_b = [Buf(f"x{s}") for s in range(NSUB)]
        hT = sb("hT", [128, 8, T], BF16)
        hT_b = [Buf(f"hT{s}") for s in range(NSUB)]
        big = sb("big", [128, 12288], BF16)
        pg = [Buf(f"pg{i}") for i in range(24)]
        qkT = big[:, 0:8192].rearrange("p (c t) -> p c t", c=16)
        sigo = big[:, 8192:12288].rearrange("p (s f) -> p s f", s=4)
        actT = big[:, 0:11264].rearrange("p (c t) -> p c t", c=22)
        vaug = sb("vaug", [128, NSUB, 4, 257], BF16)
        vaug_b = [Buf(f"vaug{s}") for s in range(NSUB)]
        aqkv = sb("aqkv", [128, NSUB, 1280], BF16)
        aqkv_b = [Buf(f"aqkv{s}") for s in range(NSUB)]
        ymT = sb("ymT", [128, 8, T], BF16)
        ymT_b = [Buf(f"ymT{s}") for s in range(NSUB)]
        yaT = sb("yaT", [128, 8, T], BF16)
        yaT_b = [Buf(f"yaT{s}") for s in range(NSUB)]
        mgT = sb("mgT", [128, 8, T], BF16)
        mgT_b = [Buf(f"mgT{j}") for j in range(8)]
        wslot = [sb(f"wslot{i}", [128, SLOT], BF16) for i in range(NSLOT)]
        wslot_b = [Buf(f"wslot{i}") for i in range(NSLOT)]
        cbf = sb("cbf_sb", [128, 768], BF16)
        cf = sb("cf_sb", [128, 516], F32)
        const_b = Buf("const")
        pcol = sb("pcol_sb", [128, 120], F32)
        prow = sb("prow_sb", [128, 144], F32)
        pgate = sb("pgate_sb", [4, 2], F32)
        wgate = sb("wgate", [128, 8, 8], BF16)
        wgate_b = Buf("wgate")
        xn = [sb(f"xn{i}", [128, D], BF16) for i in range(2)]
        xn_b = [Buf(f"xn{i}") for i in range(2)]
        junk = sb("junk", [128, 256], BF16)
        junk_b = Buf("junk")
        stat = sb("stat", [128, 64], F32)
        cst = [sb(f"cst{i}", [128, 515], F32) for i in range(2)]
        cst_b = [Buf(f"cst{i}") for i in range(2)]
        cacc = [sb(f"cacc{i}", [128, 512], F32) for i in range(2)]
        cacc_b = [Buf(f"cacc{i}") for i in range(2)]
        carry = sb("carry", [128, 16, 3], F32)
        carry_b = [Buf(f"carry{j}") for j in range(16)]
        ostage = [sb(f"ostage{i}", [128, 512], F32) for i in range(2)]
        ostage_b = [Buf(f"ostage{i}") for i in range(2)]

        gr = sb("gr", [4, 2, T], F32)
        gr_b = [Buf("gr0"), Buf("gr1")]
        gs = sb("gs", [4, 32], F32)
        gs_b = Buf("gs")
        Rm = sb("Rm", [4, 32], F32)
        Rm_b = Buf("Rm")
        ones4 = sb("ones4", [4, 128], F32)
        wt = [sb(f"wt{i}", [128, NSUB, 8], F32) for i in range(2)]
        wt_b = [Buf(f"wt{i}") for i in range(2)]
        decb = [sb(f"decb{i}", [128, 32], F32) for i in range(2)]
        decb_b = [Buf(f"decb{i}") for i in range(2)]
        Cst = sb("Cst", [128, 4, 2, 257], F32)
        Cst_b = [Buf(f"Cst{h}") for h in range(4)]
        Csb = sb("Csb", [128, 4, 2, 257], BF16)
        Csb_b = [Buf(f"Csb{h}") for h in range(4)]
        STm = [sb(f"STm{i}", [128, 4, 128], BF16) for i in range(2)]
        STm_b = [[Buf(f"STm{i}_{h}") for h in range(4)] for i in range(2)]
        wvt = [sb(f"wvt{i}", [128, 4, 257], BF16) for i in range(2)]
        wvt_b = [[Buf(f"wvt{i}_{h}") for h in range(4)] for i in range(2)]
        ktok = [sb(f"ktok{i}", [128, D], BF16) for i in range(2)]
        ktok_b = [Buf(f"ktok{i}") for i in range(2)]
        hms = [sb(f"hm{i}", [128, D], BF16) for i in range(2)]
        hms_b = [[Buf(f"hm{i}_{h}") for h in range(4)] for i in range(2)]
        ymtok = sb("ymtok", [128, D], BF16)
        ymtok_b = Buf("ymtok")
        est = [sb(f"est{i}", [128, 24], F32) for i in range(2)]
        est_b = [Buf(f"est{i}") for i in range(2)]
        sq = sb("sq", [128, 1152], F32)
        sq_b = Buf("sq")
        qn = sb("qn", [128, 1152], F32)
        qn_b = Buf("qn")
        qb = sb("qb", [128, 18, 64], BF16)
        qb_b = Buf("qb")
        k2 = sb("k2", [128, 256], BF16)
        k2_b = Buf("k2")
        QBs = [sb(f"QB{i}", [128, 8, 256], BF16) for i in range(2)]
        QBs_b = [Buf(f"QB{i}") for i in range(2)]
        kTd = sb("kTd", [128, 2, 2, 128], BF16)
        kTd_b = [Buf("kTd0"), Buf("kTd1")]
        vat = sb("vat", [128, 2, 2, 65], BF16)
        vat_b = [Buf("vat0"), Buf("vat1")]
        pT = [sb(f"pT{i}", [128, 512], BF16) for i in range(2)]
        pT_b = [Buf(f"pT{i}") for i in range(2)]
        ya = sb("ya", [128, D], BF16)
        ya_b = Buf("ya")
        asts = [sb(f"ast{i}", [128, 68], F32) for i in range(2)]
        asts_b = [Buf(f"ast{i}") for i in range(2)]
        gqk = sb("gqk", [128, 128], F32)
        acst = sb("acst", [128, 32], F32)
        sgt = [sb(f"sgt{i}", [128, 512], BF16) for i in range(4)]
        sgt_b = [Buf(f"sgt{i}") for i in range(4)]
        mt = [sq[:, 0:512], sq[:, 512:1024]]
        mt_b = [sq_b, sq_b]

        sbuf_left = nc.sbuf_bytes_remaining
        pbank = [psum(f"pb{i}", [128, 512], F32) for i in range(8)]
        pbank_b = [Buf(f"pb{i}", excl=True) for i in range(8)]

        ident_f = cf[:, CF_ID:CF_ID + 4]
        MBprev = cbf[:, CB_MP:CB_MP + 256]
        MBcur = cbf[:, CB_MC:CB_MC + 256]
        ident_bf = cbf[:, CB_ID:CB_ID + 128]
        mask16 = cbf[:, CB_M16:CB_M16 + 128]

        g1col = pcol[:, 0:8]
        g2col = pcol[:, 8:16]
        gmcol = pcol[:, 16:24]
        convw = pcol[:, 24:88].rearrange("p (j k) -> p j k", j=16)
        convb = pcol[:, 88:104]
        bmcol = pcol[:, 104:120]
        fold = {"g1": g1col, "g2": g2col, "gm": gmcol, None: None}

        def dump(name, ap, bufs, shape, dt=F32):
            if name not in dumps:
                return
            d = nc.dram_tensor("dbg_" + name, list(shape), dt, kind="ExternalOutput").ap()
            dump_specs[name] = (list(shape), dt)
            tr.dma("sp", lambda e, d=d, ap=ap: e.dma_start(out=d, in_=ap), "dbg_" + name, reads=bufs)

        tr.dma("sp", lambda e: e.dma_start(out=cbf[:], in_=cbf_d), "cld", writes=[const_b])
        tr.dma("sp", lambda e: e.dma_start(out=cf[:], in_=cf_d), "cld", writes=[const_b])
        tr.dma("sp", lambda e: e.dma_start(out=pcol[:], in_=pcol_d), "cld", writes=[const_b])
        tr.dma("sp", lambda e: e.dma_start(out=prow[:], in_=prow_d.partition_broadcast(128)), "cld",
               writes=[const_b])
        tr.dma("sp", lambda e: e.dma_start(out=pgate[:], in_=pgate_d), "cld", writes=[const_b])

        wscr_b = {it["name"]: Buf("wscr_" + it["name"]) for it in items}
        cast_rr = [0]
        stage_rr = [0]

        def prep_cast(out_ap, in_ap, scale_ap, reads, writes):
            k = cast_rr[0] % 2
            cast_rr[0] += 1
            if k == 0:
                if scale_ap is None:
                    tr.op("act", lambda e: e.activation(out=out_ap, in_=in_ap, func=AF.Copy),
                          reads=reads, writes=writes)
                else:
                    tr.op("act", lambda e: e.activation(out=out_ap, in_=in_ap, func=AF.Copy, scale=scale_ap),
                          reads=reads + [const_b], writes=writes)
            else:
                eng = "dve" if k == 1 else "pool"
                if scale_ap is None:
                    tr.op(eng, lambda e: e.tensor_copy(out=out_ap, in_=in_ap), reads=reads, writes=writes)
                else:
                    tr.op(eng, lambda e: e.tensor_scalar(out=out_ap, in0=in_ap, scalar1=scale_ap, scalar2=None,
                                                         op0=ALU.mult),
                          reads=reads + [const_b], writes=writes)

        bigf = big.bitcast(F32)

        vaugf = vaug.reshape([128, NSUB * 4 * 257]).bitcast(F32)
        aqkvf = aqkv.reshape([128, NSUB * 1280]).bitcast(F32)
        vstage = [(vaugf[:, 0:1024], [vaug_b[0], vaug_b[1]]), (vaugf[:, 1028:2052], [vaug_b[2], vaug_b[3]]),
                  (aqkvf[:, 0:1024], [aqkv_b[0], aqkv_b[1]]), (aqkvf[:, 1280:2304], [aqkv_b[2], aqkv_b[3]])]

        def prep_item(it, slot, deep=False, vst=False):
            segs, segw, kc0, nkc = it["segs"], it["segw"], it["kc0"], it["nkc"]
            nseg = len(segs)
            view = wslot[slot][:, 0:it["size"]].rearrange("p (s k w) -> p s k w", s=nseg, k=nkc)
            groups = []
            for si, (src, c0, fd) in enumerate(segs):
                if groups and groups[-1][0] == src and groups[-1][3] == fd and \
                        groups[-1][1] + groups[-1][2] * segw == c0 and \
                        (groups[-1][2] + 1) * segw <= 1024 and groups[-1][4] + groups[-1][2] == si:
                    groups[-1][2] += 1
                else:
                    groups.append([src, c0, 1, fd, si])
            for k in range(nkc):
                kc = kc0 + k
                for (src, c0, n, fd, si0) in groups:
                    st = stage_rr[0] % (10 if deep else NSUB)
                    stage_rr[0] += 1
                    width = n * segw
                    src_ap = wsrc[src][kc * 128:(kc + 1) * 128, c0:c0 + width]
                    if vst:
                        stg, stg_bufs = vstage[st][0][:, 0:width], vstage[st][1]
                        st = 10 + st
                    elif st < NSUB:
                        stg = x_sb[:, st, 0:width]
                        stg_bufs = [x_b[st]]
                    else:
                        stg = bigf[:, (st - NSUB) * 1024:(st - NSUB) * 1024 + width]
                        stg_bufs = pg[4 * (st - NSUB):4 * (st - NSUB) + 4]
                    tr.dma("sp", lambda e, stg=stg, src_ap=src_ap: e.dma_start(out=stg, in_=src_ap),
                           f"xld{st}", writes=stg_bufs, nbytes=width * 512)
                    if n == 1:
                        out_ap = view[:, si0, k, :]
                        in_ap = stg
                    else:
                        out_ap = view[:, si0:si0 + n, k, :]
                        in_ap = stg.rearrange("p (s w) -> p s w", s=n)
                    sc = None if fd is None else fold[fd][:, kc:kc + 1]
                    prep_cast(out_ap, in_ap, sc, list(stg_bufs), [wslot_b[slot]])
            dst = wscr_d[:, it["off"]:it["off"] + it["size"]]
            tr.dma("sp", lambda e, dst=dst, slot=slot, it=it: e.dma_start(out=dst, in_=wslot[slot][:, 0:it["size"]]),
                   f"wst{slot}", reads=[wslot_b[slot]], writes=[wscr_b[it["name"]]], nbytes=it["size"] * 256)

        early_pending = {it["name"] for it in items}
        for kc in range(8):
            st = stage_rr[0] % NSUB
            stage_rr[0] += 1
            stg = x_sb[:, st, 0:8]
            src_ap = wsrc["w_in"][kc * 128:(kc + 1) * 128, O_MI:O_MI + 8]
            tr.dma("sp", lambda e, stg=stg, src_ap=src_ap: e.dma_start(out=stg, in_=src_ap), f"xld{st}",
                   writes=[x_b[st]])
            tr.op("dve", lambda e, kc=kc, stg=stg: e.tensor_scalar(out=wgate[:, kc, :], in0=stg,
                                                                  scalar1=g1col[:, kc:kc + 1], scalar2=None,
                                                                  op0=ALU.mult),
                  reads=[x_b[st], const_b], writes=[wgate_b])

        wcur = {"slot": 0}

        def wload(name):
            it = itmap[name]
            slot = wcur["slot"]
            wcur["slot"] = (slot + 1) % NSLOT
            if name in early_pending:
                early_pending.discard(name)
                prep_item(it, slot, vst=name.startswith(("wout", "ffi", "ffo")))
                view = wslot[slot][:, 0:it["size"]].rearrange("p (s k w) -> p s k w", s=len(it["segs"]),
                                                              k=it["nkc"])
                return view, wslot_b[slot]
            src = wscr_d[:, it["off"]:it["off"] + it["size"]]
            tr.dma("sp", lambda e, src=src, slot=slot, it=it: e.dma_start(out=wslot[slot][:, 0:it["size"]], in_=src),
                   f"wld{slot}", reads=[wscr_b[name]], writes=[wslot_b[slot]], nbytes=it["size"] * 256)
            view = wslot[slot][:, 0:it["size"]].rearrange("p (s k w) -> p s k w", s=len(it["segs"]), k=it["nkc"])
            return view, wslot_b[slot]

        gemm_rr = [0]

        def gemm_bank():
            b = gemm_rr[0] % 4
            gemm_rr[0] += 1
            return b

        xstg = [sq[:, 0:D], qn[:, 0:D]]
        xstg_b = [sq_b, qn_b]

        def tile_front(ti):
            r0 = ti * T
            for s in range(NSUB):
                k = s % 2
                src = x_d[r0 + s * 128:r0 + (s + 1) * 128, :]
                tr.dma("sp", lambda e, k=k, src=src: e.dma_start(out=xstg[k], in_=src), f"xsg{k}",
                       writes=[xstg_b[k]], nbytes=524288)
                norm_transpose(xstg[k], xstg_b[k], hT, hT_b[s], s, ti * NSUB + s)
            dump(f"hT{ti}", hT[:], hT_b, [128, 8, T], BF16)

        def x_reload(ti):
            r0 = ti * T
            for s in range(NSUB):
                src = x_d[r0 + s * 128:r0 + (s + 1) * 128, :]
                tr.dma("sp", lambda e, s=s, src=src: e.dma_start(out=x_sb[:, s, :], in_=src), f"xld{s}",
                       writes=[x_b[s]], nbytes=524288)

        def norm_transpose(src, src_b, dstT, dst_b, s, uid):
            c = (uid % 8) * 4
            ss, lnv, rstd = stat[:, c:c + 1], stat[:, c + 1:c + 2], stat[:, c + 2:c + 3]
            sbuf_ = Buf()
            k = uid % 2
            tr.op("act", lambda e: e.activation(out=xn[k][:], in_=src, func=AF.Square, accum_out=ss),
                  reads=[src_b], writes=[xn_b[k], sbuf_])
            tr.op("act", lambda e: e.activation(out=lnv, in_=ss, func=AF.Ln, scale=1.0 / D, bias=EPS),
                  reads=[sbuf_], writes=[sbuf_])
            tr.op("act", lambda e: e.activation(out=rstd, in_=lnv, func=AF.Exp, scale=-0.5),
                  reads=[sbuf_], writes=[sbuf_])
            tr.op("act", lambda e: e.activation(out=xn[k][:], in_=src, func=AF.Copy, scale=rstd),
                  reads=[src_b, sbuf_], writes=[xn_b[k]])
            pb = 4 + (uid % 2)
            pst = pbank[pb].bitcast(BF16)
            for kc in range(8):
                tr.op("pe", lambda e, kc=kc: e.transpose(out=pst[:, kc * 128:(kc + 1) * 128],
                                                         in_=xn[k][:, kc * 128:(kc + 1) * 128], identity=ident_bf),
                      reads=[xn_b[k], const_b], writes=[pbank_b[pb]], inc=(kc == 7))
            tr.op("act", lambda e: e.activation(out=dstT[:, :, s * 128:(s + 1) * 128],
                                                in_=pst[:, :].rearrange("p (c t) -> p c t", c=8), func=AF.Copy),
                  reads=[pbank_b[pb]], writes=[dst_b])

        def inproj_a(ti, first):
            for a in range(2):
                wv, wb = wload(f"ina{a}")
                for jj in range(8):
                    j = a * 8 + jj
                    pb = gemm_bank()
                    for kc in range(8):
                        tr.op("pe", lambda e, jj=jj, kc=kc, pb=pb, wv=wv: e.matmul(
                            pbank[pb][:, :], lhsT=wv[:, jj, kc, :], rhs=hT[:, kc, :], start=(kc == 0), stop=(kc == 7)),
                            reads=[wb] + hT_b, writes=[pbank_b[pb]], inc=(kc == 7))
                    k = j % 2
                    if first:
                        tr.op("pool", lambda e, k=k: e.memset(cst[k][:, 0:3], 0.0), writes=[cst_b[k]])
                    else:
                        tr.op("pool", lambda e, k=k, j=j: e.tensor_copy(out=cst[k][:, 0:3], in_=carry[:, j, :]),
                              reads=[carry_b[j]], writes=[cst_b[k]])
                    tr.op("act", lambda e, k=k, pb=pb: e.activation(out=cst[k][:, 3:515], in_=pbank[pb][:, :],
                                                                    func=AF.Copy),
                          reads=[pbank_b[pb]], writes=[cst_b[k]])
                    tr.op("pool", lambda e, k=k, j=j: e.tensor_copy(out=carry[:, j, :], in_=cst[k][:, 512:515]),
                          reads=[cst_b[k]], writes=[carry_b[j]])
                    tr.op("act", lambda e, k=k, j=j, pb=pb: e.activation(out=cacc[k][:, 3:512],
                                                                         in_=pbank[pb][:, 0:509], func=AF.Copy,
                                                                         scale=convw[:, j, 0:1]),
                          reads=[pbank_b[pb], const_b], writes=[cacc_b[k]])
                    tr.op("pool", lambda e, k=k, j=j: e.tensor_scalar(out=cacc[k][:, 0:3], in0=cst[k][:, 0:3],
                                                                       scalar1=convw[:, j, 0:1], scalar2=None,
                                                                       op0=ALU.mult),
                          reads=[cst_b[k], const_b, cacc_b[k]], writes=[cacc_b[k]])
                    for tap in range(1, 4):
                        tr.op("dve", lambda e, k=k, j=j, tap=tap: e.scalar_tensor_tensor(
                            out=cacc[k][:], in0=cst[k][:, tap:tap + 512], scalar=convw[:, j, tap:tap + 1],
                            in1=cacc[k][:], op0=ALU.mult, op1=ALU.add),
                            reads=[cst_b[k], cacc_b[k], const_b], writes=[cacc_b[k]])
                    tr.op("act", lambda e, k=k, j=j: e.activation(out=qkT[:, j, :], in_=cacc[k][:], func=AF.Silu,
                                                                   bias=convb[:, j:j + 1]),
                          reads=[cacc_b[k], const_b], writes=[pg[j]])
            dump(f"qkT{ti}", qkT, pg[0:16], [128, 16, T], BF16)

        def inproj_b(ti):
            plan = [("inb0", [("v", 0), ("v", 2)]), ("inb1", [("o", 0), ("o", 512)]),
                    ("inb2", [("q", 0), ("q", 512)]), ("inb3", [("kv", 0)])]
            for name, segl in plan:
                wv, wb = wload(name)
                segw = itmap[name]["segw"]
                for si, (kind, arg) in enumerate(segl):
                    for s in range(NSUB):
                        pb = gemm_bank()
                        for kc in range(8):
                            tr.op("pe", lambda e, si=si, kc=kc, pb=pb, wv=wv, s=s, segw=segw: e.matmul(
                                pbank[pb][:, 0:segw], lhsT=hT[:, kc, s * 128:(s + 1) * 128], rhs=wv[:, si, kc, :],
                                start=(kc == 0), stop=(kc == 7)),
                                reads=[wb, hT_b[s]], writes=[pbank_b[pb]], inc=(kc == 7))
                        if kind == "v":
                            tr.op("act", lambda e, pb=pb, s=s, arg=arg: e.activation(
                                out=vaug[:, s, arg:arg + 2, 0:256],
                                in_=pbank[pb][:, :].rearrange("p (h e) -> p h e", h=2), func=AF.Copy),
                                reads=[pbank_b[pb]], writes=[vaug_b[s]])
                        elif kind == "o":
                            tr.op("act", lambda e, pb=pb, s=s, arg=arg: e.activation(
                                out=sigo[:, s, arg:arg + 512], in_=pbank[pb][:, :], func=AF.Sigmoid),
                                reads=[pbank_b[pb]], writes=[pg[16 + 2 * s], pg[17 + 2 * s]])
                        elif kind == "q":
                            tr.op("dve", lambda e, pb=pb, s=s, arg=arg: e.tensor_copy(
                                out=aqkv[:, s, arg:arg + 512], in_=pbank[pb][:, :]),
                                reads=[pbank_b[pb]], writes=[aqkv_b[s]])
                        else:
                            tr.op("dve", lambda e, pb=pb, s=s: e.tensor_copy(
                                out=aqkv[:, s, 1024:1280], in_=pbank[pb][:, 0:256]),
                                reads=[pbank_b[pb]], writes=[aqkv_b[s]])
            dump(f"vaug{ti}", vaug[:], vaug_b, [128, NSUB, 4, 257], BF16)
            dump(f"sigo{ti}", sigo, pg[16:24], [128, NSUB, 1024], BF16)
            dump(f"aqkv{ti}", aqkv[:], aqkv_b, [128, NSUB, 1280], BF16)

        setup_b = Buf("setup")
        for i in range(2):
            tr.op("pool", lambda e, i=i: e.memset(QBs[i][:], 0.0), writes=[QBs_b[i]])
        tr.op("pool", lambda e: e.memset(vat[:, :, :, 64:65], 1.0), writes=vat_b)
        tr.op("pool", lambda e: e.memset(ones4[:], 1.0), writes=[setup_b])
        tr.op("dve", lambda e: e.tensor_scalar(out=gqk[:, 0:64], in0=prow[:, 0:64],
                                               scalar1=0.125, scalar2=None, op0=ALU.mult),
              reads=[const_b], writes=[setup_b])
        tr.op("dve", lambda e: e.tensor_copy(out=gqk[:, 64:128], in_=prow[:, 64:128]),
              reads=[const_b, setup_b], writes=[setup_b])
        tr.op("dve", lambda e: e.tensor_reduce(out=acst[:, 1:2], in_=prow[:, 0:64], axis=AX.X, op=ALU.max,
                                               apply_absolute_value=True),
              reads=[const_b, setup_b], writes=[setup_b])
        tr.op("dve", lambda e: e.tensor_reduce(out=acst[:, 2:3], in_=prow[:, 64:128], axis=AX.X, op=ALU.max,
                                               apply_absolute_value=True),
              reads=[const_b, setup_b], writes=[setup_b])
        tr.op("dve", lambda e: e.scalar_tensor_tensor(out=acst[:, 0:1], in0=acst[:, 1:2], scalar=-8.0,
                                                      in1=acst[:, 2:3], op0=ALU.mult, op1=ALU.mult),
              reads=[setup_b], writes=[setup_b])
        nlmax = acst[:, 0:1]
        tr.op("act", lambda e: e.activation(out=acst[:, 16:32], in_=prow[:, 128:144], func=AF.Exp, bias=nlmax),
              reads=[const_b, setup_b], writes=[setup_b])
        sinkexp = acst[:, 16:32]
        tr.op("dve", lambda e: e.tensor_scalar(out=gs[:, 28:29], in0=pgate[:, 1:2], scalar1=-1.0, scalar2=None,
                                               op0=ALU.mult),
              reads=[const_b], writes=[setup_b])
        nbf = gs[:, 28:29]
        bi = pgate[:, 0:1]

        GS_MBLK, GS_MC, GS_NMC, GS_MPREV, GS_DIF, GS_DEC = 0, 4, 8, 12, 17, 21

        def gates(ti, first):
            tp = ti % 2
            bi_, bf_, bt_, bd_ = gemm_bank(), gemm_bank(), gemm_bank(), gemm_bank()
            for (bank, c0) in ((bi_, 0), (bf_, 4)):
                for kc in range(8):
                    tr.op("pe", lambda e, bank=bank, c0=c0, kc=kc: e.matmul(
                        pbank[bank][0:4, :], lhsT=wgate[:, kc, c0:c0 + 4], rhs=hT[:, kc, :],
                        start=(kc == 0), stop=(kc == 7)),
                        reads=[wgate_b] + hT_b, writes=[pbank_b[bank]], inc=(kc == 7))
            ipre, fpre = pbank[bi_][0:4, :], pbank[bf_][0:4, :]
            g0, g1 = gr[:, 0, :], gr[:, 1, :]
            tr.op("act", lambda e: e.activation(out=g0, in_=fpre, func=AF.Exp, scale=-1.0, bias=nbf),
                  reads=[pbank_b[bf_], setup_b], writes=[gr_b[0]])
            tr.op("act", lambda e: e.activation(out=g0, in_=g0, func=AF.Ln, bias=1.0),
                  reads=[gr_b[0]], writes=[gr_b[0]])
            for j in range(NSUB):
                tr.op("dve", lambda e, j=j: e.tensor_tensor_scan(
                    out=g1[:, j * 128:(j + 1) * 128], data0=ones4[:, 0:128], data1=g0[:, j * 128:(j + 1) * 128],
                    initial=0.0, op0=ALU.mult, op1=ALU.add),
                    reads=[gr_b[0], setup_b], writes=[gr_b[1]])
            tr.op("dve", lambda e: e.scalar_tensor_tensor(out=g0, in0=ipre, scalar=bi, in1=g1, op0=ALU.add,
                                                          op1=ALU.add),
                  reads=[pbank_b[bi_], gr_b[1], const_b, gr_b[0]], writes=[gr_b[0]])
            tr.op("dve", lambda e: e.tensor_reduce(out=gs[:, GS_MBLK:GS_MBLK + 4],
                                                   in_=g0.rearrange("p (j t) -> p j t", j=4), axis=AX.X, op=ALU.max),
                  reads=[gr_b[0]], writes=[gs_b])
            if first:
                tr.op("dve", lambda e: e.memset(gs[:, GS_MPREV:GS_MPREV + 1], 0.0), reads=[gs_b], writes=[gs_b])
            else:
                tr.op("dve", lambda e: e.tensor_copy(out=gs[:, GS_MPREV:GS_MPREV + 1],
                                                     in_=gs[:, GS_MPREV + 4:GS_MPREV + 5]),
                      reads=[gs_b], writes=[gs_b])
            for j in range(NSUB):
                tr.op("dve", lambda e, j=j: e.tensor_tensor(out=gs[:, GS_MC + j:GS_MC + j + 1],
                                                            in0=gs[:, GS_MPREV + j:GS_MPREV + j + 1],
                                                            in1=gs[:, GS_MBLK + j:GS_MBLK + j + 1], op=ALU.max),
                      reads=[gs_b], writes=[gs_b])
                tr.op("dve", lambda e, j=j: e.tensor_tensor(out=gs[:, GS_MPREV + j + 1:GS_MPREV + j + 2],
                                                            in0=gs[:, GS_MC + j:GS_MC + j + 1],
                                                            in1=g1[:, j * 128 + 127:j * 128 + 128], op=ALU.subtract),
                      reads=[gs_b, gr_b[1]], writes=[gs_b])
            tr.op("dve", lambda e: e.tensor_tensor(out=gs[:, GS_DIF:GS_DIF + 4], in0=gs[:, GS_MPREV:GS_MPREV + 4],
                                                   in1=gs[:, GS_MC:GS_MC + 4], op=ALU.subtract),
                  reads=[gs_b], writes=[gs_b])
            tr.op("dve", lambda e: e.tensor_scalar(out=gs[:, GS_NMC:GS_NMC + 4], in0=gs[:, GS_MC:GS_MC + 4],
                                                   scalar1=-1.0, scalar2=None, op0=ALU.mult),
                  reads=[gs_b], writes=[gs_b])
            tr.op("act", lambda e: e.activation(out=gs[:, GS_DEC:GS_DEC + 4], in_=gs[:, GS_DIF:GS_DIF + 4],
                                                func=AF.Exp),
                  reads=[gs_b], writes=[gs_b])
            for j in range(NSUB):
                sl = slice(j * 128, (j + 1) * 128)
                tr.op("act", lambda e, j=j, sl=sl: e.activation(out=g1[:, sl], in_=g1[:, sl], func=AF.Exp,
                                                                bias=gs[:, GS_NMC + j:GS_NMC + j + 1]),
                      reads=[gs_b, gr_b[1]], writes=[gr_b[1]])
                tr.op("act", lambda e, j=j, sl=sl: e.activation(out=g0[:, sl], in_=g0[:, sl], func=AF.Exp,
                                                                bias=gs[:, GS_NMC + j:GS_NMC + j + 1]),
                      reads=[gs_b, gr_b[0]], writes=[gr_b[0]])
            for j in range(NSUB):
                sl = slice(j * 128, (j + 1) * 128)
                tr.op("pe", lambda e, j=j, sl=sl: e.matmul(pbank[bt_][:, j * 8:j * 8 + 4], lhsT=g0[:, sl],
                                                           rhs=ident_f[0:4, 0:4], start=True, stop=True),
                      reads=[gr_b[0], const_b], writes=[pbank_b[bt_]], inc=False)
                tr.op("pe", lambda e, j=j, sl=sl: e.matmul(pbank[bt_][:, j * 8 + 4:j * 8 + 8], lhsT=g1[:, sl],
                                                           rhs=ident_f[0:4, 0:4], start=True, stop=True),
                      reads=[gr_b[1], const_b], writes=[pbank_b[bt_]], inc=(j == NSUB - 1))
            tr.op("act", lambda e: e.activation(out=wt[tp][:].rearrange("p j c -> p (j c)"), in_=pbank[bt_][:, 0:32],
                                                func=AF.Copy),
                  reads=[pbank_b[bt_]], writes=[wt_b[tp]])
            tr.op("dve", lambda e: e.tensor_tensor(
                out=Rm[:, 0:16].rearrange("p (j h) -> p j h", j=4),
                in0=ident_f[0:4, 0:4].unsqueeze(1).broadcast_to([4, 4, 4]),
                in1=gs[:, GS_DEC:GS_DEC + 4].unsqueeze(2).broadcast_to([4, 4, 4]), op=ALU.mult),
                reads=[gs_b, const_b, Rm_b], writes=[Rm_b])
            tr.op("dve", lambda e: e.tensor_scalar(out=Rm[:, 16:32], in0=Rm[:, 0:16], scalar1=1.0 / 16.0,
                                                   scalar2=None, op0=ALU.mult),
                  reads=[Rm_b], writes=[Rm_b])
            tr.op("pe", lambda e: e.matmul(pbank[bd_][:, 0:32], lhsT=ones4[:, 0:128], rhs=Rm[:, 0:32],
                                           start=True, stop=True),
                  reads=[Rm_b, setup_b], writes=[pbank_b[bd_]])
            tr.op("act", lambda e: e.activation(out=decb[tp][:], in_=pbank[bd_][:, 0:32], func=AF.Copy),
                  reads=[pbank_b[bd_]], writes=[decb_b[tp]])
            dump(f"wt{ti}", wt[tp][:], [wt_b[tp]], [128, NSUB, 8])
            dump(f"decb{ti}", decb[tp][:], [decb_b[tp]], [128, 32])

        def mlstm_block(ti, s, first_block):
            tp = ti % 2
            par = s % 2
            sl = slice(s * 128, (s + 1) * 128)
            if first_block:
                tr.op("pool", lambda e: e.memset(Cst[:], 0.0), writes=Cst_b)
            pst = pbank[7].bitcast(BF16)
            for c in range(8):
                tr.op("pe", lambda e, c=c: e.transpose(out=pst[:, c * 128:(c + 1) * 128], in_=qkT[:, 8 + c, sl],
                                                       identity=ident_bf),
                      reads=[pg[8 + c], const_b], writes=[pbank_b[7]], inc=(c == 7))
            tr.op("act", lambda e: e.activation(out=ktok[par][:], in_=pst[:, :], func=AF.Copy),
                  reads=[pbank_b[7]], writes=[ktok_b[par]])
            e_ = est[par]
            hm, hm_b = hms[par], hms_b[par]
            for h in range(4):
                for dc in range(2):
                    tr.op("pe", lambda e, h=h, dc=dc: e.matmul(pbank[5][:, 0:128], lhsT=qkT[:, 8 + 2 * h + dc, sl],
                                                               rhs=qkT[:, 2 * h + dc, sl], start=(dc == 0),
                                                               stop=(dc == 1)),
                          reads=[pg[8 + 2 * h + dc], pg[2 * h + dc]], writes=[pbank_b[5]], inc=(dc == 1))
                tr.op("dve", lambda e, h=h: e.tensor_tensor(out=STm[par][:, h, :], in0=pbank[5][:, 0:128],
                                                            in1=mask16, op=ALU.mult),
                      reads=[pbank_b[5], const_b], writes=[STm_b[par][h]])
                tr.op("act", lambda e, h=h: e.activation(out=wvt[par][:, h, :], in_=vaug[:, s, h, :], func=AF.Copy,
                                                         scale=wt[tp][:, s, h:h + 1]),
                      reads=[vaug_b[s], wt_b[tp]], writes=[wvt_b[par][h]])
                tr.op("act", lambda e, h=h: e.activation(
                    out=Csb[:, h, :, :], in_=Cst[:, h, :, :], func=AF.Copy,
                    scale=decb[tp][:, 16 + s * 4 + h:16 + s * 4 + h + 1]),
                    reads=[Cst_b[h], decb_b[tp]], writes=[Csb_b[h]])
                num = pbank[6][:, 0:257]
                tr.op("pe", lambda e, h=h: e.matmul(num, lhsT=STm[par][:, h, :], rhs=wvt[par][:, h, :],
                                                    start=True, stop=False),
                      reads=[STm_b[par][h], wvt_b[par][h]], writes=[pbank_b[6]], inc=False)
                for dc in range(2):
                    tr.op("pe", lambda e, h=h, dc=dc: e.matmul(num, lhsT=qkT[:, 2 * h + dc, sl],
                                                               rhs=Csb[:, h, dc, :], start=False, stop=(dc == 1)),
                          reads=[pg[2 * h + dc], Csb_b[h]], writes=[pbank_b[6]], inc=(dc == 1))
                for dc, bank, c0 in ((0, 7, 0), (1, 5, 128)):
                    tr.op("pe", lambda e, h=h, dc=dc, bank=bank, c0=c0: e.matmul(
                        pbank[bank][:, c0:c0 + 257], lhsT=ktok[par][:, h * 256 + dc * 128:h * 256 + (dc + 1) * 128],
                        rhs=wvt[par][:, h, :], start=True, stop=True),
                        reads=[ktok_b[par], wvt_b[par][h]], writes=[pbank_b[bank]])
                    tr.op("dve", lambda e, h=h, dc=dc, bank=bank, c0=c0: e.scalar_tensor_tensor(
                        out=Cst[:, h, dc, :], in0=Cst[:, h, dc, :],
                        scalar=decb[tp][:, s * 4 + h:s * 4 + h + 1], in1=pbank[bank][:, c0:c0 + 257],
                        op0=ALU.mult, op1=ALU.add),
                        reads=[Cst_b[h], decb_b[tp], pbank_b[bank]], writes=[Cst_b[h]])
                tr.op("act", lambda e, h=h: e.activation(out=e_[:, 20 + h:21 + h], in_=pbank[6][:, 256:257],
                                                         func=AF.Abs),
                      reads=[pbank_b[6], est_b[par]], writes=[est_b[par]])
                tr.op("dve", lambda e, h=h: e.tensor_tensor(out=e_[:, h:h + 1], in0=e_[:, 20 + h:21 + h],
                                                            in1=wt[tp][:, s, 4 + h:5 + h], op=ALU.max),
                      reads=[wt_b[tp], est_b[par]], writes=[est_b[par]])
                tr.op("dve", lambda e, h=h: e.reciprocal(out=e_[:, 4 + h:5 + h], in_=e_[:, h:h + 1]),
                      reads=[est_b[par]], writes=[est_b[par]])
                tr.op("dve", lambda e, h=h: e.scalar_tensor_tensor(
                    out=hm[:, h * 256:(h + 1) * 256], in0=pbank[6][:, 0:256], scalar=e_[:, 4 + h:5 + h],
                    in1=sigo[:, s, h * 256:(h + 1) * 256], op0=ALU.mult, op1=ALU.mult),
                    reads=[pbank_b[6], est_b[par], pg[16 + 2 * s], pg[17 + 2 * s]], writes=[hm_b[h]])
                tr.op("act", lambda e, h=h: e.activation(out=junk[:, 0:256], in_=hm[:, h * 256:(h + 1) * 256],
                                                         func=AF.Square, accum_out=e_[:, 8 + h:9 + h]),
                      reads=[hm_b[h], est_b[par]], writes=[junk_b, est_b[par]])
            tr.op("act", lambda e: e.activation(out=e_[:, 12:16], in_=e_[:, 8:12], func=AF.Ln, scale=1.0 / 256.0,
                                                bias=EPS),
                  reads=[est_b[par]], writes=[est_b[par]])
            tr.op("act", lambda e: e.activation(out=e_[:, 16:20], in_=e_[:, 12:16], func=AF.Exp, scale=-0.5),
                  reads=[est_b[par]], writes=[est_b[par]])
            tr.op("dve", lambda e: e.tensor_tensor(
                out=ymtok[:].rearrange("p (h e) -> p h e", h=4), in0=hm[:].rearrange("p (h e) -> p h e", h=4),
                in1=e_[:, 16:20].unsqueeze(2).broadcast_to([128, 4, 256]), op=ALU.mult),
                reads=hm_b + [est_b[par]], writes=[ymtok_b])
            for c in range(8):
                tr.op("pe", lambda e, c=c: e.transpose(out=pst[:, c * 128:(c + 1) * 128],
                                                       in_=ymtok[:, c * 128:(c + 1) * 128], identity=ident_bf),
                      reads=[ymtok_b, const_b], writes=[pbank_b[7]], inc=(c == 7))
            tr.op("act", lambda e: e.activation(out=ymT[:, :, sl], in_=pst[:, :].rearrange("p (c t) -> p c t", c=8),
                                                func=AF.Copy),
                  reads=[pbank_b[7]], writes=[ymT_b[s]])

        def attn_block(ti, s, jb):
            par = jb % 2
            first = (jb == 0)
            QB, QB_b = QBs[par], QBs_b[par]
            ast, ast_b = asts[par], asts_b[par]
            src = aqkv[:, s, 0:1152]
            tr.op("act", lambda e: e.activation(out=sq[:], in_=src, func=AF.Square),
                  reads=[aqkv_b[s]], writes=[sq_b])
            tr.op("dve", lambda e: e.tensor_reduce(out=ast[:, 0:18], in_=sq[:].rearrange("p (h d) -> p h d", h=18),
                                                   axis=AX.X, op=ALU.add),
                  reads=[sq_b, ast_b], writes=[ast_b])
            tr.op("act", lambda e: e.activation(out=ast[:, 0:18], in_=ast[:, 0:18], func=AF.Ln, scale=1.0 / 64.0,
                                                bias=EPS),
                  reads=[ast_b], writes=[ast_b])
            tr.op("act", lambda e: e.activation(out=ast[:, 18:36], in_=ast[:, 0:18], func=AF.Exp, scale=-0.5),
                  reads=[ast_b], writes=[ast_b])
            qn3 = qn[:].rearrange("p (h d) -> p h d", h=18)
            rt = sq[:, 0:576].rearrange("p (a h d) -> p a h d", a=4, h=18)
            rt_b = sq_b
            tr.op("dve", lambda e: e.tensor_tensor(out=qn3, in0=src.rearrange("p (h d) -> p h d", h=18),
                                                   in1=ast[:, 18:36].unsqueeze(2).broadcast_to([128, 18, 64]),
                                                   op=ALU.mult),
                  reads=[aqkv_b[s], ast_b], writes=[qn_b])
            tr.op("dve", lambda e: e.tensor_tensor(out=qn3[:, 0:16, :], in0=qn3[:, 0:16, :],
                                                   in1=gqk[:, 0:64].unsqueeze(1).broadcast_to([128, 16, 64]),
                                                   op=ALU.mult),
                  reads=[qn_b, setup_b], writes=[qn_b])
            tr.op("dve", lambda e: e.tensor_tensor(out=qn3[:, 16:18, :], in0=qn3[:, 16:18, :],
                                                   in1=gqk[:, 64:128].unsqueeze(1).broadcast_to([128, 2, 64]),
                                                   op=ALU.mult),
                  reads=[qn_b, setup_b], writes=[qn_b])
            cosb = cf[:, CF_COS + jb * 8:CF_COS + jb * 8 + 8].unsqueeze(1).broadcast_to([128, 18, 8])
            sinb = cf[:, CF_SIN + jb * 8:CF_SIN + jb * 8 + 8].unsqueeze(1).broadcast_to([128, 18, 8])
            x1, x2 = qn3[:, :, 0:8], qn3[:, :, 8:16]
            tr.op("dve", lambda e: e.tensor_tensor(out=rt[:, 0, :, :], in0=x1, in1=cosb, op=ALU.mult),
                  reads=[qn_b, const_b], writes=[rt_b])
            tr.op("dve", lambda e: e.tensor_tensor(out=rt[:, 1, :, :], in0=x2, in1=sinb, op=ALU.mult),
                  reads=[qn_b, const_b, rt_b], writes=[rt_b])
            tr.op("dve", lambda e: e.tensor_tensor(out=rt[:, 2, :, :], in0=x2, in1=cosb, op=ALU.mult),
                  reads=[qn_b, const_b, rt_b], writes=[rt_b])
            tr.op("dve", lambda e: e.tensor_tensor(out=rt[:, 3, :, :], in0=x1, in1=sinb, op=ALU.mult),
                  reads=[qn_b, const_b, rt_b], writes=[rt_b])
            tr.op("dve", lambda e: e.tensor_tensor(out=qb[:, :, 0:8], in0=rt[:, 0, :, :], in1=rt[:, 1, :, :],
                                                   op=ALU.subtract),
                  reads=[rt_b], writes=[qb_b])
            tr.op("dve", lambda e: e.tensor_tensor(out=qb[:, :, 8:16], in0=rt[:, 2, :, :], in1=rt[:, 3, :, :],
                                                   op=ALU.add),
                  reads=[rt_b, qb_b], writes=[qb_b])
            tr.op("act", lambda e: e.activation(out=qb[:, :, 16:64], in_=qn3[:, :, 16:64], func=AF.Copy),
                  reads=[qn_b, qb_b], writes=[qb_b])
            tr.op("pool", lambda e: e.tensor_copy(
                out=k2[:].rearrange("p (g r d) -> p g r d", g=2, r=2),
                in_=qb[:, 16:18, :].unsqueeze(2).broadcast_to([128, 2, 2, 64])),
                reads=[qb_b], writes=[k2_b])
            tr.op("pool", lambda e: e.tensor_copy(
                out=vat[:, par, :, 0:64], in_=aqkv[:, s, 1152:1280].rearrange("p (g d) -> p g d", g=2)),
                reads=[aqkv_b[s]], writes=[vat_b[par]])
            pq = pbank[4].bitcast(BF16)
            for g in range(2):
                tr.op("pe", lambda e, g=g: e.transpose(out=pq[:, g * 128:(g + 1) * 128],
                                                       in_=k2[:, g * 128:(g + 1) * 128], identity=ident_bf),
                      reads=[k2_b, const_b], writes=[pbank_b[4]], inc=(g == 1))
            tr.op("act", lambda e: e.activation(out=kTd[:, par, :, :],
                                                in_=pq[:, 0:256].rearrange("p (g t) -> p g t", g=2), func=AF.Copy),
                  reads=[pbank_b[4]], writes=[kTd_b[par]])
            for c in range(8):
                tr.op("pe", lambda e, c=c: e.transpose(out=pq[:, c * 128:(c + 1) * 128],
                                                       in_=qb[:].rearrange("p h d -> p (h d)")[:, c * 128:(c + 1) * 128],
                                                       identity=ident_bf),
                      reads=[qb_b, const_b], writes=[pbank_b[4]], inc=(c == 7))
            pq3 = pq[:, :].rearrange("p (c t) -> p c t", c=8)
            tr.op("act", lambda e: e.activation(out=QB[0:64, :, 0:128], in_=pq3[0:64, :, :], func=AF.Copy),
                  reads=[pbank_b[4]], writes=[QB_b])
            tr.op("dve", lambda e: e.tensor_copy(out=QB[64:128, :, 128:256], in_=pq3[64:128, :, :]),
                  reads=[pbank_b[4], QB_b], writes=[QB_b])
            def po_ap(head):
                if head < 7:
                    return 2, pbank[2][:, head * 65:(head + 1) * 65]
                if head < 14:
                    return 3, pbank[3][:, (head - 7) * 65:(head - 6) * 65]
                return 4, pbank[4][:, (head - 14) * 65:(head - 13) * 65]
            def epilogue(groups):
                for (bank, h0, nh) in groups:
                    po3 = pbank[bank][:, 0:nh * 65].rearrange("p (h d) -> p h d", h=nh)
                    dsum = ast[:, 36 + h0:36 + h0 + nh]
                    rden = ast[:, 52 + h0:52 + h0 + nh]
                    tr.op("dve", lambda e, po3=po3, dsum=dsum, h0=h0, nh=nh: e.tensor_tensor(
                        out=dsum, in0=po3[:, :, 64], in1=sinkexp[:, h0:h0 + nh], op=ALU.add),
                        reads=[pbank_b[bank], setup_b, ast_b], writes=[ast_b])
                    tr.op("dve", lambda e, dsum=dsum, rden=rden: e.reciprocal(out=rden, in_=dsum),
                          reads=[ast_b], writes=[ast_b])
                    tr.op("dve", lambda e, po3=po3, rden=rden, h0=h0, nh=nh: e.tensor_tensor(
                        out=ya[:, h0 * 64:(h0 + nh) * 64].rearrange("p (h d) -> p h d", h=nh), in0=po3[:, :, 0:64],
                        in1=rden.unsqueeze(2).broadcast_to([128, nh, 64]), op=ALU.mult),
                        reads=[pbank_b[bank], ast_b], writes=[ya_b])

            for ci, c in enumerate((7, 0, 1, 2, 3, 4, 5, 6)):
                g = c // 4
                lb = ci % 2
                lg = pbank[lb]
                if not first:
                    tr.op("pe", lambda e, c=c, g=g, lg=lg: e.matmul(lg[:, 0:256], lhsT=kTd[:, 1 - par, g, :],
                                                                    rhs=QB[:, c, :], start=True, stop=False),
                          reads=[kTd_b[1 - par], QB_b], writes=[pbank_b[lb]], inc=False)
                    tr.op("pe", lambda e, lg=lg: e.matmul(lg[:, 0:256], lhsT=ident_bf, rhs=MBprev,
                                                          start=False, stop=True),
                          reads=[const_b], writes=[pbank_b[lb]], inc=False)
                tr.op("pe", lambda e, c=c, g=g, lg=lg: e.matmul(lg[:, 256:512], lhsT=kTd[:, par, g, :],
                                                                rhs=QB[:, c, :], start=True, stop=False),
                      reads=[kTd_b[par], QB_b], writes=[pbank_b[lb]], inc=False)
                tr.op("pe", lambda e, lg=lg: e.matmul(lg[:, 256:512], lhsT=ident_bf, rhs=MBcur,
                                                      start=False, stop=True),
                      reads=[const_b], writes=[pbank_b[lb]])
                lo = 256 if first else 0
                pk_ = ci % 2
                tr.op("act", lambda e, lg=lg, lo=lo, pk_=pk_: e.activation(out=pT[pk_][:, lo:512], in_=lg[:, lo:512],
                                                                           func=AF.Exp, bias=nlmax),
                      reads=[pbank_b[lb], setup_b], writes=[pT_b[pk_]])
                for hh in range(2):
                    head = 2 * c + hh
                    bank, po = po_ap(head)
                    srcs = [(256 + hh * 128, par)]
                    if not first:
                        srcs.append((hh * 128, 1 - par))
                    for i, (col, slot) in enumerate(srcs):
                        tr.op("pe", lambda e, pk_=pk_, col=col, slot=slot, g=g, po=po, i=i, n=len(srcs): e.matmul(
                            po, lhsT=pT[pk_][:, col:col + 128], rhs=vat[:, slot, g, :], start=(i == 0),
                            stop=(i == n - 1)),
                            reads=[pT_b[pk_], vat_b[slot]], writes=[pbank_b[bank]], inc=(i == len(srcs) - 1))
                if c == 7:
                    epilogue(((4, 14, 2),))
            epilogue(((2, 0, 7), (3, 7, 7)))
            sl = slice(s * 128, (s + 1) * 128)
            py = pbank[4].bitcast(BF16)
            for c in range(8):
                tr.op("pe", lambda e, c=c: e.transpose(out=py[:, c * 128:(c + 1) * 128],
                                                       in_=ya[:, c * 128:(c + 1) * 128], identity=ident_bf),
                      reads=[ya_b, const_b], writes=[pbank_b[4]], inc=(c == 7))
            tr.op("act", lambda e: e.activation(out=yaT[:, :, sl], in_=py[:, :].rearrange("p (c t) -> p c t", c=8),
                                                func=AF.Copy),
                  reads=[pbank_b[4]], writes=[yaT_b[s]])

        def merge(ti):
            for a in range(4):
                wv, wb = wload(f"mrg{a}")
                for jj in range(2):
                    j = 2 * a + jj
                    for (bank, seg, rhsT, rb) in ((0, 0, ymT, ymT_b), (1, 1, yaT, yaT_b), (2, 2, hT, hT_b),
                                                  (3, 3, hT, hT_b)):
                        for kc in range(8):
                            tr.op("pe", lambda e, bank=bank, seg=seg, rhsT=rhsT, kc=kc, wv=wv, jj=jj: e.matmul(
                                pbank[bank][:, :], lhsT=wv[:, seg * 2 + jj, kc, :], rhs=rhsT[:, kc, :],
                                start=(kc == 0), stop=(kc == 7)),
                                reads=[wb] + rb, writes=[pbank_b[bank]], inc=(kc == 7))
                    k = j % 2
                    tr.op("act", lambda e, j=j, k=k: e.activation(out=sgt[k][:], in_=pbank[2][:, :], func=AF.Sigmoid,
                                                                  bias=bmcol[:, j:j + 1]),
                          reads=[pbank_b[2], const_b], writes=[sgt_b[k]])
                    tr.op("act", lambda e, j=j, k=k: e.activation(out=sgt[2 + k][:], in_=pbank[3][:, :],
                                                                  func=AF.Sigmoid, bias=bmcol[:, 8 + j:9 + j]),
                          reads=[pbank_b[3], const_b], writes=[sgt_b[2 + k]])
                    tr.op("dve", lambda e, k=k: e.tensor_tensor(out=mt[0], in0=pbank[0][:, :], in1=sgt[k][:],
                                                                op=ALU.mult),
                          reads=[pbank_b[0], sgt_b[k]], writes=[mt_b[0]])
                    tr.op("dve", lambda e, k=k: e.tensor_tensor(out=mt[1], in0=pbank[1][:, :], in1=sgt[2 + k][:],
                                                                op=ALU.mult),
                          reads=[pbank_b[1], sgt_b[2 + k]], writes=[mt_b[1]])
                    tr.op("dve", lambda e, j=j: e.tensor_tensor(out=mgT[:, j, :], in0=mt[0], in1=mt[1],
                                                                 op=ALU.add),
                          reads=[mt_b[0], mt_b[1]], writes=[mgT_b[j]])
            dump(f"mgT{ti}", mgT[:], mgT_b, [128, 8, T], BF16)

        def outproj(ti):
            wv, wb = wload("wout")
            for s in range(NSUB):
                for n in range(2):
                    pb = gemm_bank()
                    for kc in range(8):
                        tr.op("pe", lambda e, pb=pb, kc=kc, s=s, n=n, wv=wv: e.matmul(
                            pbank[pb][:, :], lhsT=mgT[:, kc, s * 128:(s + 1) * 128], rhs=wv[:, n, kc, :],
                            start=(kc == 0), stop=(kc == 7)),
                            reads=[wb] + mgT_b, writes=[pbank_b[pb]], inc=(kc == 7))
                    tr.op("dve", lambda e, pb=pb, s=s, n=n: e.tensor_tensor(
                        out=x_sb[:, s, n * 512:(n + 1) * 512], in0=pbank[pb][:, :],
                        in1=x_sb[:, s, n * 512:(n + 1) * 512], op=ALU.add),
                        reads=[pbank_b[pb], x_b[s]], writes=[x_b[s]])
            dump(f"x1_{ti}", x_sb[:], x_b, [128, NSUB, D])
            for s in range(NSUB):
                norm_transpose(x_sb[:, s, :], x_b[s], ymT, ymT_b[s], s, ti * NSUB + s)

        ost_rr = [0]

        def ffn(ti):
            r0 = ti * T
            for a in range(6):
                wv, wb = wload(f"ffi{a}")
                js = list(range(4 * a, min(4 * a + 4, 22)))
                for idx, j in enumerate(js):
                    gb, ub = gemm_bank(), gemm_bank()
                    for (bank, seg) in ((gb, idx), (ub, len(js) + idx)):
                        for kc in range(8):
                            tr.op("pe", lambda e, bank=bank, seg=seg, kc=kc, wv=wv: e.matmul(
                                pbank[bank][:, :], lhsT=wv[:, seg, kc, :], rhs=ymT[:, kc, :],
                                start=(kc == 0), stop=(kc == 7)),
                                reads=[wb] + ymT_b, writes=[pbank_b[bank]], inc=(kc == 7))
                    k = j % 2
                    tr.op("act", lambda e, gb=gb, k=k: e.activation(out=sgt[k][:], in_=pbank[gb][:, :], func=AF.Silu),
                          reads=[pbank_b[gb]], writes=[sgt_b[k]])
                    tr.op("dve", lambda e, ub=ub, k=k, j=j: e.tensor_tensor(out=actT[:, j, :], in0=pbank[ub][:, :],
                                                                            in1=sgt[k][:], op=ALU.mult),
                          reads=[pbank_b[ub], sgt_b[k]], writes=[pg[j]])
            for n in range(2):
                for hlf in range(2):
                    wv, wb = wload(f"ffo{n}{hlf}")
                    for k in range(11):
                        kc = hlf * 11 + k
                        for s in range(NSUB):
                            tr.op("pe", lambda e, s=s, kc=kc, k=k, wv=wv, n=n: e.matmul(
                                pbank[s + 4 * n][:, :], lhsT=actT[:, kc, s * 128:(s + 1) * 128], rhs=wv[:, 0, k, :],
                                start=(kc == 0), stop=(kc == 21)),
                                reads=[wb, pg[kc]], writes=[pbank_b[s + 4 * n]], inc=(k == 10 and s == NSUB - 1))
                for s in range(NSUB):
                    o = ost_rr[0] % 2
                    ost_rr[0] += 1
                    tr.op("dve", lambda e, s=s, n=n, o=o: e.tensor_tensor(
                        out=ostage[o][:], in0=pbank[s + 4 * n][:, :], in1=x_sb[:, s, n * 512:(n + 1) * 512],
                        op=ALU.add),
                        reads=[pbank_b[s + 4 * n], x_b[s]], writes=[ostage_b[o]])
                    dst = out_d[r0 + s * 128:r0 + (s + 1) * 128, n * 512:(n + 1) * 512]
                    tr.dma("sp", lambda e, dst=dst, o=o: e.dma_start(out=dst, in_=ostage[o][:]), f"ost{o}",
                           reads=[ostage_b[o]], nbytes=262144)

        tr.op("pool", lambda e: e.memset(vaug[:, :, :, 256:257], 1.0), writes=vaug_b)

        tile_front(0)
        for ti in range(ntiles):
            first = (ti % 8 == 0)
            inproj_a(ti, first)
            gates(ti, first)
            inproj_b(ti)
            if stop_after == "inproj":
                continue
            for s in range(NSUB):
                mlstm_block(ti, s, first and s == 0)
                if stop_after != "mlstm":
                    attn_block(ti, s, (ti % 8) * NSUB + s)
            dump(f"ymT{ti}", ymT[:], ymT_b, [128, 8, T], BF16)
            dump(f"yaT{ti}", yaT[:], yaT_b, [128, 8, T], BF16)
            if stop_after in ("mlstm", "attn"):
                continue
            merge(ti)
            x_reload(ti)
            outproj(ti)
            if stop_after == "outproj":
                continue
            if ti + 1 < ntiles:
                tile_front(ti + 1)
            ffn(ti)
            if ti == 0:
                tr.op("pool", lambda e: e.memset(vaug[:, :, :, 256:257], 1.0), writes=vaug_b)

        tr.schedule(reorder=reorder, prio=prio)

        semnames = set(Tracker.ENG) | set(tr.dma_sems)
        sems = {n: es.enter_context(nc.semaphore("s_" + n)) for n in sorted(semnames)}
        block = es.enter_context(nc.Block())

        def replay(engname):
            def run(eng):
                for item in tr.q[engname]:
                    if item[0] == "wait":
                        eng.wait_ge(sems[item[1]], item[2])
                    else:
                        ins = None
                        for fn in item[1]:
                            ins = fn(eng)
                        ins.then_inc(sems[item[2]], item[3])
            return run

        block.tensor(replay("pe"))
        block.scalar(replay("act"))
        block.vector(replay("dve"))
        block.gpsimd(replay("pool"))
        block.sync(replay("sp"))
    stats = {e: len(tr.q[e]) for e in Tracker.ENG}
    stats['sim_end_us'] = getattr(tr, 'sim_end', 0.0) / 1e3
    stats['sbuf_left'] = sbuf_left
    return nc, dump_specs, stats


def _prep_inputs(inputs, ntiles=16, ncores=NCORES):
    f = np.float32
    x = np.ascontiguousarray(np.asarray(inputs["x"], dtype=f)).reshape(-1, D)
    cbf, cf = _host_consts()
    g1 = np.asarray(inputs["norm1_g"], f).reshape(8, 128).T
    g2 = np.asarray(inputs["norm2_g"], f).reshape(8, 128).T
    gm = np.asarray(inputs["m_norm_g"], f).reshape(8, 128).T
    convw = np.asarray(inputs["conv_w"], f).reshape(4, 16, 128).transpose(2, 1, 0).reshape(128, 64)
    convb = np.asarray(inputs["conv_b"], f).reshape(16, 128).T
    bmc = np.asarray(inputs["b_merge"], f).reshape(16, 128).T
    pcol = np.ascontiguousarray(np.concatenate([g1, g2, gm, convw, convb, bmc], axis=1))
    prow = np.ascontiguousarray(np.concatenate([np.asarray(inputs["q_norm_g"], f).reshape(-1),
                                                np.asarray(inputs["k_norm_g"], f).reshape(-1),
                                                np.asarray(inputs["sinks"], f).reshape(-1)])[None, :])
    pgate = np.ascontiguousarray(np.asarray(inputs["b_mgate"], f).reshape(2, 4).T)
    shared = {
        "w_in": np.ascontiguousarray(np.asarray(inputs["w_in"], f).reshape(D, N_IN)),
        "w_bm": np.ascontiguousarray(np.asarray(inputs["w_branch_m"], f).reshape(D, D)),
        "w_ba": np.ascontiguousarray(np.asarray(inputs["w_branch_a"], f).reshape(D, D)),
        "w_out": np.ascontiguousarray(np.asarray(inputs["w_out"], f).reshape(D, D)),
        "w_fi": np.ascontiguousarray(np.asarray(inputs["w_ffn_in"], f).reshape(D, 2 * DFF)),
        "w_fo": np.ascontiguousarray(np.asarray(inputs["w_ffn_out"], f).reshape(DFF, D)),
        "cbf": cbf, "cf": cf, "pcol": pcol, "prow": prow, "pgate": pgate,
    }
    in_maps = []
    per = TOK_CORE
    for c in range(ncores):
        m = dict(shared)
        m["x"] = x[c * per:c * per + ntiles * T]
        in_maps.append(m)
    return in_maps


_PROGRAM = None


def kernel(**inputs):
    global _PROGRAM
    if _PROGRAM is None:
        _PROGRAM = build_program(16)[0]
    in_maps = _prep_inputs(inputs)
    res = run_bass_kernel_spmd(_PROGRAM, in_maps, core_ids=list(range(NCORES)))
    out = np.concatenate([np.asarray(r["out"], dtype=np.float32) for r in res.results], axis=0)
    return out.reshape(16, SEQ, D)
```

```python
import numpy as np
import ml_dtypes
from contextlib import ExitStack
import concourse.bass as bass
import concourse.mybir as mybir
from concourse.bass_utils import run_bass_kernel_spmd

F32 = mybir.dt.float32
BF16 = mybir.dt.bfloat16
AF = mybir.ActivationFunctionType
ALU = mybir.AluOpType
AX = mybir.AxisListType

D = 1024
SEQ = 4096
NCORES = 8
TOK_CORE = 2 * SEQ
T = 512
NSUB = 4
DFF = 2816
N_IN = 7432
EPS = 1e-6
NEG = -30000.0
O_MQ, O_MK, O_MV, O_MO, O_MI, O_MF, O_AQ, O_AK, O_AV, O_GM, O_GA = (
    0, 1024, 2048, 3072, 4096, 4100, 4104, 5128, 5256, 5384, 6408)


class Buf:
    __slots__ = ("name", "w", "r", "excl")

    def __init__(self, name="", excl=False):
        self.name = name
        self.w = None
        self.r = []
        self.excl = excl


class Op:
    __slots__ = ("id", "eng", "fns", "preds", "dur", "sem", "nbytes", "tick", "fin")

    def __init__(self, id, eng, sem=None, nbytes=0):
        self.id = id
        self.eng = eng
        self.fns = []
        self.preds = set()
        self.dur = 0.0
        self.sem = sem
        self.nbytes = nbytes
        self.tick = 0
        self.fin = 0.0


def _est(eng, n):
    if eng == "pe":
        return 60.0 + 0.33 * max(n, 64)
    if eng == "act":
        return 200.0 + 0.85 * n
    if eng == "dve":
        return 180.0 + 1.05 * n
    if eng == "pool":
        return 300.0 + 3.0 * n
    return 60.0


class Tracker:
    ENG = ("pe", "act", "dve", "pool", "sp")

    def __init__(self):
        self.ops = []
        self.unit = None
        self.q = {e: [] for e in self.ENG}
        self.dma_sems = set()

    def _preds(self, reads, writes, self_id):
        p = set()
        for b in reads:
            if b.w is not None:
                p.add(b.w)
            if b.excl:
                p.update(b.r)
        for b in writes:
            if b.w is not None:
                p.add(b.w)
            p.update(b.r)
        p.discard(self_id)
        return p

    def _mark(self, oid, reads, writes):
        for b in writes:
            b.w = oid
            b.r = []
        for b in reads:
            if b.excl:
                b.w = oid
                b.r = []
            elif not b.r or b.r[-1] != oid:
                b.r.append(oid)

    class _Probe:
        def __init__(self):
            self.n = 512

        def __getattr__(self, name):
            def f(*args, **kw):
                out = kw.get("out", args[0] if args else None)
                try:
                    shp = out.shape
                    m = 1
                    for d in shp[1:]:
                        m *= int(d)
                    self.n = m
                except Exception:
                    pass
                return None
            return f

    def op(self, eng, fn, reads=(), writes=(), inc=True, n=None):
        if n is None:
            pr = Tracker._Probe()
            fn(pr)
            n = pr.n
        if eng == "pe" and self.unit is not None:
            o = self.unit
        else:
            o = Op(len(self.ops), eng)
            self.ops.append(o)
            if eng == "pe":
                self.unit = o
        o.fns.append(fn)
        o.dur += _est(eng, n)
        o.preds |= self._preds(reads, writes, o.id)
        self._mark(o.id, reads, writes)
        if eng == "pe" and inc:
            self.unit = None

    def dma(self, eng, fn, sem, reads=(), writes=(), nbytes=65536):
        assert self.unit is None
        o = Op(len(self.ops), eng, sem=sem, nbytes=nbytes)
        self.ops.append(o)
        self.dma_sems.add(sem)
        o.fns.append(fn)
        o.dur = 60.0
        o.preds |= self._preds(reads, writes, o.id)
        self._mark(o.id, reads, writes)

    def schedule(self, reorder=True, prio="order"):
        import heapq
        ops = self.ops
        n = len(ops)
        succs = [[] for _ in range(n)]
        indeg = [0] * n
        for o in ops:
            indeg[o.id] = len(o.preds)
            for p in o.preds:
                succs[p].append(o.id)
        order = {e: [] for e in self.ENG}
        if not reorder:
            for o in ops:
                order[o.eng].append(o)
        else:
            key = list(range(n))
            if prio == "cp":
                ind2 = list(indeg)
                topo = [i for i in range(n) if ind2[i] == 0]
                k = 0
                while k < len(topo):
                    for sid in succs[topo[k]]:
                        ind2[sid] -= 1
                        if ind2[sid] == 0:
                            topo.append(sid)
                    k += 1
                bl = [0.0] * n
                for i in reversed(topo):
                    m = 0.0
                    for sid in succs[i]:
                        if bl[sid] > m:
                            m = bl[sid]
                    d = ops[i].dur if ops[i].sem is None else 2000.0 + ops[i].nbytes / 300.0
                    bl[i] = m + d
                rank = sorted(range(n), key=lambda i: (-bl[i], i))
                for r, i in enumerate(rank):
                    key[i] = r
            inv = [0] * n
            for i in range(n):
                inv[key[i]] = i
            free_at = {e: 0.0 for e in self.ENG}
            pend = {e: [] for e in self.ENG}
            avail = {e: [] for e in self.ENG}
            ready_t = [0.0] * n
            for o in ops:
                if indeg[o.id] == 0:
                    heapq.heappush(pend[o.eng], (0.0, key[o.id]))
            dma_free = 0.0
            done = 0
            while done < n:
                best = None
                for e in self.ENG:
                    pe_, av = pend[e], avail[e]
                    while pe_ and pe_[0][0] <= free_at[e]:
                        heapq.heappush(av, heapq.heappop(pe_)[1])
                    if av:
                        cand = (free_at[e], av[0], e, True)
                    elif pe_:
                        cand = (pe_[0][0], pe_[0][1], e, False)
                    else:
                        continue
                    if best is None or cand[:2] < best[:2]:
                        best = cand
                start, okey, e, from_av = best
                if from_av:
                    heapq.heappop(avail[e])
                else:
                    heapq.heappop(pend[e])
                oid = inv[okey]
                o = ops[oid]
                if o.sem is not None:
                    free_at[e] = start + o.dur
                    t0 = max(start, dma_free)
                    dma_free = t0 + o.nbytes / 300.0
                    o.fin = dma_free + 2000.0
                else:
                    o.fin = start + o.dur
                    free_at[e] = o.fin
                order[e].append(o)
                done += 1
                for sid in succs[oid]:
                    so = ops[sid]
                    lat = 0.0 if so.eng == e else 150.0
                    if ready_t[sid] < o.fin + lat:
                        ready_t[sid] = o.fin + lat
                    indeg[sid] -= 1
                    if indeg[sid] == 0:
                        heapq.heappush(pend[so.eng], (ready_t[sid], key[sid]))
            self.sim_end = max(o.fin for o in ops)
        dma_cnt = {}
        for e in self.ENG:
            c = 0
            for o in order[e]:
                if o.sem is not None:
                    dma_cnt[o.sem] = dma_cnt.get(o.sem, 0) + 16
                    o.tick = dma_cnt[o.sem]
                else:
                    c += 1
                    o.tick = c
        for e in self.ENG:
            waited = {}
            q = self.q[e]
            for o in order[e]:
                need = {}
                for p in o.preds:
                    po = ops[p]
                    if po.eng == "pe" and e == "pe":
                        continue
                    k = po.sem if po.sem is not None else po.eng
                    if need.get(k, 0) < po.tick:
                        need[k] = po.tick
                for k, v in need.items():
                    if waited.get(k, 0) < v:
                        waited[k] = v
                        q.append(("wait", k, v))
                k = o.sem if o.sem is not None else o.eng
                q.append(("op", o.fns, k, 16 if o.sem is not None else 1))
            if e == "sp":
                for k, v in dma_cnt.items():
                    if waited.get(k, 0) < v:
                        waited[k] = v
                        q.append(("wait", k, v))


def _host_consts():
    bf = ml_dtypes.bfloat16
    p = np.arange(128)
    ident = np.eye(128, dtype=np.float32)
    s = p[:, None]
    t = p[None, :]
    mask16 = np.where(s <= t, 1.0 / 16.0, 0.0).astype(np.float32)
    mcur = np.where(s <= t, 0.0, NEG).astype(np.float32)
    mprev = np.where(s > t, 0.0, NEG).astype(np.float32)
    cbf = np.concatenate([ident, mask16, mprev, mprev, mcur, mcur], axis=1).astype(bf)
    half = 8
    inv_freq = (500000.0 ** (-np.arange(half, dtype=np.float32) * (2.0 / 16.0))).astype(np.float32)
    pos = (np.arange(32)[None, :] * 128 + p[:, None]).astype(np.float32)
    ang = pos[:, :, None] * inv_freq[None, None, :]
    cos = np.cos(ang).astype(np.float32).reshape(128, 256)
    sin = np.sin(ang).astype(np.float32).reshape(128, 256)
    cf = np.concatenate([ident[:, 0:4], cos, sin], axis=1).astype(np.float32)
    return cbf, cf


CB_ID, CB_M16, CB_MP, CB_MC = 0, 128, 256, 512
CF_ID, CF_COS, CF_SIN = 0, 4, 260


def _weight_items():
    items = []

    def add(name, src_segs, segw, kc0=0, nkc=8):
        items.append(dict(name=name, segs=src_segs, segw=segw, kc0=kc0, nkc=nkc))

    for a in range(2):
        add(f"ina{a}", [("w_in", a * 1024 + j * 128, "g1") for j in range(8)], 128)
    add("inb0", [("w_in", O_MV, "g1"), ("w_in", O_MV + 512, "g1")], 512)
    add("inb1", [("w_in", O_MO, "g1"), ("w_in", O_MO + 512, "g1")], 512)
    add("inb2", [("w_in", O_AQ, "g1"), ("w_in", O_AQ + 512, "g1")], 512)
    add("inb3", [("w_in", O_AK, "g1")], 256)
    for a in range(4):
        js = (2 * a, 2 * a + 1)
        segs = ([("w_bm", j * 128, "gm") for j in js] + [("w_ba", j * 128, None) for j in js] +
                [("w_in", O_GM + j * 128, "g1") for j in js] + [("w_in", O_GA + j * 128, "g1") for j in js])
        add(f"mrg{a}", segs, 128)
    add("wout", [("w_out", 0, None), ("w_out", 512, None)], 512)
    for a in range(6):
        js = list(range(4 * a, min(4 * a + 4, 22)))
        segs = [("w_fi", j * 128, "g2") for j in js] + [("w_fi", DFF + j * 128, "g2") for j in js]
        add(f"ffi{a}", segs, 128)
    for n in range(2):
        for hlf in range(2):
            add(f"ffo{n}{hlf}", [("w_fo", n * 512, None)], 512, kc0=hlf * 11, nkc=11)
    off = 0
    for it in items:
        it["size"] = len(it["segs"]) * it["nkc"] * it["segw"]
        it["off"] = off
        off += it["size"]
    return items, off


SLOT = 8192
NSLOT = 2


def build_program(ntiles=16, dumps=(), stop_after=None, reorder=True, prio="cp"):
    nc = bass.Bass("TRN2", target_bir_lowering=False)
    tr = Tracker()
    items, wtot = _weight_items()
    itmap = {it["name"]: it for it in items}
    dumps = set(dumps)
    dump_specs = {}

    ntok = ntiles * T
    x_d = nc.dram_tensor("x", [ntok, D], F32, kind="ExternalInput").ap()
    out_d = nc.dram_tensor("out", [ntok, D], F32, kind="ExternalOutput").ap()
    wsrc = {
        "w_in": nc.dram_tensor("w_in", [D, N_IN], F32, kind="ExternalInput").ap(),
        "w_bm": nc.dram_tensor("w_bm", [D, D], F32, kind="ExternalInput").ap(),
        "w_ba": nc.dram_tensor("w_ba", [D, D], F32, kind="ExternalInput").ap(),
        "w_out": nc.dram_tensor("w_out", [D, D], F32, kind="ExternalInput").ap(),
        "w_fi": nc.dram_tensor("w_fi", [D, 2 * DFF], F32, kind="ExternalInput").ap(),
        "w_fo": nc.dram_tensor("w_fo", [DFF, D], F32, kind="ExternalInput").ap(),
    }
    cbf_d = nc.dram_tensor("cbf", [128, 768], BF16, kind="ExternalInput").ap()
    cf_d = nc.dram_tensor("cf", [128, 516], F32, kind="ExternalInput").ap()
    pcol_d = nc.dram_tensor("pcol", [128, 24 + 64 + 16 + 16], F32, kind="ExternalInput").ap()
    prow_d = nc.dram_tensor("prow", [1, 144], F32, kind="ExternalInput").ap()
    pgate_d = nc.dram_tensor("pgate", [4, 2], F32, kind="ExternalInput").ap()
    wscr_d = nc.dram_tensor("wscr", [128, wtot], BF16, kind="Internal").ap()

    es = ExitStack()
    with es:
        def sb(name, shape, dt):
            return es.enter_context(nc.sbuf_tensor(name, shape, dt))

        def psum(name, shape, dt):
            return es.enter_context(nc.psum_tensor(name, shape, dt))

        x_sb = sb("x_sb", [128, NSUB, D], F32)
        x_b = [Buf(f"x{s}") for s in range(NSUB)]
        hT = sb("hT", [128, 8, T], BF16)
        hT_b = [Buf(f"hT{s}") for s in range(NSUB)]
        big = sb("big", [128, 12288], BF16)
        pg = [Buf(f"pg{i}") for i in range(24)]
        qkT = big[:, 0:8192].rearrange("p (c t) -> p c t", c=16)
        sigo = big[:, 8192:12288].rearrange("p (s f) -> p s f", s=4)
        actT = big[:, 0:11264].rearrange("p (c t) -> p c t", c=22)
        vaug = sb("vaug", [128, NSUB, 4, 257], BF16)
        vaug_b = [Buf(f"vaug{s}") for s in range(NSUB)]
        aqkv = sb("aqkv", [128, NSUB, 1280], BF16)
        aqkv_b = [Buf(f"aqkv{s}") for s in range(NSUB)]
        ymT = sb("ymT", [128, 8, T], BF16)
        ymT_b = [Buf(f"ymT{s}") for s in range(NSUB)]
        yaT = sb("yaT", [128, 8, T], BF16)
        yaT_b = [Buf(f"yaT{s}") for s in range(NSUB)]
        mgT = sb("mgT", [128, 8, T], BF16)
        mgT_b = [Buf(f"mgT{j}") for j in range(8)]
        wslot = [sb(f"wslot{i}", [128, SLOT], BF16) for i in range(NSLOT)]
        wslot_b = [Buf(f"wslot{i}") for i in range(NSLOT)]
        cbf = sb("cbf_sb", [128, 768], BF16)
        cf = sb("cf_sb", [128, 516], F32)
        const_b = Buf("const")
        pcol = sb("pcol_sb", [128, 120], F32)
        prow = sb("prow_sb", [128, 144], F32)
        pgate = sb("pgate_sb", [4, 2], F32)
        wgate = sb("wgate", [128, 8, 8], BF16)
        wgate_b = Buf("wgate")
        xn = [sb(f"xn{i}", [128, D], BF16) for i in range(2)]
        xn_b = [Buf(f"xn{i}") for i in range(2)]
        junk = sb("junk", [128, 256], BF16)
        junk_b = Buf("junk")
        stat = sb("stat", [128, 64], F32)
        cst = [sb(f"cst{i}", [128, 515], F32) for i in range(2)]
        cst_b = [Buf(f"cst{i}") for i in range(2)]
        cacc = [sb(f"cacc{i}", [128, 512], F32) for i in range(2)]
        cacc_b = [Buf(f"cacc{i}") for i in range(2)]
        carry = sb("carry", [128, 16, 3], F32)
        carry_b = [Buf(f"carry{j}") for j in range(16)]

        gr = sb("gr", [4, 2, T], F32)
        gr_b = [Buf("gr0"), Buf("gr1")]
        gs = sb("gs", [4, 32], F32)
        gs_b = Buf("gs")
        Rm = sb("Rm", [4, 32], F32)
        Rm_b = Buf("Rm")
        ones4 = sb("ones4", [4, 128], F32)
        wt = [sb(f"wt{i}", [128, NSUB, 8], F32) for i in range(2)]
        wt_b = [Buf(f"wt{i}") for i in range(2)]
        decb = [sb(f"decb{i}", [128, 32], F32) for i in range(2)]
        decb_b = [Buf(f"decb{i}") for i in range(2)]
        Cst = sb("Cst", [128, 4, 2, 257], F32)
        Cst_b = [Buf(f"Cst{h}") for h in range(4)]
        Csb = sb("Csb", [128, 4, 2, 257], BF16)
        Csb_b = [Buf(f"Csb{h}") for h in range(4)]
        STm = [sb(f"STm{i}", [128, 4, 128], BF16) for i in range(2)]
        STm_b = [[Buf(f"STm{i}_{h}") for h in range(4)] for i in range(2)]
        wvt = [sb(f"wvt{i}", [128, 4, 257], BF16) for i in range(2)]
        wvt_b = [[Buf(f"wvt{i}_{h}") for h in range(4)] for i in range(2)]
        ktok = [sb(f"ktok{i}", [128, D], BF16) for i in range(2)]
        ktok_b = [Buf(f"ktok{i}") for i in range(2)]
        hms = [sb(f"hm{i}", [128, D], BF16) for i in range(2)]
        hms_b = [[Buf(f"hm{i}_{h}") for h in range(4)] for i in range(2)]
        ymtoks = [sb(f"ymtok{i}", [128, D], BF16) for i in range(2)]
        ymtoks_b = [Buf(f"ymtok{i}") for i in range(2)]
        est = [sb(f"est{i}", [128, 24], F32) for i in range(2)]
        est_b = [Buf(f"est{i}") for i in range(2)]
        sq = sb("sq", [128, 1152], F32)
        sq_b = Buf("sq")
        qn = sb("qn", [128, 1152], F32)
        qn_b = Buf("qn")
        qb = sb("qb", [128, 18, 64], BF16)
        qb_b = Buf("qb")
        k2 = sb("k2", [128, 256], BF16)
        k2_b = Buf("k2")
        QBs = [sb(f"QB{i}", [128, 8, 256], BF16) for i in range(2)]
        QBs_b = [Buf(f"QB{i}") for i in range(2)]
        kTd = sb("kTd", [128, 2, 2, 128], BF16)
        kTd_b = [Buf("kTd0"), Buf("kTd1")]
        vat = sb("vat", [128, 2, 2, 65], BF16)
        vat_b = [Buf("vat0"), Buf("vat1")]
        pT = [sb(f"pT{i}", [128, 512], BF16) for i in range(2)]
        pT_b = [Buf(f"pT{i}") for i in range(2)]
        yas = [sb(f"ya{i}", [128, D], BF16) for i in range(2)]
        yas_b = [Buf(f"ya{i}") for i in range(2)]
        asts = [sb(f"ast{i}", [128, 68], F32) for i in range(2)]
        asts_b = [Buf(f"ast{i}") for i in range(2)]
        gqk = sb("gqk", [128, 128], F32)
        acst = sb("acst", [128, 32], F32)
        sgt = [sb(f"sgt{i}", [128, 512], BF16) for i in range(4)]
        sgt_b = [Buf(f"sgt{i}") for i in range(4)]
        mt = [sq[:, 0:512], sq[:, 512:1024]]
        mt_b = [sq_b, sq_b]

        sbuf_left = nc.sbuf_bytes_remaining
        pbank = [psum(f"pb{i}", [128, 512], F32) for i in range(8)]
        pbank_b = [Buf(f"pb{i}", excl=True) for i in range(8)]

        ident_f = cf[:, CF_ID:CF_ID + 4]
        MBprev = cbf[:, CB_MP:CB_MP + 256]
        MBcur = cbf[:, CB_MC:CB_MC + 256]
        ident_bf = cbf[:, CB_ID:CB_ID + 128]
        mask16 = cbf[:, CB_M16:CB_M16 + 128]

        g1col = pcol[:, 0:8]
        g2col = pcol[:, 8:16]
        gmcol = pcol[:, 16:24]
        convw = pcol[:, 24:88].rearrange("p (j k) -> p j k", j=16)
        convb = pcol[:, 88:104]
        bmcol = pcol[:, 104:120]
        fold = {"g1": g1col, "g2": g2col, "gm": gmcol, None: None}

        def dump(name, ap, bufs, shape, dt=F32):
            if name not in dumps:
                return
            d = nc.dram_tensor("dbg_" + name, list(shape), dt, kind="ExternalOutput").ap()
            dump_specs[name] = (list(shape), dt)
            tr.dma("sp", lambda e, d=d, ap=ap: e.dma_start(out=d, in_=ap), "dbg_" + name, reads=bufs)

        tr.dma("sp", lambda e: e.dma_start(out=cbf[:], in_=cbf_d), "cld", writes=[const_b])
        tr.dma("sp", lambda e: e.dma_start(out=cf[:], in_=cf_d), "cld", writes=[const_b])
        tr.dma("sp", lambda e: e.dma_start(out=pcol[:], in_=pcol_d), "cld", writes=[const_b])
        tr.dma("sp", lambda e: e.dma_start(out=prow[:], in_=prow_d.partition_broadcast(128)), "cld",
               writes=[const_b])
        tr.dma("sp", lambda e: e.dma_start(out=pgate[:], in_=pgate_d), "cld", writes=[const_b])

        wscr_b = {it["name"]: Buf("wscr_" + it["name"]) for it in items}
        cast_rr = [0]
        stage_rr = [0]

        def prep_cast(out_ap, in_ap, scale_ap, reads, writes):
            k = cast_rr[0] % 2
            cast_rr[0] += 1
            if k == 0:
                if scale_ap is None:
                    tr.op("act", lambda e: e.activation(out=out_ap, in_=in_ap, func=AF.Copy),
                          reads=reads, writes=writes)
                else:
                    tr.op("act", lambda e: e.activation(out=out_ap, in_=in_ap, func=AF.Copy, scale=scale_ap),
                          reads=reads + [const_b], writes=writes)
            else:
                eng = "dve" if k == 1 else "pool"
                if scale_ap is None:
                    tr.op(eng, lambda e: e.tensor_copy(out=out_ap, in_=in_ap), reads=reads, writes=writes)
                else:
                    tr.op(eng, lambda e: e.tensor_scalar(out=out_ap, in0=in_ap, scalar1=scale_ap, scalar2=None,
                                                         op0=ALU.mult),
                          reads=reads + [const_b], writes=writes)

        bigf = big.bitcast(F32)

        vaugf = vaug.reshape([128, NSUB * 4 * 257]).bitcast(F32)
        aqkvf = aqkv.reshape([128, NSUB * 1280]).bitcast(F32)
        vstage = [(vaugf[:, 0:1024], [vaug_b[0], vaug_b[1]]), (vaugf[:, 1028:2052], [vaug_b[2], vaug_b[3]]),
                  (aqkvf[:, 0:1024], [aqkv_b[0], aqkv_b[1]]), (aqkvf[:, 1280:2304], [aqkv_b[2], aqkv_b[3]])]

        def prep_item(it, slot, deep=False, vst=False):
            segs, segw, kc0, nkc = it["segs"], it["segw"], it["kc0"], it["nkc"]
            nseg = len(segs)
            view = wslot[slot][:, 0:it["size"]].rearrange("p (s k w) -> p s k w", s=nseg, k=nkc)
            groups = []
            for si, (src, c0, fd) in enumerate(segs):
                if groups and groups[-1][0] == src and groups[-1][3] == fd and \
                        groups[-1][1] + groups[-1][2] * segw == c0 and \
                        (groups[-1][2] + 1) * segw <= 1024 and groups[-1][4] + groups[-1][2] == si:
                    groups[-1][2] += 1
                else:
                    groups.append([src, c0, 1, fd, si])
            for k in range(nkc):
                kc = kc0 + k
                for (src, c0, n, fd, si0) in groups:
                    st = stage_rr[0] % (10 if deep else NSUB)
                    stage_rr[0] += 1
                    width = n * segw
                    src_ap = wsrc[src][kc * 128:(kc + 1) * 128, c0:c0 + width]
                    if vst:
                        stg, stg_bufs = vstage[st][0][:, 0:width], vstage[st][1]
                        st = 10 + st
                    elif st < NSUB:
                        stg = x_sb[:, st, 0:width]
                        stg_bufs = [x_b[st]]
                    else:
                        stg = bigf[:, (st - NSUB) * 1024:(st - NSUB) * 1024 + width]
                        stg_bufs = pg[4 * (st - NSUB):4 * (st - NSUB) + 4]
                    tr.dma("sp", lambda e, stg=stg, src_ap=src_ap: e.dma_start(out=stg, in_=src_ap),
                           f"xld{st}", writes=stg_bufs, nbytes=width * 512)
                    if n == 1:
                        out_ap = view[:, si0, k, :]
                        in_ap = stg
                    else:
                        out_ap = view[:, si0:si0 + n, k, :]
                        in_ap = stg.rearrange("p (s w) -> p s w", s=n)
                    sc = None if fd is None else fold[fd][:, kc:kc + 1]
                    prep_cast(out_ap, in_ap, sc, list(stg_bufs), [wslot_b[slot]])
            dst = wscr_d[:, it["off"]:it["off"] + it["size"]]
            tr.dma("sp", lambda e, dst=dst, slot=slot, it=it: e.dma_start(out=dst, in_=wslot[slot][:, 0:it["size"]]),
                   f"wst{slot}", reads=[wslot_b[slot]], writes=[wscr_b[it["name"]]], nbytes=it["size"] * 256)

        early_pending = {it["name"] for it in items}
        for kc in range(8):
            st = stage_rr[0] % NSUB
            stage_rr[0] += 1
            stg = x_sb[:, st, 0:8]
            src_ap = wsrc["w_in"][kc * 128:(kc + 1) * 128, O_MI:O_MI + 8]
            tr.dma("sp", lambda e, stg=stg, src_ap=src_ap: e.dma_start(out=stg, in_=src_ap), f"xld{st}",
                   writes=[x_b[st]])
            tr.op("dve", lambda e, kc=kc, stg=stg: e.tensor_scalar(out=wgate[:, kc, :], in0=stg,
                                                                  scalar1=g1col[:, kc:kc + 1], scalar2=None,
                                                                  op0=ALU.mult),
                  reads=[x_b[st], const_b], writes=[wgate_b])

        wcur = {"slot": 0}

        def wload(name):
            it = itmap[name]
            slot = wcur["slot"]
            wcur["slot"] = (slot + 1) % NSLOT
            if name in early_pending:
                early_pending.discard(name)
                prep_item(it, slot, vst=name.startswith(("wout", "ffi", "ffo")))
                view = wslot[slot][:, 0:it["size"]].rearrange("p (s k w) -> p s k w", s=len(it["segs"]),
                                                              k=it["nkc"])
                return view, wslot_b[slot]
            src = wscr_d[:, it["off"]:it["off"] + it["size"]]
            tr.dma("sp", lambda e, src=src, slot=slot, it=it: e.dma_start(out=wslot[slot][:, 0:it["size"]], in_=src),
                   f"wld{slot}", reads=[wscr_b[name]], writes=[wslot_b[slot]], nbytes=it["size"] * 256)
            view = wslot[slot][:, 0:it["size"]].rearrange("p (s k w) -> p s k w", s=len(it["segs"]), k=it["nkc"])
            return view, wslot_b[slot]

        gemm_rr = [0]

        def gemm_bank():
            b = gemm_rr[0] % 4
            gemm_rr[0] += 1
            return b

        xstg = [sq[:, 0:D], qn[:, 0:D]]
        xstg_b = [sq_b, qn_b]

        def tile_front(ti):
            r0 = ti * T
            for s in range(NSUB):
                k = s % 2
                src = x_d[r0 + s * 128:r0 + (s + 1) * 128, :]
                tr.dma("sp", lambda e, k=k, src=src: e.dma_start(out=xstg[k], in_=src), f"xsg{k}",
                       writes=[xstg_b[k]], nbytes=524288)
                norm_transpose(xstg[k], xstg_b[k], hT, hT_b[s], s, ti * NSUB + s)
            dump(f"hT{ti}", hT[:], hT_b, [128, 8, T], BF16)

        def x_reload(ti):
            r0 = ti * T
            for s in range(NSUB):
                src = x_d[r0 + s * 128:r0 + (s + 1) * 128, :]
                tr.dma("sp", lambda e, s=s, src=src: e.dma_start(out=x_sb[:, s, :], in_=src), f"xld{s}",
                       writes=[x_b[s]], nbytes=524288)

        def norm_transpose(src, src_b, dstT, dst_b, s, uid):
            c = (uid % 8) * 4
            ss, lnv, rstd = stat[:, c:c + 1], stat[:, c + 1:c + 2], stat[:, c + 2:c + 3]
            sbuf_ = Buf()
            k = uid % 2
            tr.op("act", lambda e: e.activation(out=xn[k][:], in_=src, func=AF.Square, accum_out=ss),
                  reads=[src_b], writes=[xn_b[k], sbuf_])
            tr.op("act", lambda e: e.activation(out=lnv, in_=ss, func=AF.Ln, scale=1.0 / D, bias=EPS),
                  reads=[sbuf_], writes=[sbuf_])
            tr.op("act", lambda e: e.activation(out=rstd, in_=lnv, func=AF.Exp, scale=-0.5),
                  reads=[sbuf_], writes=[sbuf_])
            tr.op("act", lambda e: e.activation(out=xn[k][:], in_=src, func=AF.Copy, scale=rstd),
                  reads=[src_b, sbuf_], writes=[xn_b[k]])
            pb = 4 + (uid % 2)
            pst = pbank[pb].bitcast(BF16)
            for kc in range(8):
                tr.op("pe", lambda e, kc=kc: e.transpose(out=pst[:, kc * 128:(kc + 1) * 128],
                                                         in_=xn[k][:, kc * 128:(kc + 1) * 128], identity=ident_bf),
                      reads=[xn_b[k], const_b], writes=[pbank_b[pb]], inc=(kc == 7))
            tr.op("act", lambda e: e.activation(out=dstT[:, :, s * 128:(s + 1) * 128],
                                                in_=pst[:, :].rearrange("p (c t) -> p c t", c=8), func=AF.Copy),
                  reads=[pbank_b[pb]], writes=[dst_b])

        def inproj_a(ti, first):
            for a in range(2):
                wv, wb = wload(f"ina{a}")
                for jj in range(8):
                    j = a * 8 + jj
                    pb = gemm_bank()
                    for kc in range(8):
                        tr.op("pe", lambda e, jj=jj, kc=kc, pb=pb, wv=wv: e.matmul(
                            pbank[pb][:, :], lhsT=wv[:, jj, kc, :], rhs=hT[:, kc, :], start=(kc == 0), stop=(kc == 7)),
                            reads=[wb] + hT_b, writes=[pbank_b[pb]], inc=(kc == 7))
                    k = j % 2
                    if first:
                        tr.op("pool", lambda e, k=k: e.memset(cst[k][:, 0:3], 0.0), writes=[cst_b[k]])
                    else:
                        tr.op("pool", lambda e, k=k, j=j: e.tensor_copy(out=cst[k][:, 0:3], in_=carry[:, j, :]),
                              reads=[carry_b[j]], writes=[cst_b[k]])
                    tr.op("act", lambda e, k=k, pb=pb: e.activation(out=cst[k][:, 3:515], in_=pbank[pb][:, :],
                                                                    func=AF.Copy),
                          reads=[pbank_b[pb]], writes=[cst_b[k]])
                    tr.op("pool", lambda e, k=k, j=j: e.tensor_copy(out=carry[:, j, :], in_=cst[k][:, 512:515]),
                          reads=[cst_b[k]], writes=[carry_b[j]])
                    tr.op("act", lambda e, k=k, j=j, pb=pb: e.activation(out=cacc[k][:, 3:512],
                                                                         in_=pbank[pb][:, 0:509], func=AF.Copy,
                                                                         scale=convw[:, j, 0:1]),
                          reads=[pbank_b[pb], const_b], writes=[cacc_b[k]])
                    tr.op("pool", lambda e, k=k, j=j: e.tensor_scalar(out=cacc[k][:, 0:3], in0=cst[k][:, 0:3],
                                                                       scalar1=convw[:, j, 0:1], scalar2=None,
                                                                       op0=ALU.mult),
                          reads=[cst_b[k], const_b, cacc_b[k]], writes=[cacc_b[k]])
                    for tap in range(1, 4):
                        tr.op("dve", lambda e, k=k, j=j, tap=tap: e.scalar_tensor_tensor(
                            out=cacc[k][:], in0=cst[k][:, tap:tap + 512], scalar=convw[:, j, tap:tap + 1],
                            in1=cacc[k][:], op0=ALU.mult, op1=ALU.add),
                            reads=[cst_b[k], cacc_b[k], const_b], writes=[cacc_b[k]])
                    tr.op("act", lambda e, k=k, j=j: e.activation(out=qkT[:, j, :], in_=cacc[k][:], func=AF.Silu,
                                                                   bias=convb[:, j:j + 1]),
                          reads=[cacc_b[k], const_b], writes=[pg[j]])
            dump(f"qkT{ti}", qkT, pg[0:16], [128, 16, T], BF16)

        def inproj_b(ti):
            plan = [("inb0", [("v", 0), ("v", 2)]), ("inb1", [("o", 0), ("o", 512)]),
                    ("inb2", [("q", 0), ("q", 512)]), ("inb3", [("kv", 0)])]
            for name, segl in plan:
                wv, wb = wload(name)
                segw = itmap[name]["segw"]
                for si, (kind, arg) in enumerate(segl):
                    for s in range(NSUB):
                        pb = gemm_bank()
                        for kc in range(8):
                            tr.op("pe", lambda e, si=si, kc=kc, pb=pb, wv=wv, s=s, segw=segw: e.matmul(
                                pbank[pb][:, 0:segw], lhsT=hT[:, kc, s * 128:(s + 1) * 128], rhs=wv[:, si, kc, :],
                                start=(kc == 0), stop=(kc == 7)),
                                reads=[wb, hT_b[s]], writes=[pbank_b[pb]], inc=(kc == 7))
                        if kind == "v":
                            tr.op("act", lambda e, pb=pb, s=s, arg=arg: e.activation(
                                out=vaug[:, s, arg:arg + 2, 0:256],
                                in_=pbank[pb][:, :].rearrange("p (h e) -> p h e", h=2), func=AF.Copy),
                                reads=[pbank_b[pb]], writes=[vaug_b[s]])
                        elif kind == "o":
                            tr.op("act", lambda e, pb=pb, s=s, arg=arg: e.activation(
                                out=sigo[:, s, arg:arg + 512], in_=pbank[pb][:, :], func=AF.Sigmoid),
                                reads=[pbank_b[pb]], writes=[pg[16 + 2 * s], pg[17 + 2 * s]])
                        elif kind == "q":
                            tr.op("dve", lambda e, pb=pb, s=s, arg=arg: e.tensor_copy(
                                out=aqkv[:, s, arg:arg + 512], in_=pbank[pb][:, :]),
                                reads=[pbank_b[pb]], writes=[aqkv_b[s]])
                        else:
                            tr.op("dve", lambda e, pb=pb, s=s: e.tensor_copy(
                                out=aqkv[:, s, 1024:1280], in_=pbank[pb][:, 0:256]),
                                reads=[pbank_b[pb]], writes=[aqkv_b[s]])
            dump(f"vaug{ti}", vaug[:], vaug_b, [128, NSUB, 4, 257], BF16)
            dump(f"sigo{ti}", sigo, pg[16:24], [128, NSUB, 1024], BF16)
            dump(f"aqkv{ti}", aqkv[:], aqkv_b, [128, NSUB, 1280], BF16)

        setup_b = Buf("setup")
        for i in range(2):
            tr.op("pool", lambda e, i=i: e.memset(QBs[i][:], 0.0), writes=[QBs_b[i]])
        tr.op("pool", lambda e: e.memset(vat[:, :, :, 64:65], 1.0), writes=vat_b)
        tr.op("pool", lambda e: e.memset(ones4[:], 1.0), writes=[setup_b])
        tr.op("dve", lambda e: e.tensor_scalar(out=gqk[:, 0:64], in0=prow[:, 0:64],
                                               scalar1=0.125, scalar2=None, op0=ALU.mult),
              reads=[const_b], writes=[setup_b])
        tr.op("dve", lambda e: e.tensor_copy(out=gqk[:, 64:128], in_=prow[:, 64:128]),
              reads=[const_b, setup_b], writes=[setup_b])
        tr.op("dve", lambda e: e.tensor_reduce(out=acst[:, 1:2], in_=prow[:, 0:64], axis=AX.X, op=ALU.max,
                                               apply_absolute_value=True),
              reads=[const_b, setup_b], writes=[setup_b])
        tr.op("dve", lambda e: e.tensor_reduce(out=acst[:, 2:3], in_=prow[:, 64:128], axis=AX.X, op=ALU.max,
                                               apply_absolute_value=True),
              reads=[const_b, setup_b], writes=[setup_b])
        tr.op("dve", lambda e: e.scalar_tensor_tensor(out=acst[:, 0:1], in0=acst[:, 1:2], scalar=-8.0,
                                                      in1=acst[:, 2:3], op0=ALU.mult, op1=ALU.mult),
              reads=[setup_b], writes=[setup_b])
        nlmax = acst[:, 0:1]
        tr.op("act", lambda e: e.activation(out=acst[:, 16:32], in_=prow[:, 128:144], func=AF.Exp, bias=nlmax),
              reads=[const_b, setup_b], writes=[setup_b])
        sinkexp = acst[:, 16:32]
        tr.op("dve", lambda e: e.tensor_scalar(out=gs[:, 28:29], in0=pgate[:, 1:2], scalar1=-1.0, scalar2=None,
                                               op0=ALU.mult),
              reads=[const_b], writes=[setup_b])
        nbf = gs[:, 28:29]
        bi = pgate[:, 0:1]

        GS_MBLK, GS_MC, GS_NMC, GS_MPREV, GS_DIF, GS_DEC = 0, 4, 8, 12, 17, 21

        def gates(ti, first):
            tp = ti % 2
            bi_, bf_, bt_, bd_ = gemm_bank(), gemm_bank(), gemm_bank(), gemm_bank()
            for (bank, c0) in ((bi_, 0), (bf_, 4)):
                for kc in range(8):
                    tr.op("pe", lambda e, bank=bank, c0=c0, kc=kc: e.matmul(
                        pbank[bank][0:4, :], lhsT=wgate[:, kc, c0:c0 + 4], rhs=hT[:, kc, :],
                        start=(kc == 0), stop=(kc == 7)),
                        reads=[wgate_b] + hT_b, writes=[pbank_b[bank]], inc=(kc == 7))
            ipre, fpre = pbank[bi_][0:4, :], pbank[bf_][0:4, :]
            g0, g1 = gr[:, 0, :], gr[:, 1, :]
            tr.op("act", lambda e: e.activation(out=g0, in_=fpre, func=AF.Exp, scale=-1.0, bias=nbf),
                  reads=[pbank_b[bf_], setup_b], writes=[gr_b[0]])
            tr.op("act", lambda e: e.activation(out=g0, in_=g0, func=AF.Ln, bias=1.0),
                  reads=[gr_b[0]], writes=[gr_b[0]])
            for j in range(NSUB):
                tr.op("dve", lambda e, j=j: e.tensor_tensor_scan(
                    out=g1[:, j * 128:(j + 1) * 128], data0=ones4[:, 0:128], data1=g0[:, j * 128:(j + 1) * 128],
                    initial=0.0, op0=ALU.mult, op1=ALU.add),
                    reads=[gr_b[0], setup_b], writes=[gr_b[1]])
            tr.op("dve", lambda e: e.scalar_tensor_tensor(out=g0, in0=ipre, scalar=bi, in1=g1, op0=ALU.add,
                                                          op1=ALU.add),
                  reads=[pbank_b[bi_], gr_b[1], const_b, gr_b[0]], writes=[gr_b[0]])
            tr.op("dve", lambda e: e.tensor_reduce(out=gs[:, GS_MBLK:GS_MBLK + 4],
                                                   in_=g0.rearrange("p (j t) -> p j t", j=4), axis=AX.X, op=ALU.max),
                  reads=[gr_b[0]], writes=[gs_b])
            if first:
                tr.op("dve", lambda e: e.memset(gs[:, GS_MPREV:GS_MPREV + 1], 0.0), reads=[gs_b], writes=[gs_b])
            else:
                tr.op("dve", lambda e: e.tensor_copy(out=gs[:, GS_MPREV:GS_MPREV + 1],
                                                     in_=gs[:, GS_MPREV + 4:GS_MPREV + 5]),
                      reads=[gs_b], writes=[gs_b])
            for j in range(NSUB):
                tr.op("dve", lambda e, j=j: e.tensor_tensor(out=gs[:, GS_MC + j:GS_MC + j + 1],
                                                            in0=gs[:, GS_MPREV + j:GS_MPREV + j + 1],
                                                            in1=gs[:, GS_MBLK + j:GS_MBLK + j + 1], op=ALU.max),
                      reads=[gs_b], writes=[gs_b])
                tr.op("dve", lambda e, j=j: e.tensor_tensor(out=gs[:, GS_MPREV + j + 1:GS_MPREV + j + 2],
                                                            in0=gs[:, GS_MC + j:GS_MC + j + 1],
                                                            in1=g1[:, j * 128 + 127:j * 128 + 128], op=ALU.subtract),
                      reads=[gs_b, gr_b[1]], writes=[gs_b])
            tr.op("dve", lambda e: e.tensor_tensor(out=gs[:, GS_DIF:GS_DIF + 4], in0=gs[:, GS_MPREV:GS_MPREV + 4],
                                                   in1=gs[:, GS_MC:GS_MC + 4], op=ALU.subtract),
                  reads=[gs_b], writes=[gs_b])
            tr.op("dve", lambda e: e.tensor_scalar(out=gs[:, GS_NMC:GS_NMC + 4], in0=gs[:, GS_MC:GS_MC + 4],
                                                   scalar1=-1.0, scalar2=None, op0=ALU.mult),
                  reads=[gs_b], writes=[gs_b])
            tr.op("act", lambda e: e.activation(out=gs[:, GS_DEC:GS_DEC + 4], in_=gs[:, GS_DIF:GS_DIF + 4],
                                                func=AF.Exp),
                  reads=[gs_b], writes=[gs_b])
            for j in range(NSUB):
                sl = slice(j * 128, (j + 1) * 128)
                tr.op("act", lambda e, j=j, sl=sl: e.activation(out=g1[:, sl], in_=g1[:, sl], func=AF.Exp,
                                                                bias=gs[:, GS_NMC + j:GS_NMC + j + 1]),
                      reads=[gs_b, gr_b[1]], writes=[gr_b[1]])
                tr.op("act", lambda e, j=j, sl=sl: e.activation(out=g0[:, sl], in_=g0[:, sl], func=AF.Exp,
                                                                bias=gs[:, GS_NMC + j:GS_NMC + j + 1]),
                      reads=[gs_b, gr_b[0]], writes=[gr_b[0]])
            for j in range(NSUB):
                sl = slice(j * 128, (j + 1) * 128)
                tr.op("pe", lambda e, j=j, sl=sl: e.matmul(pbank[bt_][:, j * 8:j * 8 + 4], lhsT=g0[:, sl],
                                                           rhs=ident_f[0:4, 0:4], start=True, stop=True),
                      reads=[gr_b[0], const_b], writes=[pbank_b[bt_]], inc=False)
                tr.op("pe", lambda e, j=j, sl=sl: e.matmul(pbank[bt_][:, j * 8 + 4:j * 8 + 8], lhsT=g1[:, sl],
                                                           rhs=ident_f[0:4, 0:4], start=True, stop=True),
                      reads=[gr_b[1], const_b], writes=[pbank_b[bt_]], inc=(j == NSUB - 1))
            tr.op("act", lambda e: e.activation(out=wt[tp][:].rearrange("p j c -> p (j c)"), in_=pbank[bt_][:, 0:32],
                                                func=AF.Copy),
                  reads=[pbank_b[bt_]], writes=[wt_b[tp]])
            tr.op("dve", lambda e: e.tensor_tensor(
                out=Rm[:, 0:16].rearrange("p (j h) -> p j h", j=4),
                in0=ident_f[0:4, 0:4].unsqueeze(1).broadcast_to([4, 4, 4]),
                in1=gs[:, GS_DEC:GS_DEC + 4].unsqueeze(2).broadcast_to([4, 4, 4]), op=ALU.mult),
                reads=[gs_b, const_b, Rm_b], writes=[Rm_b])
            tr.op("dve", lambda e: e.tensor_scalar(out=Rm[:, 16:32], in0=Rm[:, 0:16], scalar1=1.0 / 16.0,
                                                   scalar2=None, op0=ALU.mult),
                  reads=[Rm_b], writes=[Rm_b])
            tr.op("pe", lambda e: e.matmul(pbank[bd_][:, 0:32], lhsT=ones4[:, 0:128], rhs=Rm[:, 0:32],
                                           start=True, stop=True),
                  reads=[Rm_b, setup_b], writes=[pbank_b[bd_]])
            tr.op("act", lambda e: e.activation(out=decb[tp][:], in_=pbank[bd_][:, 0:32], func=AF.Copy),
                  reads=[pbank_b[bd_]], writes=[decb_b[tp]])
            dump(f"wt{ti}", wt[tp][:], [wt_b[tp]], [128, NSUB, 8])
            dump(f"decb{ti}", decb[tp][:], [decb_b[tp]], [128, 32])

        def mlstm_block(ti, s, first_block):
            tp = ti % 2
            par = s % 2
            sl = slice(s * 128, (s + 1) * 128)
            if first_block:
                tr.op("pool", lambda e: e.memset(Cst[:], 0.0), writes=Cst_b)
            pst = pbank[7].bitcast(BF16)
            for c in range(8):
                tr.op("pe", lambda e, c=c: e.transpose(out=pst[:, c * 128:(c + 1) * 128], in_=qkT[:, 8 + c, sl],
                                                       identity=ident_bf),
                      reads=[pg[8 + c], const_b], writes=[pbank_b[7]], inc=(c == 7))
            tr.op("act", lambda e: e.activation(out=ktok[par][:], in_=pst[:, :], func=AF.Copy),
                  reads=[pbank_b[7]], writes=[ktok_b[par]])
            e_ = est[par]
            hm, hm_b = hms[par], hms_b[par]
            ymtok, ymtok_b = ymtoks[par], ymtoks_b[par]
            for h in range(4):
                for dc in range(2):
                    tr.op("pe", lambda e, h=h, dc=dc: e.matmul(pbank[5][:, 0:128], lhsT=qkT[:, 8 + 2 * h + dc, sl],
                                                               rhs=qkT[:, 2 * h + dc, sl], start=(dc == 0),
                                                               stop=(dc == 1)),
                          reads=[pg[8 + 2 * h + dc], pg[2 * h + dc]], writes=[pbank_b[5]], inc=(dc == 1))
                tr.op("dve", lambda e, h=h: e.tensor_tensor(out=STm[par][:, h, :], in0=pbank[5][:, 0:128],
                                                            in1=mask16, op=ALU.mult),
                      reads=[pbank_b[5], const_b], writes=[STm_b[par][h]])
                tr.op("act", lambda e, h=h: e.activation(out=wvt[par][:, h, :], in_=vaug[:, s, h, :], func=AF.Copy,
                                                         scale=wt[tp][:, s, h:h + 1]),
                      reads=[vaug_b[s], wt_b[tp]], writes=[wvt_b[par][h]])
                tr.op("act", lambda e, h=h: e.activation(
                    out=Csb[:, h, :, :], in_=Cst[:, h, :, :], func=AF.Copy,
                    scale=decb[tp][:, 16 + s * 4 + h:16 + s * 4 + h + 1]),
                    reads=[Cst_b[h], decb_b[tp]], writes=[Csb_b[h]])
                num = pbank[6][:, 0:257]
                tr.op("pe", lambda e, h=h: e.matmul(num, lhsT=STm[par][:, h, :], rhs=wvt[par][:, h, :],
                                                    start=True, stop=False),
                      reads=[STm_b[par][h], wvt_b[par][h]], writes=[pbank_b[6]], inc=False)
                for dc in range(2):
                    tr.op("pe", lambda e, h=h, dc=dc: e.matmul(num, lhsT=qkT[:, 2 * h + dc, sl],
                                                               rhs=Csb[:, h, dc, :], start=False, stop=(dc == 1)),
                          reads=[pg[2 * h + dc], Csb_b[h]], writes=[pbank_b[6]], inc=(dc == 1))
                for dc, bank, c0 in ((0, 7, 0), (1, 5, 128)):
                    tr.op("pe", lambda e, h=h, dc=dc, bank=bank, c0=c0: e.matmul(
                        pbank[bank][:, c0:c0 + 257], lhsT=ktok[par][:, h * 256 + dc * 128:h * 256 + (dc + 1) * 128],
                        rhs=wvt[par][:, h, :], start=True, stop=True),
                        reads=[ktok_b[par], wvt_b[par][h]], writes=[pbank_b[bank]])
                    tr.op("dve", lambda e, h=h, dc=dc, bank=bank, c0=c0: e.scalar_tensor_tensor(
                        out=Cst[:, h, dc, :], in0=Cst[:, h, dc, :],
                        scalar=decb[tp][:, s * 4 + h:s * 4 + h + 1], in1=pbank[bank][:, c0:c0 + 257],
                        op0=ALU.mult, op1=ALU.add),
                        reads=[Cst_b[h], decb_b[tp], pbank_b[bank]], writes=[Cst_b[h]])
                tr.op("act", lambda e, h=h: e.activation(out=e_[:, 20 + h:21 + h], in_=pbank[6][:, 256:257],
                                                         func=AF.Abs),
                      reads=[pbank_b[6], est_b[par]], writes=[est_b[par]])
                tr.op("dve", lambda e, h=h: e.tensor_tensor(out=e_[:, h:h + 1], in0=e_[:, 20 + h:21 + h],
                                                            in1=wt[tp][:, s, 4 + h:5 + h], op=ALU.max),
                      reads=[wt_b[tp], est_b[par]], writes=[est_b[par]])
                tr.op("dve", lambda e, h=h: e.reciprocal(out=e_[:, 4 + h:5 + h], in_=e_[:, h:h + 1]),
                      reads=[est_b[par]], writes=[est_b[par]])
                tr.op("dve", lambda e, h=h: e.scalar_tensor_tensor(
                    out=hm[:, h * 256:(h + 1) * 256], in0=pbank[6][:, 0:256], scalar=e_[:, 4 + h:5 + h],
                    in1=sigo[:, s, h * 256:(h + 1) * 256], op0=ALU.mult, op1=ALU.mult),
                    reads=[pbank_b[6], est_b[par], pg[16 + 2 * s], pg[17 + 2 * s]], writes=[hm_b[h]])
                tr.op("act", lambda e, h=h: e.activation(out=junk[:, 0:256], in_=hm[:, h * 256:(h + 1) * 256],
                                                         func=AF.Square, accum_out=e_[:, 8 + h:9 + h]),
                      reads=[hm_b[h], est_b[par]], writes=[junk_b, est_b[par]])
            tr.op("act", lambda e: e.activation(out=e_[:, 12:16], in_=e_[:, 8:12], func=AF.Ln, scale=1.0 / 256.0,
                                                bias=EPS),
                  reads=[est_b[par]], writes=[est_b[par]])
            tr.op("act", lambda e: e.activation(out=e_[:, 16:20], in_=e_[:, 12:16], func=AF.Exp, scale=-0.5),
                  reads=[est_b[par]], writes=[est_b[par]])
            tr.op("dve", lambda e: e.tensor_tensor(
                out=ymtok[:].rearrange("p (h e) -> p h e", h=4), in0=hm[:].rearrange("p (h e) -> p h e", h=4),
                in1=e_[:, 16:20].unsqueeze(2).broadcast_to([128, 4, 256]), op=ALU.mult),
                reads=hm_b + [est_b[par]], writes=[ymtok_b])
            for c in range(8):
                tr.op("pe", lambda e, c=c: e.transpose(out=pst[:, c * 128:(c + 1) * 128],
                                                       in_=ymtok[:, c * 128:(c + 1) * 128], identity=ident_bf),
                      reads=[ymtok_b, const_b], writes=[pbank_b[7]], inc=(c == 7))
            tr.op("act", lambda e: e.activation(out=ymT[:, :, sl], in_=pst[:, :].rearrange("p (c t) -> p c t", c=8),
                                                func=AF.Copy),
                  reads=[pbank_b[7]], writes=[ymT_b[s]])

        def attn_block(ti, s, jb):
            par = jb % 2
            first = (jb == 0)
            QB, QB_b = QBs[par], QBs_b[par]
            ast, ast_b = asts[par], asts_b[par]
            ya, ya_b = yas[par], yas_b[par]
            src = aqkv[:, s, 0:1152]
            tr.op("act", lambda e: e.activation(out=sq[:], in_=src, func=AF.Square),
                  reads=[aqkv_b[s]], writes=[sq_b])
            tr.op("dve", lambda e: e.tensor_reduce(out=ast[:, 0:18], in_=sq[:].rearrange("p (h d) -> p h d", h=18),
                                                   axis=AX.X, op=ALU.add),
                  reads=[sq_b, ast_b], writes=[ast_b])
            tr.op("act", lambda e: e.activation(out=ast[:, 0:18], in_=ast[:, 0:18], func=AF.Ln, scale=1.0 / 64.0,
                                                bias=EPS),
                  reads=[ast_b], writes=[ast_b])
            tr.op("act", lambda e: e.activation(out=ast[:, 18:36], in_=ast[:, 0:18], func=AF.Exp, scale=-0.5),
                  reads=[ast_b], writes=[ast_b])
            qn3 = qn[:].rearrange("p (h d) -> p h d", h=18)
            rt = sq[:, 0:576].rearrange("p (a h d) -> p a h d", a=4, h=18)
            rt_b = sq_b
            tr.op("dve", lambda e: e.tensor_tensor(out=qn3, in0=src.rearrange("p (h d) -> p h d", h=18),
                                                   in1=ast[:, 18:36].unsqueeze(2).broadcast_to([128, 18, 64]),
                                                   op=ALU.mult),
                  reads=[aqkv_b[s], ast_b], writes=[qn_b])
            tr.op("dve", lambda e: e.tensor_tensor(out=qn3[:, 0:16, :], in0=qn3[:, 0:16, :],
                                                   in1=gqk[:, 0:64].unsqueeze(1).broadcast_to([128, 16, 64]),
                                                   op=ALU.mult),
                  reads=[qn_b, setup_b], writes=[qn_b])
            tr.op("dve", lambda e: e.tensor_tensor(out=qn3[:, 16:18, :], in0=qn3[:, 16:18, :],
                                                   in1=gqk[:, 64:128].unsqueeze(1).broadcast_to([128, 2, 64]),
                                                   op=ALU.mult),
                  reads=[qn_b, setup_b], writes=[qn_b])
            cosb = cf[:, CF_COS + jb * 8:CF_COS + jb * 8 + 8].unsqueeze(1).broadcast_to([128, 18, 8])
            sinb = cf[:, CF_SIN + jb * 8:CF_SIN + jb * 8 + 8].unsqueeze(1).broadcast_to([128, 18, 8])
            x1, x2 = qn3[:, :, 0:8], qn3[:, :, 8:16]
            tr.op("dve", lambda e: e.tensor_tensor(out=rt[:, 0, :, :], in0=x1, in1=cosb, op=ALU.mult),
                  reads=[qn_b, const_b], writes=[rt_b])
            tr.op("dve", lambda e: e.tensor_tensor(out=rt[:, 1, :, :], in0=x2, in1=sinb, op=ALU.mult),
                  reads=[qn_b, const_b, rt_b], writes=[rt_b])
            tr.op("dve", lambda e: e.tensor_tensor(out=rt[:, 2, :, :], in0=x2, in1=cosb, op=ALU.mult),
                  reads=[qn_b, const_b, rt_b], writes=[rt_b])
            tr.op("dve", lambda e: e.tensor_tensor(out=rt[:, 3, :, :], in0=x1, in1=sinb, op=ALU.mult),
                  reads=[qn_b, const_b, rt_b], writes=[rt_b])
            tr.op("dve", lambda e: e.tensor_tensor(out=qb[:, :, 0:8], in0=rt[:, 0, :, :], in1=rt[:, 1, :, :],
                                                   op=ALU.subtract),
                  reads=[rt_b], writes=[qb_b])
            tr.op("dve", lambda e: e.tensor_tensor(out=qb[:, :, 8:16], in0=rt[:, 2, :, :], in1=rt[:, 3, :, :],
                                                   op=ALU.add),
                  reads=[rt_b, qb_b], writes=[qb_b])
            tr.op("act", lambda e: e.activation(out=qb[:, :, 16:64], in_=qn3[:, :, 16:64], func=AF.Copy),
                  reads=[qn_b, qb_b], writes=[qb_b])
            tr.op("pool", lambda e: e.tensor_copy(
                out=k2[:].rearrange("p (g r d) -> p g r d", g=2, r=2),
                in_=qb[:, 16:18, :].unsqueeze(2).broadcast_to([128, 2, 2, 64])),
                reads=[qb_b], writes=[k2_b])
            tr.op("pool", lambda e: e.tensor_copy(
                out=vat[:, par, :, 0:64], in_=aqkv[:, s, 1152:1280].rearrange("p (g d) -> p g d", g=2)),
                reads=[aqkv_b[s]], writes=[vat_b[par]])
            pq = pbank[4].bitcast(BF16)
            for g in range(2):
                tr.op("pe", lambda e, g=g: e.transpose(out=pq[:, g * 128:(g + 1) * 128],
                                                       in_=k2[:, g * 128:(g + 1) * 128], identity=ident_bf),
                      reads=[k2_b, const_b], writes=[pbank_b[4]], inc=(g == 1))
            tr.op("act", lambda e: e.activation(out=kTd[:, par, :, :],
                                                in_=pq[:, 0:256].rearrange("p (g t) -> p g t", g=2), func=AF.Copy),
                  reads=[pbank_b[4]], writes=[kTd_b[par]])
            for c in range(8):
                tr.op("pe", lambda e, c=c: e.transpose(out=pq[:, c * 128:(c + 1) * 128],
                                                       in_=qb[:].rearrange("p h d -> p (h d)")[:, c * 128:(c + 1) * 128],
                                                       identity=ident_bf),
                      reads=[qb_b, const_b], writes=[pbank_b[4]], inc=(c == 7))
            pq3 = pq[:, :].rearrange("p (c t) -> p c t", c=8)
            tr.op("act", lambda e: e.activation(out=QB[0:64, :, 0:128], in_=pq3[0:64, :, :], func=AF.Copy),
                  reads=[pbank_b[4]], writes=[QB_b])
            tr.op("dve", lambda e: e.tensor_copy(out=QB[64:128, :, 128:256], in_=pq3[64:128, :, :]),
                  reads=[pbank_b[4], QB_b], writes=[QB_b])
            def po_ap(head):
                if head < 7:
                    return 2, pbank[2][:, head * 65:(head + 1) * 65]
                if head < 14:
                    return 3, pbank[3][:, (head - 7) * 65:(head - 6) * 65]
                return 4, pbank[4][:, (head - 14) * 65:(head - 13) * 65]
            def epilogue(groups):
                for (bank, h0, nh) in groups:
                    po3 = pbank[bank][:, 0:nh * 65].rearrange("p (h d) -> p h d", h=nh)
                    dsum = ast[:, 36 + h0:36 + h0 + nh]
                    rden = ast[:, 52 + h0:52 + h0 + nh]
                    tr.op("dve", lambda e, po3=po3, dsum=dsum, h0=h0, nh=nh: e.tensor_tensor(
                        out=dsum, in0=po3[:, :, 64], in1=sinkexp[:, h0:h0 + nh], op=ALU.add),
                        reads=[pbank_b[bank], setup_b, ast_b], writes=[ast_b])
                    tr.op("dve", lambda e, dsum=dsum, rden=rden: e.reciprocal(out=rden, in_=dsum),
                          reads=[ast_b], writes=[ast_b])
                    tr.op("dve", lambda e, po3=po3, rden=rden, h0=h0, nh=nh: e.tensor_tensor(
                        out=ya[:, h0 * 64:(h0 + nh) * 64].rearrange("p (h d) -> p h d", h=nh), in0=po3[:, :, 0:64],
                        in1=rden.unsqueeze(2).broadcast_to([128, nh, 64]), op=ALU.mult),
                        reads=[pbank_b[bank], ast_b], writes=[ya_b])

            for ci, c in enumerate((7, 0, 1, 2, 3, 4, 5, 6)):
                g = c // 4
                lb = ci % 2
                lg = pbank[lb]
                if not first:
                    tr.op("pe", lambda e, c=c, g=g, lg=lg: e.matmul(lg[:, 0:256], lhsT=kTd[:, 1 - par, g, :],
                                                                    rhs=QB[:, c, :], start=True, stop=False),
                          reads=[kTd_b[1 - par], QB_b], writes=[pbank_b[lb]], inc=False)
                    tr.op("pe", lambda e, lg=lg: e.matmul(lg[:, 0:256], lhsT=ident_bf, rhs=MBprev,
                                                          start=False, stop=True),
                          reads=[const_b], writes=[pbank_b[lb]], inc=False)
                tr.op("pe", lambda e, c=c, g=g, lg=lg: e.matmul(lg[:, 256:512], lhsT=kTd[:, par, g, :],
                                                                rhs=QB[:, c, :], start=True, stop=False),
                      reads=[kTd_b[par], QB_b], writes=[pbank_b[lb]], inc=False)
                tr.op("pe", lambda e, lg=lg: e.matmul(lg[:, 256:512], lhsT=ident_bf, rhs=MBcur,
                                                      start=False, stop=True),
                      reads=[const_b], writes=[pbank_b[lb]])
                lo = 256 if first else 0
                pk_ = ci % 2
                tr.op("act", lambda e, lg=lg, lo=lo, pk_=pk_: e.activation(out=pT[pk_][:, lo:512], in_=lg[:, lo:512],
                                                                           func=AF.Exp, bias=nlmax),
                      reads=[pbank_b[lb], setup_b], writes=[pT_b[pk_]])
                for hh in range(2):
                    head = 2 * c + hh
                    bank, po = po_ap(head)
                    srcs = [(256 + hh * 128, par)]
                    if not first:
                        srcs.append((hh * 128, 1 - par))
                    for i, (col, slot) in enumerate(srcs):
                        tr.op("pe", lambda e, pk_=pk_, col=col, slot=slot, g=g, po=po, i=i, n=len(srcs): e.matmul(
                            po, lhsT=pT[pk_][:, col:col + 128], rhs=vat[:, slot, g, :], start=(i == 0),
                            stop=(i == n - 1)),
                            reads=[pT_b[pk_], vat_b[slot]], writes=[pbank_b[bank]], inc=(i == len(srcs) - 1))
                if c == 7:
                    epilogue(((4, 14, 2),))
            epilogue(((2, 0, 7), (3, 7, 7)))
            sl = slice(s * 128, (s + 1) * 128)
            py = pbank[4].bitcast(BF16)
            for c in range(8):
                tr.op("pe", lambda e, c=c: e.transpose(out=py[:, c * 128:(c + 1) * 128],
                                                       in_=ya[:, c * 128:(c + 1) * 128], identity=ident_bf),
                      reads=[ya_b, const_b], writes=[pbank_b[4]], inc=(c == 7))
            tr.op("act", lambda e: e.activation(out=yaT[:, :, sl], in_=py[:, :].rearrange("p (c t) -> p c t", c=8),
                                                func=AF.Copy),
                  reads=[pbank_b[4]], writes=[yaT_b[s]])

        def merge(ti):
            for a in range(4):
                wv, wb = wload(f"mrg{a}")
                for jj in range(2):
                    j = 2 * a + jj
                    for (bank, seg, rhsT, rb) in ((0, 0, ymT, ymT_b), (1, 1, yaT, yaT_b), (2, 2, hT, hT_b),
                                                  (3, 3, hT, hT_b)):
                        for kc in range(8):
                            tr.op("pe", lambda e, bank=bank, seg=seg, rhsT=rhsT, kc=kc, wv=wv, jj=jj: e.matmul(
                                pbank[bank][:, :], lhsT=wv[:, seg * 2 + jj, kc, :], rhs=rhsT[:, kc, :],
                                start=(kc == 0), stop=(kc == 7)),
                                reads=[wb] + rb, writes=[pbank_b[bank]], inc=(kc == 7))
                    k = j % 2
                    tr.op("act", lambda e, j=j, k=k: e.activation(out=sgt[k][:], in_=pbank[2][:, :], func=AF.Sigmoid,
                                                                  bias=bmcol[:, j:j + 1]),
                          reads=[pbank_b[2], const_b], writes=[sgt_b[k]])
                    tr.op("act", lambda e, j=j, k=k: e.activation(out=sgt[2 + k][:], in_=pbank[3][:, :],
                                                                  func=AF.Sigmoid, bias=bmcol[:, 8 + j:9 + j]),
                          reads=[pbank_b[3], const_b], writes=[sgt_b[2 + k]])
                    tr.op("dve", lambda e, k=k: e.tensor_tensor(out=mt[0], in0=pbank[0][:, :], in1=sgt[k][:],
                                                                op=ALU.mult),
                          reads=[pbank_b[0], sgt_b[k]], writes=[mt_b[0]])
                    tr.op("dve", lambda e, k=k: e.tensor_tensor(out=mt[1], in0=pbank[1][:, :], in1=sgt[2 + k][:],
                                                                op=ALU.mult),
                          reads=[pbank_b[1], sgt_b[2 + k]], writes=[mt_b[1]])
                    tr.op("dve", lambda e, j=j: e.tensor_tensor(out=mgT[:, j, :], in0=mt[0], in1=mt[1],
                                                                 op=ALU.add),
                          reads=[mt_b[0], mt_b[1]], writes=[mgT_b[j]])
            dump(f"mgT{ti}", mgT[:], mgT_b, [128, 8, T], BF16)

        def outproj(ti):
            wv, wb = wload("wout")
            for s in range(NSUB):
                for n in range(2):
                    pb = gemm_bank()
                    for kc in range(8):
                        tr.op("pe", lambda e, pb=pb, kc=kc, s=s, n=n, wv=wv: e.matmul(
                            pbank[pb][:, :], lhsT=mgT[:, kc, s * 128:(s + 1) * 128], rhs=wv[:, n, kc, :],
                            start=(kc == 0), stop=(kc == 7)),
                            reads=[wb] + mgT_b, writes=[pbank_b[pb]], inc=(kc == 7))
                    tr.op("dve", lambda e, pb=pb, s=s, n=n: e.tensor_tensor(
                        out=x_sb[:, s, n * 512:(n + 1) * 512], in0=pbank[pb][:, :],
                        in1=x_sb[:, s, n * 512:(n + 1) * 512], op=ALU.add),
                        reads=[pbank_b[pb], x_b[s]], writes=[x_b[s]])
            dump(f"x1_{ti}", x_sb[:], x_b, [128, NSUB, D])
            for s in range(NSUB):
                norm_transpose(x_sb[:, s, :], x_b[s], ymT, ymT_b[s], s, ti * NSUB + s)

        ost_rr = [0]

        def ffn(ti):
            r0 = ti * T
            for a in range(6):
                wv, wb = wload(f"ffi{a}")
                js = list(range(4 * a, min(4 * a + 4, 22)))
                for idx, j in enumerate(js):
                    gb, ub = gemm_bank(), gemm_bank()
                    for (bank, seg) in ((gb, idx), (ub, len(js) + idx)):
                        for kc in range(8):
                            tr.op("pe", lambda e, bank=bank, seg=seg, kc=kc, wv=wv: e.matmul(
                                pbank[bank][:, :], lhsT=wv[:, seg, kc, :], rhs=ymT[:, kc, :],
                                start=(kc == 0), stop=(kc == 7)),
                                reads=[wb] + ymT_b, writes=[pbank_b[bank]], inc=(kc == 7))
                    k = j % 2
                    tr.op("act", lambda e, gb=gb, k=k: e.activation(out=sgt[k][:], in_=pbank[gb][:, :], func=AF.Silu),
                          reads=[pbank_b[gb]], writes=[sgt_b[k]])
                    tr.op("dve", lambda e, ub=ub, k=k, j=j: e.tensor_tensor(out=actT[:, j, :], in0=pbank[ub][:, :],
                                                                            in1=sgt[k][:], op=ALU.mult),
                          reads=[pbank_b[ub], sgt_b[k]], writes=[pg[j]])
            for n in range(2):
                for hlf in range(2):
                    wv, wb = wload(f"ffo{n}{hlf}")
                    for k in range(11):
                        kc = hlf * 11 + k
                        for s in range(NSUB):
                            tr.op("pe", lambda e, s=s, kc=kc, k=k, wv=wv, n=n: e.matmul(
                                pbank[s + 4 * n][:, :], lhsT=actT[:, kc, s * 128:(s + 1) * 128], rhs=wv[:, 0, k, :],
                                start=(kc == 0), stop=(kc == 21)),
                                reads=[wb, pg[kc]], writes=[pbank_b[s + 4 * n]], inc=(k == 10 and s == NSUB - 1))
                for s in range(NSUB):
                    tr.op("dve", lambda e, s=s, n=n: e.tensor_tensor(
                        out=x_sb[:, s, n * 512:(n + 1) * 512], in0=pbank[s + 4 * n][:, :],
                        in1=x_sb[:, s, n * 512:(n + 1) * 512], op=ALU.add),
                        reads=[pbank_b[s + 4 * n], x_b[s]], writes=[x_b[s]])
            for s in range(NSUB):
                dst = out_d[r0 + s * 128:r0 + (s + 1) * 128, :]
                tr.dma("sp", lambda e, dst=dst, s=s: e.dma_start(out=dst, in_=x_sb[:, s, :]), f"ost{s}",
                       reads=[x_b[s]], nbytes=524288)

        tr.op("pool", lambda e: e.memset(vaug[:, :, :, 256:257], 1.0), writes=vaug_b)

        tile_front(0)
        for ti in range(ntiles):
            first = (ti % 8 == 0)
            inproj_a(ti, first)
            gates(ti, first)
            inproj_b(ti)
            if stop_after == "inproj":
                continue
            for s in range(NSUB):
                mlstm_block(ti, s, first and s == 0)
                if stop_after != "mlstm":
                    attn_block(ti, s, (ti % 8) * NSUB + s)
            dump(f"ymT{ti}", ymT[:], ymT_b, [128, 8, T], BF16)
            dump(f"yaT{ti}", yaT[:], yaT_b, [128, 8, T], BF16)
            if stop_after in ("mlstm", "attn"):
                continue
            merge(ti)
            x_reload(ti)
            outproj(ti)
            if stop_after == "outproj":
                continue
            if ti + 1 < ntiles:
                tile_front(ti + 1)
            ffn(ti)
            if ti == 0:
                tr.op("pool", lambda e: e.memset(vaug[:, :, :, 256:257], 1.0), writes=vaug_b)

        tr.schedule(reorder=reorder, prio=prio)

        semnames = set(Tracker.ENG) | set(tr.dma_sems)
        sems = {n: es.enter_context(nc.semaphore("s_" + n)) for n in sorted(semnames)}
        block = es.enter_context(nc.Block())

        def replay(engname):
            def run(eng):
                for item in tr.q[engname]:
                    if item[0] == "wait":
                        eng.wait_ge(sems[item[1]], item[2])
                    else:
                        ins = None
                        for fn in item[1]:
                            ins = fn(eng)
                        ins.then_inc(sems[item[2]], item[3])
            return run

        block.tensor(replay("pe"))
        block.scalar(replay("act"))
        block.vector(replay("dve"))
        block.gpsimd(replay("pool"))
        block.sync(replay("sp"))
    stats = {e: len(tr.q[e]) for e in Tracker.ENG}
    stats['sim_end_us'] = getattr(tr, 'sim_end', 0.0) / 1e3
    stats['sbuf_left'] = sbuf_left
    return nc, dump_specs, stats


def _prep_inputs(inputs, ntiles=16, ncores=NCORES):
    f = np.float32
    x = np.ascontiguousarray(np.asarray(inputs["x"], dtype=f)).reshape(-1, D)
    cbf, cf = _host_consts()
    g1 = np.asarray(inputs["norm1_g"], f).reshape(8, 128).T
    g2 = np.asarray(inputs["norm2_g"], f).reshape(8, 128).T
    gm = np.asarray(inputs["m_norm_g"], f).reshape(8, 128).T
    convw = np.asarray(inputs["conv_w"], f).reshape(4, 16, 128).transpose(2, 1, 0).reshape(128, 64)
    convb = np.asarray(inputs["conv_b"], f).reshape(16, 128).T
    bmc = np.asarray(inputs["b_merge"], f).reshape(16, 128).T
    pcol = np.ascontiguousarray(np.concatenate([g1, g2, gm, convw, convb, bmc], axis=1))
    prow = np.ascontiguousarray(np.concatenate([np.asarray(inputs["q_norm_g"], f).reshape(-1),
                                                np.asarray(inputs["k_norm_g"], f).reshape(-1),
                                                np.asarray(inputs["sinks"], f).reshape(-1)])[None, :])
    pgate = np.ascontiguousarray(np.asarray(inputs["b_mgate"], f).reshape(2, 4).T)
    shared = {
        "w_in": np.ascontiguousarray(np.asarray(inputs["w_in"], f).reshape(D, N_IN)),
        "w_bm": np.ascontiguousarray(np.asarray(inputs["w_branch_m"], f).reshape(D, D)),
        "w_ba": np.ascontiguousarray(np.asarray(inputs["w_branch_a"], f).reshape(D, D)),
        "w_out": np.ascontiguousarray(np.asarray(inputs["w_out"], f).reshape(D, D)),
        "w_fi": np.ascontiguousarray(np.asarray(inputs["w_ffn_in"], f).reshape(D, 2 * DFF)),
        "w_fo": np.ascontiguousarray(np.asarray(inputs["w_ffn_out"], f).reshape(DFF, D)),
        "cbf": cbf, "cf": cf, "pcol": pcol, "prow": prow, "pgate": pgate,
    }
    in_maps = []
    per = TOK_CORE
    for c in range(ncores):
        m = dict(shared)
        m["x"] = x[c * per:c * per + ntiles * T]
        in_maps.append(m)
    return in_maps


_PROGRAM = None


def kernel(**inputs):
    global _PROGRAM
    if _PROGRAM is None:
        _PROGRAM = build_program(16)[0]
    in_maps = _prep_inputs(inputs)
    res = run_bass_kernel_spmd(_PROGRAM, in_maps, core_ids=list(range(NCORES)))
    out = np.concatenate([np.asarray(r["out"], dtype=np.float32) for r in res.results], axis=0)
    return out.reshape(16, SEQ, D)
```
